# Optimizing a Trainium2 kernel written in Bass

```python
import jax
import jax.numpy as jnp
from jax import lax
import numpy as np

D_MODEL = 1024
BATCH = 8
SEQ = 4096
DEPTH = 2

N_MEM = 256
HEAD_DIM = 64
ROPE_THETA = 500000.0
ROPE_FRACTION = 4
RMS_EPS = 1e-6
NEG_INF = -1e30
TINY = 1e-20

DSA_HEADS = 8
DSA_IDX_HEADS = 8
DSA_IDX_DIM = 32
DSA_TOPK = 256
DSA_QBLOCK = 128

MOBA_HEADS = 8
MOBA_BLOCK = 256
MOBA_TOPK = 3
MOBA_QBLOCK = 32

NSA_HEADS = 16
NSA_GROUPS = 4
NSA_CMP_LEN = 32
NSA_CMP_STRIDE = 16
NSA_SEL_LEN = 64
NSA_SEL_TOPK = 16
NSA_WINDOW = 512
NSA_QBLOCK = 32
NSA_FORCE = 1e4

MEM_HEADS = 4
MEM_HEAD_DIM = 128

D_FF = ((8 * D_MODEL + 3 * 256 - 1) // (3 * 256)) * 256

AB_SIZES = (DSA_HEADS * HEAD_DIM, HEAD_DIM, HEAD_DIM, DSA_IDX_HEADS * DSA_IDX_DIM, DSA_IDX_DIM, DSA_IDX_HEADS, MOBA_HEADS * HEAD_DIM, MOBA_HEADS * HEAD_DIM, MOBA_HEADS * HEAD_DIM)
AB_WIDTH = sum(AB_SIZES)
AB_MIX_WIDTH = (DSA_HEADS + MOBA_HEADS) * HEAD_DIM
C_SIZES = (NSA_HEADS * HEAD_DIM,) + (NSA_GROUPS * HEAD_DIM,) * 6 + (NSA_HEADS * 3,)
C_WIDTH = sum(C_SIZES)
C_MIX_WIDTH = NSA_HEADS * HEAD_DIM

kernel_name = 'hybrid_dsa_moba_nsa_mem_block'


def rms_norm(x, gain):
    xf = x.astype(jnp.float32)
    y = xf * lax.rsqrt(jnp.mean(xf * xf, axis=-1, keepdims=True) + RMS_EPS)
    return (y * gain.astype(jnp.float32)).astype(x.dtype)


def partial_rope(x, positions):
    dh = x.shape[-1]
    rot = dh // ROPE_FRACTION
    half = rot // 2
    inv_freq = ROPE_THETA ** (-(jnp.arange(half, dtype=jnp.float32) * 2.0 / rot))
    ang = positions.astype(jnp.float32)[:, :, None] * inv_freq
    cos = jnp.cos(ang)[:, :, None, :]
    sin = jnp.sin(ang)[:, :, None, :]
    xf = x.astype(jnp.float32)
    x1 = xf[..., :half]
    x2 = xf[..., half:rot]
    out = jnp.concatenate([x1 * cos - x2 * sin, x2 * cos + x1 * sin, xf[..., rot:]], axis=-1)
    return out.astype(x.dtype)


def masked_softmax(s, mask):
    s = jnp.where(mask, s, NEG_INF)
    m = jnp.max(s, axis=-1, keepdims=True)
    p = jnp.where(mask, jnp.exp(s - m), 0.0)
    return p / jnp.maximum(jnp.sum(p, axis=-1, keepdims=True), TINY)


def _rows(t, start, size):
    return lax.dynamic_slice_in_dim(t, start, size, axis=1)


def _split(x, sizes):
    out = []
    start = 0
    for n in sizes:
        out.append(x[..., start:start + n])
        start += n
    return out


def _sweep_queries(fn, n_blocks):
    out = lax.map(fn, jnp.arange(n_blocks, dtype=jnp.int32))
    out = jnp.moveaxis(out, 0, 1)
    return out.reshape((out.shape[0], -1) + out.shape[3:])


def dsa_attention(q, k, v, q_idx, k_idx, w_idx):
    B, S, H, Dh = q.shape
    k_top = min(DSA_TOPK, S // 4)
    key_pos = jnp.arange(S)
    b_idx = jnp.arange(B)[:, None, None]
    scale = Dh ** -0.5

    def block(c):
        t0 = c * DSA_QBLOCK
        qpos = t0 + jnp.arange(DSA_QBLOCK)
        logits = jnp.einsum('bqhd,bsd->bqhs', _rows(q_idx, t0, DSA_QBLOCK), k_idx).astype(jnp.float32)
        score = jnp.einsum('bqh,bqhs->bqs', _rows(w_idx, t0, DSA_QBLOCK).astype(jnp.float32), jax.nn.relu(logits))
        score = jnp.where(key_pos[None, None, :] <= qpos[None, :, None], score, NEG_INF)
        _, sel = lax.top_k(score, k_top)
        k_sel = k[b_idx, sel]
        v_sel = v[b_idx, sel]
        s = jnp.einsum('bqhd,bqkd->bhqk', _rows(q, t0, DSA_QBLOCK), k_sel).astype(jnp.float32) * scale
        p = masked_softmax(s, (sel <= qpos[None, :, None])[:, None])
        return jnp.einsum('bhqk,bqkd->bqhd', p.astype(v.dtype), v_sel)

    return _sweep_queries(block, S // DSA_QBLOCK)


def moba_attention(q, k, v):
    B, S, H, Dh = q.shape
    n_blk = -(-S // MOBA_BLOCK)
    pad = n_blk * MOBA_BLOCK - S
    k_pad = jnp.pad(k, ((0, 0), (0, pad), (0, 0), (0, 0)))
    v_pad = jnp.pad(v, ((0, 0), (0, pad), (0, 0), (0, 0)))
    k_blk = k_pad.reshape(B, n_blk, MOBA_BLOCK, H, Dh).transpose(0, 3, 1, 2, 4)
    v_blk = v_pad.reshape(B, n_blk, MOBA_BLOCK, H, Dh).transpose(0, 3, 1, 2, 4)
    k_mean = jnp.mean(k_blk.astype(jnp.float32), axis=3)
    n_top = max(1, min(MOBA_TOPK, n_blk - 1))
    n_past = n_top * MOBA_BLOCK
    b_idx = jnp.arange(B)[:, None, None, None]
    h_idx = jnp.arange(H)[None, :, None, None]
    blk_ids = jnp.arange(n_blk)
    scale = Dh ** -0.5

    def block(c):
        t0 = c * MOBA_QBLOCK
        qpos = t0 + jnp.arange(MOBA_QBLOCK)
        own = t0 // MOBA_BLOCK
        qc = _rows(q, t0, MOBA_QBLOCK)
        gate = jnp.einsum('bqhd,bhnd->bhqn', qc.astype(jnp.float32), k_mean)
        gate = jnp.where(blk_ids < own, gate, NEG_INF)
        _, sel = lax.top_k(gate, n_top)
        k_sel = k_blk[b_idx, h_idx, sel]
        v_sel = v_blk[b_idx, h_idx, sel]
        s_past = jnp.einsum('bqhd,bhqnkd->bhqnk', qc, k_sel).astype(jnp.float32)
        m_past = jnp.broadcast_to((sel < own)[..., None], s_past.shape)
        k_own = _rows(k_pad, own * MOBA_BLOCK, MOBA_BLOCK)
        v_own = _rows(v_pad, own * MOBA_BLOCK, MOBA_BLOCK)
        kpos = own * MOBA_BLOCK + jnp.arange(MOBA_BLOCK)
        s_own = jnp.einsum('bqhd,bkhd->bhqk', qc, k_own).astype(jnp.float32)
        m_own = jnp.broadcast_to(kpos[None, :] <= qpos[:, None], s_own.shape)
        s = jnp.concatenate([s_past.reshape(B, H, MOBA_QBLOCK, n_past), s_own], axis=-1) * scale
        m = jnp.concatenate([m_past.reshape(B, H, MOBA_QBLOCK, n_past), m_own], axis=-1)
        p = masked_softmax(s, m).astype(v.dtype)
        p_past = p[..., :n_past].reshape(B, H, MOBA_QBLOCK, n_top, MOBA_BLOCK)
        return (jnp.einsum('bhqnk,bhqnkd->bqhd', p_past, v_sel)
                + jnp.einsum('bhqk,bkhd->bqhd', p[..., n_past:], v_own))

    return _sweep_queries(block, S // MOBA_QBLOCK)


def nsa_compress(x, pos_emb, w1, w2):
    B, S, G, Dh = x.shape
    n_cmp = (S - NSA_CMP_LEN) // NSA_CMP_STRIDE + 1
    idx = jnp.arange(n_cmp)[:, None] * NSA_CMP_STRIDE + jnp.arange(NSA_CMP_LEN)[None, :]
    blk = x[:, idx] + pos_emb[None, None, :, None, :]
    blk = blk.transpose(0, 1, 3, 2, 4).reshape(B, n_cmp, G, NSA_CMP_LEN * Dh)
    return jax.nn.silu(blk @ w1) @ w2


def nsa_attention(q, q_rot, k_cmp, v_cmp, k_sel, v_sel, k_win, v_win, gates):
    B, S, H, Dh = q.shape
    G = k_sel.shape[2]
    HG = H // G
    qg = q.reshape(B, S, G, HG, Dh)
    qr = q_rot.reshape(B, S, G, HG, Dh)
    n_cmp = k_cmp.shape[1]
    cmp_start = jnp.arange(n_cmp) * NSA_CMP_STRIDE
    cmp_end = cmp_start + NSA_CMP_LEN - 1
    n_sel = S // NSA_SEL_LEN
    n_top = min(NSA_SEL_TOPK, n_sel)
    sel_start = jnp.arange(n_sel) * NSA_SEL_LEN
    cover = ((cmp_start[:, None] < sel_start[None, :] + NSA_SEL_LEN)
             & (cmp_start[:, None] + NSA_CMP_LEN > sel_start[None, :])).astype(jnp.float32)
    ks_blk = k_sel.reshape(B, n_sel, NSA_SEL_LEN, G, Dh).transpose(0, 3, 1, 2, 4)
    vs_blk = v_sel.reshape(B, n_sel, NSA_SEL_LEN, G, Dh).transpose(0, 3, 1, 2, 4)
    kw_pad = jnp.pad(k_win, ((0, 0), (NSA_WINDOW, 0), (0, 0), (0, 0)))
    vw_pad = jnp.pad(v_win, ((0, 0), (NSA_WINDOW, 0), (0, 0), (0, 0)))
    b_idx = jnp.arange(B)[:, None, None, None]
    g_idx = jnp.arange(G)[None, :, None, None]
    blk_ids = jnp.arange(n_sel)[None, :]
    scale = Dh ** -0.5
    n_gather = n_top * NSA_SEL_LEN

    def block(c):
        t0 = c * NSA_QBLOCK
        qpos = t0 + jnp.arange(NSA_QBLOCK)
        qp = _rows(qg, t0, NSA_QBLOCK)
        qrc = _rows(qr, t0, NSA_QBLOCK)
        gc = _rows(gates, t0, NSA_QBLOCK)
        s_c = jnp.einsum('bqgjd,bngd->bgjqn', qp, k_cmp).astype(jnp.float32) * scale
        p_c = masked_softmax(s_c, cmp_end[None, :] <= qpos[:, None])
        o_c = jnp.einsum('bgjqn,bngd->bqgjd', p_c.astype(v_cmp.dtype), v_cmp)
        imp = jnp.einsum('bgjqn,ns->bgqs', p_c, cover)
        cur = (qpos // NSA_SEL_LEN)[:, None]
        forced = (blk_ids == 0) | (blk_ids == cur) | (blk_ids == cur - 1)
        imp = jnp.where(forced, NSA_FORCE, imp)
        imp = jnp.where(blk_ids <= cur, imp, NEG_INF)
        _, sel = lax.top_k(imp, n_top)
        kg = ks_blk[b_idx, g_idx, sel].reshape(B, G, NSA_QBLOCK, n_gather, Dh)
        vg = vs_blk[b_idx, g_idx, sel].reshape(B, G, NSA_QBLOCK, n_gather, Dh)
        kpos = (sel[..., None] * NSA_SEL_LEN + jnp.arange(NSA_SEL_LEN)).reshape(B, G, NSA_QBLOCK, n_gather)
        s_s = jnp.einsum('bqgjd,bgqmd->bgjqm', qrc, kg).astype(jnp.float32) * scale
        p_s = masked_softmax(s_s, (kpos <= qpos[None, None, :, None])[:, :, None])
        o_s = jnp.einsum('bgjqm,bgqmd->bqgjd', p_s.astype(vg.dtype), vg)
        kw = _rows(kw_pad, t0, NSA_WINDOW + NSA_QBLOCK)
        vw = _rows(vw_pad, t0, NSA_WINDOW + NSA_QBLOCK)
        wpos = t0 - NSA_WINDOW + jnp.arange(NSA_WINDOW + NSA_QBLOCK)
        dist = qpos[:, None] - wpos[None, :]
        m_w = (dist >= 0) & (dist < NSA_WINDOW) & (wpos[None, :] >= 0)
        s_w = jnp.einsum('bqgjd,bkgd->bgjqk', qrc, kw).astype(jnp.float32) * scale
        p_w = masked_softmax(s_w, m_w)
        o_w = jnp.einsum('bgjqk,bkgd->bqgjd', p_w.astype(vw.dtype), vw)
        o = gc[..., 0:1] * o_c + gc[..., 1:2] * o_s + gc[..., 2:3] * o_w
        return o.reshape(B, NSA_QBLOCK, H, Dh)

    return _sweep_queries(block, S // NSA_QBLOCK)


def dsa_moba_mixer(h, positions, w_in, w_out, a_q_norm, a_k_norm, b_q_norm, b_k_norm):
    B, S, _ = h.shape
    aq, ak, av, iq, ik, iw, bq, bk, bv = _split(h @ w_in, AB_SIZES)
    aq = partial_rope(rms_norm(aq.reshape(B, S, DSA_HEADS, HEAD_DIM), a_q_norm), positions)
    ak = partial_rope(rms_norm(ak.reshape(B, S, 1, HEAD_DIM), a_k_norm), positions)[:, :, 0]
    iq = partial_rope(iq.reshape(B, S, DSA_IDX_HEADS, DSA_IDX_DIM), positions)
    ik = partial_rope(ik.reshape(B, S, 1, DSA_IDX_DIM), positions)[:, :, 0]
    o_a = dsa_attention(aq, ak, av, iq, ik, iw)
    bq = partial_rope(rms_norm(bq.reshape(B, S, MOBA_HEADS, HEAD_DIM), b_q_norm), positions)
    bk = partial_rope(rms_norm(bk.reshape(B, S, MOBA_HEADS, HEAD_DIM), b_k_norm), positions)
    o_b = moba_attention(bq, bk, bv.reshape(B, S, MOBA_HEADS, HEAD_DIM))
    o = jnp.concatenate([o_a.reshape(B, S, -1), o_b.reshape(B, S, -1)], axis=-1)
    return o @ w_out


def nsa_mixer(h, positions, w_in, w_out, q_norm, kcmp_norm, ksel_norm, kwin_norm,
              pos_k, pos_v, w1_k, w2_k, w1_v, w2_v):
    B, S, _ = h.shape
    G = NSA_GROUPS
    HG = NSA_HEADS // NSA_GROUPS
    q, kc, vc, ks, vs, kw, vw, gt = _split(h @ w_in, C_SIZES)
    grp = lambda t: t.reshape(B, S, G, HEAD_DIM)
    q = rms_norm(q.reshape(B, S, NSA_HEADS, HEAD_DIM), q_norm)
    q_rot = partial_rope(q, positions)
    kc = rms_norm(nsa_compress(grp(kc), pos_k, w1_k, w2_k), kcmp_norm)
    vc = nsa_compress(grp(vc), pos_v, w1_v, w2_v)
    ks = partial_rope(rms_norm(grp(ks), ksel_norm), positions)
    kw = partial_rope(rms_norm(grp(kw), kwin_norm), positions)
    gates = jax.nn.sigmoid(gt.astype(jnp.float32)).reshape(B, S, G, HG, 3).astype(h.dtype)
    o = nsa_attention(q, q_rot, kc, vc, ks, grp(vs), kw, grp(vw), gates)
    return o.reshape(B, S, C_MIX_WIDTH) @ w_out


def memory_cross_attention(h, m, w_q, w_kv, w_o, q_norm, k_norm):
    B, S, _ = h.shape
    M = m.shape[1]
    q = rms_norm((h @ w_q).reshape(B, S, MEM_HEADS, MEM_HEAD_DIM), q_norm)
    k, v = _split(m @ w_kv, (MEM_HEADS * MEM_HEAD_DIM, MEM_HEADS * MEM_HEAD_DIM))
    k = rms_norm(k.reshape(B, M, MEM_HEADS, MEM_HEAD_DIM), k_norm)
    v = v.reshape(B, M, MEM_HEADS, MEM_HEAD_DIM)
    s = jnp.einsum('bshd,bmhd->bhsm', q, k).astype(jnp.float32) * (MEM_HEAD_DIM ** -0.5)
    p = jax.nn.softmax(s, axis=-1).astype(v.dtype)
    o = jnp.einsum('bhsm,bmhd->bshd', p, v).reshape(B, S, MEM_HEADS * MEM_HEAD_DIM)
    return o @ w_o


def swiglu(h, w_in, w_out):
    g, u = _split(h @ w_in, (D_FF, D_FF))
    return (jax.nn.silu(g) * u) @ w_out


def setup_inputs(seed: int = 0) -> dict:
    key = jax.random.key(seed)
    keys = jax.random.split(key, 48)
    ctr = [0]

    def nxt():
        k = keys[ctr[0]]
        ctr[0] += 1
        return k

    def nrm(shape, scale):
        return scale * jax.random.normal(nxt(), shape, jnp.float32)

    def gain(shape):
        return 1.0 + 0.02 * jax.random.normal(nxt(), shape, jnp.float32)

    n_even = (DEPTH + 1) // 2
    n_odd = DEPTH // 2
    d = D_MODEL
    x = nrm((BATCH, SEQ, d), 1.0)
    mem = nrm((BATCH, N_MEM, d), 1.0)
    offset = jax.random.randint(nxt(), (BATCH, 1), 0, SEQ)
    positions = (offset + jnp.arange(SEQ)[None, :]).astype(jnp.int32)
    cmp_in = NSA_CMP_LEN * HEAD_DIM
    return {
        'x': x,
        'mem': mem,
        'positions': positions,
        'norm_mix': gain((DEPTH, d)),
        'norm_mem': gain((DEPTH, d)),
        'norm_mem_src': gain((DEPTH, d)),
        'norm_ffn': gain((DEPTH, d)),
        'ab_w_in': nrm((n_even, d, AB_WIDTH), d ** -0.5),
        'ab_w_out': nrm((n_even, AB_MIX_WIDTH, d), AB_MIX_WIDTH ** -0.5),
        'dsa_q_norm': gain((n_even, HEAD_DIM)),
        'dsa_k_norm': gain((n_even, HEAD_DIM)),
        'moba_q_norm': gain((n_even, HEAD_DIM)),
        'moba_k_norm': gain((n_even, HEAD_DIM)),
        'nsa_w_in': nrm((n_odd, d, C_WIDTH), d ** -0.5),
        'nsa_w_out': nrm((n_odd, C_MIX_WIDTH, d), C_MIX_WIDTH ** -0.5),
        'nsa_q_norm': gain((n_odd, HEAD_DIM)),
        'nsa_kcmp_norm': gain((n_odd, HEAD_DIM)),
        'nsa_ksel_norm': gain((n_odd, HEAD_DIM)),
        'nsa_kwin_norm': gain((n_odd, HEAD_DIM)),
        'nsa_cmp_pos_k': nrm((n_odd, NSA_CMP_LEN, HEAD_DIM), 0.1),
        'nsa_cmp_pos_v': nrm((n_odd, NSA_CMP_LEN, HEAD_DIM), 0.1),
        'nsa_cmp_w1_k': nrm((n_odd, cmp_in, HEAD_DIM), cmp_in ** -0.5),
        'nsa_cmp_w2_k': nrm((n_odd, HEAD_DIM, HEAD_DIM), HEAD_DIM ** -0.5),
        'nsa_cmp_w1_v': nrm((n_odd, cmp_in, HEAD_DIM), cmp_in ** -0.5),
        'nsa_cmp_w2_v': nrm((n_odd, HEAD_DIM, HEAD_DIM), HEAD_DIM ** -0.5),
        'mem_w_q': nrm((DEPTH, d, MEM_HEADS * MEM_HEAD_DIM), d ** -0.5),
        'mem_w_kv': nrm((DEPTH, d, 2 * MEM_HEADS * MEM_HEAD_DIM), d ** -0.5),
        'mem_w_o': nrm((DEPTH, MEM_HEADS * MEM_HEAD_DIM, d), (MEM_HEADS * MEM_HEAD_DIM) ** -0.5),
        'mem_q_norm': gain((DEPTH, MEM_HEAD_DIM)),
        'mem_k_norm': gain((DEPTH, MEM_HEAD_DIM)),
        'ffn_w_in': nrm((DEPTH, d, 2 * D_FF), d ** -0.5),
        'ffn_w_out': nrm((DEPTH, D_FF, d), D_FF ** -0.5),
    }


def reference(x, mem, positions, norm_mix, norm_mem, norm_mem_src, norm_ffn,
              ab_w_in, ab_w_out, dsa_q_norm, dsa_k_norm, moba_q_norm, moba_k_norm,
              nsa_w_in, nsa_w_out, nsa_q_norm, nsa_kcmp_norm, nsa_ksel_norm, nsa_kwin_norm,
              nsa_cmp_pos_k, nsa_cmp_pos_v, nsa_cmp_w1_k, nsa_cmp_w2_k, nsa_cmp_w1_v, nsa_cmp_w2_v,
              mem_w_q, mem_w_kv, mem_w_o, mem_q_norm, mem_k_norm, ffn_w_in, ffn_w_out):
    for i in range(DEPTH):
        j = i // 2
        h = rms_norm(x, norm_mix[i])
        if i % 2 == 0:
            x = x + dsa_moba_mixer(h, positions, ab_w_in[j], ab_w_out[j], dsa_q_norm[j], dsa_k_norm[j],
                                   moba_q_norm[j], moba_k_norm[j])
        else:
            x = x + nsa_mixer(h, positions, nsa_w_in[j], nsa_w_out[j], nsa_q_norm[j], nsa_kcmp_norm[j],
                              nsa_ksel_norm[j], nsa_kwin_norm[j], nsa_cmp_pos_k[j], nsa_cmp_pos_v[j],
                              nsa_cmp_w1_k[j], nsa_cmp_w2_k[j], nsa_cmp_w1_v[j], nsa_cmp_w2_v[j])
        x = x + memory_cross_attention(rms_norm(x, norm_mem[i]), rms_norm(mem, norm_mem_src[i]),
                                       mem_w_q[i], mem_w_kv[i], mem_w_o[i], mem_q_norm[i], mem_k_norm[i])
        x = x + swiglu(rms_norm(x, norm_ffn[i]), ffn_w_in[i], ffn_w_out[i])
    return x
```

```python
from contextlib import ExitStack
import numpy as np
import concourse.bass as bass
import concourse.mybir as mybir

F32 = mybir.dt.float32
BF16 = mybir.dt.bfloat16
I32 = mybir.dt.int32
AF = mybir.ActivationFunctionType
ALU = mybir.AluOpType
AX = mybir.AxisListType

N_DMA_SLOTS = 24
N_HW_SLOTS = 14


class Buf:
    __slots__ = ("name", "w", "r")

    def __init__(self, name):
        self.name = name
        self.w = None
        self.r = {}


class T:
    def __init__(self, t, name):
        self.t = t
        self.b = Buf(name)

    def __getitem__(self, k):
        return self.t[k]


class Ctx:
    def __init__(self, nc):
        self.nc = nc
        self.es = ExitStack()
        self.eng = {"pe": nc.tensor, "act": nc.scalar, "dve": nc.vector, "pool": nc.gpsimd, "sp": nc.sync}
        self.sem = {}
        for k in ("pe", "act", "dve", "pool"):
            self.sem[k] = self.es.enter_context(nc.semaphore("sem_" + k))
        for i in range(N_DMA_SLOTS):
            self.sem[("d", i)] = self.es.enter_context(nc.semaphore("semd%d" % i))
        self.cnt = {k: 0 for k in ("pe", "act", "dve", "pool")}
        self.dval = [0] * N_DMA_SLOTS
        self.dnext = 0
        self.dnext_sw = 0
        self.seen = {k: {} for k in self.eng}
        self.uid = 0
        self.phase_stack = None
        self.ninstr = 0

    def _nm(self, name):
        self.uid += 1
        return "%s_%d" % (name, self.uid)

    def sb(self, name, shape, dtype, stack=None):
        st = stack if stack is not None else (self.phase_stack or self.es)
        nm = self._nm(name)
        return T(st.enter_context(self.nc.sbuf_tensor(nm, list(shape), dtype)), nm)

    def ps(self, name, shape, dtype, stack=None):
        st = stack if stack is not None else (self.phase_stack or self.es)
        nm = self._nm(name)
        return T(st.enter_context(self.nc.psum_tensor(nm, list(shape), dtype)), nm)

    def dram(self, name, shape, dtype, kind="Internal"):
        return self.nc.dram_tensor(name, list(shape), dtype, kind=kind).ap()

    def _waits(self, E, reads, writes):
        deps = {}

        def need(k, v):
            if v > deps.get(k, 0):
                deps[k] = v

        for t in reads:
            b = t.b
            if b.w is not None:
                need(*b.w)
        for t in writes:
            b = t.b
            if b.w is not None:
                need(*b.w)
            for k, v in b.r.items():
                need(k, v)
        eng = self.eng[E]
        seen = self.seen[E]
        for k, v in deps.items():
            if k == E and E == "pe":
                continue
            if seen.get(k, 0) >= v:
                continue
            eng.wait_ge(self.sem[k], v)
            self.ninstr += 1
            seen[k] = v

    def op(self, E, fn, r=(), w=()):
        self._waits(E, r, w)
        inst = fn(self.eng[E])
        self.cnt[E] += 1
        self.ninstr += 1
        inst.then_inc(self.sem[E], 1)
        ev = self.cnt[E]
        for t in r:
            if t.b.r.get(E, 0) < ev:
                t.b.r[E] = ev
        for t in w:
            t.b.w = (E, ev)
            t.b.r = {}
        return inst

    def dma(self, Q, out, in_, r=(), w=(), **kw):
        self._waits(Q, r, w)
        slot = self.dnext
        self.dnext = (self.dnext + 1) % N_DMA_SLOTS
        k = ("d", slot)
        eng = self.eng[Q]
        if self.seen[Q].get(k, 0) < self.dval[slot]:
            eng.wait_ge(self.sem[k], self.dval[slot])
            self.seen[Q][k] = self.dval[slot]
            self.ninstr += 1
        inst = eng.dma_start(out=out, in_=in_, **kw)
        self.dval[slot] += 16
        inst.then_inc(self.sem[k], 16)
        self.ninstr += 1
        v = self.dval[slot]
        for t in r:
            if t.b.r.get(k, 0) < v:
                t.b.r[k] = v
        for t in w:
            t.b.w = (k, v)
            t.b.r = {}
        return inst

    def barrier(self):
        for E, eng in self.eng.items():
            seen = self.seen[E]
            for k in ("pe", "act", "dve", "pool"):
                v = self.cnt[k]
                if v == 0 or seen.get(k, 0) >= v or (k == E and E == "pe"):
                    continue
                eng.wait_ge(self.sem[k], v)
                seen[k] = v
                self.ninstr += 1
            for i in range(N_DMA_SLOTS):
                k = ("d", i)
                v = self.dval[i]
                if v == 0 or seen.get(k, 0) >= v:
                    continue
                eng.wait_ge(self.sem[k], v)
                seen[k] = v
                self.ninstr += 1

    def phase(self):
        ctx = self

        class _P:
            def __enter__(s):
                ctx.barrier()
                s.prev = ctx.phase_stack
                s.st = ExitStack()
                ctx.phase_stack = s.st
                return s

            def __exit__(s, *a):
                ctx.barrier()
                ctx.phase_stack = s.prev
                s.st.close()
                return False

        return _P()

    def close(self):
        self.barrier()
        self.es.close()


S = 4096
D = 1024
NT = 32
NEG = -30000.0
EPS = 1e-6
THETA = 500000.0
DFF = 2816


def ins_bc(ap, axis, n):
    l = [list(p) for p in ap.ap]
    l.insert(axis, [0, n])
    return bass.AP(ap.tensor, ap.offset, l)


class K:
    pass


def build(dbg=False, upto=99):
    nc = bass.Bass("TRN2", target_bir_lowering=False)
    k = K()
    inp = {}

    def IN(name, shape, dt=F32):
        inp[name] = nc.dram_tensor(name, list(shape), dt, kind="ExternalInput").ap()
        return inp[name]

    x_in = IN("x", [S, D]); mem_in = IN("mem", [256, D]); pos_in = IN("post", [128, NT], I32)
    for n in ("norm_mix", "norm_mem", "norm_mem_src", "norm_ffn"):
        IN(n, [2, D])
    IN("ab_w_in", [D, 2472]); IN("ab_w_out", [D, D])
    for n in ("dsa_q_norm", "dsa_k_norm", "moba_q_norm", "moba_k_norm", "nsa_q_norm", "nsa_kcmp_norm", "nsa_ksel_norm", "nsa_kwin_norm"):
        IN(n, [64])
    IN("nsa_w_in", [D, 2608]); IN("nsa_w_out", [D, D])
    IN("nsa_cmp_pos_k", [32, 64]); IN("nsa_cmp_pos_v", [32, 64])
    IN("nsa_cmp_w1_k", [2048, 64]); IN("nsa_cmp_w2_k", [64, 64]); IN("nsa_cmp_w1_v", [2048, 64]); IN("nsa_cmp_w2_v", [64, 64])
    IN("mem_w_q", [2, D, 512]); IN("mem_w_kv", [2, D, 1024]); IN("mem_w_o", [2, 512, D])
    IN("mem_q_norm", [2, 128]); IN("mem_k_norm", [2, 128])
    IN("ffn_w_in", [2, D, 2 * DFF]); IN("ffn_w_out", [2, DFF, D])
    y_out = nc.dram_tensor("y", [S, D], F32, kind="ExternalOutput").ap()

    C = Ctx(nc)
    skind = "ExternalOutput" if dbg else "Internal"
    dbg_names = []

    def SCR(name, shape, dt):
        if dbg:
            dbg_names.append(name)
        return nc.dram_tensor(name, list(shape), dt, kind=skind).ap()

    ident = C.sb("ident", [128, 128], BF16)
    I4 = C.sb("I4", [128, 4, 128], BF16)
    epsG = C.sb("epsG", [128, 1], F32)
    C.op("pool", lambda e: e.memset(epsG[:], EPS), w=[epsG])
    TRI4 = C.sb("TRI4", [128, 4, 128], BF16)
    BLO4 = C.sb("BLO4", [128, 4, 128], BF16)
    CAUS = C.sb("CAUS", [128, 4, 4, 128], BF16)
    cs64 = C.sb("cs64", [128, NT, 8], F32); sn64 = C.sb("sn64", [128, NT, 8], F32)
    cs32 = C.sb("cs32", [128, NT, 4], F32); sn32 = C.sb("sn32", [128, NT, 4], F32)
    with C.phase():
        cf = C.sb("cf", [128, 128], F32)
        C.op("pool", lambda e: e.memset(cf[:], 0.0), w=[cf])
        C.op("pool", lambda e: e.affine_select(out=cf[:], in_=cf[:], pattern=[[-1, 128]], compare_op=ALU.not_equal, fill=1.0, base=0, channel_multiplier=1), r=[cf], w=[cf])
        C.op("dve", lambda e: e.tensor_copy(out=ident[:], in_=cf[:]), r=[cf], w=[ident])
        C.op("dve", lambda e: e.tensor_copy(out=I4[:], in_=ins_bc(cf[:], 1, 4)), r=[cf], w=[I4])
        tf = C.sb("tf", [128, 128], F32)
        C.op("pool", lambda e: e.memset(tf[:], 0.0), w=[tf])
        C.op("pool", lambda e: e.affine_select(out=tf[:], in_=tf[:], pattern=[[1, 128]], compare_op=ALU.is_ge, fill=NEG, base=0, channel_multiplier=-1), r=[tf], w=[tf])
        C.op("dve", lambda e: e.tensor_copy(out=TRI4[:], in_=ins_bc(tf[:], 1, 4)), r=[tf], w=[TRI4])
        for j in range(4):
            for qi in range(4):
                if qi > j:
                    C.op("dve", lambda e: e.memset(CAUS[:, j, qi, :], 0.0), w=[CAUS])
                elif qi == j:
                    C.op("dve", lambda e: e.tensor_copy(out=CAUS[:, j, qi, :], in_=tf[:]), r=[tf], w=[CAUS])
                else:
                    C.op("dve", lambda e: e.memset(CAUS[:, j, qi, :], NEG), w=[CAUS])
        tf2 = C.sb("tf2", [128, 128], F32)
        C.op("pool", lambda e: e.memset(tf2[:], 0.0), w=[tf2])
        C.op("pool", lambda e: e.affine_select(out=tf2[:], in_=tf2[:], pattern=[[-1, 128]], compare_op=ALU.is_gt, fill=NEG, base=0, channel_multiplier=1), r=[tf2], w=[tf2])
        C.op("dve", lambda e: e.tensor_copy(out=BLO4[:], in_=ins_bc(tf2[:], 1, 4)), r=[tf2], w=[BLO4])
        pi_ = C.sb("posi", [128, NT], I32); pf = C.sb("posf", [128, NT], F32)
        C.dma("sp", pi_[:], pos_in, w=[pi_])
        C.op("dve", lambda e: e.tensor_copy(out=pf[:], in_=pi_[:]), r=[pi_], w=[pf])
        u = C.sb("u", [128, NT], F32); ui = C.sb("ui", [128, NT], I32); uf = C.sb("uf", [128, NT], F32); fr = C.sb("fr", [128, NT], F32)

        def sincol(dst, col, freq, shift):
            C.op("dve", lambda e: e.tensor_scalar(out=u[:], in0=pf[:], scalar1=float(freq / (2 * np.pi)), scalar2=float(shift), op0=ALU.mult, op1=ALU.add), r=[pf], w=[u])
            C.op("dve", lambda e: e.tensor_copy(out=ui[:], in_=u[:]), r=[u], w=[ui])
            C.op("dve", lambda e: e.tensor_copy(out=uf[:], in_=ui[:]), r=[ui], w=[uf])
            C.op("dve", lambda e: e.tensor_tensor(out=fr[:], in0=u[:], in1=uf[:], op=ALU.subtract), r=[u, uf], w=[fr])
            C.op("dve", lambda e: e.tensor_scalar(out=fr[:], in0=fr[:], scalar1=0.4999, scalar2=-0.4999, op0=ALU.min, op1=ALU.max), r=[fr], w=[fr])
            C.op("act", lambda e: e.activation(out=dst[:, :, col], in_=fr[:], func=AF.Sin, scale=float(2 * np.pi)), r=[fr], w=[dst])

        for i in range(8):
            f = THETA ** (-(i * 2.0 / 16))
            sincol(sn64, i, f, 0.0); sincol(cs64, i, f, 0.25)
        for i in range(4):
            f = THETA ** (-(i * 2.0 / 8))
            sincol(sn32, i, f, 0.0); sincol(cs32, i, f, 0.25)

    def load_gain(dst, src_ap):
        C.dma("sp", dst[:], src_ap.rearrange("(kt p) -> p kt", p=128), w=[dst], allow_slow_non_contiguous=True)

    def load_w(dst, src_ap, KT, N, gain=None):
        srcv = src_ap.rearrange("(kt p) n -> p kt n", p=128)
        with C.phase():
            st = [C.sb("wst", [128, 4, 512], F32) for _ in range(3)]
            i = 0
            for k0 in range(0, KT, 4):
                k1 = min(KT, k0 + 4)
                for c0 in range(0, N, 512):
                    c1 = min(N, c0 + 512)
                    s = st[i % 3]
                    C.dma("sp" if i % 2 == 0 else "pool", s[:, 0:k1 - k0, 0:c1 - c0], srcv[:, k0:k1, c0:c1], w=[s])
                    for kt in range(k0, k1):
                        if kt % 2 == 0:
                            if gain is not None:
                                C.op("dve", lambda e: e.tensor_scalar(out=dst[:, kt, c0:c1], in0=s[:, kt - k0, 0:c1 - c0], scalar1=gain[:, kt:kt + 1], scalar2=None, op0=ALU.mult), r=[s, gain], w=[dst])
                            else:
                                C.op("dve", lambda e: e.tensor_copy(out=dst[:, kt, c0:c1], in_=s[:, kt - k0, 0:c1 - c0]), r=[s], w=[dst])
                        else:
                            if gain is not None:
                                C.op("act", lambda e: e.activation(out=dst[:, kt, c0:c1], in_=s[:, kt - k0, 0:c1 - c0], func=AF.Copy, scale=gain[:, kt:kt + 1]), r=[s, gain], w=[dst])
                            else:
                                C.op("act", lambda e: e.activation(out=dst[:, kt, c0:c1], in_=s[:, kt - k0, 0:c1 - c0], func=AF.Copy), r=[s], w=[dst])
                    i += 1

    class NormT:
        def __init__(s, pT=None):
            s.junk = C.sb("junk", [128, D], F32)
            s.ss = C.sb("ss", [128, 1], F32); s.rs = C.sb("rs", [128, 1], F32); s.epsT = epsG
            s.hn = C.sb("hn", [128, D], BF16)
            s.pT = pT if pT is not None else C.ps("pT", [128, 8, 128], BF16)

        def run(s, X, hT_ap, hT):
            C.op("act", lambda e: e.activation(out=s.junk[:], in_=X[:], func=AF.Square, accum_out=s.ss[:]), r=[X], w=[s.junk, s.ss])
            C.op("act", lambda e: e.activation(out=s.rs[:], in_=s.ss[:], func=AF.Ln, scale=1.0 / D, bias=s.epsT[:]), r=[s.ss, s.epsT], w=[s.rs])
            C.op("act", lambda e: e.activation(out=s.rs[:], in_=s.rs[:], func=AF.Exp, scale=-0.5), r=[s.rs], w=[s.rs])
            C.op("act", lambda e: e.activation(out=s.hn[:], in_=X[:], func=AF.Copy, scale=s.rs[:]), r=[X, s.rs], w=[s.hn])
            for kt in range(8):
                C.op("pe", lambda e: e.transpose(out=s.pT[:, kt, :], in_=s.hn[:, kt * 128:(kt + 1) * 128], identity=ident[:]), r=[s.hn, ident], w=[s.pT])
            C.op("dve", lambda e: e.tensor_copy(out=hT_ap, in_=s.pT[:]), r=[s.pT], w=[hT])

    class HeadNorm:
        def __init__(s, Hmax, Dh, rope_eng="dve"):
            s.sq = C.sb("hsq", [128, Hmax, Dh], F32)
            s.nrms = [C.sb("hnrm", [128, Hmax, Dh], F32) for _ in range(2)]
            s.ssh = C.sb("hss", [128, Hmax], F32); s.rsh = C.sb("hrs", [128, Hmax], F32)
            hf = Dh // 8
            s.t1s = [C.sb("ht1", [128, Hmax, hf], F32) for _ in range(2)]; s.t2s = [C.sb("ht2", [128, Hmax, hf], F32) for _ in range(2)]
            s.Dh = Dh; s.k = 0; s.re = rope_eng

        def run(s, src_ap, srcT, H, gain, out_ap, outT, rope=None, norm=True, out2_ap=None, out2T=None):
            Dh = s.Dh
            s.k += 1
            nrmT = s.nrms[s.k % 2]; t1T = s.t1s[s.k % 2]; t2T = s.t2s[s.k % 2]
            if norm:
                sq = s.sq[:, 0:H, :]; nrm = nrmT[:, 0:H, :]
                C.op("dve", lambda e: e.tensor_tensor(out=sq, in0=src_ap, in1=src_ap, op=ALU.mult), r=[srcT], w=[s.sq])
                C.op("dve", lambda e: e.tensor_reduce(out=s.ssh[:, 0:H], in_=sq, axis=AX.X, op=ALU.add), r=[s.sq], w=[s.ssh])
                C.op("act", lambda e: e.activation(out=s.rsh[:, 0:H], in_=s.ssh[:, 0:H], func=AF.Ln, scale=1.0 / Dh, bias=epsG[:]), r=[s.ssh, epsG], w=[s.rsh])
                C.op("act", lambda e: e.activation(out=s.rsh[:, 0:H], in_=s.rsh[:, 0:H], func=AF.Exp, scale=-0.5), r=[s.rsh], w=[s.rsh])
                C.op("dve", lambda e: e.tensor_tensor(out=nrm, in0=src_ap, in1=ins_bc(s.rsh[:, 0:H], 2, Dh), op=ALU.mult), r=[srcT, s.rsh], w=[nrmT])
                C.op("dve", lambda e: e.tensor_tensor(out=nrm, in0=nrm, in1=ins_bc(gain[:], 1, H), op=ALU.mult), r=[nrmT, gain], w=[nrmT])
                nT = nrmT
            else:
                nrm = src_ap; nT = srcT
            if out2_ap is not None:
                C.op("act", lambda e: e.activation(out=out2_ap, in_=nrm, func=AF.Copy), r=[nT], w=[out2T])
            C.op("act", lambda e: e.activation(out=out_ap, in_=nrm, func=AF.Copy), r=[nT], w=[outT])
            if rope is not None:
                RE = s.re
                cs, sn, hf = rope
                x1 = nrm[:, :, 0:hf]; x2 = nrm[:, :, hf:2 * hf]
                csb = ins_bc(cs, 1, H); snb = ins_bc(sn, 1, H)
                t1 = t1T[:, 0:H, :]; t2 = t2T[:, 0:H, :]
                C.op(RE, lambda e: e.tensor_tensor(out=t1, in0=x1, in1=csb, op=ALU.mult), r=[nT], w=[t1T])
                C.op(RE, lambda e: e.tensor_tensor(out=t2, in0=x2, in1=snb, op=ALU.mult), r=[nT], w=[t2T])
                C.op(RE, lambda e: e.tensor_tensor(out=out_ap[:, :, 0:hf], in0=t1, in1=t2, op=ALU.subtract), r=[t1T, t2T], w=[outT])
                C.op(RE, lambda e: e.tensor_tensor(out=t1, in0=x2, in1=csb, op=ALU.mult), r=[nT], w=[t1T])
                C.op(RE, lambda e: e.tensor_tensor(out=t2, in0=x1, in1=snb, op=ALU.mult), r=[nT], w=[t2T])
                C.op(RE, lambda e: e.tensor_tensor(out=out_ap[:, :, hf:2 * hf], in0=t1, in1=t2, op=ALU.add), r=[t1T, t2T], w=[outT])

    def run_proj(x_src, W, chunks, make_job, npo=3):
        nt = NormT()
        xt = [C.sb("xt", [128, D], F32) for _ in range(2)]
        hT = [C.sb("hT", [128, 8, 128], BF16) for _ in range(2)]
        po = [C.ps("po", [128, 512], F32) for _ in range(npo)]

        def norm(t):
            X = xt[t % 2]
            C.dma("sp", X[:], x_src[t * 128:(t + 1) * 128, :], w=[X])
            nt.run(X, hT[t % 2][:], hT[t % 2])

        jobs = [(t, c) for t in range(NT) for c in range(len(chunks))]

        def mm(k):
            t, c = jobs[k]
            c0, c1 = chunks[c]
            P = po[k % npo]
            H_ = hT[t % 2]
            for kt in range(8):
                C.op("pe", lambda e: e.matmul(P[:, 0:c1 - c0], lhsT=H_[:, kt, :], rhs=W[:, kt, c0:c1], start=(kt == 0), stop=(kt == 7)), r=[H_, W], w=[P])
            return P

        norm(0)
        Ps = {0: mm(0)}
        prev_trans = None
        for k, (t, c) in enumerate(jobs):
            if c == 0 and t + 1 < NT:
                norm(t + 1)
            if k + 1 < len(jobs):
                Ps[k + 1] = mm(k + 1)
            if prev_trans is not None:
                prev_trans()
            post, trans = make_job(k, t, c, Ps.pop(k))
            post()
            prev_trans = trans
        if prev_trans is not None:
            prev_trans()

    def bgain(src_ap, n=64):
        t = C.sb("bg", [128, n], F32)
        C.dma("sp", t[:], src_ap.partition_broadcast(128), w=[t])
        return t


    QA_T = SCR("QA_T", [8, 64, S], BF16); KA_T = SCR("KA_T", [64, S], BF16); VA = SCR("VA", [S, 64], BF16)
    IQ_T = SCR("IQ_T", [8, 32, S], BF16); IK_T = SCR("IK_T", [32, S], BF16); IW = SCR("IW", [S, 8], F32)
    QB_T = SCR("QB_T", [8, 64, S], BF16); KB_T = SCR("KB_T", [8, 64, S], BF16); VB = SCR("VB", [S, 512], BF16)
    OM = SCR("OM", [S, D], BF16)
    XA = SCR("XA", [S, D], F32); XB = SCR("XB", [S, D], F32); XC = SCR("XC", [S, D], F32)

    def store(dst_ap, src_ap, srcT, **kw):
        C.dma("pool", dst_ap, src_ap, r=[srcT], **kw)

    def phase_A0():
        with C.phase():
            g = C.sb("g", [128, 8], F32); load_gain(g, inp["norm_mix"][0])
            W = C.sb("W0", [128, 8, 2472], BF16)
            load_w(W, inp["ab_w_in"], 8, 2472, g)
            gq = bgain(inp["dsa_q_norm"]); gk = bgain(inp["dsa_k_norm"]); gbq = bgain(inp["moba_q_norm"]); gbk = bgain(inp["moba_k_norm"])
            hnm = HeadNorm(8, 64, "pool"); hn32 = HeadNorm(9, 32, "pool")
            pr = [C.sb("pr", [128, 512], F32) for _ in range(2)]
            qn = [C.sb("qn", [128, 512], BF16) for _ in range(3)]
            ptq = [C.ps("ptq", [64, 8, 128], BF16) for _ in range(2)]
            qT = [C.sb("qT", [64, 8, 128], BF16) for _ in range(3)]
            chunks = [(0, 512), (512, 936), (936, 1448), (1448, 1960), (1960, 2472)]
            cnt = {"ti": 0}

            def tr_heads(Q, H, Dh, src_off, dst_ap):
                i = cnt["ti"]; cnt["ti"] += 1
                PT = ptq[i % 2]; QT = qT[i % 3]
                for h in range(H):
                    C.op("pe", lambda e: e.transpose(out=PT[0:Dh, h, :], in_=Q[:, src_off + h * Dh: src_off + (h + 1) * Dh], identity=ident[:]), r=[Q, ident], w=[PT])
                C.op("dve", lambda e: e.tensor_copy(out=QT[0:Dh, 0:H, :], in_=PT[0:Dh, 0:H, :]), r=[PT], w=[QT])
                store(dst_ap, QT[0:Dh, 0:H, :], QT)

            def make_job(k, t, c, P):
                R = pr[k % 2]; Q = qn[k % 3]
                ts = slice(t * 128, (t + 1) * 128)
                cs8 = cs64[:, t, :]; sn8 = sn64[:, t, :]; cs4 = cs32[:, t, :]; sn4 = sn32[:, t, :]
                wd = chunks[c][1] - chunks[c][0]

                def post():
                    C.op("act", lambda e: e.activation(out=R[:, 0:wd], in_=P[:, 0:wd], func=AF.Copy), r=[P], w=[R])
                    if c in (0, 2, 3):
                        gg = (gq, None, gbq, gbk)[c]
                        hnm.run(R[:].rearrange("p (h d) -> p h d", h=8), R, 8, gg, Q[:].rearrange("p (h d) -> p h d", h=8), Q, rope=(cs8, sn8, 8))
                    elif c == 1:
                        hnm.run(R[:, 0:64].rearrange("p (h d) -> p h d", h=1), R, 1, gk, Q[:, 0:64].rearrange("p (h d) -> p h d", h=1), Q, rope=(cs8, sn8, 8))
                        C.op("act", lambda e: e.activation(out=Q[:, 64:128], in_=R[:, 64:128], func=AF.Copy), r=[R], w=[Q])
                        hn32.run(R[:, 128:416].rearrange("p (h d) -> p h d", h=9), R, 9, None, Q[:, 128:416].rearrange("p (h d) -> p h d", h=9), Q, rope=(cs4, sn4, 4), norm=False)
                        store(IW[ts, :], R[:, 416:424], R)
                    else:
                        C.op("act", lambda e: e.activation(out=Q[:], in_=R[:], func=AF.Copy), r=[R], w=[Q])

                def trans():
                    if c in (0, 2, 3):
                        dst = (QA_T, None, QB_T, KB_T)[c]
                        tr_heads(Q, 8, 64, 0, dst[:, :, ts].rearrange("h d s -> d h s"))
                    elif c == 1:
                        store(VA[ts, :], Q[:, 64:128], Q)
                        tr_heads(Q, 1, 64, 0, KA_T[:, ts].rearrange("(h d) s -> d h s", h=1))
                        tr_heads(Q, 8, 32, 128, IQ_T[:, :, ts].rearrange("h d s -> d h s"))
                        tr_heads(Q, 1, 32, 384, IK_T[:, ts].rearrange("(h d) s -> d h s", h=1))
                    else:
                        store(VB[ts, :], Q[:], Q)
                return post, trans

            run_proj(x_in, W, chunks, make_job)

    NIT = 16

    def run_merged(gA, nA, gB, nB):
        a = b = 0
        doneA = gA is None
        doneB = gB is None
        while not (doneA and doneB):
            pickA = (not doneA) and (doneB or a * nB <= b * nA)
            if pickA:
                try:
                    next(gA); a += 1
                except StopIteration:
                    doneA = True
            else:
                try:
                    next(gB); b += 1
                except StopIteration:
                    doneB = True

    def phase_A1():
        with C.phase():
            KT_ = C.sb("KAT", [128, S], BF16)
            C.op("pool", lambda e: e.memset(KT_[64:128, :], 0.0), w=[KT_])
            C.dma("sp", KT_[0:64, :], KA_T, w=[KT_])
            IKT = C.sb("IKT", [128, S], BF16)
            C.op("pool", lambda e: e.memset(IKT[:], 0.0), w=[IKT])
            C.dma("sp", IKT[0:32, :], IK_T, w=[IKT])
            Va = C.sb("VAa", [128, NT, 65], BF16)
            C.op("pool", lambda e: e.memset(Va[:], 1.0), w=[Va])
            C.dma("sp", Va[:, :, 0:64], VA.rearrange("(t p) c -> p t c", p=128), w=[Va])
            IWs = C.sb("IWs", [128, NT, 8], F32); C.dma("sp", IWs[:], IW.rearrange("(t p) c -> p t c", p=128), w=[IWs])
            score = [C.sb("score", [128, S], F32) for _ in range(2)]
            junk = C.sb("junkb", [128, S], BF16)
            nmask = [C.sb("nmask", [128, S], BF16) for _ in range(2)]
            rl = [C.sb("rl", [128, 512], F32) for _ in range(2)]
            qa = [C.sb("qa", [128, 8, 128], BF16) for _ in range(2)]
            iqt = [C.sb("iqt", [128, 8, 128], BF16) for _ in range(2)]
            for t_ in qa + iqt:
                C.op("pool", lambda e: e.memset(t_[:], 0.0), w=[t_])
            psi = [C.ps("psi", [128, 512], F32) for _ in range(2)]
            pss = [C.ps("pss", [128, 512], F32) for _ in range(2)]
            pacc = [C.ps("pacc", [128, 512], F32) for _ in range(4)]
            PTs = [C.sb("PTs", [128, 512], BF16) for _ in range(3)]
            sm = {n: C.sb(n, [128, 1], F32) for n in ("lo", "hi", "mid", "cnt", "ge", "rng", "c255", "sgn")}
            cthr = [C.sb("cthr", [128, 1], F32) for _ in range(2)]
            junkA = C.sb("junkA", [128, S], BF16)
            rzs = [C.sb("rz", [128, 1], F32) for _ in range(2)]
            steps = C.sb("steps", [128, NIT], F32); pw = C.sb("pw", [128, NIT], F32)
            for kk in range(NIT):
                C.op("pool", lambda e: e.memset(pw[:, kk:kk + 1], float(2.0 ** -(kk + 1))), w=[pw])
            C.op("pool", lambda e: e.memset(sm["c255"][:], 255.5), w=[sm["c255"]])
            oq = [C.sb("oq", [128, 512], BF16) for _ in range(2)]
            st_ = {"ii": 0, "si": 0, "pi": 0, "ri": 0}

            def gen_idx(qt):
                nk = (qt + 1) * 128
                IQ = iqt[qt % 2]; SC = score[qt % 2]; NM = nmask[qt % 2]
                ts = slice(qt * 128, (qt + 1) * 128)
                C.dma("sp", IQ[0:32, :, :], IQ_T[:, :, ts].rearrange("h d s -> d h s"), w=[IQ])
                jobs_ = [(c0, min(512, nk - c0), h) for c0 in range(0, nk, 512) for h in range(8)]

                def imm(j):
                    c0, wd, h = jobs_[j]
                    P = psi[st_["ii"] % 2]; st_["ii"] += 1
                    C.op("pe", lambda e: e.matmul(P[:, 0:wd], lhsT=IQ[:, h, :], rhs=IKT[:, c0:c0 + wd], start=True, stop=True), r=[IQ, IKT], w=[P])
                    return P

                Pcur = imm(0)
                for j, (c0, wd, h) in enumerate(jobs_):
                    Pnext = imm(j + 1) if j + 1 < len(jobs_) else None
                    P = Pcur
                    if h == 0:
                        C.op("dve", lambda e: e.tensor_scalar(out=SC[:, c0:c0 + wd], in0=P[:, 0:wd], scalar1=0.0, scalar2=IWs[:, qt, 0:1], op0=ALU.max, op1=ALU.mult), r=[P, IWs], w=[SC])
                    else:
                        R_ = rl[j % 2]
                        C.op("act", lambda e: e.activation(out=R_[:, 0:wd], in_=P[:, 0:wd], func=AF.Relu), r=[P], w=[R_])
                        C.op("dve", lambda e: e.scalar_tensor_tensor(out=SC[:, c0:c0 + wd], in0=R_[:, 0:wd], scalar=IWs[:, qt, h:h + 1], in1=SC[:, c0:c0 + wd], op0=ALU.mult, op1=ALU.add), r=[R_, IWs, SC], w=[SC])
                    Pcur = Pnext
                    if h == 7:
                        yield
                sc = SC[:, 0:nk]
                C.op("dve", lambda e: e.tensor_reduce(out=sm["hi"][:], in_=sc, axis=AX.X, op=ALU.max), r=[SC], w=[sm["hi"]])
                C.op("dve", lambda e: e.tensor_reduce(out=sm["lo"][:], in_=sc, axis=AX.X, op=ALU.min), r=[SC], w=[sm["lo"]])
                C.op("pool", lambda e: e.affine_select(out=SC[:, nk - 128:nk], in_=SC[:, nk - 128:nk], pattern=[[-1, 128]], compare_op=ALU.is_ge, fill=-1e30, base=0, channel_multiplier=1), r=[SC], w=[SC])
                C.op("dve", lambda e: e.tensor_tensor(out=sm["rng"][:], in0=sm["hi"][:], in1=sm["lo"][:], op=ALU.subtract), r=[sm["lo"], sm["hi"]], w=[sm["rng"]])
                C.op("dve", lambda e: e.tensor_scalar(out=steps[:], in0=pw[:], scalar1=sm["rng"][:], scalar2=None, op0=ALU.mult), r=[pw, sm["rng"]], w=[steps])
                yield
                cD = max(8, int(0.42 * nk) // 8 * 8)
                nA = nk - cD
                CT = cthr[qt % 2]
                C.op("pool", lambda e: e.memset(CT[:], float(255.5 - 0.5 * nA)), w=[CT])
                for it in range(NIT):
                    C.op("dve", lambda e: e.tensor_tensor(out=sm["mid"][:], in0=sm["lo"][:], in1=steps[:, it:it + 1], op=ALU.add), r=[sm["lo"], steps], w=[sm["mid"]])
                    C.op("act", lambda e: e.activation(out=junkA[:, 0:nA], in_=SC[:, cD:nk], func=AF.Sign, scale=-1.0, bias=sm["mid"][:], accum_out=sm["sgn"][:]), r=[SC, sm["mid"]], w=[junkA, sm["sgn"]])
                    C.op("dve", lambda e: e.tensor_scalar(out=junk[:, 0:cD], in0=SC[:, 0:cD], scalar1=sm["mid"][:], scalar2=None, op0=ALU.is_ge, op1=ALU.add, accum_out=sm["cnt"][:]), r=[SC, sm["mid"]], w=[junk, sm["cnt"]])
                    C.op("dve", lambda e: e.scalar_tensor_tensor(out=sm["cnt"][:], in0=sm["sgn"][:], scalar=-0.5, in1=sm["cnt"][:], op0=ALU.mult, op1=ALU.add), r=[sm["sgn"], sm["cnt"]], w=[sm["cnt"]])
                    C.op("dve", lambda e: e.tensor_scalar(out=sm["ge"][:], in0=sm["cnt"][:], scalar1=CT[:], scalar2=steps[:, it:it + 1], op0=ALU.is_ge, op1=ALU.mult), r=[sm["cnt"], CT, steps], w=[sm["ge"]])
                    C.op("dve", lambda e: e.tensor_tensor(out=sm["lo"][:], in0=sm["lo"][:], in1=sm["ge"][:], op=ALU.add), r=[sm["lo"], sm["ge"]], w=[sm["lo"]])
                    if it % 2 == 1:
                        yield
                C.op("dve", lambda e: e.tensor_scalar(out=NM[:, 0:nk], in0=sc, scalar1=sm["lo"][:], scalar2=NEG, op0=ALU.is_lt, op1=ALU.mult), r=[SC, sm["lo"]], w=[NM])
                yield

            def n_idx(qt):
                return ((qt + 1) * 128 + 511) // 512 + NIT // 2 + 2

            def gen_att(qt):
                QA = qa[qt % 2]; NM = nmask[qt % 2]; OQ = oq[qt % 2]
                ts = slice(qt * 128, (qt + 1) * 128)
                C.dma("sp", QA[0:64, :, :], QA_T[:, :, ts].rearrange("h d s -> d h s"), w=[QA])

                def qk(hg, st):
                    P = pss[st_["si"] % 2]; st_["si"] += 1
                    ss_ = slice(st * 128, (st + 1) * 128)
                    C.op("pe", lambda e: e.matmul(P[:], lhsT=KT_[:, ss_], rhs=QA[:, hg * 4:(hg + 1) * 4, :], start=True, stop=False), r=[KT_, QA], w=[P])
                    C.op("pe", lambda e: e.matmul(P[:], lhsT=NM[:, ss_], rhs=I4[:], start=False, stop=True), r=[NM, I4], w=[P])
                    return P

                for hg in range(2):
                    Pc = qk(hg, 0)
                    for st in range(qt + 1):
                        Pn = qk(hg, st + 1) if st < qt else None
                        PT = PTs[st_["pi"] % 3]; st_["pi"] += 1
                        C.op("act", lambda e: e.activation(out=PT[:], in_=Pc[:], func=AF.Exp, scale=0.125), r=[Pc], w=[PT])
                        for h in range(4):
                            C.op("pe", lambda e: e.matmul(pacc[h][:, 0:65], lhsT=PT[:, h * 128:(h + 1) * 128], rhs=Va[:, st, :], start=(st == 0), stop=(st == qt)), r=[PT, Va], w=[pacc[h]])
                        Pc = Pn
                        if st % 4 == 3:
                            yield
                    for h in range(4):
                        hd = hg * 4 + h
                        rz = rzs[st_["ri"] % 2]; st_["ri"] += 1
                        C.op("dve", lambda e: e.reciprocal(out=rz[:], in_=pacc[h][:, 64:65]), r=[pacc[h]], w=[rz])
                        C.op("act", lambda e: e.activation(out=OQ[:, hd * 64:(hd + 1) * 64], in_=pacc[h][:, 0:64], func=AF.Copy, scale=rz[:]), r=[pacc[h], rz], w=[OQ])
                    yield
                store(OM[ts, 0:512], OQ[:], OQ)
                yield

            def n_att(qt):
                return 2 * ((qt + 1) // 4 + 1) + 1

            run_merged(gen_idx(0), 1, None, 1)
            for qt in range(NT):
                gB = gen_idx(qt + 1) if qt + 1 < NT else None
                run_merged(gen_att(qt), n_att(qt), gB, n_idx(qt + 1) if gB else 1)

    def phase_A2():
        with C.phase():
            KT_ = C.sb("KBT", [128, S], BF16); QT_ = C.sb("QBT", [128, S], BF16)
            C.op("pool", lambda e: e.memset(KT_[64:128, :], 0.0), w=[KT_])
            C.op("pool", lambda e: e.memset(QT_[64:128, :], 0.0), w=[QT_])
            with C.phase():
                Ef = C.sb("Ef", [80, S], F32)
                C.op("pool", lambda e: e.memset(Ef[64:80, :], 1.0), w=[Ef])
                C.op("pool", lambda e: e.affine_select(out=Ef[64:80, :], in_=Ef[64:80, :], pattern=[[1, S]], compare_op=ALU.is_ge, fill=0.0, base=0, channel_multiplier=-256), r=[Ef], w=[Ef])
                C.op("pool", lambda e: e.affine_select(out=Ef[64:80, :], in_=Ef[64:80, :], pattern=[[-1, S]], compare_op=ALU.is_ge, fill=0.0, base=255, channel_multiplier=256), r=[Ef], w=[Ef])
                C.op("dve", lambda e: e.tensor_copy(out=KT_[64:80, :], in_=Ef[64:80, :]), r=[Ef], w=[KT_])
            cm = C.sb("cm", [128, NT, 16], F32); om = C.sb("om", [128, NT, 16], F32)
            C.op("pool", lambda e: e.memset(cm[:], 0.0), w=[cm])
            C.op("pool", lambda e: e.memset(om[:], 0.0), w=[om])
            for qt in range(NT):
                own = qt // 2
                C.op("pool", lambda e: e.memset(cm[:, qt, own:16], -1e30), w=[cm])
                C.op("pool", lambda e: e.memset(om[:, qt, own:own + 1], 1.0), w=[om])
            Va = C.sb("VBa", [128, NT, 65], BF16)
            C.op("pool", lambda e: e.memset(Va[:], 1.0), w=[Va])
            kmf = C.sb("kmf", [64, 16], F32); kmb = C.sb("kmb", [64, 16], BF16)
            pg = C.ps("pg", [128, NT, 16], F32)
            gm = C.sb("gm", [128, NT, 16], F32); m8 = C.sb("m8", [128, NT, 8], F32)
            sel = C.sb("sel", [128, NT, 16], F32)
            nbx = C.sb("nbx", [128, NT, 80], BF16)
            C.op("pool", lambda e: e.memset(nbx[:], 0.0), w=[nbx])
            pnt = C.ps("pnt", [80, 8, 128], BF16)
            pss = [C.ps("pss", [128, 512], F32) for _ in range(2)]
            pacc = [C.ps("pacc", [128, 512], F32) for _ in range(4)]
            PTs = [C.sb("PTs", [128, 512], BF16) for _ in range(3)]
            rzs = [C.sb("rz", [128, 1], F32) for _ in range(2)]
            ob = [C.sb("ob", [128, 64], BF16) for _ in range(3)]
            si = 0; pi = 0; oi = 0; ri = 0
            for h in range(8):
                C.dma("sp", KT_[0:64, :], KB_T[h], w=[KT_])
                C.dma("sp", QT_[0:64, :], QB_T[h], w=[QT_])
                C.dma("sp", Va[:, :, 0:64], VB[:, h * 64:(h + 1) * 64].rearrange("(t p) c -> p t c", p=128), w=[Va])
                C.op("dve", lambda e: e.tensor_reduce(out=kmf[:], in_=KT_[0:64, :].rearrange("d (n s) -> d n s", n=16), axis=AX.X, op=ALU.add), r=[KT_], w=[kmf])
                C.op("dve", lambda e: e.tensor_scalar(out=kmb[:], in0=kmf[:], scalar1=1.0 / 256, scalar2=None, op0=ALU.mult), r=[kmf], w=[kmb])
                for qt in range(NT):
                    C.op("pe", lambda e: e.matmul(pg[:, qt, :], lhsT=QT_[0:64, qt * 128:(qt + 1) * 128], rhs=kmb[:], start=True, stop=True), r=[QT_, kmb], w=[pg])
                C.op("dve", lambda e: e.tensor_tensor(out=gm[:], in0=pg[:], in1=cm[:], op=ALU.add), r=[pg, cm], w=[gm])
                for qt in range(NT):
                    C.op("dve", lambda e: e.max(out=m8[:, qt, :], in_=gm[:, qt, :]), r=[gm], w=[m8])
                C.op("dve", lambda e: e.tensor_tensor(out=sel[:], in0=gm[:], in1=ins_bc(m8[:, :, 2], 2, 16), op=ALU.is_ge), r=[gm, m8], w=[sel])
                C.op("dve", lambda e: e.tensor_tensor(out=sel[:], in0=sel[:], in1=om[:], op=ALU.max), r=[sel, om], w=[sel])
                C.op("dve", lambda e: e.tensor_scalar(out=nbx[:, :, 64:80], in0=sel[:], scalar1=1.0, scalar2=-NEG, op0=ALU.subtract, op1=ALU.mult), r=[sel], w=[nbx])
                for r4 in range(4):
                    for j in range(8):
                        qt = r4 * 8 + j
                        C.op("pe", lambda e: e.transpose(out=pnt[:, j, :], in_=nbx[:, qt, :], identity=ident[:]), r=[nbx, ident], w=[pnt])
                    C.op("act", lambda e: e.activation(out=QT_[64:80, r4 * 1024:(r4 + 1) * 1024], in_=pnt[64:80, :, :].rearrange("n j q -> n (j q)"), func=AF.Copy), r=[pnt], w=[QT_])
                steps_ = [(G, st) for G in range(8) for st in range(4 * G + 4)]

                def qk(G, st):
                    nonlocal si
                    P = pss[si % 2]; si += 1
                    ss_ = slice(st * 128, (st + 1) * 128); qs = slice(G * 512, (G + 1) * 512)
                    diag = st >= 4 * G
                    C.op("pe", lambda e: e.matmul(P[:], lhsT=KT_[:, ss_], rhs=QT_[:, qs], start=True, stop=not diag), r=[KT_, QT_], w=[P])
                    if diag:
                        C.op("pe", lambda e: e.matmul(P[:], lhsT=ident[:], rhs=CAUS[:, st - 4 * G, :, :], start=False, stop=True), r=[ident, CAUS], w=[P])
                    return P

                Pc = qk(*steps_[0])
                for k_, (G, st) in enumerate(steps_):
                    Pn = qk(*steps_[k_ + 1]) if k_ + 1 < len(steps_) else None
                    PT = PTs[pi % 3]; pi += 1
                    C.op("act", lambda e: e.activation(out=PT[:], in_=Pc[:], func=AF.Exp, scale=0.125), r=[Pc], w=[PT])
                    for qi in range(4):
                        qt = 4 * G + qi
                        if qt < st:
                            continue
                        C.op("pe", lambda e: e.matmul(pacc[qi][:, 0:65], lhsT=PT[:, qi * 128:(qi + 1) * 128], rhs=Va[:, st, :], start=(st == 0), stop=(st == qt)), r=[PT, Va], w=[pacc[qi]])
                    Pc = Pn
                    if st == 4 * G + 3:
                        for qi in range(4):
                            qt = 4 * G + qi
                            O_ = ob[oi % 3]; oi += 1
                            rz = rzs[ri % 2]; ri += 1
                            C.op("dve", lambda e: e.reciprocal(out=rz[:], in_=pacc[qi][:, 64:65]), r=[pacc[qi]], w=[rz])
                            C.op("act", lambda e: e.activation(out=O_[:], in_=pacc[qi][:, 0:64], func=AF.Copy, scale=rz[:]), r=[pacc[qi], rz], w=[O_])
                            store(OM[qt * 128:(qt + 1) * 128, 512 + h * 64:512 + (h + 1) * 64], O_[:], O_)

    def phase_A3(L, w_out_ap, x_src, x_dst):
        with C.phase():
            Wo = C.sb("Wo", [128, 8, D], BF16); load_w(Wo, w_out_ap, 8, D)
            g = C.sb("g", [128, 8], F32); load_gain(g, inp["norm_mem"][L])
            Wq = C.sb("Wq", [128, 8, 512], BF16); load_w(Wq, inp["mem_w_q"][L], 8, 512, g)
            Wom = C.sb("Wom", [128, 4, D], BF16); load_w(Wom, inp["mem_w_o"][L], 4, D)
            gqn = bgain(inp["mem_q_norm"][L], 128); gkn = bgain(inp["mem_k_norm"][L], 128)
            mK = C.sb("mK", [128, 4, 256], BF16); mV = C.sb("mV", [128, 2, 4, 129], BF16)
            C.op("pool", lambda e: e.memset(mV[:], 1.0), w=[mV])
            nt = NormT(); hnm = HeadNorm(4, 128)
            hT = C.sb("hT", [128, 8, 128], BF16)
            po = [C.ps("po", [128, 512], F32) for _ in range(2)]
            pr = C.sb("pr", [128, 512], F32); qn = C.sb("qn", [128, 512], BF16)
            ptq8 = C.ps("ptq", [128, 8, 128], BF16)
            ptq = T(ptq8.t, "x"); ptq.b = ptq8.b
            ptq_ap = ptq8[:, 0:4, :]
            with C.phase():
                gs = C.sb("gs", [128, 8], F32); load_gain(gs, inp["norm_mem_src"][L])
                Wkv = C.sb("Wkv", [128, 8, D], BF16); load_w(Wkv, inp["mem_w_kv"][L], 8, D, gs)
                mt_ = C.sb("mt", [128, D], F32)
                for mt in range(2):
                    C.dma("sp", mt_[:], mem_in[mt * 128:(mt + 1) * 128, :], w=[mt_])
                    nt.run(mt_, hT[:], hT)
                    for c in range(2):
                        P = po[c]
                        for kt in range(8):
                            C.op("pe", lambda e: e.matmul(P[:], lhsT=hT[:, kt, :], rhs=Wkv[:, kt, c * 512:(c + 1) * 512], start=(kt == 0), stop=(kt == 7)), r=[hT, Wkv], w=[P])
                        if c == 0:
                            C.op("act", lambda e: e.activation(out=pr[:], in_=P[:], func=AF.Copy), r=[P], w=[pr])
                            hnm.run(pr[:].rearrange("p (h d) -> p h d", h=4), pr, 4, gkn, qn[:].rearrange("p (h d) -> p h d", h=4), qn)
                            for h in range(4):
                                C.op("pe", lambda e: e.transpose(out=ptq[:, h, :], in_=qn[:, h * 128:(h + 1) * 128], identity=ident[:]), r=[qn, ident], w=[ptq])
                            C.op("dve", lambda e: e.tensor_copy(out=mK[:, :, mt * 128:(mt + 1) * 128], in_=ptq_ap), r=[ptq], w=[mK])
                        else:
                            C.op("act", lambda e: e.activation(out=mV[:, mt, :, 0:128], in_=P[:].rearrange("p (h d) -> p h d", h=4), func=AF.Copy), r=[P], w=[mV])
            pT = nt.pT
            lanes = []
            for ln in range(2):
                Lb = K()
                Lb.nt = nt if ln == 0 else NormT(pT=pT)
                Lb.hnm = hnm if ln == 0 else HeadNorm(4, 128)
                Lb.ot = C.sb("ot", [128, D], BF16); Lb.xt = C.sb("xt", [128, D], F32)
                Lb.oT = C.sb("oT", [128, 8, 128], BF16); Lb.x1 = C.sb("x1", [128, D], F32); Lb.x2 = C.sb("x2", [128, D], F32)
                Lb.hT = hT if ln == 0 else C.sb("hT", [128, 8, 128], BF16)
                Lb.pr = pr if ln == 0 else C.sb("pr", [128, 512], F32)
                Lb.qn = qn if ln == 0 else C.sb("qn", [128, 512], BF16)
                Lb.qT = C.sb("qT", [128, 4, 128], BF16)
                Lb.po = po[ln]
                Lb.pss = C.ps("pss", [128, 512], F32)
                Lb.PTm = [C.sb("PTm", [128, 512], BF16) for _ in range(2)]
                Lb.pov = C.ps("pov", [128, 2, 256], F32)
                Lb.rz = [C.sb("rz", [128, 1], F32) for _ in range(2)]
                Lb.omx = C.sb("omx", [128, 512], BF16); Lb.omT = C.sb("omT", [128, 4, 128], BF16)
                lanes.append(Lb)

            def gen_tile(Lb, t):
                ts = slice(t * 128, (t + 1) * 128)
                O_ = Lb.ot; X = Lb.xt; X2 = Lb.x2; x1 = Lb.x1; oT = Lb.oT; P = Lb.po
                C.dma("sp", O_[:], OM[ts, :], w=[O_])
                C.dma("sp", X[:], x_src[ts, :], w=[X])
                for kt in range(8):
                    C.op("pe", lambda e: e.transpose(out=pT[:, kt, :], in_=O_[:, kt * 128:(kt + 1) * 128], identity=ident[:]), r=[O_, ident], w=[pT])
                C.op("dve", lambda e: e.tensor_copy(out=oT[:], in_=pT[:]), r=[pT], w=[oT])
                yield
                for c in range(2):
                    for kt in range(8):
                        C.op("pe", lambda e: e.matmul(P[:], lhsT=oT[:, kt, :], rhs=Wo[:, kt, c * 512:(c + 1) * 512], start=(kt == 0), stop=(kt == 7)), r=[oT, Wo], w=[P])
                    C.op("dve", lambda e: e.tensor_tensor(out=x1[:, c * 512:(c + 1) * 512], in0=P[:], in1=X[:, c * 512:(c + 1) * 512], op=ALU.add), r=[P, X], w=[x1])
                    yield
                Lb.nt.run(x1, Lb.hT[:], Lb.hT)
                yield
                for kt in range(8):
                    C.op("pe", lambda e: e.matmul(P[:], lhsT=Lb.hT[:, kt, :], rhs=Wq[:, kt, :], start=(kt == 0), stop=(kt == 7)), r=[Lb.hT, Wq], w=[P])
                C.op("act", lambda e: e.activation(out=Lb.pr[:], in_=P[:], func=AF.Copy), r=[P], w=[Lb.pr])
                yield
                Lb.hnm.run(Lb.pr[:].rearrange("p (h d) -> p h d", h=4), Lb.pr, 4, gqn, Lb.qn[:].rearrange("p (h d) -> p h d", h=4), Lb.qn)
                yield
                for h in range(4):
                    C.op("pe", lambda e: e.transpose(out=ptq[:, h, :], in_=Lb.qn[:, h * 128:(h + 1) * 128], identity=ident[:]), r=[Lb.qn, ident], w=[ptq])
                C.op("dve", lambda e: e.tensor_copy(out=Lb.qT[:], in_=ptq_ap), r=[ptq], w=[Lb.qT])
                yield
                for mt in range(2):
                    PS = Lb.pss
                    for h in range(4):
                        C.op("pe", lambda e: e.matmul(PS[:, h * 128:(h + 1) * 128], lhsT=mK[:, h, mt * 128:(mt + 1) * 128], rhs=Lb.qT[:, h, :], start=True, stop=True), r=[mK, Lb.qT], w=[PS])
                    C.op("act", lambda e: e.activation(out=Lb.PTm[mt][:], in_=PS[:], func=AF.Exp, scale=float(128 ** -0.5)), r=[PS], w=[Lb.PTm[mt]])
                    yield
                for h in range(4):
                    A = Lb.pov
                    for mt in range(2):
                        C.op("pe", lambda e: e.matmul(A[:, h % 2, 0:129], lhsT=Lb.PTm[mt][:, h * 128:(h + 1) * 128], rhs=mV[:, mt, h, :], start=(mt == 0), stop=(mt == 1)), r=[Lb.PTm[mt], mV], w=[A])
                    rz = Lb.rz[h % 2]
                    C.op("dve", lambda e: e.reciprocal(out=rz[:], in_=A[:, h % 2, 128:129]), r=[A], w=[rz])
                    C.op("act", lambda e: e.activation(out=Lb.omx[:, h * 128:(h + 1) * 128], in_=A[:, h % 2, 0:128], func=AF.Copy, scale=rz[:]), r=[A, rz], w=[Lb.omx])
                    yield
                for h in range(4):
                    C.op("pe", lambda e: e.transpose(out=ptq[:, h, :], in_=Lb.omx[:, h * 128:(h + 1) * 128], identity=ident[:]), r=[Lb.omx, ident], w=[ptq])
                C.op("dve", lambda e: e.tensor_copy(out=Lb.omT[:], in_=ptq_ap), r=[ptq], w=[Lb.omT])
                yield
                for c in range(2):
                    for kt in range(4):
                        C.op("pe", lambda e: e.matmul(P[:], lhsT=Lb.omT[:, kt, :], rhs=Wom[:, kt, c * 512:(c + 1) * 512], start=(kt == 0), stop=(kt == 3)), r=[Lb.omT, Wom], w=[P])
                    C.op("dve", lambda e: e.tensor_tensor(out=X2[:, c * 512:(c + 1) * 512], in0=P[:], in1=x1[:, c * 512:(c + 1) * 512], op=ALU.add), r=[P, x1], w=[X2])
                    yield
                store(x_dst[ts, :], X2[:], X2)
                yield

            def lane_gen(ln):
                if ln == 1:
                    for _ in range(9):
                        yield
                for t in range(ln, NT, 2):
                    yield from gen_tile(lanes[ln], t)

            run_merged(lane_gen(0), 1, lane_gen(1), 1)

    def phase_A4(L, x_src, x_dst):
        with C.phase():
            g = C.sb("g", [128, 8], F32); load_gain(g, inp["norm_ffn"][L])
            Wi = C.sb("Wi", [128, 8, 2 * DFF], BF16); load_w(Wi, inp["ffn_w_in"][L], 8, 2 * DFF, g)
            Wd = C.sb("Wd", [128, 22, D], BF16); load_w(Wd, inp["ffn_w_out"][L], 22, D)
            nt = NormT()
            xt = [C.sb("xt", [128, D], F32) for _ in range(4)]
            hT = C.sb("hT", [128, 8, 512], BF16)
            hid = C.sb("hid", [128, 22, 512], BF16)
            psg = [C.ps("psg", [128, 512], F32) for _ in range(2)]
            psu = [C.ps("psu", [128, 512], F32) for _ in range(2)]
            po = [C.ps("po", [128, 512], F32) for _ in range(2)]
            sg = [C.sb("sg", [128, 512], BF16) for _ in range(2)]
            oi = 0
            for G in range(8):
                for tt in range(4):
                    t = G * 4 + tt
                    C.dma("sp", xt[tt][:], x_src[t * 128:(t + 1) * 128, :], w=[xt[tt]])
                    nt.run(xt[tt], hT[:, :, tt * 128:(tt + 1) * 128], hT)
                for ft in range(22):
                    Pg = psg[ft % 2]; Pu = psu[ft % 2]; Sg = sg[ft % 2]
                    for kt in range(8):
                        C.op("pe", lambda e: e.matmul(Pg[:], lhsT=Wi[:, kt, ft * 128:(ft + 1) * 128], rhs=hT[:, kt, :], start=(kt == 0), stop=(kt == 7)), r=[Wi, hT], w=[Pg])
                    for kt in range(8):
                        C.op("pe", lambda e: e.matmul(Pu[:], lhsT=Wi[:, kt, DFF + ft * 128:DFF + (ft + 1) * 128], rhs=hT[:, kt, :], start=(kt == 0), stop=(kt == 7)), r=[Wi, hT], w=[Pu])
                    C.op("act", lambda e: e.activation(out=Sg[:], in_=Pg[:], func=AF.Silu), r=[Pg], w=[Sg])
                    C.op("dve", lambda e: e.tensor_tensor(out=hid[:, ft, :], in0=Sg[:], in1=Pu[:], op=ALU.mult), r=[Sg, Pu], w=[hid])
                for tt in range(4):
                    t = G * 4 + tt
                    XO = xt[tt]
                    for c in range(2):
                        P = po[c]
                        for ft in range(22):
                            C.op("pe", lambda e: e.matmul(P[:], lhsT=hid[:, ft, tt * 128:(tt + 1) * 128], rhs=Wd[:, ft, c * 512:(c + 1) * 512], start=(ft == 0), stop=(ft == 21)), r=[hid, Wd], w=[P])
                        C.op("dve", lambda e: e.tensor_tensor(out=XO[:, c * 512:(c + 1) * 512], in0=P[:], in1=xt[tt][:, c * 512:(c + 1) * 512], op=ALU.add), r=[P, xt[tt]], w=[XO])
                    store(x_dst[t * 128:(t + 1) * 128, :], XO[:], XO)


    Q_T = SCR("Q_T", [16, 64, S], BF16); QR_T = SCR("QR_T", [16, 64, S], BF16)
    KCr_T = SCR("KCr_T", [4, 64, S], BF16); VCr_T = SCR("VCr_T", [4, 64, S], BF16)
    KS_T = SCR("KS_T", [4, 64, S], BF16); VS = SCR("VS", [S, 256], BF16)
    KW_T = SCR("KW_T", [4, 64, S], BF16); VW = SCR("VW", [S, 256], BF16)
    GT = SCR("GT", [S, 48], F32)
    KC_T = C.sb("KC_T", [128, 4, 256], BF16, stack=C.es)
    VCa = C.sb("VCa", [128, 2, 4, 129], BF16, stack=C.es)

    def phase_B0():
        with C.phase():
            g = C.sb("g", [128, 8], F32); load_gain(g, inp["norm_mix"][1])
            W = C.sb("W1", [128, 8, 2608], BF16)
            load_w(W, inp["nsa_w_in"], 8, 2608, g)
            gq = bgain(inp["nsa_q_norm"]); gks = bgain(inp["nsa_ksel_norm"]); gkw = bgain(inp["nsa_kwin_norm"])
            hnm = HeadNorm(8, 64, "pool")
            pr = [C.sb("pr", [128, 512], F32) for _ in range(2)]
            qn = [C.sb("qn", [128, 512], BF16) for _ in range(3)]
            qn2 = [C.sb("qn2", [128, 512], BF16) for _ in range(3)]
            ptq = [C.ps("ptq", [64, 8, 128], BF16) for _ in range(2)]
            qT = [C.sb("qT", [64, 8, 128], BF16) for _ in range(3)]
            chunks = [(0, 512), (512, 1024), (1024, 1536), (1536, 2048), (2048, 2560), (2560, 2608)]
            cnt = {"ti": 0}

            def tr_heads(SRC, H, src_off, dst_ap):
                i = cnt["ti"]; cnt["ti"] += 1
                PT = ptq[i % 2]; QT = qT[i % 3]
                for h in range(H):
                    C.op("pe", lambda e: e.transpose(out=PT[:, h, :], in_=SRC[:, src_off + h * 64: src_off + (h + 1) * 64], identity=ident[:]), r=[SRC, ident], w=[PT])
                C.op("dve", lambda e: e.tensor_copy(out=QT[:, 0:H, :], in_=PT[:, 0:H, :]), r=[PT], w=[QT])
                store(dst_ap, QT[:, 0:H, :], QT)

            def make_job(k, t, c, P):
                R = pr[k % 2]; Q = qn[k % 3]; Q2 = qn2[k % 3]
                ts = slice(t * 128, (t + 1) * 128)
                cs8 = cs64[:, t, :]; sn8 = sn64[:, t, :]
                v8 = lambda A_: A_[:].rearrange("p (h d) -> p h d", h=8)
                v4 = lambda A_: A_[:, 0:256].rearrange("p (h d) -> p h d", h=4)

                def post():
                    if c == 5:
                        C.op("act", lambda e: e.activation(out=R[:, 0:48], in_=P[:, 0:48], func=AF.Exp, scale=-1.0), r=[P], w=[R])
                        C.op("dve", lambda e: e.tensor_scalar(out=R[:, 0:48], in0=R[:, 0:48], scalar1=1.0, scalar2=None, op0=ALU.add), r=[R], w=[R])
                        C.op("dve", lambda e: e.reciprocal(out=R[:, 0:48], in_=R[:, 0:48]), r=[R], w=[R])
                        store(GT[ts, :], R[:, 0:48], R)
                        return
                    C.op("act", lambda e: e.activation(out=R[:], in_=P[:], func=AF.Copy), r=[P], w=[R])
                    if c in (0, 1):
                        hnm.run(v8(R), R, 8, gq, v8(Q), Q, rope=(cs8, sn8, 8), out2_ap=v8(Q2), out2T=Q2)
                    elif c == 2:
                        C.op("act", lambda e: e.activation(out=Q[:], in_=R[:], func=AF.Copy), r=[R], w=[Q])
                    else:
                        gg = gks if c == 3 else gkw
                        hnm.run(v4(R), R, 4, gg, v4(Q), Q, rope=(cs8, sn8, 8))
                        C.op("act", lambda e: e.activation(out=Q[:, 256:512], in_=R[:, 256:512], func=AF.Copy), r=[R], w=[Q])

                def trans():
                    if c == 5:
                        return
                    if c in (0, 1):
                        tr_heads(Q, 8, 0, QR_T[c * 8:(c + 1) * 8, :, ts].rearrange("h d s -> d h s"))
                        tr_heads(Q2, 8, 0, Q_T[c * 8:(c + 1) * 8, :, ts].rearrange("h d s -> d h s"))
                    elif c == 2:
                        tr_heads(Q, 4, 0, KCr_T[:, :, ts].rearrange("h d s -> d h s"))
                        tr_heads(Q, 4, 256, VCr_T[:, :, ts].rearrange("h d s -> d h s"))
                    else:
                        store((VS if c == 3 else VW)[ts, :], Q[:, 256:512], Q)
                        tr_heads(Q, 4, 0, (KS_T if c == 3 else KW_T)[:, :, ts].rearrange("h d s -> d h s"))
                return post, trans

            run_proj(XB, W, chunks, make_job)

    def phase_B1():
        with C.phase():
            C.op("pool", lambda e: e.memset(KC_T[:], 0.0), w=[KC_T])
            C.op("pool", lambda e: e.memset(VCa[:], 0.0), w=[VCa])
            C.op("pool", lambda e: e.memset(VCa[:, :, :, 64:65], 1.0), w=[VCa])
            cov = C.sb("cov", [128, 2, 64], F32)
            C.op("pool", lambda e: e.memset(cov[:], 1.0), w=[cov])
            for half in range(2):
                C.op("pool", lambda e: e.affine_select(out=cov[:, half, :], in_=cov[:, half, :], pattern=[[-64, 64]], compare_op=ALU.is_gt, fill=0.0, base=2048 * half + 32, channel_multiplier=16), r=[cov], w=[cov])
                C.op("pool", lambda e: e.affine_select(out=cov[:, half, :], in_=cov[:, half, :], pattern=[[64, 64]], compare_op=ALU.is_gt, fill=0.0, base=64 - 2048 * half, channel_multiplier=-16), r=[cov], w=[cov])
                C.op("dve", lambda e: e.tensor_copy(out=VCa[:, half, :, 65:129], in_=ins_bc(cov[:, half, :], 1, 4)), r=[cov], w=[VCa])
            gkc = bgain(inp["nsa_kcmp_norm"])
            hnm = HeadNorm(1, 64)
            XT = C.sb("XT", [64, 4, S], BF16)
            w1f = C.sb("w1f", [64, 32, 64], F32); w1b = C.sb("w1b", [64, 32, 64], BF16)
            w2f = C.sb("w2f", [64, 64], F32); w2b = C.sb("w2b", [64, 64], BF16)
            posf = C.sb("posf", [64, 32], F32); posrep = C.sb("posrep", [64, 32, 128], BF16)
            cpos = C.sb("cpos", [128, 64], F32)
            ph = [C.ps("ph", [128, 512], F32) for _ in range(2)]
            pt_ = C.ps("pt_", [64, 8, 128], BF16)
            hs = C.sb("hs", [128, 64], F32); hsb = C.sb("hsb", [128, 64], BF16); hsT = C.sb("hsT", [64, 128], BF16)
            o2 = C.sb("o2", [128, 64], F32); o2b = C.sb("o2b", [128, 64], BF16)
            pi = 0
            for kv in range(2):
                src = (KCr_T, VCr_T)[kv]
                C.dma("sp", XT[:], src.rearrange("g d s -> d g s"), w=[XT])
                C.dma("sp", w1f[:], inp[("nsa_cmp_w1_k", "nsa_cmp_w1_v")[kv]].rearrange("(l d) o -> d l o", d=64), w=[w1f])
                C.dma("sp", w2f[:], inp[("nsa_cmp_w2_k", "nsa_cmp_w2_v")[kv]], w=[w2f])
                C.dma("sp", posf[:], inp[("nsa_cmp_pos_k", "nsa_cmp_pos_v")[kv]].rearrange("l d -> d l"), w=[posf], allow_slow_non_contiguous=True)
                C.op("dve", lambda e: e.tensor_copy(out=w1b[:], in_=w1f[:]), r=[w1f], w=[w1b])
                C.op("dve", lambda e: e.tensor_copy(out=w2b[:], in_=w2f[:]), r=[w2f], w=[w2b])
                C.op("dve", lambda e: e.tensor_copy(out=posrep[:], in_=ins_bc(posf[:], 2, 128)), r=[posf], w=[posrep])
                P = ph[pi % 2]; pi += 1
                for l in range(32):
                    C.op("pe", lambda e: e.matmul(P[:, 0:64], lhsT=posrep[:, l, :], rhs=w1b[:, l, :], start=(l == 0), stop=(l == 31)), r=[posrep, w1b], w=[P])
                C.op("dve", lambda e: e.tensor_copy(out=cpos[:], in_=P[:, 0:64]), r=[P], w=[cpos])
                for g_ in range(4):
                    for half in range(2):
                        M = 128 if half == 0 else 127
                        P = ph[pi % 2]; pi += 1
                        for l in range(32):
                            a0 = 2048 * half + l
                            C.op("pe", lambda e: e.matmul(P[0:M, 0:64], lhsT=XT[:, g_, a0:a0 + 16 * (M - 1) + 1:16], rhs=w1b[:, l, :], start=(l == 0), stop=(l == 31)), r=[XT, w1b], w=[P])
                        C.op("dve", lambda e: e.tensor_tensor(out=hs[0:M, :], in0=P[0:M, 0:64], in1=cpos[0:M, :], op=ALU.add), r=[P, cpos], w=[hs])
                        C.op("act", lambda e: e.activation(out=hsb[0:M, :], in_=hs[0:M, :], func=AF.Silu), r=[hs], w=[hsb])
                        C.op("pe", lambda e: e.transpose(out=pt_[:, 0, 0:M], in_=hsb[0:M, :], identity=ident[0:M, 0:M]), r=[hsb, ident], w=[pt_])
                        C.op("dve", lambda e: e.tensor_copy(out=hsT[:, 0:M], in_=pt_[:, 0, 0:M]), r=[pt_], w=[hsT])
                        P2 = ph[pi % 2]; pi += 1
                        C.op("pe", lambda e: e.matmul(P2[0:M, 0:64], lhsT=hsT[:, 0:M], rhs=w2b[:], start=True, stop=True), r=[hsT, w2b], w=[P2])
                        if kv == 0:
                            C.op("act", lambda e: e.activation(out=o2[0:M, :], in_=P2[0:M, 0:64], func=AF.Copy), r=[P2], w=[o2])
                            hnm.run(o2[:].rearrange("p (h d) -> p h d", h=1), o2, 1, gkc, o2b[:].rearrange("p (h d) -> p h d", h=1), o2b)
                            C.op("pe", lambda e: e.transpose(out=pt_[:, 1, 0:M], in_=o2b[0:M, :], identity=ident[0:M, 0:M]), r=[o2b, ident], w=[pt_])
                            C.op("dve", lambda e: e.tensor_copy(out=KC_T[0:64, g_, half * 128:half * 128 + M], in_=pt_[:, 1, 0:M]), r=[pt_], w=[KC_T])
                        else:
                            C.op("act", lambda e: e.activation(out=VCa[0:M, half, g_, 0:64], in_=P2[0:M, 0:64], func=AF.Copy), r=[P2], w=[VCa])

    def phase_B2():
        with C.phase():
            KS = C.sb("KS", [128, 4, S], BF16); C.dma("sp", KS[0:64, :, :], KS_T.rearrange("g d s -> d g s"), w=[KS])
            KW = C.sb("KW", [128, 4, S], BF16)
            C.op("pool", lambda e: e.memset(KW[64:128, :, :], 0.0), w=[KW])
            C.dma("sp", KW[0:64, :, :], KW_T.rearrange("g d s -> d g s"), w=[KW])
            VSa = C.sb("VSa", [128, NT, 4, 65], BF16); VWa = C.sb("VWa", [128, NT, 4, 65], BF16)
            C.op("pool", lambda e: e.memset(VSa[:], 1.0), w=[VSa])
            C.op("pool", lambda e: e.memset(VWa[:], 1.0), w=[VWa])
            for t in range(NT):
                C.dma("sp", VSa[:, t, :, 0:64], VS[t * 128:(t + 1) * 128, :].rearrange("p (g d) -> p g d", g=4), w=[VSa])
                C.dma("sp", VWa[:, t, :, 0:64], VW[t * 128:(t + 1) * 128, :].rearrange("p (g d) -> p g d", g=4), w=[VWa])
            with C.phase():
                Ef = C.sb("E2f", [128, S], F32)
                C.op("pool", lambda e: e.memset(Ef[64:128, :], 1.0), w=[Ef])
                C.op("pool", lambda e: e.affine_select(out=Ef[64:128, :], in_=Ef[64:128, :], pattern=[[1, S]], compare_op=ALU.is_ge, fill=0.0, base=0, channel_multiplier=-64), r=[Ef], w=[Ef])
                C.op("pool", lambda e: e.affine_select(out=Ef[64:128, :], in_=Ef[64:128, :], pattern=[[-1, S]], compare_op=ALU.is_ge, fill=0.0, base=63, channel_multiplier=64), r=[Ef], w=[Ef])
                for g_ in range(4):
                    C.op("dve" if g_ % 2 == 0 else "act", (lambda e: e.tensor_copy(out=KS[64:128, g_, :], in_=Ef[64:128, :])) if g_ % 2 == 0 else (lambda e: e.activation(out=KS[64:128, g_, :], in_=Ef[64:128, :], func=AF.Copy)), r=[Ef], w=[KS])
            V0 = C.sb("V0", [128, 128], F32)
            C.op("pool", lambda e: e.iota(out=V0[:], pattern=[[-1, 128]], base=0, channel_multiplier=16, allow_small_or_imprecise_dtypes=True), w=[V0])
            J = C.sb("J", [128, 64], F32)
            C.op("pool", lambda e: e.iota(out=J[:], pattern=[[1, 64]], base=0, channel_multiplier=0, allow_small_or_imprecise_dtypes=True), w=[J])
            f0 = C.sb("f0", [128, 64], F32)
            C.op("dve", lambda e: e.tensor_scalar(out=f0[:], in0=J[:], scalar1=0.0, scalar2=None, op0=ALU.is_equal), r=[J], w=[f0])
            CURb = C.sb("CURb", [128, 1], F32)
            C.op("pool", lambda e: e.iota(out=CURb[:], pattern=[[0, 1]], base=0, channel_multiplier=1, allow_small_or_imprecise_dtypes=True), w=[CURb])
            C.op("dve", lambda e: e.tensor_scalar(out=CURb[:], in0=CURb[:], scalar1=64.0, scalar2=None, op0=ALU.is_ge), r=[CURb], w=[CURb])
            sm = {n: C.sb(n, [128, 1], F32) for n in ("curq", "cm1")}
            rz4s = [C.sb("rz4", [128, 4], F32) for _ in range(2)]; cf4s = [C.sb("cf4", [128, 4], F32) for _ in range(2)]
            f1 = C.sb("f1", [128, 64], F32); f2 = C.sb("f2", [128, 64], F32)
            FORCE = C.sb("FORCE", [128, 64], F32); FUT = C.sb("FUT", [128, 64], F32)
            imp = C.sb("imp", [128, 64], F32); imp3 = C.sb("imp3", [128, 64], F32); impr = C.sb("impr", [128, 64], F32)
            imt = C.sb("imt", [128, 4, 64], F32)
            m8a = C.sb("m8a", [128, 8], F32); m8b = C.sb("m8b", [128, 8], F32)
            nm = C.sb("nm", [128, 128], BF16)
            C.op("pool", lambda e: e.memset(nm[:], 0.0), w=[nm])
            pnm = C.ps("pnm", [128, 8, 128], BF16)
            cb = [C.sb("cb", [128, 4, 128], BF16) for _ in range(2)]
            qnt = [C.sb("qnt", [128, 16, 128], BF16) for _ in range(2)]
            qrt = [C.sb("qrt", [128, 16, 128], BF16) for _ in range(2)]
            for t_ in qnt + qrt:
                C.op("pool", lambda e: e.memset(t_[:], 0.0), w=[t_])
            gts = [C.sb("gts", [128, 48], F32) for _ in range(2)]
            acc = C.sb("acc", [128, D], F32)
            obf = [C.sb("obf", [128, D], BF16) for _ in range(2)]
            pss = [C.ps("pss", [128, 512], F32) for _ in range(2)]
            pacc = C.ps("pacc", [128, 4, 512], F32)
            PTs = [C.sb("PTs", [128, 512], BF16) for _ in range(3)]
            si = 0; pi = 0; zi = 0
            for qt in range(NT):
                ts = slice(qt * 128, (qt + 1) * 128)
                QN = qnt[qt % 2]; QR = qrt[qt % 2]; Gt = gts[qt % 2]; OB = obf[qt % 2]
                C.dma("sp", QN[0:64, :, :], Q_T[:, :, ts].rearrange("h d s -> d h s"), w=[QN])
                C.dma("sp", QR[0:64, :, :], QR_T[:, :, ts].rearrange("h d s -> d h s"), w=[QR])
                C.dma("sp", Gt[:], GT[ts, :], w=[Gt])
                nhalf = 1 if qt < 16 else 2
                for half in range(nhalf):
                    C.op("dve", lambda e: e.tensor_scalar(out=cb[half][:], in0=ins_bc(V0[:], 1, 4), scalar1=float(128 * qt - 31 - 2048 * half), scalar2=NEG, op0=ALU.is_gt, op1=ALU.mult), r=[V0], w=[cb[half]])
                C.op("dve", lambda e: e.tensor_scalar(out=sm["curq"][:], in0=CURb[:], scalar1=float(2 * qt), scalar2=None, op0=ALU.add), r=[CURb], w=[sm["curq"]])
                C.op("dve", lambda e: e.tensor_scalar(out=sm["cm1"][:], in0=CURb[:], scalar1=float(2 * qt - 1), scalar2=None, op0=ALU.add), r=[CURb], w=[sm["cm1"]])
                C.op("dve", lambda e: e.tensor_scalar(out=f1[:], in0=J[:], scalar1=sm["curq"][:], scalar2=None, op0=ALU.is_equal), r=[J, sm["curq"]], w=[f1])
                C.op("dve", lambda e: e.tensor_scalar(out=f2[:], in0=J[:], scalar1=sm["cm1"][:], scalar2=None, op0=ALU.is_equal), r=[J, sm["cm1"]], w=[f2])
                C.op("dve", lambda e: e.tensor_tensor(out=f1[:], in0=f1[:], in1=f2[:], op=ALU.add), r=[f1, f2], w=[f1])
                C.op("dve", lambda e: e.tensor_tensor(out=f1[:], in0=f1[:], in1=f0[:], op=ALU.add), r=[f1, f0], w=[f1])
                C.op("dve", lambda e: e.tensor_scalar(out=FORCE[:], in0=f1[:], scalar1=1e4, scalar2=None, op0=ALU.mult), r=[f1], w=[FORCE])
                C.op("dve", lambda e: e.tensor_scalar(out=FUT[:], in0=J[:], scalar1=sm["curq"][:], scalar2=-1e30, op0=ALU.is_gt, op1=ALU.mult), r=[J, sm["curq"]], w=[FUT])

                def branch(g_, qk_fn, sts, Vrhs_fn, ncol, gidx, first):
                    nonlocal si, pi, zi
                    n = len(sts)

                    def qk(st):
                        nonlocal si
                        P = pss[si % 2]; si += 1
                        mm = qk_fn(st)
                        for ei, (l_ap, lT, r_ap, rT) in enumerate(mm):
                            C.op("pe", lambda e: e.matmul(P[:], lhsT=l_ap, rhs=r_ap, start=(ei == 0), stop=(ei == len(mm) - 1)), r=[lT, rT], w=[P])
                        return P

                    Pc = qk(sts[0])
                    for ix, st in enumerate(sts):
                        Pn = qk(sts[ix + 1]) if ix + 1 < n else None
                        PT = PTs[pi % 3]; pi += 1
                        C.op("act", lambda e: e.activation(out=PT[:], in_=Pc[:], func=AF.Exp, scale=0.125), r=[Pc], w=[PT])
                        v_ap, vT = Vrhs_fn(st)
                        for j in range(4):
                            C.op("pe", lambda e: e.matmul(pacc[:, j, 0:ncol], lhsT=PT[:, j * 128:(j + 1) * 128], rhs=v_ap, start=(ix == 0), stop=(ix == n - 1)), r=[PT, vT], w=[pacc])
                        Pc = Pn
                    rz4 = rz4s[zi % 2]; cf4 = cf4s[zi % 2]; zi += 1
                    C.op("dve", lambda e: e.tensor_scalar(out=rz4[:], in0=pacc[:, :, 64], scalar1=1e-20, scalar2=None, op0=ALU.max), r=[pacc], w=[rz4])
                    C.op("dve", lambda e: e.reciprocal(out=rz4[:], in_=rz4[:]), r=[rz4], w=[rz4])
                    C.op("dve", lambda e: e.tensor_tensor(out=cf4[:], in0=rz4[:], in1=Gt[:, 12 * g_ + gidx:12 * g_ + gidx + 10:3], op=ALU.mult), r=[rz4, Gt], w=[cf4])
                    for j in range(4):
                        hd = 4 * g_ + j
                        ah = acc[:, hd * 64:(hd + 1) * 64]
                        if first:
                            C.op("act", lambda e: e.activation(out=ah, in_=pacc[:, j, 0:64], func=AF.Copy, scale=cf4[:, j:j + 1]), r=[pacc, cf4], w=[acc])
                        else:
                            C.op("dve", lambda e: e.scalar_tensor_tensor(out=ah, in0=pacc[:, j, 0:64], scalar=cf4[:, j:j + 1], in1=ah, op0=ALU.mult, op1=ALU.add), r=[pacc, cf4, acc], w=[acc])
                    if first:
                        C.op("dve", lambda e: e.tensor_tensor(out=imt[:], in0=pacc[:, :, 65:129], in1=ins_bc(rz4[:], 2, 64), op=ALU.mult), r=[pacc, rz4], w=[imt])
                        C.op("dve", lambda e: e.tensor_reduce(out=imp[:], in_=imt[:].rearrange("p j d -> p d j"), axis=AX.X, op=ALU.add), r=[imt], w=[imp])

                for g_ in range(4):
                    branch(g_, lambda st: [(KC_T[:, g_, st * 128:(st + 1) * 128], KC_T, QN[:, 4 * g_:4 * g_ + 4, :], QN), (ident[:], ident, cb[st][:], cb[st])],
                           list(range(nhalf)), lambda st: (VCa[:, st, g_, :], VCa), 129, 0, True)
                    C.op("dve", lambda e: e.tensor_tensor(out=imp3[:], in0=imp[:], in1=FORCE[:], op=ALU.max), r=[imp, FORCE], w=[imp3])
                    C.op("dve", lambda e: e.tensor_tensor(out=imp3[:], in0=imp3[:], in1=FUT[:], op=ALU.add), r=[imp3, FUT], w=[imp3])
                    C.op("dve", lambda e: e.max(out=m8a[:], in_=imp3[:]), r=[imp3], w=[m8a])
                    C.op("dve", lambda e: e.match_replace(out=impr[:], in_to_replace=m8a[:], in_values=imp3[:], imm_value=-1e30), r=[imp3, m8a], w=[impr])
                    C.op("dve", lambda e: e.max(out=m8b[:], in_=impr[:]), r=[impr], w=[m8b])
                    C.op("dve", lambda e: e.tensor_scalar(out=nm[:, 64:128], in0=imp3[:], scalar1=m8b[:, 7:8], scalar2=NEG, op0=ALU.is_lt, op1=ALU.mult), r=[imp3, m8b], w=[nm])
                    def selqk(st):
                        m = [(KS[:, g_, st * 128:(st + 1) * 128], KS, QR[:, 4 * g_:4 * g_ + 4, :], QR)]
                        if st == qt:
                            m.append((ident[:], ident, TRI4[:], TRI4))
                        return m

                    def winqk(st):
                        m = [(KW[:, g_, st * 128:(st + 1) * 128], KW, QR[:, 4 * g_:4 * g_ + 4, :], QR)]
                        if st == qt:
                            m.append((ident[:], ident, TRI4[:], TRI4))
                        elif st == qt - 4:
                            m.append((ident[:], ident, BLO4[:], BLO4))
                        return m
                    branch(g_, winqk, list(range(max(0, qt - 4), qt + 1)), lambda st: (VWa[:, st, g_, :], VWa), 65, 2, False)
                    C.op("pe", lambda e: e.transpose(out=pnm[:, 0, :], in_=nm[:], identity=ident[:]), r=[nm, ident], w=[pnm])
                    C.op("dve", lambda e: e.tensor_copy(out=QR[64:128, 4 * g_:4 * g_ + 4, :], in_=ins_bc(pnm[64:128, 0, :], 1, 4)), r=[pnm], w=[QR])

                    branch(g_, selqk, list(range(qt + 1)), lambda st: (VSa[:, st, g_, :], VSa), 65, 1, False)
                C.op("act", lambda e: e.activation(out=OB[:], in_=acc[:], func=AF.Copy), r=[acc], w=[OB])
                store(OM[ts, :], OB[:], OB)

    if upto >= 1:
        phase_A0()
    if upto >= 2:
        phase_A1()
    if upto >= 3:
        phase_A2()
    if upto >= 4:
        phase_A3(0, inp["ab_w_out"], x_in, XA)
    if upto >= 5:
        phase_A4(0, XA, XB if (dbg or upto > 5) else y_out)
    if upto >= 6:
        phase_B0()
    if upto >= 7:
        phase_B1()
    if upto >= 8:
        phase_B2()
    if upto >= 9:
        phase_A3(1, inp["nsa_w_out"], XB, XC)
    if upto >= 10:
        phase_A4(1, XC, y_out)
    C.close()
    k.nc = nc; k.dbg_names = dbg_names; k.ninstr = C.ninstr; k.inp_names = list(inp)
    return k


_W_NAMES = ["norm_mix", "norm_mem", "norm_mem_src", "norm_ffn", "mem_w_q", "mem_w_kv", "mem_w_o", "mem_q_norm", "mem_k_norm", "ffn_w_in", "ffn_w_out"]
_W0_NAMES = ["ab_w_in", "ab_w_out", "dsa_q_norm", "dsa_k_norm", "moba_q_norm", "moba_k_norm", "nsa_w_in", "nsa_w_out", "nsa_q_norm", "nsa_kcmp_norm",
             "nsa_ksel_norm", "nsa_kwin_norm", "nsa_cmp_pos_k", "nsa_cmp_pos_v", "nsa_cmp_w1_k", "nsa_cmp_w2_k", "nsa_cmp_w1_v", "nsa_cmp_w2_v"]


def make_in_map(inputs, b):
    m = {"x": np.ascontiguousarray(inputs["x"][b], dtype=np.float32),
         "mem": np.ascontiguousarray(inputs["mem"][b], dtype=np.float32),
         "post": np.ascontiguousarray(np.asarray(inputs["positions"][b]).astype(np.int32).reshape(NT, 128).T)}
    for n in _W_NAMES:
        m[n] = np.ascontiguousarray(inputs[n], dtype=np.float32)
    for n in _W0_NAMES:
        m[n] = np.ascontiguousarray(np.asarray(inputs[n])[0], dtype=np.float32)
    return m


def kernel(**inputs):
    from concourse.bass_utils import run_bass_kernel_spmd
    k = build(dbg=False)
    in_maps = [make_in_map(inputs, b) for b in range(8)]
    res = run_bass_kernel_spmd(k.nc, in_maps, core_ids=list(range(8)))
    return np.stack([np.asarray(r["y"], dtype=np.float32) for r in res.results], axis=0)
```

```python
from contextlib import ExitStack
import numpy as np
import concourse.bass as bass
import concourse.mybir as mybir

F32 = mybir.dt.float32
BF16 = mybir.dt.bfloat16
I32 = mybir.dt.int32
AF = mybir.ActivationFunctionType
ALU = mybir.AluOpType
AX = mybir.AxisListType

N_DMA_SLOTS = 24
N_HW_SLOTS = 14


class Buf:
    __slots__ = ("name", "w", "r")

    def __init__(self, name):
        self.name = name
        self.w = None
        self.r = {}


class T:
    def __init__(self, t, name):
        self.t = t
        self.b = Buf(name)

    def __getitem__(self, k):
        return self.t[k]


class Ctx:
    def __init__(self, nc):
        self.nc = nc
        self.es = ExitStack()
        self.eng = {"pe": nc.tensor, "act": nc.scalar, "dve": nc.vector, "pool": nc.gpsimd, "sp": nc.sync}
        self.sem = {}
        for k in ("pe", "act", "dve", "pool"):
            self.sem[k] = self.es.enter_context(nc.semaphore("sem_" + k))
        for i in range(N_DMA_SLOTS):
            self.sem[("d", i)] = self.es.enter_context(nc.semaphore("semd%d" % i))
        self.cnt = {k: 0 for k in ("pe", "act", "dve", "pool")}
        self.dval = [0] * N_DMA_SLOTS
        self.dnext = 0
        self.dnext_sw = 0
        self.seen = {k: {} for k in self.eng}
        self.uid = 0
        self.phase_stack = None
        self.ninstr = 0

    def _nm(self, name):
        self.uid += 1
        return "%s_%d" % (name, self.uid)

    def sb(self, name, shape, dtype, stack=None):
        st = stack if stack is not None else (self.phase_stack or self.es)
        nm = self._nm(name)
        return T(st.enter_context(self.nc.sbuf_tensor(nm, list(shape), dtype)), nm)

    def ps(self, name, shape, dtype, stack=None):
        st = stack if stack is not None else (self.phase_stack or self.es)
        nm = self._nm(name)
        return T(st.enter_context(self.nc.psum_tensor(nm, list(shape), dtype)), nm)

    def dram(self, name, shape, dtype, kind="Internal"):
        return self.nc.dram_tensor(name, list(shape), dtype, kind=kind).ap()

    def _waits(self, E, reads, writes):
        deps = {}

        def need(k, v):
            if v > deps.get(k, 0):
                deps[k] = v

        for t in reads:
            b = t.b
            if b.w is not None:
                need(*b.w)
        for t in writes:
            b = t.b
            if b.w is not None:
                need(*b.w)
            for k, v in b.r.items():
                need(k, v)
        eng = self.eng[E]
        seen = self.seen[E]
        for k, v in deps.items():
            if k == E and E == "pe":
                continue
            if seen.get(k, 0) >= v:
                continue
            eng.wait_ge(self.sem[k], v)
            self.ninstr += 1
            seen[k] = v

    def op(self, E, fn, r=(), w=()):
        self._waits(E, r, w)
        inst = fn(self.eng[E])
        self.cnt[E] += 1
        self.ninstr += 1
        inst.then_inc(self.sem[E], 1)
        ev = self.cnt[E]
        for t in r:
            if t.b.r.get(E, 0) < ev:
                t.b.r[E] = ev
        for t in w:
            t.b.w = (E, ev)
            t.b.r = {}
        return inst

    def dma(self, Q, out, in_, r=(), w=(), **kw):
        self._waits(Q, r, w)
        slot = self.dnext
        self.dnext = (self.dnext + 1) % N_DMA_SLOTS
        k = ("d", slot)
        eng = self.eng[Q]
        if self.seen[Q].get(k, 0) < self.dval[slot]:
            eng.wait_ge(self.sem[k], self.dval[slot])
            self.seen[Q][k] = self.dval[slot]
            self.ninstr += 1
        inst = eng.dma_start(out=out, in_=in_, **kw)
        self.dval[slot] += 16
        inst.then_inc(self.sem[k], 16)
        self.ninstr += 1
        v = self.dval[slot]
        for t in r:
            if t.b.r.get(k, 0) < v:
                t.b.r[k] = v
        for t in w:
            t.b.w = (k, v)
            t.b.r = {}
        return inst

    def barrier(self):
        for E, eng in self.eng.items():
            seen = self.seen[E]
            for k in ("pe", "act", "dve", "pool"):
                v = self.cnt[k]
                if v == 0 or seen.get(k, 0) >= v or (k == E and E == "pe"):
                    continue
                eng.wait_ge(self.sem[k], v)
                seen[k] = v
                self.ninstr += 1
            for i in range(N_DMA_SLOTS):
                k = ("d", i)
                v = self.dval[i]
                if v == 0 or seen.get(k, 0) >= v:
                    continue
                eng.wait_ge(self.sem[k], v)
                seen[k] = v
                self.ninstr += 1

    def phase(self):
        ctx = self

        class _P:
            def __enter__(s):
                ctx.barrier()
                s.prev = ctx.phase_stack
                s.st = ExitStack()
                ctx.phase_stack = s.st
                return s

            def __exit__(s, *a):
                ctx.barrier()
                ctx.phase_stack = s.prev
                s.st.close()
                return False

        return _P()

    def close(self):
        self.barrier()
        self.es.close()


S = 4096
D = 1024
NT = 32
NEG = -30000.0
EPS = 1e-6
THETA = 500000.0
DFF = 2816


def ins_bc(ap, axis, n):
    l = [list(p) for p in ap.ap]
    l.insert(axis, [0, n])
    return bass.AP(ap.tensor, ap.offset, l)


class K:
    pass


def build(dbg=False, upto=99):
    nc = bass.Bass("TRN2", target_bir_lowering=False)
    k = K()
    inp = {}

    def IN(name, shape, dt=F32):
        inp[name] = nc.dram_tensor(name, list(shape), dt, kind="ExternalInput").ap()
        return inp[name]

    x_in = IN("x", [S, D]); mem_in = IN("mem", [256, D]); pos_in = IN("post", [128, NT], I32)
    for n in ("norm_mix", "norm_mem", "norm_mem_src", "norm_ffn"):
        IN(n, [2, D])
    IN("ab_w_in", [D, 2472]); IN("ab_w_out", [D, D])
    for n in ("dsa_q_norm", "dsa_k_norm", "moba_q_norm", "moba_k_norm", "nsa_q_norm", "nsa_kcmp_norm", "nsa_ksel_norm", "nsa_kwin_norm"):
        IN(n, [64])
    IN("nsa_w_in", [D, 2608]); IN("nsa_w_out", [D, D])
    IN("nsa_cmp_pos_k", [32, 64]); IN("nsa_cmp_pos_v", [32, 64])
    IN("nsa_cmp_w1_k", [2048, 64]); IN("nsa_cmp_w2_k", [64, 64]); IN("nsa_cmp_w1_v", [2048, 64]); IN("nsa_cmp_w2_v", [64, 64])
    IN("mem_w_q", [2, D, 512]); IN("mem_w_kv", [2, D, 1024]); IN("mem_w_o", [2, 512, D])
    IN("mem_q_norm", [2, 128]); IN("mem_k_norm", [2, 128])
    IN("ffn_w_in", [2, D, 2 * DFF]); IN("ffn_w_out", [2, DFF, D])
    y_out = nc.dram_tensor("y", [S, D], F32, kind="ExternalOutput").ap()

    C = Ctx(nc)
    skind = "ExternalOutput" if dbg else "Internal"
    dbg_names = []

    def SCR(name, shape, dt):
        if dbg:
            dbg_names.append(name)
        return nc.dram_tensor(name, list(shape), dt, kind=skind).ap()

    ident = C.sb("ident", [128, 128], BF16)
    I4 = C.sb("I4", [128, 4, 128], BF16)
    epsG = C.sb("epsG", [128, 1], F32)
    C.op("pool", lambda e: e.memset(epsG[:], EPS), w=[epsG])
    TRI4 = C.sb("TRI4", [128, 4, 128], BF16)
    BLO4 = C.sb("BLO4", [128, 4, 128], BF16)
    CAUS = C.sb("CAUS", [128, 4, 4, 128], BF16)
    cs64 = C.sb("cs64", [128, NT, 8], F32); sn64 = C.sb("sn64", [128, NT, 8], F32)
    cs32 = C.sb("cs32", [128, NT, 4], F32); sn32 = C.sb("sn32", [128, NT, 4], F32)
    with C.phase():
        cf = C.sb("cf", [128, 128], F32)
        C.op("pool", lambda e: e.memset(cf[:], 0.0), w=[cf])
        C.op("pool", lambda e: e.affine_select(out=cf[:], in_=cf[:], pattern=[[-1, 128]], compare_op=ALU.not_equal, fill=1.0, base=0, channel_multiplier=1), r=[cf], w=[cf])
        C.op("dve", lambda e: e.tensor_copy(out=ident[:], in_=cf[:]), r=[cf], w=[ident])
        C.op("dve", lambda e: e.tensor_copy(out=I4[:], in_=ins_bc(cf[:], 1, 4)), r=[cf], w=[I4])
        tf = C.sb("tf", [128, 128], F32)
        C.op("pool", lambda e: e.memset(tf[:], 0.0), w=[tf])
        C.op("pool", lambda e: e.affine_select(out=tf[:], in_=tf[:], pattern=[[1, 128]], compare_op=ALU.is_ge, fill=NEG, base=0, channel_multiplier=-1), r=[tf], w=[tf])
        C.op("dve", lambda e: e.tensor_copy(out=TRI4[:], in_=ins_bc(tf[:], 1, 4)), r=[tf], w=[TRI4])
        for j in range(4):
            for qi in range(4):
                if qi > j:
                    C.op("dve", lambda e: e.memset(CAUS[:, j, qi, :], 0.0), w=[CAUS])
                elif qi == j:
                    C.op("dve", lambda e: e.tensor_copy(out=CAUS[:, j, qi, :], in_=tf[:]), r=[tf], w=[CAUS])
                else:
                    C.op("dve", lambda e: e.memset(CAUS[:, j, qi, :], NEG), w=[CAUS])
        tf2 = C.sb("tf2", [128, 128], F32)
        C.op("pool", lambda e: e.memset(tf2[:], 0.0), w=[tf2])
        C.op("pool", lambda e: e.affine_select(out=tf2[:], in_=tf2[:], pattern=[[-1, 128]], compare_op=ALU.is_gt, fill=NEG, base=0, channel_multiplier=1), r=[tf2], w=[tf2])
        C.op("dve", lambda e: e.tensor_copy(out=BLO4[:], in_=ins_bc(tf2[:], 1, 4)), r=[tf2], w=[BLO4])
        pi_ = C.sb("posi", [128, NT], I32); pf = C.sb("posf", [128, NT], F32)
        C.dma("sp", pi_[:], pos_in, w=[pi_])
        C.op("dve", lambda e: e.tensor_copy(out=pf[:], in_=pi_[:]), r=[pi_], w=[pf])
        u = C.sb("u", [128, NT], F32); ui = C.sb("ui", [128, NT], I32); uf = C.sb("uf", [128, NT], F32); fr = C.sb("fr", [128, NT], F32)

        def sincol(dst, col, freq, shift):
            C.op("dve", lambda e: e.tensor_scalar(out=u[:], in0=pf[:], scalar1=float(freq / (2 * np.pi)), scalar2=float(shift), op0=ALU.mult, op1=ALU.add), r=[pf], w=[u])
            C.op("dve", lambda e: e.tensor_copy(out=ui[:], in_=u[:]), r=[u], w=[ui])
            C.op("dve", lambda e: e.tensor_copy(out=uf[:], in_=ui[:]), r=[ui], w=[uf])
            C.op("dve", lambda e: e.tensor_tensor(out=fr[:], in0=u[:], in1=uf[:], op=ALU.subtract), r=[u, uf], w=[fr])
            C.op("dve", lambda e: e.tensor_scalar(out=fr[:], in0=fr[:], scalar1=0.4999, scalar2=-0.4999, op0=ALU.min, op1=ALU.max), r=[fr], w=[fr])
            C.op("act", lambda e: e.activation(out=dst[:, :, col], in_=fr[:], func=AF.Sin, scale=float(2 * np.pi)), r=[fr], w=[dst])

        for i in range(8):
            f = THETA ** (-(i * 2.0 / 16))
            sincol(sn64, i, f, 0.0); sincol(cs64, i, f, 0.25)
        for i in range(4):
            f = THETA ** (-(i * 2.0 / 8))
            sincol(sn32, i, f, 0.0); sincol(cs32, i, f, 0.25)

    def load_gain(dst, src_ap):
        C.dma("sp", dst[:], src_ap.rearrange("(kt p) -> p kt", p=128), w=[dst], allow_slow_non_contiguous=True)

    def load_w(dst, src_ap, KT, N, gain=None):
        srcv = src_ap.rearrange("(kt p) n -> p kt n", p=128)
        with C.phase():
            st = [C.sb("wst", [128, 4, 512], F32) for _ in range(3)]
            i = 0
            for k0 in range(0, KT, 4):
                k1 = min(KT, k0 + 4)
                for c0 in range(0, N, 512):
                    c1 = min(N, c0 + 512)
                    s = st[i % 3]
                    C.dma("sp" if i % 2 == 0 else "pool", s[:, 0:k1 - k0, 0:c1 - c0], srcv[:, k0:k1, c0:c1], w=[s])
                    for kt in range(k0, k1):
                        if kt % 2 == 0:
                            if gain is not None:
                                C.op("dve", lambda e: e.tensor_scalar(out=dst[:, kt, c0:c1], in0=s[:, kt - k0, 0:c1 - c0], scalar1=gain[:, kt:kt + 1], scalar2=None, op0=ALU.mult), r=[s, gain], w=[dst])
                            else:
                                C.op("dve", lambda e: e.tensor_copy(out=dst[:, kt, c0:c1], in_=s[:, kt - k0, 0:c1 - c0]), r=[s], w=[dst])
                        else:
                            if gain is not None:
                                C.op("act", lambda e: e.activation(out=dst[:, kt, c0:c1], in_=s[:, kt - k0, 0:c1 - c0], func=AF.Copy, scale=gain[:, kt:kt + 1]), r=[s, gain], w=[dst])
                            else:
                                C.op("act", lambda e: e.activation(out=dst[:, kt, c0:c1], in_=s[:, kt - k0, 0:c1 - c0], func=AF.Copy), r=[s], w=[dst])
                    i += 1

    class NormT:
        def __init__(s, pT=None):
            s.junk = C.sb("junk", [128, D], F32)
            s.ss = C.sb("ss", [128, 1], F32); s.rs = C.sb("rs", [128, 1], F32); s.epsT = epsG
            s.hn = C.sb("hn", [128, D], BF16)
            s.pT = pT if pT is not None else C.ps("pT", [128, 8, 128], BF16)

        def run(s, X, hT_ap, hT):
            C.op("act", lambda e: e.activation(out=s.junk[:], in_=X[:], func=AF.Square, accum_out=s.ss[:]), r=[X], w=[s.junk, s.ss])
            C.op("act", lambda e: e.activation(out=s.rs[:], in_=s.ss[:], func=AF.Ln, scale=1.0 / D, bias=s.epsT[:]), r=[s.ss, s.epsT], w=[s.rs])
            C.op("act", lambda e: e.activation(out=s.rs[:], in_=s.rs[:], func=AF.Exp, scale=-0.5), r=[s.rs], w=[s.rs])
            C.op("act", lambda e: e.activation(out=s.hn[:], in_=X[:], func=AF.Copy, scale=s.rs[:]), r=[X, s.rs], w=[s.hn])
            for kt in range(8):
                C.op("pe", lambda e: e.transpose(out=s.pT[:, kt, :], in_=s.hn[:, kt * 128:(kt + 1) * 128], identity=ident[:]), r=[s.hn, ident], w=[s.pT])
            C.op("dve", lambda e: e.tensor_copy(out=hT_ap, in_=s.pT[:]), r=[s.pT], w=[hT])

    class HeadNorm:
        def __init__(s, Hmax, Dh, rope_eng="dve"):
            s.sq = C.sb("hsq", [128, Hmax, Dh], F32)
            s.nrms = [C.sb("hnrm", [128, Hmax, Dh], F32) for _ in range(2)]
            s.ssh = C.sb("hss", [128, Hmax], F32); s.rsh = C.sb("hrs", [128, Hmax], F32)
            hf = Dh // 8
            s.t1s = [C.sb("ht1", [128, Hmax, hf], F32) for _ in range(2)]; s.t2s = [C.sb("ht2", [128, Hmax, hf], F32) for _ in range(2)]
            s.Dh = Dh; s.k = 0; s.re = rope_eng

        def run(s, src_ap, srcT, H, gain, out_ap, outT, rope=None, norm=True, out2_ap=None, out2T=None):
            Dh = s.Dh
            s.k += 1
            nrmT = s.nrms[s.k % 2]; t1T = s.t1s[s.k % 2]; t2T = s.t2s[s.k % 2]
            if norm:
                sq = s.sq[:, 0:H, :]; nrm = nrmT[:, 0:H, :]
                C.op("dve", lambda e: e.tensor_tensor(out=sq, in0=src_ap, in1=src_ap, op=ALU.mult), r=[srcT], w=[s.sq])
                C.op("dve", lambda e: e.tensor_reduce(out=s.ssh[:, 0:H], in_=sq, axis=AX.X, op=ALU.add), r=[s.sq], w=[s.ssh])
                C.op("act", lambda e: e.activation(out=s.rsh[:, 0:H], in_=s.ssh[:, 0:H], func=AF.Ln, scale=1.0 / Dh, bias=epsG[:]), r=[s.ssh, epsG], w=[s.rsh])
                C.op("act", lambda e: e.activation(out=s.rsh[:, 0:H], in_=s.rsh[:, 0:H], func=AF.Exp, scale=-0.5), r=[s.rsh], w=[s.rsh])
                C.op("dve", lambda e: e.tensor_tensor(out=nrm, in0=src_ap, in1=ins_bc(s.rsh[:, 0:H], 2, Dh), op=ALU.mult), r=[srcT, s.rsh], w=[nrmT])
                C.op("dve", lambda e: e.tensor_tensor(out=nrm, in0=nrm, in1=ins_bc(gain[:], 1, H), op=ALU.mult), r=[nrmT, gain], w=[nrmT])
                nT = nrmT
            else:
                nrm = src_ap; nT = srcT
            if out2_ap is not None:
                C.op("act", lambda e: e.activation(out=out2_ap, in_=nrm, func=AF.Copy), r=[nT], w=[out2T])
            C.op("act", lambda e: e.activation(out=out_ap, in_=nrm, func=AF.Copy), r=[nT], w=[outT])
            if rope is not None:
                RE = s.re
                cs, sn, hf = rope
                x1 = nrm[:, :, 0:hf]; x2 = nrm[:, :, hf:2 * hf]
                csb = ins_bc(cs, 1, H); snb = ins_bc(sn, 1, H)
                t1 = t1T[:, 0:H, :]; t2 = t2T[:, 0:H, :]
                C.op(RE, lambda e: e.tensor_tensor(out=t1, in0=x1, in1=csb, op=ALU.mult), r=[nT], w=[t1T])
                C.op(RE, lambda e: e.tensor_tensor(out=t2, in0=x2, in1=snb, op=ALU.mult), r=[nT], w=[t2T])
                C.op(RE, lambda e: e.tensor_tensor(out=out_ap[:, :, 0:hf], in0=t1, in1=t2, op=ALU.subtract), r=[t1T, t2T], w=[outT])
                C.op(RE, lambda e: e.tensor_tensor(out=t1, in0=x2, in1=csb, op=ALU.mult), r=[nT], w=[t1T])
                C.op(RE, lambda e: e.tensor_tensor(out=t2, in0=x1, in1=snb, op=ALU.mult), r=[nT], w=[t2T])
                C.op(RE, lambda e: e.tensor_tensor(out=out_ap[:, :, hf:2 * hf], in0=t1, in1=t2, op=ALU.add), r=[t1T, t2T], w=[outT])

    def run_proj(x_src, W, chunks, make_job, npo=3):
        nt = NormT()
        xt = [C.sb("xt", [128, D], F32) for _ in range(2)]
        hT = [C.sb("hT", [128, 8, 128], BF16) for _ in range(2)]
        po = [C.ps("po", [128, 512], F32) for _ in range(npo)]

        def norm(t):
            X = xt[t % 2]
            C.dma("sp", X[:], x_src[t * 128:(t + 1) * 128, :], w=[X])
            nt.run(X, hT[t % 2][:], hT[t % 2])

        jobs = [(t, c) for t in range(NT) for c in range(len(chunks))]

        def mm(k):
            t, c = jobs[k]
            c0, c1 = chunks[c]
            P = po[k % npo]
            H_ = hT[t % 2]
            for kt in range(8):
                C.op("pe", lambda e: e.matmul(P[:, 0:c1 - c0], lhsT=H_[:, kt, :], rhs=W[:, kt, c0:c1], start=(kt == 0), stop=(kt == 7)), r=[H_, W], w=[P])
            return P

        norm(0)
        Ps = {0: mm(0)}
        prev_trans = None
        for k, (t, c) in enumerate(jobs):
            if c == 0 and t + 1 < NT:
                norm(t + 1)
            if k + 1 < len(jobs):
                Ps[k + 1] = mm(k + 1)
            if prev_trans is not None:
                prev_trans()
            post, trans = make_job(k, t, c, Ps.pop(k))
            post()
            prev_trans = trans
        if prev_trans is not None:
            prev_trans()

    def bgain(src_ap, n=64):
        t = C.sb("bg", [128, n], F32)
        C.dma("sp", t[:], src_ap.partition_broadcast(128), w=[t])
        return t


    QA_T = SCR("QA_T", [8, 64, S], BF16); KA_T = SCR("KA_T", [64, S], BF16); VA = SCR("VA", [S, 64], BF16)
    IQ_T = SCR("IQ_T", [8, 32, S], BF16); IK_T = SCR("IK_T", [32, S], BF16); IW = SCR("IW", [S, 8], F32)
    QB_T = SCR("QB_T", [8, 64, S], BF16); KB_T = SCR("KB_T", [8, 64, S], BF16); VB = SCR("VB", [S, 512], BF16)
    OM = SCR("OM", [S, D], BF16)
    XA = SCR("XA", [S, D], F32); XB = SCR("XB", [S, D], F32); XC = SCR("XC", [S, D], F32)

    def store(dst_ap, src_ap, srcT, **kw):
        C.dma("pool", dst_ap, src_ap, r=[srcT], **kw)

    def phase_A0():
        with C.phase():
            g = C.sb("g", [128, 8], F32); load_gain(g, inp["norm_mix"][0])
            W = C.sb("W0", [128, 8, 2472], BF16)
            load_w(W, inp["ab_w_in"], 8, 2472, g)
            gq = bgain(inp["dsa_q_norm"]); gk = bgain(inp["dsa_k_norm"]); gbq = bgain(inp["moba_q_norm"]); gbk = bgain(inp["moba_k_norm"])
            hnm = HeadNorm(8, 64, "pool"); hn32 = HeadNorm(9, 32, "pool")
            pr = [C.sb("pr", [128, 512], F32) for _ in range(2)]
            qn = [C.sb("qn", [128, 512], BF16) for _ in range(3)]
            ptq = [C.ps("ptq", [64, 8, 128], BF16) for _ in range(2)]
            qT = [C.sb("qT", [64, 8, 128], BF16) for _ in range(3)]
            chunks = [(0, 512), (512, 936), (936, 1448), (1448, 1960), (1960, 2472)]
            cnt = {"ti": 0}

            def tr_heads(Q, H, Dh, src_off, dst_ap):
                i = cnt["ti"]; cnt["ti"] += 1
                PT = ptq[i % 2]; QT = qT[i % 3]
                for h in range(H):
                    C.op("pe", lambda e: e.transpose(out=PT[0:Dh, h, :], in_=Q[:, src_off + h * Dh: src_off + (h + 1) * Dh], identity=ident[:]), r=[Q, ident], w=[PT])
                C.op("dve", lambda e: e.tensor_copy(out=QT[0:Dh, 0:H, :], in_=PT[0:Dh, 0:H, :]), r=[PT], w=[QT])
                store(dst_ap, QT[0:Dh, 0:H, :], QT)

            def make_job(k, t, c, P):
                R = pr[k % 2]; Q = qn[k % 3]
                ts = slice(t * 128, (t + 1) * 128)
                cs8 = cs64[:, t, :]; sn8 = sn64[:, t, :]; cs4 = cs32[:, t, :]; sn4 = sn32[:, t, :]
                wd = chunks[c][1] - chunks[c][0]

                def post():
                    C.op("act", lambda e: e.activation(out=R[:, 0:wd], in_=P[:, 0:wd], func=AF.Copy), r=[P], w=[R])
                    if c in (0, 2, 3):
                        gg = (gq, None, gbq, gbk)[c]
                        hnm.run(R[:].rearrange("p (h d) -> p h d", h=8), R, 8, gg, Q[:].rearrange("p (h d) -> p h d", h=8), Q, rope=(cs8, sn8, 8))
                    elif c == 1:
                        hnm.run(R[:, 0:64].rearrange("p (h d) -> p h d", h=1), R, 1, gk, Q[:, 0:64].rearrange("p (h d) -> p h d", h=1), Q, rope=(cs8, sn8, 8))
                        C.op("act", lambda e: e.activation(out=Q[:, 64:128], in_=R[:, 64:128], func=AF.Copy), r=[R], w=[Q])
                        hn32.run(R[:, 128:416].rearrange("p (h d) -> p h d", h=9), R, 9, None, Q[:, 128:416].rearrange("p (h d) -> p h d", h=9), Q, rope=(cs4, sn4, 4), norm=False)
                        store(IW[ts, :], R[:, 416:424], R)
                    else:
                        C.op("act", lambda e: e.activation(out=Q[:], in_=R[:], func=AF.Copy), r=[R], w=[Q])

                def trans():
                    if c in (0, 2, 3):
                        dst = (QA_T, None, QB_T, KB_T)[c]
                        tr_heads(Q, 8, 64, 0, dst[:, :, ts].rearrange("h d s -> d h s"))
                    elif c == 1:
                        store(VA[ts, :], Q[:, 64:128], Q)
                        tr_heads(Q, 1, 64, 0, KA_T[:, ts].rearrange("(h d) s -> d h s", h=1))
                        tr_heads(Q, 8, 32, 128, IQ_T[:, :, ts].rearrange("h d s -> d h s"))
                        tr_heads(Q, 1, 32, 384, IK_T[:, ts].rearrange("(h d) s -> d h s", h=1))
                    else:
                        store(VB[ts, :], Q[:], Q)
                return post, trans

            run_proj(x_in, W, chunks, make_job)

    NIT = 16

    def run_merged(gA, nA, gB, nB):
        a = b = 0
        doneA = gA is None
        doneB = gB is None
        while not (doneA and doneB):
            pickA = (not doneA) and (doneB or a * nB <= b * nA)
            if pickA:
                try:
                    next(gA); a += 1
                except StopIteration:
                    doneA = True
            else:
                try:
                    next(gB); b += 1
                except StopIteration:
                    doneB = True

    def phase_A1():
        with C.phase():
            KT_ = C.sb("KAT", [128, S], BF16)
            C.op("pool", lambda e: e.memset(KT_[64:128, :], 0.0), w=[KT_])
            C.dma("sp", KT_[0:64, :], KA_T, w=[KT_])
            IKT = C.sb("IKT", [128, S], BF16)
            C.op("pool", lambda e: e.memset(IKT[:], 0.0), w=[IKT])
            C.dma("sp", IKT[0:32, :], IK_T, w=[IKT])
            Va = C.sb("VAa", [128, NT, 65], BF16)
            C.op("pool", lambda e: e.memset(Va[:], 1.0), w=[Va])
            C.dma("sp", Va[:, :, 0:64], VA.rearrange("(t p) c -> p t c", p=128), w=[Va])
            IWs = C.sb("IWs", [128, NT, 8], F32); C.dma("sp", IWs[:], IW.rearrange("(t p) c -> p t c", p=128), w=[IWs])
            score = [C.sb("score", [128, S], F32) for _ in range(2)]
            junk = C.sb("junkb", [128, S], BF16)
            nmask = [C.sb("nmask", [128, S], BF16) for _ in range(2)]
            rl = [C.sb("rl", [128, 512], F32) for _ in range(2)]
            qa = [C.sb("qa", [128, 8, 128], BF16) for _ in range(2)]
            iqt = [C.sb("iqt", [128, 8, 128], BF16) for _ in range(2)]
            for t_ in qa + iqt:
                C.op("pool", lambda e: e.memset(t_[:], 0.0), w=[t_])
            psi = [C.ps("psi", [128, 512], F32) for _ in range(2)]
            pss = [C.ps("pss", [128, 512], F32) for _ in range(2)]
            pacc = [C.ps("pacc", [128, 512], F32) for _ in range(4)]
            PTs = [C.sb("PTs", [128, 512], BF16) for _ in range(3)]
            sm = {n: C.sb(n, [128, 1], F32) for n in ("lo", "hi", "mid", "cnt", "ge", "rng", "c255", "sgn")}
            cthr = [C.sb("cthr", [128, 1], F32) for _ in range(2)]
            junkA = C.sb("junkA", [128, S], BF16)
            rzs = [C.sb("rz", [128, 1], F32) for _ in range(2)]
            steps = C.sb("steps", [128, NIT], F32); pw = C.sb("pw", [128, NIT], F32); steps2 = C.sb("steps2", [128, NIT], F32)
            for kk in range(NIT):
                C.op("pool", lambda e: e.memset(pw[:, kk:kk + 1], float(2.0 ** -(kk + 1))), w=[pw])
            C.op("pool", lambda e: e.memset(sm["c255"][:], 255.5), w=[sm["c255"]])
            oq = [C.sb("oq", [128, 512], BF16) for _ in range(2)]
            st_ = {"ii": 0, "si": 0, "pi": 0, "ri": 0}

            def gen_idx(qt):
                nk = (qt + 1) * 128
                IQ = iqt[qt % 2]; SC = score[qt % 2]; NM = nmask[qt % 2]
                ts = slice(qt * 128, (qt + 1) * 128)
                C.dma("sp", IQ[0:32, :, :], IQ_T[:, :, ts].rearrange("h d s -> d h s"), w=[IQ])
                jobs_ = [(c0, min(512, nk - c0), h) for c0 in range(0, nk, 512) for h in range(8)]

                def imm(j):
                    c0, wd, h = jobs_[j]
                    P = psi[st_["ii"] % 2]; st_["ii"] += 1
                    C.op("pe", lambda e: e.matmul(P[:, 0:wd], lhsT=IQ[:, h, :], rhs=IKT[:, c0:c0 + wd], start=True, stop=True), r=[IQ, IKT], w=[P])
                    return P

                Pcur = imm(0)
                for j, (c0, wd, h) in enumerate(jobs_):
                    Pnext = imm(j + 1) if j + 1 < len(jobs_) else None
                    P = Pcur
                    if h == 0:
                        C.op("dve", lambda e: e.tensor_scalar(out=SC[:, c0:c0 + wd], in0=P[:, 0:wd], scalar1=0.0, scalar2=IWs[:, qt, 0:1], op0=ALU.max, op1=ALU.mult), r=[P, IWs], w=[SC])
                    else:
                        R_ = rl[j % 2]
                        C.op("act", lambda e: e.activation(out=R_[:, 0:wd], in_=P[:, 0:wd], func=AF.Relu), r=[P], w=[R_])
                        C.op("dve", lambda e: e.scalar_tensor_tensor(out=SC[:, c0:c0 + wd], in0=R_[:, 0:wd], scalar=IWs[:, qt, h:h + 1], in1=SC[:, c0:c0 + wd], op0=ALU.mult, op1=ALU.add), r=[R_, IWs, SC], w=[SC])
                    Pcur = Pnext
                    if h == 7:
                        yield
                sc = SC[:, 0:nk]
                C.op("dve", lambda e: e.tensor_reduce(out=sm["hi"][:], in_=sc, axis=AX.X, op=ALU.max), r=[SC], w=[sm["hi"]])
                C.op("dve", lambda e: e.tensor_reduce(out=sm["lo"][:], in_=sc, axis=AX.X, op=ALU.min), r=[SC], w=[sm["lo"]])
                C.op("pool", lambda e: e.affine_select(out=SC[:, nk - 128:nk], in_=SC[:, nk - 128:nk], pattern=[[-1, 128]], compare_op=ALU.is_ge, fill=-1e30, base=0, channel_multiplier=1), r=[SC], w=[SC])
                C.op("dve", lambda e: e.tensor_tensor(out=sm["rng"][:], in0=sm["hi"][:], in1=sm["lo"][:], op=ALU.subtract), r=[sm["lo"], sm["hi"]], w=[sm["rng"]])
                C.op("dve", lambda e: e.tensor_scalar(out=steps[:], in0=pw[:], scalar1=sm["rng"][:], scalar2=None, op0=ALU.mult), r=[pw, sm["rng"]], w=[steps])
                yield
                cD = max(8, int(0.52 * nk) // 8 * 8)
                nA = nk - cD
                CT = cthr[qt % 2]
                C.op("pool", lambda e: e.memset(CT[:], float(255.5 - 0.5 * nA)), w=[CT])
                C.op("dve", lambda e: e.tensor_scalar(out=steps2[:], in0=steps[:], scalar1=2.0, scalar2=None, op0=ALU.mult), r=[steps], w=[steps2])
                C.op("dve", lambda e: e.tensor_tensor(out=sm["mid"][:], in0=sm["lo"][:], in1=steps[:, 0:1], op=ALU.add), r=[sm["lo"], steps], w=[sm["mid"]])
                for it in range(NIT):
                    last = it == NIT - 1
                    mul_ap = steps[:, it:it + 1] if last else steps2[:, it + 1:it + 2]
                    sub_ap = steps[:, it:it + 1] if last else steps[:, it + 1:it + 2]
                    C.op("act", lambda e: e.activation(out=junkA[:, 0:nA], in_=SC[:, cD:nk], func=AF.Sign, scale=-1.0, bias=sm["mid"][:], accum_out=sm["sgn"][:]), r=[SC, sm["mid"]], w=[junkA, sm["sgn"]])
                    C.op("dve", lambda e: e.tensor_scalar(out=junk[:, 0:cD], in0=SC[:, 0:cD], scalar1=sm["mid"][:], scalar2=None, op0=ALU.is_ge, op1=ALU.add, accum_out=sm["cnt"][:]), r=[SC, sm["mid"]], w=[junk, sm["cnt"]])
                    C.op("dve", lambda e: e.scalar_tensor_tensor(out=sm["cnt"][:], in0=sm["sgn"][:], scalar=-0.5, in1=sm["cnt"][:], op0=ALU.mult, op1=ALU.add), r=[sm["sgn"], sm["cnt"]], w=[sm["cnt"]])
                    C.op("dve", lambda e: e.tensor_scalar(out=sm["ge"][:], in0=sm["cnt"][:], scalar1=CT[:], scalar2=mul_ap, op0=ALU.is_ge, op1=ALU.mult), r=[sm["cnt"], CT, steps, steps2], w=[sm["ge"]])
                    C.op("dve", lambda e: e.scalar_tensor_tensor(out=sm["mid"][:], in0=sm["ge"][:], scalar=sub_ap, in1=sm["mid"][:], op0=ALU.subtract, op1=ALU.add), r=[sm["ge"], steps, sm["mid"]], w=[sm["mid"]])
                    if it % 2 == 1:
                        yield
                C.op("dve", lambda e: e.scalar_tensor_tensor(out=sm["mid"][:], in0=steps[:, NIT - 1:NIT], scalar=-0.5, in1=sm["mid"][:], op0=ALU.mult, op1=ALU.add), r=[steps, sm["mid"]], w=[sm["mid"]])
                C.op("dve", lambda e: e.tensor_scalar(out=NM[:, 0:nk], in0=sc, scalar1=sm["mid"][:], scalar2=NEG, op0=ALU.is_lt, op1=ALU.mult), r=[SC, sm["mid"]], w=[NM])
                yield

            def n_idx(qt):
                return ((qt + 1) * 128 + 511) // 512 + NIT // 2 + 2

            def gen_att(qt):
                QA = qa[qt % 2]; NM = nmask[qt % 2]; OQ = oq[qt % 2]
                ts = slice(qt * 128, (qt + 1) * 128)
                C.dma("sp", QA[0:64, :, :], QA_T[:, :, ts].rearrange("h d s -> d h s"), w=[QA])

                def qk(hg, st):
                    P = pss[st_["si"] % 2]; st_["si"] += 1
                    ss_ = slice(st * 128, (st + 1) * 128)
                    C.op("pe", lambda e: e.matmul(P[:], lhsT=KT_[:, ss_], rhs=QA[:, hg * 4:(hg + 1) * 4, :], start=True, stop=False), r=[KT_, QA], w=[P])
                    C.op("pe", lambda e: e.matmul(P[:], lhsT=NM[:, ss_], rhs=I4[:], start=False, stop=True), r=[NM, I4], w=[P])
                    return P

                for hg in range(2):
                    Pc = qk(hg, 0)
                    for st in range(qt + 1):
                        Pn = qk(hg, st + 1) if st < qt else None
                        PT = PTs[st_["pi"] % 3]; st_["pi"] += 1
                        C.op("act", lambda e: e.activation(out=PT[:], in_=Pc[:], func=AF.Exp, scale=0.125), r=[Pc], w=[PT])
                        for h in range(4):
                            C.op("pe", lambda e: e.matmul(pacc[h][:, 0:65], lhsT=PT[:, h * 128:(h + 1) * 128], rhs=Va[:, st, :], start=(st == 0), stop=(st == qt)), r=[PT, Va], w=[pacc[h]])
                        Pc = Pn
                        if st % 4 == 3:
                            yield
                    for h in range(4):
                        hd = hg * 4 + h
                        rz = rzs[st_["ri"] % 2]; st_["ri"] += 1
                        C.op("dve", lambda e: e.reciprocal(out=rz[:], in_=pacc[h][:, 64:65]), r=[pacc[h]], w=[rz])
                        C.op("act", lambda e: e.activation(out=OQ[:, hd * 64:(hd + 1) * 64], in_=pacc[h][:, 0:64], func=AF.Copy, scale=rz[:]), r=[pacc[h], rz], w=[OQ])
                    yield
                store(OM[ts, 0:512], OQ[:], OQ)
                yield

            def n_att(qt):
                return 2 * ((qt + 1) // 4 + 1) + 1

            run_merged(gen_idx(0), 1, None, 1)
            for qt in range(NT):
                gB = gen_idx(qt + 1) if qt + 1 < NT else None
                run_merged(gen_att(qt), n_att(qt), gB, n_idx(qt + 1) if gB else 1)

    def phase_A2():
        with C.phase():
            KT_ = C.sb("KBT", [128, S], BF16); QT_ = C.sb("QBT", [128, S], BF16)
            C.op("pool", lambda e: e.memset(KT_[64:128, :], 0.0), w=[KT_])
            C.op("pool", lambda e: e.memset(QT_[64:128, :], 0.0), w=[QT_])
            with C.phase():
                Ef = C.sb("Ef", [80, S], F32)
                C.op("pool", lambda e: e.memset(Ef[64:80, :], 1.0), w=[Ef])
                C.op("pool", lambda e: e.affine_select(out=Ef[64:80, :], in_=Ef[64:80, :], pattern=[[1, S]], compare_op=ALU.is_ge, fill=0.0, base=0, channel_multiplier=-256), r=[Ef], w=[Ef])
                C.op("pool", lambda e: e.affine_select(out=Ef[64:80, :], in_=Ef[64:80, :], pattern=[[-1, S]], compare_op=ALU.is_ge, fill=0.0, base=255, channel_multiplier=256), r=[Ef], w=[Ef])
                C.op("dve", lambda e: e.tensor_copy(out=KT_[64:80, :], in_=Ef[64:80, :]), r=[Ef], w=[KT_])
            cm = C.sb("cm", [128, NT, 16], F32); om = C.sb("om", [128, NT, 16], F32)
            C.op("pool", lambda e: e.memset(cm[:], 0.0), w=[cm])
            C.op("pool", lambda e: e.memset(om[:], 0.0), w=[om])
            for qt in range(NT):
                own = qt // 2
                C.op("pool", lambda e: e.memset(cm[:, qt, own:16], -1e30), w=[cm])
                C.op("pool", lambda e: e.memset(om[:, qt, own:own + 1], 1.0), w=[om])
            Va = C.sb("VBa", [128, NT, 65], BF16)
            C.op("pool", lambda e: e.memset(Va[:], 1.0), w=[Va])
            kmf = C.sb("kmf", [64, 16], F32); kmb = C.sb("kmb", [64, 16], BF16)
            pg = C.ps("pg", [128, NT, 16], F32)
            gm = C.sb("gm", [128, NT, 16], F32); m8 = C.sb("m8", [128, NT, 8], F32)
            sel = C.sb("sel", [128, NT, 16], F32)
            nbx = C.sb("nbx", [128, NT, 80], BF16)
            C.op("pool", lambda e: e.memset(nbx[:], 0.0), w=[nbx])
            pnt = C.ps("pnt", [80, 8, 128], BF16)
            pss = [C.ps("pss", [128, 512], F32) for _ in range(2)]
            pacc = [C.ps("pacc", [128, 512], F32) for _ in range(4)]
            PTs = [C.sb("PTs", [128, 512], BF16) for _ in range(3)]
            rzs = [C.sb("rz", [128, 1], F32) for _ in range(2)]
            ob = [C.sb("ob", [128, 64], BF16) for _ in range(3)]
            si = 0; pi = 0; oi = 0; ri = 0
            for h in range(8):
                C.dma("sp", KT_[0:64, :], KB_T[h], w=[KT_])
                C.dma("sp", QT_[0:64, :], QB_T[h], w=[QT_])
                C.dma("sp", Va[:, :, 0:64], VB[:, h * 64:(h + 1) * 64].rearrange("(t p) c -> p t c", p=128), w=[Va])
                C.op("dve", lambda e: e.tensor_reduce(out=kmf[:], in_=KT_[0:64, :].rearrange("d (n s) -> d n s", n=16), axis=AX.X, op=ALU.add), r=[KT_], w=[kmf])
                C.op("dve", lambda e: e.tensor_scalar(out=kmb[:], in0=kmf[:], scalar1=1.0 / 256, scalar2=None, op0=ALU.mult), r=[kmf], w=[kmb])
                for qt in range(NT):
                    C.op("pe", lambda e: e.matmul(pg[:, qt, :], lhsT=QT_[0:64, qt * 128:(qt + 1) * 128], rhs=kmb[:], start=True, stop=True), r=[QT_, kmb], w=[pg])
                C.op("dve", lambda e: e.tensor_tensor(out=gm[:], in0=pg[:], in1=cm[:], op=ALU.add), r=[pg, cm], w=[gm])
                for qt in range(NT):
                    C.op("dve", lambda e: e.max(out=m8[:, qt, :], in_=gm[:, qt, :]), r=[gm], w=[m8])
                C.op("dve", lambda e: e.tensor_tensor(out=sel[:], in0=gm[:], in1=ins_bc(m8[:, :, 2], 2, 16), op=ALU.is_ge), r=[gm, m8], w=[sel])
                C.op("dve", lambda e: e.tensor_tensor(out=sel[:], in0=sel[:], in1=om[:], op=ALU.max), r=[sel, om], w=[sel])
                C.op("dve", lambda e: e.tensor_scalar(out=nbx[:, :, 64:80], in0=sel[:], scalar1=1.0, scalar2=-NEG, op0=ALU.subtract, op1=ALU.mult), r=[sel], w=[nbx])
                for r4 in range(4):
                    for j in range(8):
                        qt = r4 * 8 + j
                        C.op("pe", lambda e: e.transpose(out=pnt[:, j, :], in_=nbx[:, qt, :], identity=ident[:]), r=[nbx, ident], w=[pnt])
                    C.op("act", lambda e: e.activation(out=QT_[64:80, r4 * 1024:(r4 + 1) * 1024], in_=pnt[64:80, :, :].rearrange("n j q -> n (j q)"), func=AF.Copy), r=[pnt], w=[QT_])
                steps_ = [(G, st) for G in range(8) for st in range(4 * G + 4)]

                def qk(G, st):
                    nonlocal si
                    P = pss[si % 2]; si += 1
                    ss_ = slice(st * 128, (st + 1) * 128); qs = slice(G * 512, (G + 1) * 512)
                    diag = st >= 4 * G
                    C.op("pe", lambda e: e.matmul(P[:], lhsT=KT_[:, ss_], rhs=QT_[:, qs], start=True, stop=not diag), r=[KT_, QT_], w=[P])
                    if diag:
                        C.op("pe", lambda e: e.matmul(P[:], lhsT=ident[:], rhs=CAUS[:, st - 4 * G, :, :], start=False, stop=True), r=[ident, CAUS], w=[P])
                    return P

                Pc = qk(*steps_[0])
                for k_, (G, st) in enumerate(steps_):
                    Pn = qk(*steps_[k_ + 1]) if k_ + 1 < len(steps_) else None
                    PT = PTs[pi % 3]; pi += 1
                    C.op("act", lambda e: e.activation(out=PT[:], in_=Pc[:], func=AF.Exp, scale=0.125), r=[Pc], w=[PT])
                    for qi in range(4):
                        qt = 4 * G + qi
                        if qt < st:
                            continue
                        C.op("pe", lambda e: e.matmul(pacc[qi][:, 0:65], lhsT=PT[:, qi * 128:(qi + 1) * 128], rhs=Va[:, st, :], start=(st == 0), stop=(st == qt)), r=[PT, Va], w=[pacc[qi]])
                    Pc = Pn
                    if st == 4 * G + 3:
                        for qi in range(4):
                            qt = 4 * G + qi
                            O_ = ob[oi % 3]; oi += 1
                            rz = rzs[ri % 2]; ri += 1
                            C.op("dve", lambda e: e.reciprocal(out=rz[:], in_=pacc[qi][:, 64:65]), r=[pacc[qi]], w=[rz])
                            C.op("act", lambda e: e.activation(out=O_[:], in_=pacc[qi][:, 0:64], func=AF.Copy, scale=rz[:]), r=[pacc[qi], rz], w=[O_])
                            store(OM[qt * 128:(qt + 1) * 128, 512 + h * 64:512 + (h + 1) * 64], O_[:], O_)

    def phase_A3(L, w_out_ap, x_src, x_dst):
        with C.phase():
            Wo = C.sb("Wo", [128, 8, D], BF16); load_w(Wo, w_out_ap, 8, D)
            g = C.sb("g", [128, 8], F32); load_gain(g, inp["norm_mem"][L])
            Wq = C.sb("Wq", [128, 8, 512], BF16); load_w(Wq, inp["mem_w_q"][L], 8, 512, g)
            Wom = C.sb("Wom", [128, 4, D], BF16); load_w(Wom, inp["mem_w_o"][L], 4, D)
            gqn = bgain(inp["mem_q_norm"][L], 128); gkn = bgain(inp["mem_k_norm"][L], 128)
            mK = C.sb("mK", [128, 4, 256], BF16); mV = C.sb("mV", [128, 2, 4, 129], BF16)
            C.op("pool", lambda e: e.memset(mV[:], 1.0), w=[mV])
            nt = NormT(); hnm = HeadNorm(4, 128)
            hT = C.sb("hT", [128, 8, 128], BF16)
            po = [C.ps("po", [128, 512], F32) for _ in range(2)]
            pr = C.sb("pr", [128, 512], F32); qn = C.sb("qn", [128, 512], BF16)
            ptq8 = C.ps("ptq", [128, 8, 128], BF16)
            ptq = T(ptq8.t, "x"); ptq.b = ptq8.b
            ptq_ap = ptq8[:, 0:4, :]
            with C.phase():
                gs = C.sb("gs", [128, 8], F32); load_gain(gs, inp["norm_mem_src"][L])
                Wkv = C.sb("Wkv", [128, 8, D], BF16); load_w(Wkv, inp["mem_w_kv"][L], 8, D, gs)
                mt_ = C.sb("mt", [128, D], F32)
                for mt in range(2):
                    C.dma("sp", mt_[:], mem_in[mt * 128:(mt + 1) * 128, :], w=[mt_])
                    nt.run(mt_, hT[:], hT)
                    for c in range(2):
                        P = po[c]
                        for kt in range(8):
                            C.op("pe", lambda e: e.matmul(P[:], lhsT=hT[:, kt, :], rhs=Wkv[:, kt, c * 512:(c + 1) * 512], start=(kt == 0), stop=(kt == 7)), r=[hT, Wkv], w=[P])
                        if c == 0:
                            C.op("act", lambda e: e.activation(out=pr[:], in_=P[:], func=AF.Copy), r=[P], w=[pr])
                            hnm.run(pr[:].rearrange("p (h d) -> p h d", h=4), pr, 4, gkn, qn[:].rearrange("p (h d) -> p h d", h=4), qn)
                            for h in range(4):
                                C.op("pe", lambda e: e.transpose(out=ptq[:, h, :], in_=qn[:, h * 128:(h + 1) * 128], identity=ident[:]), r=[qn, ident], w=[ptq])
                            C.op("dve", lambda e: e.tensor_copy(out=mK[:, :, mt * 128:(mt + 1) * 128], in_=ptq_ap), r=[ptq], w=[mK])
                        else:
                            C.op("act", lambda e: e.activation(out=mV[:, mt, :, 0:128], in_=P[:].rearrange("p (h d) -> p h d", h=4), func=AF.Copy), r=[P], w=[mV])
            pT = nt.pT
            lanes = []
            for ln in range(2):
                Lb = K()
                Lb.nt = nt if ln == 0 else NormT(pT=pT)
                Lb.hnm = hnm if ln == 0 else HeadNorm(4, 128)
                Lb.ot = C.sb("ot", [128, D], BF16); Lb.xt = C.sb("xt", [128, D], F32)
                Lb.oT = C.sb("oT", [128, 8, 128], BF16); Lb.x1 = C.sb("x1", [128, D], F32); Lb.x2 = C.sb("x2", [128, D], F32)
                Lb.hT = hT if ln == 0 else C.sb("hT", [128, 8, 128], BF16)
                Lb.pr = pr if ln == 0 else C.sb("pr", [128, 512], F32)
                Lb.qn = qn if ln == 0 else C.sb("qn", [128, 512], BF16)
                Lb.qT = C.sb("qT", [128, 4, 128], BF16)
                Lb.po = po[ln]
                Lb.pss = C.ps("pss", [128, 512], F32)
                Lb.PTm = [C.sb("PTm", [128, 512], BF16) for _ in range(2)]
                Lb.pov = C.ps("pov", [128, 2, 256], F32)
                Lb.rz = [C.sb("rz", [128, 1], F32) for _ in range(2)]
                Lb.omx = C.sb("omx", [128, 512], BF16); Lb.omT = C.sb("omT", [128, 4, 128], BF16)
                lanes.append(Lb)

            def gen_tile(Lb, t):
                ts = slice(t * 128, (t + 1) * 128)
                O_ = Lb.ot; X = Lb.xt; X2 = Lb.x2; x1 = Lb.x1; oT = Lb.oT; P = Lb.po
                C.dma("sp", O_[:], OM[ts, :], w=[O_])
                C.dma("sp", X[:], x_src[ts, :], w=[X])
                for kt in range(8):
                    C.op("pe", lambda e: e.transpose(out=pT[:, kt, :], in_=O_[:, kt * 128:(kt + 1) * 128], identity=ident[:]), r=[O_, ident], w=[pT])
                C.op("dve", lambda e: e.tensor_copy(out=oT[:], in_=pT[:]), r=[pT], w=[oT])
                yield
                for c in range(2):
                    for kt in range(8):
                        C.op("pe", lambda e: e.matmul(P[:], lhsT=oT[:, kt, :], rhs=Wo[:, kt, c * 512:(c + 1) * 512], start=(kt == 0), stop=(kt == 7)), r=[oT, Wo], w=[P])
                    C.op("dve", lambda e: e.tensor_tensor(out=x1[:, c * 512:(c + 1) * 512], in0=P[:], in1=X[:, c * 512:(c + 1) * 512], op=ALU.add), r=[P, X], w=[x1])
                    yield
                Lb.nt.run(x1, Lb.hT[:], Lb.hT)
                yield
                for kt in range(8):
                    C.op("pe", lambda e: e.matmul(P[:], lhsT=Lb.hT[:, kt, :], rhs=Wq[:, kt, :], start=(kt == 0), stop=(kt == 7)), r=[Lb.hT, Wq], w=[P])
                C.op("act", lambda e: e.activation(out=Lb.pr[:], in_=P[:], func=AF.Copy), r=[P], w=[Lb.pr])
                yield
                Lb.hnm.run(Lb.pr[:].rearrange("p (h d) -> p h d", h=4), Lb.pr, 4, gqn, Lb.qn[:].rearrange("p (h d) -> p h d", h=4), Lb.qn)
                yield
                for h in range(4):
                    C.op("pe", lambda e: e.transpose(out=ptq[:, h, :], in_=Lb.qn[:, h * 128:(h + 1) * 128], identity=ident[:]), r=[Lb.qn, ident], w=[ptq])
                C.op("dve", lambda e: e.tensor_copy(out=Lb.qT[:], in_=ptq_ap), r=[ptq], w=[Lb.qT])
                yield
                for mt in range(2):
                    PS = Lb.pss
                    for h in range(4):
                        C.op("pe", lambda e: e.matmul(PS[:, h * 128:(h + 1) * 128], lhsT=mK[:, h, mt * 128:(mt + 1) * 128], rhs=Lb.qT[:, h, :], start=True, stop=True), r=[mK, Lb.qT], w=[PS])
                    C.op("act", lambda e: e.activation(out=Lb.PTm[mt][:], in_=PS[:], func=AF.Exp, scale=float(128 ** -0.5)), r=[PS], w=[Lb.PTm[mt]])
                    yield
                for h in range(4):
                    A = Lb.pov
                    for mt in range(2):
                        C.op("pe", lambda e: e.matmul(A[:, h % 2, 0:129], lhsT=Lb.PTm[mt][:, h * 128:(h + 1) * 128], rhs=mV[:, mt, h, :], start=(mt == 0), stop=(mt == 1)), r=[Lb.PTm[mt], mV], w=[A])
                    rz = Lb.rz[h % 2]
                    C.op("dve", lambda e: e.reciprocal(out=rz[:], in_=A[:, h % 2, 128:129]), r=[A], w=[rz])
                    C.op("act", lambda e: e.activation(out=Lb.omx[:, h * 128:(h + 1) * 128], in_=A[:, h % 2, 0:128], func=AF.Copy, scale=rz[:]), r=[A, rz], w=[Lb.omx])
                    yield
                for h in range(4):
                    C.op("pe", lambda e: e.transpose(out=ptq[:, h, :], in_=Lb.omx[:, h * 128:(h + 1) * 128], identity=ident[:]), r=[Lb.omx, ident], w=[ptq])
                C.op("dve", lambda e: e.tensor_copy(out=Lb.omT[:], in_=ptq_ap), r=[ptq], w=[Lb.omT])
                yield
                for c in range(2):
                    for kt in range(4):
                        C.op("pe", lambda e: e.matmul(P[:], lhsT=Lb.omT[:, kt, :], rhs=Wom[:, kt, c * 512:(c + 1) * 512], start=(kt == 0), stop=(kt == 3)), r=[Lb.omT, Wom], w=[P])
                    C.op("dve", lambda e: e.tensor_tensor(out=X2[:, c * 512:(c + 1) * 512], in0=P[:], in1=x1[:, c * 512:(c + 1) * 512], op=ALU.add), r=[P, x1], w=[X2])
                    yield
                store(x_dst[ts, :], X2[:], X2)
                yield

            def lane_gen(ln):
                if ln == 1:
                    for _ in range(9):
                        yield
                for t in range(ln, NT, 2):
                    yield from gen_tile(lanes[ln], t)

            run_merged(lane_gen(0), 1, lane_gen(1), 1)

    def phase_A4(L, x_src, x_dst):
        with C.phase():
            g = C.sb("g", [128, 8], F32); load_gain(g, inp["norm_ffn"][L])
            Wi = C.sb("Wi", [128, 8, 2 * DFF], BF16); load_w(Wi, inp["ffn_w_in"][L], 8, 2 * DFF, g)
            Wd = C.sb("Wd", [128, 22, D], BF16); load_w(Wd, inp["ffn_w_out"][L], 22, D)
            nt = NormT()
            xt = [C.sb("xt", [128, D], F32) for _ in range(4)]
            hT = C.sb("hT", [128, 8, 512], BF16)
            hid = C.sb("hid", [128, 22, 512], BF16)
            psg = [C.ps("psg", [128, 512], F32) for _ in range(2)]
            psu = [C.ps("psu", [128, 512], F32) for _ in range(2)]
            po = [C.ps("po", [128, 512], F32) for _ in range(2)]
            sg = [C.sb("sg", [128, 512], BF16) for _ in range(2)]
            oi = 0
            for G in range(8):
                for tt in range(4):
                    t = G * 4 + tt
                    C.dma("sp", xt[tt][:], x_src[t * 128:(t + 1) * 128, :], w=[xt[tt]])
                    nt.run(xt[tt], hT[:, :, tt * 128:(tt + 1) * 128], hT)
                for ft in range(22):
                    Pg = psg[ft % 2]; Pu = psu[ft % 2]; Sg = sg[ft % 2]
                    for kt in range(8):
                        C.op("pe", lambda e: e.matmul(Pg[:], lhsT=Wi[:, kt, ft * 128:(ft + 1) * 128], rhs=hT[:, kt, :], start=(kt == 0), stop=(kt == 7)), r=[Wi, hT], w=[Pg])
                    for kt in range(8):
                        C.op("pe", lambda e: e.matmul(Pu[:], lhsT=Wi[:, kt, DFF + ft * 128:DFF + (ft + 1) * 128], rhs=hT[:, kt, :], start=(kt == 0), stop=(kt == 7)), r=[Wi, hT], w=[Pu])
                    C.op("act", lambda e: e.activation(out=Sg[:], in_=Pg[:], func=AF.Silu), r=[Pg], w=[Sg])
                    C.op("dve", lambda e: e.tensor_tensor(out=hid[:, ft, :], in0=Sg[:], in1=Pu[:], op=ALU.mult), r=[Sg, Pu], w=[hid])
                for tt in range(4):
                    t = G * 4 + tt
                    XO = xt[tt]
                    for c in range(2):
                        P = po[c]
                        for ft in range(22):
                            C.op("pe", lambda e: e.matmul(P[:], lhsT=hid[:, ft, tt * 128:(tt + 1) * 128], rhs=Wd[:, ft, c * 512:(c + 1) * 512], start=(ft == 0), stop=(ft == 21)), r=[hid, Wd], w=[P])
                        C.op("dve", lambda e: e.tensor_tensor(out=XO[:, c * 512:(c + 1) * 512], in0=P[:], in1=xt[tt][:, c * 512:(c + 1) * 512], op=ALU.add), r=[P, xt[tt]], w=[XO])
                    store(x_dst[t * 128:(t + 1) * 128, :], XO[:], XO)


    Q_T = SCR("Q_T", [16, 64, S], BF16); QR_T = SCR("QR_T", [16, 64, S], BF16)
    KCr_T = SCR("KCr_T", [4, 64, S], BF16); VCr_T = SCR("VCr_T", [4, 64, S], BF16)
    KS_T = SCR("KS_T", [4, 64, S], BF16); VS = SCR("VS", [S, 256], BF16)
    KW_T = SCR("KW_T", [4, 64, S], BF16); VW = SCR("VW", [S, 256], BF16)
    GT = SCR("GT", [S, 48], F32)
    KC_T = C.sb("KC_T", [128, 4, 256], BF16, stack=C.es)
    VCa = C.sb("VCa", [128, 2, 4, 129], BF16, stack=C.es)

    def phase_B0():
        with C.phase():
            g = C.sb("g", [128, 8], F32); load_gain(g, inp["norm_mix"][1])
            W = C.sb("W1", [128, 8, 2608], BF16)
            load_w(W, inp["nsa_w_in"], 8, 2608, g)
            gq = bgain(inp["nsa_q_norm"]); gks = bgain(inp["nsa_ksel_norm"]); gkw = bgain(inp["nsa_kwin_norm"])
            hnm = HeadNorm(8, 64, "pool")
            pr = [C.sb("pr", [128, 512], F32) for _ in range(2)]
            qn = [C.sb("qn", [128, 512], BF16) for _ in range(3)]
            qn2 = [C.sb("qn2", [128, 512], BF16) for _ in range(3)]
            ptq = [C.ps("ptq", [64, 8, 128], BF16) for _ in range(2)]
            qT = [C.sb("qT", [64, 8, 128], BF16) for _ in range(3)]
            chunks = [(0, 512), (512, 1024), (1024, 1536), (1536, 2048), (2048, 2560), (2560, 2608)]
            cnt = {"ti": 0}

            def tr_heads(SRC, H, src_off, dst_ap):
                i = cnt["ti"]; cnt["ti"] += 1
                PT = ptq[i % 2]; QT = qT[i % 3]
                for h in range(H):
                    C.op("pe", lambda e: e.transpose(out=PT[:, h, :], in_=SRC[:, src_off + h * 64: src_off + (h + 1) * 64], identity=ident[:]), r=[SRC, ident], w=[PT])
                C.op("dve", lambda e: e.tensor_copy(out=QT[:, 0:H, :], in_=PT[:, 0:H, :]), r=[PT], w=[QT])
                store(dst_ap, QT[:, 0:H, :], QT)

            def make_job(k, t, c, P):
                R = pr[k % 2]; Q = qn[k % 3]; Q2 = qn2[k % 3]
                ts = slice(t * 128, (t + 1) * 128)
                cs8 = cs64[:, t, :]; sn8 = sn64[:, t, :]
                v8 = lambda A_: A_[:].rearrange("p (h d) -> p h d", h=8)
                v4 = lambda A_: A_[:, 0:256].rearrange("p (h d) -> p h d", h=4)

                def post():
                    if c == 5:
                        C.op("act", lambda e: e.activation(out=R[:, 0:48], in_=P[:, 0:48], func=AF.Exp, scale=-1.0), r=[P], w=[R])
                        C.op("dve", lambda e: e.tensor_scalar(out=R[:, 0:48], in0=R[:, 0:48], scalar1=1.0, scalar2=None, op0=ALU.add), r=[R], w=[R])
                        C.op("dve", lambda e: e.reciprocal(out=R[:, 0:48], in_=R[:, 0:48]), r=[R], w=[R])
                        store(GT[ts, :], R[:, 0:48], R)
                        return
                    C.op("act", lambda e: e.activation(out=R[:], in_=P[:], func=AF.Copy), r=[P], w=[R])
                    if c in (0, 1):
                        hnm.run(v8(R), R, 8, gq, v8(Q), Q, rope=(cs8, sn8, 8), out2_ap=v8(Q2), out2T=Q2)
                    elif c == 2:
                        C.op("act", lambda e: e.activation(out=Q[:], in_=R[:], func=AF.Copy), r=[R], w=[Q])
                    else:
                        gg = gks if c == 3 else gkw
                        hnm.run(v4(R), R, 4, gg, v4(Q), Q, rope=(cs8, sn8, 8))
                        C.op("act", lambda e: e.activation(out=Q[:, 256:512], in_=R[:, 256:512], func=AF.Copy), r=[R], w=[Q])

                def trans():
                    if c == 5:
                        return
                    if c in (0, 1):
                        tr_heads(Q, 8, 0, QR_T[c * 8:(c + 1) * 8, :, ts].rearrange("h d s -> d h s"))
                        tr_heads(Q2, 8, 0, Q_T[c * 8:(c + 1) * 8, :, ts].rearrange("h d s -> d h s"))
                    elif c == 2:
                        tr_heads(Q, 4, 0, KCr_T[:, :, ts].rearrange("h d s -> d h s"))
                        tr_heads(Q, 4, 256, VCr_T[:, :, ts].rearrange("h d s -> d h s"))
                    else:
                        store((VS if c == 3 else VW)[ts, :], Q[:, 256:512], Q)
                        tr_heads(Q, 4, 0, (KS_T if c == 3 else KW_T)[:, :, ts].rearrange("h d s -> d h s"))
                return post, trans

            run_proj(XB, W, chunks, make_job)

    def phase_B1():
        with C.phase():
            C.op("pool", lambda e: e.memset(KC_T[:], 0.0), w=[KC_T])
            C.op("pool", lambda e: e.memset(VCa[:], 0.0), w=[VCa])
            C.op("pool", lambda e: e.memset(VCa[:, :, :, 64:65], 1.0), w=[VCa])
            cov = C.sb("cov", [128, 2, 64], F32)
            C.op("pool", lambda e: e.memset(cov[:], 1.0), w=[cov])
            for half in range(2):
                C.op("pool", lambda e: e.affine_select(out=cov[:, half, :], in_=cov[:, half, :], pattern=[[-64, 64]], compare_op=ALU.is_gt, fill=0.0, base=2048 * half + 32, channel_multiplier=16), r=[cov], w=[cov])
                C.op("pool", lambda e: e.affine_select(out=cov[:, half, :], in_=cov[:, half, :], pattern=[[64, 64]], compare_op=ALU.is_gt, fill=0.0, base=64 - 2048 * half, channel_multiplier=-16), r=[cov], w=[cov])
                C.op("dve", lambda e: e.tensor_copy(out=VCa[:, half, :, 65:129], in_=ins_bc(cov[:, half, :], 1, 4)), r=[cov], w=[VCa])
            gkc = bgain(inp["nsa_kcmp_norm"])
            hnm = HeadNorm(1, 64)
            XT = C.sb("XT", [64, 4, S], BF16)
            w1f = C.sb("w1f", [64, 32, 64], F32); w1b = C.sb("w1b", [64, 32, 64], BF16)
            w2f = C.sb("w2f", [64, 64], F32); w2b = C.sb("w2b", [64, 64], BF16)
            posf = C.sb("posf", [64, 32], F32); posrep = C.sb("posrep", [64, 32, 128], BF16)
            cpos = C.sb("cpos", [128, 64], F32)
            ph = [C.ps("ph", [128, 512], F32) for _ in range(2)]
            pt_ = C.ps("pt_", [64, 8, 128], BF16)
            hs = C.sb("hs", [128, 64], F32); hsb = C.sb("hsb", [128, 64], BF16); hsT = C.sb("hsT", [64, 128], BF16)
            o2 = C.sb("o2", [128, 64], F32); o2b = C.sb("o2b", [128, 64], BF16)
            pi = 0
            for kv in range(2):
                src = (KCr_T, VCr_T)[kv]
                C.dma("sp", XT[:], src.rearrange("g d s -> d g s"), w=[XT])
                C.dma("sp", w1f[:], inp[("nsa_cmp_w1_k", "nsa_cmp_w1_v")[kv]].rearrange("(l d) o -> d l o", d=64), w=[w1f])
                C.dma("sp", w2f[:], inp[("nsa_cmp_w2_k", "nsa_cmp_w2_v")[kv]], w=[w2f])
                C.dma("sp", posf[:], inp[("nsa_cmp_pos_k", "nsa_cmp_pos_v")[kv]].rearrange("l d -> d l"), w=[posf], allow_slow_non_contiguous=True)
                C.op("dve", lambda e: e.tensor_copy(out=w1b[:], in_=w1f[:]), r=[w1f], w=[w1b])
                C.op("dve", lambda e: e.tensor_copy(out=w2b[:], in_=w2f[:]), r=[w2f], w=[w2b])
                C.op("dve", lambda e: e.tensor_copy(out=posrep[:], in_=ins_bc(posf[:], 2, 128)), r=[posf], w=[posrep])
                P = ph[pi % 2]; pi += 1
                for l in range(32):
                    C.op("pe", lambda e: e.matmul(P[:, 0:64], lhsT=posrep[:, l, :], rhs=w1b[:, l, :], start=(l == 0), stop=(l == 31)), r=[posrep, w1b], w=[P])
                C.op("dve", lambda e: e.tensor_copy(out=cpos[:], in_=P[:, 0:64]), r=[P], w=[cpos])
                for g_ in range(4):
                    for half in range(2):
                        M = 128 if half == 0 else 127
                        P = ph[pi % 2]; pi += 1
                        for l in range(32):
                            a0 = 2048 * half + l
                            C.op("pe", lambda e: e.matmul(P[0:M, 0:64], lhsT=XT[:, g_, a0:a0 + 16 * (M - 1) + 1:16], rhs=w1b[:, l, :], start=(l == 0), stop=(l == 31)), r=[XT, w1b], w=[P])
                        C.op("dve", lambda e: e.tensor_tensor(out=hs[0:M, :], in0=P[0:M, 0:64], in1=cpos[0:M, :], op=ALU.add), r=[P, cpos], w=[hs])
                        C.op("act", lambda e: e.activation(out=hsb[0:M, :], in_=hs[0:M, :], func=AF.Silu), r=[hs], w=[hsb])
                        C.op("pe", lambda e: e.transpose(out=pt_[:, 0, 0:M], in_=hsb[0:M, :], identity=ident[0:M, 0:M]), r=[hsb, ident], w=[pt_])
                        C.op("dve", lambda e: e.tensor_copy(out=hsT[:, 0:M], in_=pt_[:, 0, 0:M]), r=[pt_], w=[hsT])
                        P2 = ph[pi % 2]; pi += 1
                        C.op("pe", lambda e: e.matmul(P2[0:M, 0:64], lhsT=hsT[:, 0:M], rhs=w2b[:], start=True, stop=True), r=[hsT, w2b], w=[P2])
                        if kv == 0:
                            C.op("act", lambda e: e.activation(out=o2[0:M, :], in_=P2[0:M, 0:64], func=AF.Copy), r=[P2], w=[o2])
                            hnm.run(o2[:].rearrange("p (h d) -> p h d", h=1), o2, 1, gkc, o2b[:].rearrange("p (h d) -> p h d", h=1), o2b)
                            C.op("pe", lambda e: e.transpose(out=pt_[:, 1, 0:M], in_=o2b[0:M, :], identity=ident[0:M, 0:M]), r=[o2b, ident], w=[pt_])
                            C.op("dve", lambda e: e.tensor_copy(out=KC_T[0:64, g_, half * 128:half * 128 + M], in_=pt_[:, 1, 0:M]), r=[pt_], w=[KC_T])
                        else:
                            C.op("act", lambda e: e.activation(out=VCa[0:M, half, g_, 0:64], in_=P2[0:M, 0:64], func=AF.Copy), r=[P2], w=[VCa])

    def phase_B2():
        with C.phase():
            KS = C.sb("KS", [128, 4, S], BF16); C.dma("sp", KS[0:64, :, :], KS_T.rearrange("g d s -> d g s"), w=[KS])
            KW = C.sb("KW", [128, 4, S], BF16)
            C.op("pool", lambda e: e.memset(KW[64:128, :, :], 0.0), w=[KW])
            C.dma("sp", KW[0:64, :, :], KW_T.rearrange("g d s -> d g s"), w=[KW])
            VSa = C.sb("VSa", [128, NT, 4, 65], BF16); VWa = C.sb("VWa", [128, NT, 4, 65], BF16)
            C.op("pool", lambda e: e.memset(VSa[:], 1.0), w=[VSa])
            C.op("pool", lambda e: e.memset(VWa[:], 1.0), w=[VWa])
            for t in range(NT):
                C.dma("sp", VSa[:, t, :, 0:64], VS[t * 128:(t + 1) * 128, :].rearrange("p (g d) -> p g d", g=4), w=[VSa])
                C.dma("sp", VWa[:, t, :, 0:64], VW[t * 128:(t + 1) * 128, :].rearrange("p (g d) -> p g d", g=4), w=[VWa])
            with C.phase():
                Ef = C.sb("E2f", [128, S], F32)
                C.op("pool", lambda e: e.memset(Ef[64:128, :], 1.0), w=[Ef])
                C.op("pool", lambda e: e.affine_select(out=Ef[64:128, :], in_=Ef[64:128, :], pattern=[[1, S]], compare_op=ALU.is_ge, fill=0.0, base=0, channel_multiplier=-64), r=[Ef], w=[Ef])
                C.op("pool", lambda e: e.affine_select(out=Ef[64:128, :], in_=Ef[64:128, :], pattern=[[-1, S]], compare_op=ALU.is_ge, fill=0.0, base=63, channel_multiplier=64), r=[Ef], w=[Ef])
                for g_ in range(4):
                    C.op("dve" if g_ % 2 == 0 else "act", (lambda e: e.tensor_copy(out=KS[64:128, g_, :], in_=Ef[64:128, :])) if g_ % 2 == 0 else (lambda e: e.activation(out=KS[64:128, g_, :], in_=Ef[64:128, :], func=AF.Copy)), r=[Ef], w=[KS])
            V0 = C.sb("V0", [128, 128], F32)
            C.op("pool", lambda e: e.iota(out=V0[:], pattern=[[-1, 128]], base=0, channel_multiplier=16, allow_small_or_imprecise_dtypes=True), w=[V0])
            J = C.sb("J", [128, 64], F32)
            C.op("pool", lambda e: e.iota(out=J[:], pattern=[[1, 64]], base=0, channel_multiplier=0, allow_small_or_imprecise_dtypes=True), w=[J])
            f0 = C.sb("f0", [128, 64], F32)
            C.op("dve", lambda e: e.tensor_scalar(out=f0[:], in0=J[:], scalar1=0.0, scalar2=None, op0=ALU.is_equal), r=[J], w=[f0])
            CURb = C.sb("CURb", [128, 1], F32)
            C.op("pool", lambda e: e.iota(out=CURb[:], pattern=[[0, 1]], base=0, channel_multiplier=1, allow_small_or_imprecise_dtypes=True), w=[CURb])
            C.op("dve", lambda e: e.tensor_scalar(out=CURb[:], in0=CURb[:], scalar1=64.0, scalar2=None, op0=ALU.is_ge), r=[CURb], w=[CURb])
            sm = {n: C.sb(n, [128, 1], F32) for n in ("curq", "cm1")}
            rz4s = [C.sb("rz4", [128, 4], F32) for _ in range(2)]; cf4s = [C.sb("cf4", [128, 4], F32) for _ in range(2)]
            f1 = C.sb("f1", [128, 64], F32); f2 = C.sb("f2", [128, 64], F32)
            FORCE = C.sb("FORCE", [128, 64], F32); FUT = C.sb("FUT", [128, 64], F32)
            imp = C.sb("imp", [128, 64], F32); imp3 = C.sb("imp3", [128, 64], F32); impr = C.sb("impr", [128, 64], F32)
            imt = C.sb("imt", [128, 4, 64], F32)
            m8a = C.sb("m8a", [128, 8], F32); m8b = C.sb("m8b", [128, 8], F32)
            nm = C.sb("nm", [128, 128], BF16)
            C.op("pool", lambda e: e.memset(nm[:], 0.0), w=[nm])
            pnm = C.ps("pnm", [128, 8, 128], BF16)
            cb = [C.sb("cb", [128, 4, 128], BF16) for _ in range(2)]
            qnt = [C.sb("qnt", [128, 16, 128], BF16) for _ in range(2)]
            qrt = [C.sb("qrt", [128, 16, 128], BF16) for _ in range(2)]
            for t_ in qnt + qrt:
                C.op("pool", lambda e: e.memset(t_[:], 0.0), w=[t_])
            gts = [C.sb("gts", [128, 48], F32) for _ in range(2)]
            acc = C.sb("acc", [128, D], F32)
            obf = [C.sb("obf", [128, D], BF16) for _ in range(2)]
            pss = [C.ps("pss", [128, 512], F32) for _ in range(2)]
            pacc = C.ps("pacc", [128, 4, 512], F32)
            PTs = [C.sb("PTs", [128, 512], BF16) for _ in range(3)]
            si = 0; pi = 0; zi = 0
            for qt in range(NT):
                ts = slice(qt * 128, (qt + 1) * 128)
                QN = qnt[qt % 2]; QR = qrt[qt % 2]; Gt = gts[qt % 2]; OB = obf[qt % 2]
                C.dma("sp", QN[0:64, :, :], Q_T[:, :, ts].rearrange("h d s -> d h s"), w=[QN])
                C.dma("sp", QR[0:64, :, :], QR_T[:, :, ts].rearrange("h d s -> d h s"), w=[QR])
                C.dma("sp", Gt[:], GT[ts, :], w=[Gt])
                nhalf = 1 if qt < 16 else 2
                for half in range(nhalf):
                    C.op("dve", lambda e: e.tensor_scalar(out=cb[half][:], in0=ins_bc(V0[:], 1, 4), scalar1=float(128 * qt - 31 - 2048 * half), scalar2=NEG, op0=ALU.is_gt, op1=ALU.mult), r=[V0], w=[cb[half]])
                C.op("dve", lambda e: e.tensor_scalar(out=sm["curq"][:], in0=CURb[:], scalar1=float(2 * qt), scalar2=None, op0=ALU.add), r=[CURb], w=[sm["curq"]])
                C.op("dve", lambda e: e.tensor_scalar(out=sm["cm1"][:], in0=CURb[:], scalar1=float(2 * qt - 1), scalar2=None, op0=ALU.add), r=[CURb], w=[sm["cm1"]])
                C.op("dve", lambda e: e.tensor_scalar(out=f1[:], in0=J[:], scalar1=sm["curq"][:], scalar2=None, op0=ALU.is_equal), r=[J, sm["curq"]], w=[f1])
                C.op("dve", lambda e: e.tensor_scalar(out=f2[:], in0=J[:], scalar1=sm["cm1"][:], scalar2=None, op0=ALU.is_equal), r=[J, sm["cm1"]], w=[f2])
                C.op("dve", lambda e: e.tensor_tensor(out=f1[:], in0=f1[:], in1=f2[:], op=ALU.add), r=[f1, f2], w=[f1])
                C.op("dve", lambda e: e.tensor_tensor(out=f1[:], in0=f1[:], in1=f0[:], op=ALU.add), r=[f1, f0], w=[f1])
                C.op("dve", lambda e: e.tensor_scalar(out=FORCE[:], in0=f1[:], scalar1=1e4, scalar2=None, op0=ALU.mult), r=[f1], w=[FORCE])
                C.op("dve", lambda e: e.tensor_scalar(out=FUT[:], in0=J[:], scalar1=sm["curq"][:], scalar2=-1e30, op0=ALU.is_gt, op1=ALU.mult), r=[J, sm["curq"]], w=[FUT])

                def branch(g_, qk_fn, sts, Vrhs_fn, ncol, gidx, first):
                    nonlocal si, pi, zi
                    n = len(sts)

                    def qk(st):
                        nonlocal si
                        P = pss[si % 2]; si += 1
                        mm = qk_fn(st)
                        for ei, (l_ap, lT, r_ap, rT) in enumerate(mm):
                            C.op("pe", lambda e: e.matmul(P[:], lhsT=l_ap, rhs=r_ap, start=(ei == 0), stop=(ei == len(mm) - 1)), r=[lT, rT], w=[P])
                        return P

                    Pc = qk(sts[0])
                    for ix, st in enumerate(sts):
                        Pn = qk(sts[ix + 1]) if ix + 1 < n else None
                        PT = PTs[pi % 3]; pi += 1
                        C.op("act", lambda e: e.activation(out=PT[:], in_=Pc[:], func=AF.Exp, scale=0.125), r=[Pc], w=[PT])
                        v_ap, vT = Vrhs_fn(st)
                        for j in range(4):
                            C.op("pe", lambda e: e.matmul(pacc[:, j, 0:ncol], lhsT=PT[:, j * 128:(j + 1) * 128], rhs=v_ap, start=(ix == 0), stop=(ix == n - 1)), r=[PT, vT], w=[pacc])
                        Pc = Pn
                    rz4 = rz4s[zi % 2]; cf4 = cf4s[zi % 2]; zi += 1
                    C.op("dve", lambda e: e.tensor_scalar(out=rz4[:], in0=pacc[:, :, 64], scalar1=1e-20, scalar2=None, op0=ALU.max), r=[pacc], w=[rz4])
                    C.op("dve", lambda e: e.reciprocal(out=rz4[:], in_=rz4[:]), r=[rz4], w=[rz4])
                    C.op("dve", lambda e: e.tensor_tensor(out=cf4[:], in0=rz4[:], in1=Gt[:, 12 * g_ + gidx:12 * g_ + gidx + 10:3], op=ALU.mult), r=[rz4, Gt], w=[cf4])
                    for j in range(4):
                        hd = 4 * g_ + j
                        ah = acc[:, hd * 64:(hd + 1) * 64]
                        if first:
                            C.op("act", lambda e: e.activation(out=ah, in_=pacc[:, j, 0:64], func=AF.Copy, scale=cf4[:, j:j + 1]), r=[pacc, cf4], w=[acc])
                        else:
                            C.op("dve", lambda e: e.scalar_tensor_tensor(out=ah, in0=pacc[:, j, 0:64], scalar=cf4[:, j:j + 1], in1=ah, op0=ALU.mult, op1=ALU.add), r=[pacc, cf4, acc], w=[acc])
                    if first:
                        C.op("dve", lambda e: e.tensor_tensor(out=imt[:], in0=pacc[:, :, 65:129], in1=ins_bc(rz4[:], 2, 64), op=ALU.mult), r=[pacc, rz4], w=[imt])
                        C.op("dve", lambda e: e.tensor_reduce(out=imp[:], in_=imt[:].rearrange("p j d -> p d j"), axis=AX.X, op=ALU.add), r=[imt], w=[imp])

                for g_ in range(4):
                    branch(g_, lambda st: [(KC_T[:, g_, st * 128:(st + 1) * 128], KC_T, QN[:, 4 * g_:4 * g_ + 4, :], QN), (ident[:], ident, cb[st][:], cb[st])],
                           list(range(nhalf)), lambda st: (VCa[:, st, g_, :], VCa), 129, 0, True)
                    C.op("dve", lambda e: e.tensor_tensor(out=imp3[:], in0=imp[:], in1=FORCE[:], op=ALU.max), r=[imp, FORCE], w=[imp3])
                    C.op("dve", lambda e: e.tensor_tensor(out=imp3[:], in0=imp3[:], in1=FUT[:], op=ALU.add), r=[imp3, FUT], w=[imp3])
                    C.op("dve", lambda e: e.max(out=m8a[:], in_=imp3[:]), r=[imp3], w=[m8a])
                    C.op("dve", lambda e: e.match_replace(out=impr[:], in_to_replace=m8a[:], in_values=imp3[:], imm_value=-1e30), r=[imp3, m8a], w=[impr])
                    C.op("dve", lambda e: e.max(out=m8b[:], in_=impr[:]), r=[impr], w=[m8b])
                    C.op("dve", lambda e: e.tensor_scalar(out=nm[:, 64:128], in0=imp3[:], scalar1=m8b[:, 7:8], scalar2=NEG, op0=ALU.is_lt, op1=ALU.mult), r=[imp3, m8b], w=[nm])
                    def selqk(st):
                        m = [(KS[:, g_, st * 128:(st + 1) * 128], KS, QR[:, 4 * g_:4 * g_ + 4, :], QR)]
                        if st == qt:
                            m.append((ident[:], ident, TRI4[:], TRI4))
                        return m

                    def winqk(st):
                        m = [(KW[:, g_, st * 128:(st + 1) * 128], KW, QR[:, 4 * g_:4 * g_ + 4, :], QR)]
                        if st == qt:
                            m.append((ident[:], ident, TRI4[:], TRI4))
                        elif st == qt - 4:
                            m.append((ident[:], ident, BLO4[:], BLO4))
                        return m
                    branch(g_, winqk, list(range(max(0, qt - 4), qt + 1)), lambda st: (VWa[:, st, g_, :], VWa), 65, 2, False)
                    C.op("pe", lambda e: e.transpose(out=pnm[:, 0, :], in_=nm[:], identity=ident[:]), r=[nm, ident], w=[pnm])
                    C.op("dve", lambda e: e.tensor_copy(out=QR[64:128, 4 * g_:4 * g_ + 4, :], in_=ins_bc(pnm[64:128, 0, :], 1, 4)), r=[pnm], w=[QR])

                    branch(g_, selqk, list(range(qt + 1)), lambda st: (VSa[:, st, g_, :], VSa), 65, 1, False)
                C.op("act", lambda e: e.activation(out=OB[:], in_=acc[:], func=AF.Copy), r=[acc], w=[OB])
                store(OM[ts, :], OB[:], OB)

    if upto >= 1:
        phase_A0()
    if upto >= 2:
        phase_A1()
    if upto >= 3:
        phase_A2()
    if upto >= 4:
        phase_A3(0, inp["ab_w_out"], x_in, XA)
    if upto >= 5:
        phase_A4(0, XA, XB if (dbg or upto > 5) else y_out)
    if upto >= 6:
        phase_B0()
    if upto >= 7:
        phase_B1()
    if upto >= 8:
        phase_B2()
    if upto >= 9:
        phase_A3(1, inp["nsa_w_out"], XB, XC)
    if upto >= 10:
        phase_A4(1, XC, y_out)
    C.close()
    k.nc = nc; k.dbg_names = dbg_names; k.ninstr = C.ninstr; k.inp_names = list(inp)
    return k


_W_NAMES = ["norm_mix", "norm_mem", "norm_mem_src", "norm_ffn", "mem_w_q", "mem_w_kv", "mem_w_o", "mem_q_norm", "mem_k_norm", "ffn_w_in", "ffn_w_out"]
_W0_NAMES = ["ab_w_in", "ab_w_out", "dsa_q_norm", "dsa_k_norm", "moba_q_norm", "moba_k_norm", "nsa_w_in", "nsa_w_out", "nsa_q_norm", "nsa_kcmp_norm",
             "nsa_ksel_norm", "nsa_kwin_norm", "nsa_cmp_pos_k", "nsa_cmp_pos_v", "nsa_cmp_w1_k", "nsa_cmp_w2_k", "nsa_cmp_w1_v", "nsa_cmp_w2_v"]


def make_in_map(inputs, b):
    m = {"x": np.ascontiguousarray(inputs["x"][b], dtype=np.float32),
         "mem": np.ascontiguousarray(inputs["mem"][b], dtype=np.float32),
         "post": np.ascontiguousarray(np.asarray(inputs["positions"][b]).astype(np.int32).reshape(NT, 128).T)}
    for n in _W_NAMES:
        m[n] = np.ascontiguousarray(inputs[n], dtype=np.float32)
    for n in _W0_NAMES:
        m[n] = np.ascontiguousarray(np.asarray(inputs[n])[0], dtype=np.float32)
    return m


def kernel(**inputs):
    from concourse.bass_utils import run_bass_kernel_spmd
    k = build(dbg=False)
    in_maps = [make_in_map(inputs, b) for b in range(8)]
    res = run_bass_kernel_spmd(k.nc, in_maps, core_ids=list(range(8)))
    return np.stack([np.asarray(r["y"], dtype=np.float32) for r in res.results], axis=0)
```

```python
from contextlib import ExitStack
import numpy as np
import concourse.bass as bass
import concourse.mybir as mybir

F32 = mybir.dt.float32
BF16 = mybir.dt.bfloat16
I32 = mybir.dt.int32
AF = mybir.ActivationFunctionType
ALU = mybir.AluOpType
AX = mybir.AxisListType

N_DMA_SLOTS = 24
N_HW_SLOTS = 14


class Buf:
    __slots__ = ("name", "w", "r")

    def __init__(self, name):
        self.name = name
        self.w = None
        self.r = {}


class T:
    def __init__(self, t, name):
        self.t = t
        self.b = Buf(name)

    def __getitem__(self, k):
        return self.t[k]


class Ctx:
    def __init__(self, nc):
        self.nc = nc
        self.es = ExitStack()
        self.eng = {"pe": nc.tensor, "act": nc.scalar, "dve": nc.vector, "pool": nc.gpsimd, "sp": nc.sync}
        self.sem = {}
        for k in ("pe", "act", "dve", "pool"):
            self.sem[k] = self.es.enter_context(nc.semaphore("sem_" + k))
        for i in range(N_DMA_SLOTS):
            self.sem[("d", i)] = self.es.enter_context(nc.semaphore("semd%d" % i))
        self.cnt = {k: 0 for k in ("pe", "act", "dve", "pool")}
        self.dval = [0] * N_DMA_SLOTS
        self.dnext = 0
        self.dnext_sw = 0
        self.seen = {k: {} for k in self.eng}
        self.uid = 0
        self.phase_stack = None
        self.ninstr = 0

    def _nm(self, name):
        self.uid += 1
        return "%s_%d" % (name, self.uid)

    def sb(self, name, shape, dtype, stack=None):
        st = stack if stack is not None else (self.phase_stack or self.es)
        nm = self._nm(name)
        return T(st.enter_context(self.nc.sbuf_tensor(nm, list(shape), dtype)), nm)

    def ps(self, name, shape, dtype, stack=None):
        st = stack if stack is not None else (self.phase_stack or self.es)
        nm = self._nm(name)
        return T(st.enter_context(self.nc.psum_tensor(nm, list(shape), dtype)), nm)

    def dram(self, name, shape, dtype, kind="Internal"):
        return self.nc.dram_tensor(name, list(shape), dtype, kind=kind).ap()

    def _waits(self, E, reads, writes):
        deps = {}

        def need(k, v):
            if v > deps.get(k, 0):
                deps[k] = v

        for t in reads:
            b = t.b
            if b.w is not None:
                need(*b.w)
        for t in writes:
            b = t.b
            if b.w is not None:
                need(*b.w)
            for k, v in b.r.items():
                need(k, v)
        eng = self.eng[E]
        seen = self.seen[E]
        for k, v in deps.items():
            if k == E and E == "pe":
                continue
            if seen.get(k, 0) >= v:
                continue
            eng.wait_ge(self.sem[k], v)
            self.ninstr += 1
            seen[k] = v

    def op(self, E, fn, r=(), w=()):
        self._waits(E, r, w)
        inst = fn(self.eng[E])
        self.cnt[E] += 1
        self.ninstr += 1
        inst.then_inc(self.sem[E], 1)
        ev = self.cnt[E]
        for t in r:
            if t.b.r.get(E, 0) < ev:
                t.b.r[E] = ev
        for t in w:
            t.b.w = (E, ev)
            t.b.r = {}
        return inst

    def dma(self, Q, out, in_, r=(), w=(), **kw):
        self._waits(Q, r, w)
        slot = self.dnext
        self.dnext = (self.dnext + 1) % N_DMA_SLOTS
        k = ("d", slot)
        eng = self.eng[Q]
        if self.seen[Q].get(k, 0) < self.dval[slot]:
            eng.wait_ge(self.sem[k], self.dval[slot])
            self.seen[Q][k] = self.dval[slot]
            self.ninstr += 1
        inst = eng.dma_start(out=out, in_=in_, **kw)
        self.dval[slot] += 16
        inst.then_inc(self.sem[k], 16)
        self.ninstr += 1
        v = self.dval[slot]
        for t in r:
            if t.b.r.get(k, 0) < v:
                t.b.r[k] = v
        for t in w:
            t.b.w = (k, v)
            t.b.r = {}
        return inst

    def barrier(self):
        for E, eng in self.eng.items():
            seen = self.seen[E]
            for k in ("pe", "act", "dve", "pool"):
                v = self.cnt[k]
                if v == 0 or seen.get(k, 0) >= v or (k == E and E == "pe"):
                    continue
                eng.wait_ge(self.sem[k], v)
                seen[k] = v
                self.ninstr += 1
            for i in range(N_DMA_SLOTS):
                k = ("d", i)
                v = self.dval[i]
                if v == 0 or seen.get(k, 0) >= v:
                    continue
                eng.wait_ge(self.sem[k], v)
                seen[k] = v
                self.ninstr += 1

    def phase(self):
        ctx = self

        class _P:
            def __enter__(s):
                ctx.barrier()
                s.prev = ctx.phase_stack
                s.st = ExitStack()
                ctx.phase_stack = s.st
                return s

            def __exit__(s, *a):
                ctx.barrier()
                ctx.phase_stack = s.prev
                s.st.close()
                return False

        return _P()

    def close(self):
        self.barrier()
        self.es.close()


S = 4096
D = 1024
NT = 32
NEG = -30000.0
EPS = 1e-6
THETA = 500000.0
DFF = 2816


def ins_bc(ap, axis, n):
    l = [list(p) for p in ap.ap]
    l.insert(axis, [0, n])
    return bass.AP(ap.tensor, ap.offset, l)


class K:
    pass


def build(dbg=False, upto=99):
    nc = bass.Bass("TRN2", target_bir_lowering=False)
    k = K()
    inp = {}

    def IN(name, shape, dt=F32):
        inp[name] = nc.dram_tensor(name, list(shape), dt, kind="ExternalInput").ap()
        return inp[name]

    x_in = IN("x", [S, D]); mem_in = IN("mem", [256, D]); pos_in = IN("post", [128, NT], I32)
    for n in ("norm_mix", "norm_mem", "norm_mem_src", "norm_ffn"):
        IN(n, [2, D])
    IN("ab_w_in", [D, 2472]); IN("ab_w_out", [D, D])
    for n in ("dsa_q_norm", "dsa_k_norm", "moba_q_norm", "moba_k_norm", "nsa_q_norm", "nsa_kcmp_norm", "nsa_ksel_norm", "nsa_kwin_norm"):
        IN(n, [64])
    IN("nsa_w_in", [D, 2608]); IN("nsa_w_out", [D, D])
    IN("nsa_cmp_pos_k", [32, 64]); IN("nsa_cmp_pos_v", [32, 64])
    IN("nsa_cmp_w1_k", [2048, 64]); IN("nsa_cmp_w2_k", [64, 64]); IN("nsa_cmp_w1_v", [2048, 64]); IN("nsa_cmp_w2_v", [64, 64])
    IN("mem_w_q", [2, D, 512]); IN("mem_w_kv", [2, D, 1024]); IN("mem_w_o", [2, 512, D])
    IN("mem_q_norm", [2, 128]); IN("mem_k_norm", [2, 128])
    IN("ffn_w_in", [2, D, 2 * DFF]); IN("ffn_w_out", [2, DFF, D])
    y_out = nc.dram_tensor("y", [S, D], F32, kind="ExternalOutput").ap()

    C = Ctx(nc)
    skind = "ExternalOutput" if dbg else "Internal"
    dbg_names = []

    def SCR(name, shape, dt):
        if dbg:
            dbg_names.append(name)
        return nc.dram_tensor(name, list(shape), dt, kind=skind).ap()

    ident = C.sb("ident", [128, 128], BF16)
    I4 = C.sb("I4", [128, 4, 128], BF16)
    epsG = C.sb("epsG", [128, 1], F32)
    C.op("pool", lambda e: e.memset(epsG[:], EPS), w=[epsG])
    TRI4 = C.sb("TRI4", [128, 4, 128], BF16)
    BLO4 = C.sb("BLO4", [128, 4, 128], BF16)
    CAUS = C.sb("CAUS", [128, 4, 4, 128], BF16)
    cs64 = C.sb("cs64", [128, NT, 8], F32); sn64 = C.sb("sn64", [128, NT, 8], F32)
    cs32 = C.sb("cs32", [128, NT, 4], F32); sn32 = C.sb("sn32", [128, NT, 4], F32)
    with C.phase():
        cf = C.sb("cf", [128, 128], F32)
        C.op("pool", lambda e: e.memset(cf[:], 0.0), w=[cf])
        C.op("pool", lambda e: e.affine_select(out=cf[:], in_=cf[:], pattern=[[-1, 128]], compare_op=ALU.not_equal, fill=1.0, base=0, channel_multiplier=1), r=[cf], w=[cf])
        C.op("dve", lambda e: e.tensor_copy(out=ident[:], in_=cf[:]), r=[cf], w=[ident])
        C.op("dve", lambda e: e.tensor_copy(out=I4[:], in_=ins_bc(cf[:], 1, 4)), r=[cf], w=[I4])
        tf = C.sb("tf", [128, 128], F32)
        C.op("pool", lambda e: e.memset(tf[:], 0.0), w=[tf])
        C.op("pool", lambda e: e.affine_select(out=tf[:], in_=tf[:], pattern=[[1, 128]], compare_op=ALU.is_ge, fill=NEG, base=0, channel_multiplier=-1), r=[tf], w=[tf])
        C.op("dve", lambda e: e.tensor_copy(out=TRI4[:], in_=ins_bc(tf[:], 1, 4)), r=[tf], w=[TRI4])
        for j in range(4):
            for qi in range(4):
                if qi > j:
                    C.op("dve", lambda e: e.memset(CAUS[:, j, qi, :], 0.0), w=[CAUS])
                elif qi == j:
                    C.op("dve", lambda e: e.tensor_copy(out=CAUS[:, j, qi, :], in_=tf[:]), r=[tf], w=[CAUS])
                else:
                    C.op("dve", lambda e: e.memset(CAUS[:, j, qi, :], NEG), w=[CAUS])
        tf2 = C.sb("tf2", [128, 128], F32)
        C.op("pool", lambda e: e.memset(tf2[:], 0.0), w=[tf2])
        C.op("pool", lambda e: e.affine_select(out=tf2[:], in_=tf2[:], pattern=[[-1, 128]], compare_op=ALU.is_gt, fill=NEG, base=0, channel_multiplier=1), r=[tf2], w=[tf2])
        C.op("dve", lambda e: e.tensor_copy(out=BLO4[:], in_=ins_bc(tf2[:], 1, 4)), r=[tf2], w=[BLO4])
        pi_ = C.sb("posi", [128, NT], I32); pf = C.sb("posf", [128, NT], F32)
        C.dma("sp", pi_[:], pos_in, w=[pi_])
        C.op("dve", lambda e: e.tensor_copy(out=pf[:], in_=pi_[:]), r=[pi_], w=[pf])
        u = C.sb("u", [128, NT], F32); ui = C.sb("ui", [128, NT], I32); uf = C.sb("uf", [128, NT], F32); fr = C.sb("fr", [128, NT], F32)

        def sincol(dst, col, freq, shift):
            C.op("dve", lambda e: e.tensor_scalar(out=u[:], in0=pf[:], scalar1=float(freq / (2 * np.pi)), scalar2=float(shift), op0=ALU.mult, op1=ALU.add), r=[pf], w=[u])
            C.op("dve", lambda e: e.tensor_copy(out=ui[:], in_=u[:]), r=[u], w=[ui])
            C.op("dve", lambda e: e.tensor_copy(out=uf[:], in_=ui[:]), r=[ui], w=[uf])
            C.op("dve", lambda e: e.tensor_tensor(out=fr[:], in0=u[:], in1=uf[:], op=ALU.subtract), r=[u, uf], w=[fr])
            C.op("dve", lambda e: e.tensor_scalar(out=fr[:], in0=fr[:], scalar1=0.4999, scalar2=-0.4999, op0=ALU.min, op1=ALU.max), r=[fr], w=[fr])
            C.op("act", lambda e: e.activation(out=dst[:, :, col], in_=fr[:], func=AF.Sin, scale=float(2 * np.pi)), r=[fr], w=[dst])

        for i in range(8):
            f = THETA ** (-(i * 2.0 / 16))
            sincol(sn64, i, f, 0.0); sincol(cs64, i, f, 0.25)
        for i in range(4):
            f = THETA ** (-(i * 2.0 / 8))
            sincol(sn32, i, f, 0.0); sincol(cs32, i, f, 0.25)

    def load_gain(dst, src_ap):
        C.dma("sp", dst[:], src_ap.rearrange("(kt p) -> p kt", p=128), w=[dst], allow_slow_non_contiguous=True)

    def load_w(dst, src_ap, KT, N, gain=None):
        srcv = src_ap.rearrange("(kt p) n -> p kt n", p=128)
        with C.phase():
            st = [C.sb("wst", [128, 4, 512], F32) for _ in range(3)]
            i = 0
            for k0 in range(0, KT, 4):
                k1 = min(KT, k0 + 4)
                for c0 in range(0, N, 512):
                    c1 = min(N, c0 + 512)
                    s = st[i % 3]
                    C.dma("sp" if i % 2 == 0 else "pool", s[:, 0:k1 - k0, 0:c1 - c0], srcv[:, k0:k1, c0:c1], w=[s])
                    for kt in range(k0, k1):
                        if kt % 2 == 0:
                            if gain is not None:
                                C.op("dve", lambda e: e.tensor_scalar(out=dst[:, kt, c0:c1], in0=s[:, kt - k0, 0:c1 - c0], scalar1=gain[:, kt:kt + 1], scalar2=None, op0=ALU.mult), r=[s, gain], w=[dst])
                            else:
                                C.op("dve", lambda e: e.tensor_copy(out=dst[:, kt, c0:c1], in_=s[:, kt - k0, 0:c1 - c0]), r=[s], w=[dst])
                        else:
                            if gain is not None:
                                C.op("act", lambda e: e.activation(out=dst[:, kt, c0:c1], in_=s[:, kt - k0, 0:c1 - c0], func=AF.Copy, scale=gain[:, kt:kt + 1]), r=[s, gain], w=[dst])
                            else:
                                C.op("act", lambda e: e.activation(out=dst[:, kt, c0:c1], in_=s[:, kt - k0, 0:c1 - c0], func=AF.Copy), r=[s], w=[dst])
                    i += 1

    class NormT:
        def __init__(s, pT=None):
            s.junk = C.sb("junk", [128, D], F32)
            s.ss = C.sb("ss", [128, 1], F32); s.rs = C.sb("rs", [128, 1], F32); s.epsT = epsG
            s.hn = C.sb("hn", [128, D], BF16)
            s.pT = pT if pT is not None else C.ps("pT", [128, 8, 128], BF16)

        def run(s, X, hT_ap, hT):
            C.op("act", lambda e: e.activation(out=s.junk[:], in_=X[:], func=AF.Square, accum_out=s.ss[:]), r=[X], w=[s.junk, s.ss])
            C.op("act", lambda e: e.activation(out=s.rs[:], in_=s.ss[:], func=AF.Ln, scale=1.0 / D, bias=s.epsT[:]), r=[s.ss, s.epsT], w=[s.rs])
            C.op("act", lambda e: e.activation(out=s.rs[:], in_=s.rs[:], func=AF.Exp, scale=-0.5), r=[s.rs], w=[s.rs])
            C.op("act", lambda e: e.activation(out=s.hn[:], in_=X[:], func=AF.Copy, scale=s.rs[:]), r=[X, s.rs], w=[s.hn])
            for kt in range(8):
                C.op("pe", lambda e: e.transpose(out=s.pT[:, kt, :], in_=s.hn[:, kt * 128:(kt + 1) * 128], identity=ident[:]), r=[s.hn, ident], w=[s.pT])
            C.op("dve", lambda e: e.tensor_copy(out=hT_ap, in_=s.pT[:]), r=[s.pT], w=[hT])

    class HeadNorm:
        def __init__(s, Hmax, Dh, rope_eng="dve"):
            s.sq = C.sb("hsq", [128, Hmax, Dh], F32)
            s.nrms = [C.sb("hnrm", [128, Hmax, Dh], F32) for _ in range(2)]
            s.ssh = C.sb("hss", [128, Hmax], F32); s.rsh = C.sb("hrs", [128, Hmax], F32)
            hf = Dh // 8
            s.t1s = [C.sb("ht1", [128, Hmax, hf], F32) for _ in range(2)]; s.t2s = [C.sb("ht2", [128, Hmax, hf], F32) for _ in range(2)]
            s.Dh = Dh; s.k = 0; s.re = rope_eng

        def run(s, src_ap, srcT, H, gain, out_ap, outT, rope=None, norm=True, out2_ap=None, out2T=None):
            Dh = s.Dh
            s.k += 1
            nrmT = s.nrms[s.k % 2]; t1T = s.t1s[s.k % 2]; t2T = s.t2s[s.k % 2]
            if norm:
                sq = s.sq[:, 0:H, :]; nrm = nrmT[:, 0:H, :]
                C.op("dve", lambda e: e.tensor_tensor(out=sq, in0=src_ap, in1=src_ap, op=ALU.mult), r=[srcT], w=[s.sq])
                C.op("dve", lambda e: e.tensor_reduce(out=s.ssh[:, 0:H], in_=sq, axis=AX.X, op=ALU.add), r=[s.sq], w=[s.ssh])
                C.op("act", lambda e: e.activation(out=s.rsh[:, 0:H], in_=s.ssh[:, 0:H], func=AF.Ln, scale=1.0 / Dh, bias=epsG[:]), r=[s.ssh, epsG], w=[s.rsh])
                C.op("act", lambda e: e.activation(out=s.rsh[:, 0:H], in_=s.rsh[:, 0:H], func=AF.Exp, scale=-0.5), r=[s.rsh], w=[s.rsh])
                C.op("dve", lambda e: e.tensor_tensor(out=nrm, in0=src_ap, in1=ins_bc(s.rsh[:, 0:H], 2, Dh), op=ALU.mult), r=[srcT, s.rsh], w=[nrmT])
                C.op("dve", lambda e: e.tensor_tensor(out=nrm, in0=nrm, in1=ins_bc(gain[:], 1, H), op=ALU.mult), r=[nrmT, gain], w=[nrmT])
                nT = nrmT
            else:
                nrm = src_ap; nT = srcT
            if out2_ap is not None:
                C.op("act", lambda e: e.activation(out=out2_ap, in_=nrm, func=AF.Copy), r=[nT], w=[out2T])
            C.op("act", lambda e: e.activation(out=out_ap, in_=nrm, func=AF.Copy), r=[nT], w=[outT])
            if rope is not None:
                RE = s.re
                cs, sn, hf = rope
                x1 = nrm[:, :, 0:hf]; x2 = nrm[:, :, hf:2 * hf]
                csb = ins_bc(cs, 1, H); snb = ins_bc(sn, 1, H)
                t1 = t1T[:, 0:H, :]; t2 = t2T[:, 0:H, :]
                C.op(RE, lambda e: e.tensor_tensor(out=t1, in0=x1, in1=csb, op=ALU.mult), r=[nT], w=[t1T])
                C.op(RE, lambda e: e.tensor_tensor(out=t2, in0=x2, in1=snb, op=ALU.mult), r=[nT], w=[t2T])
                C.op(RE, lambda e: e.tensor_tensor(out=out_ap[:, :, 0:hf], in0=t1, in1=t2, op=ALU.subtract), r=[t1T, t2T], w=[outT])
                C.op(RE, lambda e: e.tensor_tensor(out=t1, in0=x2, in1=csb, op=ALU.mult), r=[nT], w=[t1T])
                C.op(RE, lambda e: e.tensor_tensor(out=t2, in0=x1, in1=snb, op=ALU.mult), r=[nT], w=[t2T])
                C.op(RE, lambda e: e.tensor_tensor(out=out_ap[:, :, hf:2 * hf], in0=t1, in1=t2, op=ALU.add), r=[t1T, t2T], w=[outT])

    def run_proj(x_src, W, chunks, make_job, npo=3):
        nt = NormT()
        xt = [C.sb("xt", [128, D], F32) for _ in range(2)]
        hT = [C.sb("hT", [128, 8, 128], BF16) for _ in range(2)]
        po = [C.ps("po", [128, 512], F32) for _ in range(npo)]

        def norm(t):
            X = xt[t % 2]
            C.dma("sp", X[:], x_src[t * 128:(t + 1) * 128, :], w=[X])
            nt.run(X, hT[t % 2][:], hT[t % 2])

        jobs = [(t, c) for t in range(NT) for c in range(len(chunks))]

        def mm(k):
            t, c = jobs[k]
            c0, c1 = chunks[c]
            P = po[k % npo]
            H_ = hT[t % 2]
            for kt in range(8):
                C.op("pe", lambda e: e.matmul(P[:, 0:c1 - c0], lhsT=H_[:, kt, :], rhs=W[:, kt, c0:c1], start=(kt == 0), stop=(kt == 7)), r=[H_, W], w=[P])
            return P

        norm(0)
        Ps = {0: mm(0)}
        prev_trans = None
        for k, (t, c) in enumerate(jobs):
            if c == 0 and t + 1 < NT:
                norm(t + 1)
            if k + 1 < len(jobs):
                Ps[k + 1] = mm(k + 1)
            if prev_trans is not None:
                prev_trans()
            post, trans = make_job(k, t, c, Ps.pop(k))
            post()
            prev_trans = trans
        if prev_trans is not None:
            prev_trans()

    def bgain(src_ap, n=64):
        t = C.sb("bg", [128, n], F32)
        C.dma("sp", t[:], src_ap.partition_broadcast(128), w=[t])
        return t


    QA_T = SCR("QA_T", [8, 64, S], BF16); KA_T = SCR("KA_T", [64, S], BF16); VA = SCR("VA", [S, 64], BF16)
    IQ_T = SCR("IQ_T", [8, 32, S], BF16); IK_T = SCR("IK_T", [32, S], BF16); IW = SCR("IW", [S, 8], F32)
    QB_T = SCR("QB_T", [8, 64, S], BF16); KB_T = SCR("KB_T", [8, 64, S], BF16); VB = SCR("VB", [S, 512], BF16)
    OM = SCR("OM", [S, D], BF16)
    XA = SCR("XA", [S, D], F32); XB = SCR("XB", [S, D], F32); XC = SCR("XC", [S, D], F32)

    def store(dst_ap, src_ap, srcT, **kw):
        C.dma("pool", dst_ap, src_ap, r=[srcT], **kw)

    def phase_A0():
        with C.phase():
            g = C.sb("g", [128, 8], F32); load_gain(g, inp["norm_mix"][0])
            W = C.sb("W0", [128, 8, 2472], BF16)
            load_w(W, inp["ab_w_in"], 8, 2472, g)
            gq = bgain(inp["dsa_q_norm"]); gk = bgain(inp["dsa_k_norm"]); gbq = bgain(inp["moba_q_norm"]); gbk = bgain(inp["moba_k_norm"])
            hnm = HeadNorm(8, 64, "pool"); hn32 = HeadNorm(9, 32, "pool")
            pr = [C.sb("pr", [128, 512], F32) for _ in range(2)]
            qn = [C.sb("qn", [128, 512], BF16) for _ in range(3)]
            ptq = [C.ps("ptq", [64, 8, 128], BF16) for _ in range(2)]
            qT = [C.sb("qT", [64, 8, 128], BF16) for _ in range(3)]
            chunks = [(0, 512), (512, 936), (936, 1448), (1448, 1960), (1960, 2472)]
            cnt = {"ti": 0}

            def tr_heads(Q, H, Dh, src_off, dst_ap):
                i = cnt["ti"]; cnt["ti"] += 1
                PT = ptq[i % 2]; QT = qT[i % 3]
                for h in range(H):
                    C.op("pe", lambda e: e.transpose(out=PT[0:Dh, h, :], in_=Q[:, src_off + h * Dh: src_off + (h + 1) * Dh], identity=ident[:]), r=[Q, ident], w=[PT])
                C.op("dve", lambda e: e.tensor_copy(out=QT[0:Dh, 0:H, :], in_=PT[0:Dh, 0:H, :]), r=[PT], w=[QT])
                store(dst_ap, QT[0:Dh, 0:H, :], QT)

            def make_job(k, t, c, P):
                R = pr[k % 2]; Q = qn[k % 3]
                ts = slice(t * 128, (t + 1) * 128)
                cs8 = cs64[:, t, :]; sn8 = sn64[:, t, :]; cs4 = cs32[:, t, :]; sn4 = sn32[:, t, :]
                wd = chunks[c][1] - chunks[c][0]

                def post():
                    C.op("act", lambda e: e.activation(out=R[:, 0:wd], in_=P[:, 0:wd], func=AF.Copy), r=[P], w=[R])
                    if c in (0, 2, 3):
                        gg = (gq, None, gbq, gbk)[c]
                        hnm.run(R[:].rearrange("p (h d) -> p h d", h=8), R, 8, gg, Q[:].rearrange("p (h d) -> p h d", h=8), Q, rope=(cs8, sn8, 8))
                    elif c == 1:
                        hnm.run(R[:, 0:64].rearrange("p (h d) -> p h d", h=1), R, 1, gk, Q[:, 0:64].rearrange("p (h d) -> p h d", h=1), Q, rope=(cs8, sn8, 8))
                        C.op("act", lambda e: e.activation(out=Q[:, 64:128], in_=R[:, 64:128], func=AF.Copy), r=[R], w=[Q])
                        hn32.run(R[:, 128:416].rearrange("p (h d) -> p h d", h=9), R, 9, None, Q[:, 128:416].rearrange("p (h d) -> p h d", h=9), Q, rope=(cs4, sn4, 4), norm=False)
                        store(IW[ts, :], R[:, 416:424], R)
                    else:
                        C.op("act", lambda e: e.activation(out=Q[:], in_=R[:], func=AF.Copy), r=[R], w=[Q])

                def trans():
                    if c in (0, 2, 3):
                        dst = (QA_T, None, QB_T, KB_T)[c]
                        tr_heads(Q, 8, 64, 0, dst[:, :, ts].rearrange("h d s -> d h s"))
                    elif c == 1:
                        store(VA[ts, :], Q[:, 64:128], Q)
                        tr_heads(Q, 1, 64, 0, KA_T[:, ts].rearrange("(h d) s -> d h s", h=1))
                        tr_heads(Q, 8, 32, 128, IQ_T[:, :, ts].rearrange("h d s -> d h s"))
                        tr_heads(Q, 1, 32, 384, IK_T[:, ts].rearrange("(h d) s -> d h s", h=1))
                    else:
                        store(VB[ts, :], Q[:], Q)
                return post, trans

            run_proj(x_in, W, chunks, make_job)

    NIT = 16

    def run_merged(gA, nA, gB, nB):
        a = b = 0
        doneA = gA is None
        doneB = gB is None
        while not (doneA and doneB):
            pickA = (not doneA) and (doneB or a * nB <= b * nA)
            if pickA:
                try:
                    next(gA); a += 1
                except StopIteration:
                    doneA = True
            else:
                try:
                    next(gB); b += 1
                except StopIteration:
                    doneB = True

    def phase_A1():
        with C.phase():
            KT_ = C.sb("KAT", [128, S], BF16)
            C.op("pool", lambda e: e.memset(KT_[64:128, :], 0.0), w=[KT_])
            C.dma("sp", KT_[0:64, :], KA_T, w=[KT_])
            IKT = C.sb("IKT", [128, S], BF16)
            C.op("pool", lambda e: e.memset(IKT[:], 0.0), w=[IKT])
            C.dma("sp", IKT[0:32, :], IK_T, w=[IKT])
            Va = C.sb("VAa", [128, NT, 65], BF16)
            C.op("pool", lambda e: e.memset(Va[:], 1.0), w=[Va])
            C.dma("sp", Va[:, :, 0:64], VA.rearrange("(t p) c -> p t c", p=128), w=[Va])
            IWs = C.sb("IWs", [128, NT, 8], F32); C.dma("sp", IWs[:], IW.rearrange("(t p) c -> p t c", p=128), w=[IWs])
            score = [C.sb("score", [128, S], F32) for _ in range(2)]
            junk = C.sb("junkb", [128, S], BF16)
            nmask = [C.sb("nmask", [128, S], BF16) for _ in range(2)]
            rl = [C.sb("rl", [128, 512], F32) for _ in range(2)]
            qa = [C.sb("qa", [128, 8, 128], BF16) for _ in range(2)]
            iqt = [C.sb("iqt", [128, 8, 128], BF16) for _ in range(2)]
            for t_ in qa + iqt:
                C.op("pool", lambda e: e.memset(t_[:], 0.0), w=[t_])
            psi = [C.ps("psi", [128, 512], F32) for _ in range(2)]
            pss = [C.ps("pss", [128, 512], F32) for _ in range(2)]
            pacc = [C.ps("pacc", [128, 512], F32) for _ in range(4)]
            PTs = [C.sb("PTs", [128, 512], BF16) for _ in range(3)]
            sm = {n: C.sb(n, [128, 1], F32) for n in ("lo", "hi", "mid", "cnt", "ge", "rng", "c255", "sgn")}
            cthr = [C.sb("cthr", [128, 1], F32) for _ in range(2)]
            junkA = C.sb("junkA", [128, S], BF16)
            rzs = [C.sb("rz", [128, 1], F32) for _ in range(2)]
            steps = C.sb("steps", [128, NIT], F32); pw = C.sb("pw", [128, NIT], F32); steps2 = C.sb("steps2", [128, NIT], F32)
            for kk in range(NIT):
                C.op("pool", lambda e: e.memset(pw[:, kk:kk + 1], float(2.0 ** -(kk + 1))), w=[pw])
            C.op("pool", lambda e: e.memset(sm["c255"][:], 255.5), w=[sm["c255"]])
            oq = [C.sb("oq", [128, 512], BF16) for _ in range(2)]
            st_ = {"ii": 0, "si": 0, "pi": 0, "ri": 0}

            def gen_idx(qt):
                nk = (qt + 1) * 128
                IQ = iqt[qt % 2]; SC = score[qt % 2]; NM = nmask[qt % 2]
                ts = slice(qt * 128, (qt + 1) * 128)
                C.dma("sp", IQ[0:32, :, :], IQ_T[:, :, ts].rearrange("h d s -> d h s"), w=[IQ])
                jobs_ = [(c0, min(512, nk - c0), h) for c0 in range(0, nk, 512) for h in range(8)]

                def imm(j):
                    c0, wd, h = jobs_[j]
                    P = psi[st_["ii"] % 2]; st_["ii"] += 1
                    C.op("pe", lambda e: e.matmul(P[:, 0:wd], lhsT=IQ[:, h, :], rhs=IKT[:, c0:c0 + wd], start=True, stop=True), r=[IQ, IKT], w=[P])
                    return P

                Pcur = imm(0)
                for j, (c0, wd, h) in enumerate(jobs_):
                    Pnext = imm(j + 1) if j + 1 < len(jobs_) else None
                    P = Pcur
                    if h == 0:
                        C.op("dve", lambda e: e.tensor_scalar(out=SC[:, c0:c0 + wd], in0=P[:, 0:wd], scalar1=0.0, scalar2=IWs[:, qt, 0:1], op0=ALU.max, op1=ALU.mult), r=[P, IWs], w=[SC])
                    else:
                        R_ = rl[j % 2]
                        C.op("act", lambda e: e.activation(out=R_[:, 0:wd], in_=P[:, 0:wd], func=AF.Relu), r=[P], w=[R_])
                        C.op("dve", lambda e: e.scalar_tensor_tensor(out=SC[:, c0:c0 + wd], in0=R_[:, 0:wd], scalar=IWs[:, qt, h:h + 1], in1=SC[:, c0:c0 + wd], op0=ALU.mult, op1=ALU.add), r=[R_, IWs, SC], w=[SC])
                    Pcur = Pnext
                    if h == 7:
                        yield
                sc = SC[:, 0:nk]
                C.op("dve", lambda e: e.tensor_reduce(out=sm["hi"][:], in_=sc, axis=AX.X, op=ALU.max), r=[SC], w=[sm["hi"]])
                C.op("dve", lambda e: e.tensor_reduce(out=sm["lo"][:], in_=sc, axis=AX.X, op=ALU.min), r=[SC], w=[sm["lo"]])
                C.op("pool", lambda e: e.affine_select(out=SC[:, nk - 128:nk], in_=SC[:, nk - 128:nk], pattern=[[-1, 128]], compare_op=ALU.is_ge, fill=-1e30, base=0, channel_multiplier=1), r=[SC], w=[SC])
                C.op("dve", lambda e: e.tensor_tensor(out=sm["rng"][:], in0=sm["hi"][:], in1=sm["lo"][:], op=ALU.subtract), r=[sm["lo"], sm["hi"]], w=[sm["rng"]])
                C.op("dve", lambda e: e.tensor_scalar(out=steps[:], in0=pw[:], scalar1=sm["rng"][:], scalar2=None, op0=ALU.mult), r=[pw, sm["rng"]], w=[steps])
                yield
                cD = max(8, int(0.52 * nk) // 8 * 8)
                nA = nk - cD
                CT = cthr[qt % 2]
                C.op("pool", lambda e: e.memset(CT[:], float(255.5 - 0.5 * nA)), w=[CT])
                C.op("dve", lambda e: e.tensor_scalar(out=steps2[:], in0=steps[:], scalar1=2.0, scalar2=None, op0=ALU.mult), r=[steps], w=[steps2])
                C.op("dve", lambda e: e.tensor_tensor(out=sm["mid"][:], in0=sm["lo"][:], in1=steps[:, 0:1], op=ALU.add), r=[sm["lo"], steps], w=[sm["mid"]])
                for it in range(NIT):
                    last = it == NIT - 1
                    mul_ap = steps[:, it:it + 1] if last else steps2[:, it + 1:it + 2]
                    sub_ap = steps[:, it:it + 1] if last else steps[:, it + 1:it + 2]
                    C.op("act", lambda e: e.activation(out=junkA[:, 0:nA], in_=SC[:, cD:nk], func=AF.Sign, scale=-1.0, bias=sm["mid"][:], accum_out=sm["sgn"][:]), r=[SC, sm["mid"]], w=[junkA, sm["sgn"]])
                    C.op("dve", lambda e: e.tensor_scalar(out=junk[:, 0:cD], in0=SC[:, 0:cD], scalar1=sm["mid"][:], scalar2=None, op0=ALU.is_ge, op1=ALU.add, accum_out=sm["cnt"][:]), r=[SC, sm["mid"]], w=[junk, sm["cnt"]])
                    C.op("dve", lambda e: e.scalar_tensor_tensor(out=sm["cnt"][:], in0=sm["sgn"][:], scalar=-0.5, in1=sm["cnt"][:], op0=ALU.mult, op1=ALU.add), r=[sm["sgn"], sm["cnt"]], w=[sm["cnt"]])
                    C.op("dve", lambda e: e.tensor_scalar(out=sm["ge"][:], in0=sm["cnt"][:], scalar1=CT[:], scalar2=mul_ap, op0=ALU.is_ge, op1=ALU.mult), r=[sm["cnt"], CT, steps, steps2], w=[sm["ge"]])
                    C.op("dve", lambda e: e.scalar_tensor_tensor(out=sm["mid"][:], in0=sm["ge"][:], scalar=sub_ap, in1=sm["mid"][:], op0=ALU.subtract, op1=ALU.add), r=[sm["ge"], steps, sm["mid"]], w=[sm["mid"]])
                    if it % 2 == 1:
                        yield
                C.op("dve", lambda e: e.scalar_tensor_tensor(out=sm["mid"][:], in0=steps[:, NIT - 1:NIT], scalar=-0.5, in1=sm["mid"][:], op0=ALU.mult, op1=ALU.add), r=[steps, sm["mid"]], w=[sm["mid"]])
                C.op("dve", lambda e: e.tensor_scalar(out=NM[:, 0:nk], in0=sc, scalar1=sm["mid"][:], scalar2=NEG, op0=ALU.is_lt, op1=ALU.mult), r=[SC, sm["mid"]], w=[NM])
                yield

            def n_idx(qt):
                return ((qt + 1) * 128 + 511) // 512 + NIT // 2 + 2

            def gen_att(qt):
                QA = qa[qt % 2]; NM = nmask[qt % 2]; OQ = oq[qt % 2]
                ts = slice(qt * 128, (qt + 1) * 128)
                C.dma("sp", QA[0:64, :, :], QA_T[:, :, ts].rearrange("h d s -> d h s"), w=[QA])

                def qk(hg, st):
                    P = pss[st_["si"] % 2]; st_["si"] += 1
                    ss_ = slice(st * 128, (st + 1) * 128)
                    C.op("pe", lambda e: e.matmul(P[:], lhsT=KT_[:, ss_], rhs=QA[:, hg * 4:(hg + 1) * 4, :], start=True, stop=False), r=[KT_, QA], w=[P])
                    C.op("pe", lambda e: e.matmul(P[:], lhsT=NM[:, ss_], rhs=I4[:], start=False, stop=True), r=[NM, I4], w=[P])
                    return P

                for hg in range(2):
                    Pc = qk(hg, 0)
                    for st in range(qt + 1):
                        Pn = qk(hg, st + 1) if st < qt else None
                        PT = PTs[st_["pi"] % 3]; st_["pi"] += 1
                        C.op("act", lambda e: e.activation(out=PT[:], in_=Pc[:], func=AF.Exp, scale=0.125), r=[Pc], w=[PT])
                        for h in range(4):
                            C.op("pe", lambda e: e.matmul(pacc[h][:, 0:65], lhsT=PT[:, h * 128:(h + 1) * 128], rhs=Va[:, st, :], start=(st == 0), stop=(st == qt)), r=[PT, Va], w=[pacc[h]])
                        Pc = Pn
                        if st % 4 == 3:
                            yield
                    for h in range(4):
                        hd = hg * 4 + h
                        rz = rzs[st_["ri"] % 2]; st_["ri"] += 1
                        C.op("dve", lambda e: e.reciprocal(out=rz[:], in_=pacc[h][:, 64:65]), r=[pacc[h]], w=[rz])
                        C.op("act", lambda e: e.activation(out=OQ[:, hd * 64:(hd + 1) * 64], in_=pacc[h][:, 0:64], func=AF.Copy, scale=rz[:]), r=[pacc[h], rz], w=[OQ])
                    yield
                store(OM[ts, 0:512], OQ[:], OQ)
                yield

            def n_att(qt):
                return 2 * ((qt + 1) // 4 + 1) + 1

            run_merged(gen_idx(0), 1, None, 1)
            for qt in range(NT):
                gB = gen_idx(qt + 1) if qt + 1 < NT else None
                run_merged(gen_att(qt), n_att(qt), gB, n_idx(qt + 1) if gB else 1)

    def phase_A2():
        with C.phase():
            KT_ = C.sb("KBT", [128, S], BF16); QT_ = C.sb("QBT", [128, S], BF16)
            C.op("pool", lambda e: e.memset(KT_[64:128, :], 0.0), w=[KT_])
            C.op("pool", lambda e: e.memset(QT_[64:128, :], 0.0), w=[QT_])
            with C.phase():
                Ef = C.sb("Ef", [80, S], F32)
                C.op("pool", lambda e: e.memset(Ef[64:80, :], 1.0), w=[Ef])
                C.op("pool", lambda e: e.affine_select(out=Ef[64:80, :], in_=Ef[64:80, :], pattern=[[1, S]], compare_op=ALU.is_ge, fill=0.0, base=0, channel_multiplier=-256), r=[Ef], w=[Ef])
                C.op("pool", lambda e: e.affine_select(out=Ef[64:80, :], in_=Ef[64:80, :], pattern=[[-1, S]], compare_op=ALU.is_ge, fill=0.0, base=255, channel_multiplier=256), r=[Ef], w=[Ef])
                C.op("dve", lambda e: e.tensor_copy(out=KT_[64:80, :], in_=Ef[64:80, :]), r=[Ef], w=[KT_])
            cm = C.sb("cm", [128, NT, 16], F32); om = C.sb("om", [128, NT, 16], F32)
            C.op("pool", lambda e: e.memset(cm[:], 0.0), w=[cm])
            C.op("pool", lambda e: e.memset(om[:], 0.0), w=[om])
            for qt in range(NT):
                own = qt // 2
                C.op("pool", lambda e: e.memset(cm[:, qt, own:16], -1e30), w=[cm])
                C.op("pool", lambda e: e.memset(om[:, qt, own:own + 1], 1.0), w=[om])
            Va = C.sb("VBa", [128, NT, 65], BF16)
            C.op("pool", lambda e: e.memset(Va[:], 1.0), w=[Va])
            kmf = C.sb("kmf", [64, 16], F32); kmb = C.sb("kmb", [64, 16], BF16)
            pg = C.ps("pg", [128, NT, 16], F32)
            gm = C.sb("gm", [128, NT, 16], F32); m8 = C.sb("m8", [128, NT, 8], F32)
            sel = C.sb("sel", [128, NT, 16], F32)
            nbx = C.sb("nbx", [128, NT, 80], BF16)
            C.op("pool", lambda e: e.memset(nbx[:], 0.0), w=[nbx])
            pnt = C.ps("pnt", [80, 8, 128], BF16)
            pss = [C.ps("pss", [128, 512], F32) for _ in range(2)]
            pacc = [C.ps("pacc", [128, 512], F32) for _ in range(4)]
            PTs = [C.sb("PTs", [128, 512], BF16) for _ in range(3)]
            rzs = [C.sb("rz", [128, 1], F32) for _ in range(2)]
            ob = [C.sb("ob", [128, 64], BF16) for _ in range(3)]
            si = 0; pi = 0; oi = 0; ri = 0
            for h in range(8):
                C.dma("sp", KT_[0:64, :], KB_T[h], w=[KT_])
                C.dma("sp", QT_[0:64, :], QB_T[h], w=[QT_])
                C.dma("sp", Va[:, :, 0:64], VB[:, h * 64:(h + 1) * 64].rearrange("(t p) c -> p t c", p=128), w=[Va])
                C.op("dve", lambda e: e.tensor_reduce(out=kmf[:], in_=KT_[0:64, :].rearrange("d (n s) -> d n s", n=16), axis=AX.X, op=ALU.add), r=[KT_], w=[kmf])
                C.op("dve", lambda e: e.tensor_scalar(out=kmb[:], in0=kmf[:], scalar1=1.0 / 256, scalar2=None, op0=ALU.mult), r=[kmf], w=[kmb])
                for qt in range(NT):
                    C.op("pe", lambda e: e.matmul(pg[:, qt, :], lhsT=QT_[0:64, qt * 128:(qt + 1) * 128], rhs=kmb[:], start=True, stop=True), r=[QT_, kmb], w=[pg])
                C.op("dve", lambda e: e.tensor_tensor(out=gm[:], in0=pg[:], in1=cm[:], op=ALU.add), r=[pg, cm], w=[gm])
                for qt in range(NT):
                    C.op("dve", lambda e: e.max(out=m8[:, qt, :], in_=gm[:, qt, :]), r=[gm], w=[m8])
                C.op("dve", lambda e: e.tensor_tensor(out=sel[:], in0=gm[:], in1=ins_bc(m8[:, :, 2], 2, 16), op=ALU.is_ge), r=[gm, m8], w=[sel])
                C.op("dve", lambda e: e.tensor_tensor(out=sel[:], in0=sel[:], in1=om[:], op=ALU.max), r=[sel, om], w=[sel])
                C.op("dve", lambda e: e.tensor_scalar(out=nbx[:, :, 64:80], in0=sel[:], scalar1=1.0, scalar2=-NEG, op0=ALU.subtract, op1=ALU.mult), r=[sel], w=[nbx])
                for r4 in range(4):
                    for j in range(8):
                        qt = r4 * 8 + j
                        C.op("pe", lambda e: e.transpose(out=pnt[:, j, :], in_=nbx[:, qt, :], identity=ident[:]), r=[nbx, ident], w=[pnt])
                    C.op("act", lambda e: e.activation(out=QT_[64:80, r4 * 1024:(r4 + 1) * 1024], in_=pnt[64:80, :, :].rearrange("n j q -> n (j q)"), func=AF.Copy), r=[pnt], w=[QT_])
                steps_ = [(G, st) for G in range(8) for st in range(4 * G + 4)]

                def qk(G, st):
                    nonlocal si
                    P = pss[si % 2]; si += 1
                    ss_ = slice(st * 128, (st + 1) * 128); qs = slice(G * 512, (G + 1) * 512)
                    diag = st >= 4 * G
                    C.op("pe", lambda e: e.matmul(P[:], lhsT=KT_[:, ss_], rhs=QT_[:, qs], start=True, stop=not diag), r=[KT_, QT_], w=[P])
                    if diag:
                        C.op("pe", lambda e: e.matmul(P[:], lhsT=ident[:], rhs=CAUS[:, st - 4 * G, :, :], start=False, stop=True), r=[ident, CAUS], w=[P])
                    return P

                Pc = qk(*steps_[0])
                for k_, (G, st) in enumerate(steps_):
                    Pn = qk(*steps_[k_ + 1]) if k_ + 1 < len(steps_) else None
                    PT = PTs[pi % 3]; pi += 1
                    C.op("act", lambda e: e.activation(out=PT[:], in_=Pc[:], func=AF.Exp, scale=0.125), r=[Pc], w=[PT])
                    for qi in range(4):
                        qt = 4 * G + qi
                        if qt < st:
                            continue
                        C.op("pe", lambda e: e.matmul(pacc[qi][:, 0:65], lhsT=PT[:, qi * 128:(qi + 1) * 128], rhs=Va[:, st, :], start=(st == 0), stop=(st == qt)), r=[PT, Va], w=[pacc[qi]])
                    Pc = Pn
                    if st == 4 * G + 3:
                        for qi in range(4):
                            qt = 4 * G + qi
                            O_ = ob[oi % 3]; oi += 1
                            rz = rzs[ri % 2]; ri += 1
                            C.op("dve", lambda e: e.reciprocal(out=rz[:], in_=pacc[qi][:, 64:65]), r=[pacc[qi]], w=[rz])
                            C.op("act", lambda e: e.activation(out=O_[:], in_=pacc[qi][:, 0:64], func=AF.Copy, scale=rz[:]), r=[pacc[qi], rz], w=[O_])
                            store(OM[qt * 128:(qt + 1) * 128, 512 + h * 64:512 + (h + 1) * 64], O_[:], O_)

    def phase_A3(L, w_out_ap, x_src, x_dst):
        with C.phase():
            Wo = C.sb("Wo", [128, 8, D], BF16); load_w(Wo, w_out_ap, 8, D)
            g = C.sb("g", [128, 8], F32); load_gain(g, inp["norm_mem"][L])
            Wq = C.sb("Wq", [128, 8, 512], BF16); load_w(Wq, inp["mem_w_q"][L], 8, 512, g)
            Wom = C.sb("Wom", [128, 4, D], BF16); load_w(Wom, inp["mem_w_o"][L], 4, D)
            gqn = bgain(inp["mem_q_norm"][L], 128); gkn = bgain(inp["mem_k_norm"][L], 128)
            mK = C.sb("mK", [128, 4, 256], BF16); mV = C.sb("mV", [128, 2, 4, 129], BF16)
            C.op("pool", lambda e: e.memset(mV[:], 1.0), w=[mV])
            nt = NormT(); hnm = HeadNorm(4, 128)
            hT = C.sb("hT", [128, 8, 128], BF16)
            po = [C.ps("po", [128, 512], F32) for _ in range(2)]
            pr = C.sb("pr", [128, 512], F32); qn = C.sb("qn", [128, 512], BF16)
            ptq8 = C.ps("ptq", [128, 8, 128], BF16)
            ptq = T(ptq8.t, "x"); ptq.b = ptq8.b
            ptq_ap = ptq8[:, 0:4, :]
            with C.phase():
                gs = C.sb("gs", [128, 8], F32); load_gain(gs, inp["norm_mem_src"][L])
                Wkv = C.sb("Wkv", [128, 8, D], BF16); load_w(Wkv, inp["mem_w_kv"][L], 8, D, gs)
                mt_ = C.sb("mt", [128, D], F32)
                for mt in range(2):
                    C.dma("sp", mt_[:], mem_in[mt * 128:(mt + 1) * 128, :], w=[mt_])
                    nt.run(mt_, hT[:], hT)
                    for c in range(2):
                        P = po[c]
                        for kt in range(8):
                            C.op("pe", lambda e: e.matmul(P[:], lhsT=hT[:, kt, :], rhs=Wkv[:, kt, c * 512:(c + 1) * 512], start=(kt == 0), stop=(kt == 7)), r=[hT, Wkv], w=[P])
                        if c == 0:
                            C.op("act", lambda e: e.activation(out=pr[:], in_=P[:], func=AF.Copy), r=[P], w=[pr])
                            hnm.run(pr[:].rearrange("p (h d) -> p h d", h=4), pr, 4, gkn, qn[:].rearrange("p (h d) -> p h d", h=4), qn)
                            for h in range(4):
                                C.op("pe", lambda e: e.transpose(out=ptq[:, h, :], in_=qn[:, h * 128:(h + 1) * 128], identity=ident[:]), r=[qn, ident], w=[ptq])
                            C.op("dve", lambda e: e.tensor_copy(out=mK[:, :, mt * 128:(mt + 1) * 128], in_=ptq_ap), r=[ptq], w=[mK])
                        else:
                            C.op("act", lambda e: e.activation(out=mV[:, mt, :, 0:128], in_=P[:].rearrange("p (h d) -> p h d", h=4), func=AF.Copy), r=[P], w=[mV])
            pT = nt.pT
            lanes = []
            for ln in range(2):
                Lb = K()
                Lb.nt = nt if ln == 0 else NormT(pT=pT)
                Lb.hnm = hnm if ln == 0 else HeadNorm(4, 128)
                Lb.ot = C.sb("ot", [128, D], BF16); Lb.xt = C.sb("xt", [128, D], F32)
                Lb.oT = C.sb("oT", [128, 8, 128], BF16); Lb.x1 = C.sb("x1", [128, D], F32); Lb.x2 = C.sb("x2", [128, D], F32)
                Lb.hT = hT if ln == 0 else C.sb("hT", [128, 8, 128], BF16)
                Lb.pr = pr if ln == 0 else C.sb("pr", [128, 512], F32)
                Lb.qn = qn if ln == 0 else C.sb("qn", [128, 512], BF16)
                Lb.qT = C.sb("qT", [128, 4, 128], BF16)
                Lb.po = po[ln]
                Lb.pss = C.ps("pss", [128, 512], F32)
                Lb.PTm = [C.sb("PTm", [128, 512], BF16) for _ in range(2)]
                Lb.pov = C.ps("pov", [128, 2, 256], F32)
                Lb.rz = [C.sb("rz", [128, 1], F32) for _ in range(2)]
                Lb.omx = C.sb("omx", [128, 512], BF16); Lb.omT = C.sb("omT", [128, 4, 128], BF16)
                lanes.append(Lb)

            def gen_tile(Lb, t):
                ts = slice(t * 128, (t + 1) * 128)
                O_ = Lb.ot; X = Lb.xt; X2 = Lb.x2; x1 = Lb.x1; oT = Lb.oT; P = Lb.po
                C.dma("sp", O_[:], OM[ts, :], w=[O_])
                C.dma("sp", X[:], x_src[ts, :], w=[X])
                for kt in range(8):
                    C.op("pe", lambda e: e.transpose(out=pT[:, kt, :], in_=O_[:, kt * 128:(kt + 1) * 128], identity=ident[:]), r=[O_, ident], w=[pT])
                C.op("dve", lambda e: e.tensor_copy(out=oT[:], in_=pT[:]), r=[pT], w=[oT])
                yield
                for c in range(2):
                    for kt in range(8):
                        C.op("pe", lambda e: e.matmul(P[:], lhsT=oT[:, kt, :], rhs=Wo[:, kt, c * 512:(c + 1) * 512], start=(kt == 0), stop=(kt == 7)), r=[oT, Wo], w=[P])
                    C.op("dve", lambda e: e.tensor_tensor(out=x1[:, c * 512:(c + 1) * 512], in0=P[:], in1=X[:, c * 512:(c + 1) * 512], op=ALU.add), r=[P, X], w=[x1])
                    yield
                Lb.nt.run(x1, Lb.hT[:], Lb.hT)
                yield
                for kt in range(8):
                    C.op("pe", lambda e: e.matmul(P[:], lhsT=Lb.hT[:, kt, :], rhs=Wq[:, kt, :], start=(kt == 0), stop=(kt == 7)), r=[Lb.hT, Wq], w=[P])
                C.op("act", lambda e: e.activation(out=Lb.pr[:], in_=P[:], func=AF.Copy), r=[P], w=[Lb.pr])
                yield
                Lb.hnm.run(Lb.pr[:].rearrange("p (h d) -> p h d", h=4), Lb.pr, 4, gqn, Lb.qn[:].rearrange("p (h d) -> p h d", h=4), Lb.qn)
                yield
                for h in range(4):
                    C.op("pe", lambda e: e.transpose(out=ptq[:, h, :], in_=Lb.qn[:, h * 128:(h + 1) * 128], identity=ident[:]), r=[Lb.qn, ident], w=[ptq])
                C.op("dve", lambda e: e.tensor_copy(out=Lb.qT[:], in_=ptq_ap), r=[ptq], w=[Lb.qT])
                yield
                for mt in range(2):
                    PS = Lb.pss
                    for h in range(4):
                        C.op("pe", lambda e: e.matmul(PS[:, h * 128:(h + 1) * 128], lhsT=mK[:, h, mt * 128:(mt + 1) * 128], rhs=Lb.qT[:, h, :], start=True, stop=True), r=[mK, Lb.qT], w=[PS])
                    C.op("act", lambda e: e.activation(out=Lb.PTm[mt][:], in_=PS[:], func=AF.Exp, scale=float(128 ** -0.5)), r=[PS], w=[Lb.PTm[mt]])
                    yield
                for h in range(4):
                    A = Lb.pov
                    for mt in range(2):
                        C.op("pe", lambda e: e.matmul(A[:, h % 2, 0:129], lhsT=Lb.PTm[mt][:, h * 128:(h + 1) * 128], rhs=mV[:, mt, h, :], start=(mt == 0), stop=(mt == 1)), r=[Lb.PTm[mt], mV], w=[A])
                    rz = Lb.rz[h % 2]
                    C.op("dve", lambda e: e.reciprocal(out=rz[:], in_=A[:, h % 2, 128:129]), r=[A], w=[rz])
                    C.op("act", lambda e: e.activation(out=Lb.omx[:, h * 128:(h + 1) * 128], in_=A[:, h % 2, 0:128], func=AF.Copy, scale=rz[:]), r=[A, rz], w=[Lb.omx])
                    yield
                for h in range(4):
                    C.op("pe", lambda e: e.transpose(out=ptq[:, h, :], in_=Lb.omx[:, h * 128:(h + 1) * 128], identity=ident[:]), r=[Lb.omx, ident], w=[ptq])
                C.op("dve", lambda e: e.tensor_copy(out=Lb.omT[:], in_=ptq_ap), r=[ptq], w=[Lb.omT])
                yield
                for c in range(2):
                    for kt in range(4):
                        C.op("pe", lambda e: e.matmul(P[:], lhsT=Lb.omT[:, kt, :], rhs=Wom[:, kt, c * 512:(c + 1) * 512], start=(kt == 0), stop=(kt == 3)), r=[Lb.omT, Wom], w=[P])
                    C.op("dve", lambda e: e.tensor_tensor(out=X2[:, c * 512:(c + 1) * 512], in0=P[:], in1=x1[:, c * 512:(c + 1) * 512], op=ALU.add), r=[P, x1], w=[X2])
                    yield
                store(x_dst[ts, :], X2[:], X2)
                yield

            def lane_gen(ln):
                if ln == 1:
                    for _ in range(9):
                        yield
                for t in range(ln, NT, 2):
                    yield from gen_tile(lanes[ln], t)

            run_merged(lane_gen(0), 1, lane_gen(1), 1)

    def phase_A4(L, x_src, x_dst):
        with C.phase():
            g = C.sb("g", [128, 8], F32); load_gain(g, inp["norm_ffn"][L])
            Wi = C.sb("Wi", [128, 8, 2 * DFF], BF16); load_w(Wi, inp["ffn_w_in"][L], 8, 2 * DFF, g)
            Wd = C.sb("Wd", [128, 22, D], BF16); load_w(Wd, inp["ffn_w_out"][L], 22, D)
            nt = NormT()
            xt = [C.sb("xt", [128, D], F32) for _ in range(4)]
            hT = C.sb("hT", [128, 8, 512], BF16)
            hid = C.sb("hid", [128, 22, 512], BF16)
            psg = [C.ps("psg", [128, 512], F32) for _ in range(2)]
            psu = [C.ps("psu", [128, 512], F32) for _ in range(2)]
            po = [C.ps("po", [128, 512], F32) for _ in range(2)]
            sg = [C.sb("sg", [128, 512], BF16) for _ in range(2)]
            oi = 0
            for G in range(8):
                for tt in range(4):
                    t = G * 4 + tt
                    C.dma("sp", xt[tt][:], x_src[t * 128:(t + 1) * 128, :], w=[xt[tt]])
                    nt.run(xt[tt], hT[:, :, tt * 128:(tt + 1) * 128], hT)
                for ft in range(22):
                    Pg = psg[ft % 2]; Pu = psu[ft % 2]; Sg = sg[ft % 2]
                    for kt in range(8):
                        C.op("pe", lambda e: e.matmul(Pg[:], lhsT=Wi[:, kt, ft * 128:(ft + 1) * 128], rhs=hT[:, kt, :], start=(kt == 0), stop=(kt == 7)), r=[Wi, hT], w=[Pg])
                    for kt in range(8):
                        C.op("pe", lambda e: e.matmul(Pu[:], lhsT=Wi[:, kt, DFF + ft * 128:DFF + (ft + 1) * 128], rhs=hT[:, kt, :], start=(kt == 0), stop=(kt == 7)), r=[Wi, hT], w=[Pu])
                    C.op("act", lambda e: e.activation(out=Sg[:], in_=Pg[:], func=AF.Silu), r=[Pg], w=[Sg])
                    C.op("dve", lambda e: e.tensor_tensor(out=hid[:, ft, :], in0=Sg[:], in1=Pu[:], op=ALU.mult), r=[Sg, Pu], w=[hid])
                for tt in range(4):
                    t = G * 4 + tt
                    XO = xt[tt]
                    for c in range(2):
                        P = po[c]
                        for ft in range(22):
                            C.op("pe", lambda e: e.matmul(P[:], lhsT=hid[:, ft, tt * 128:(tt + 1) * 128], rhs=Wd[:, ft, c * 512:(c + 1) * 512], start=(ft == 0), stop=(ft == 21)), r=[hid, Wd], w=[P])
                        C.op("dve", lambda e: e.tensor_tensor(out=XO[:, c * 512:(c + 1) * 512], in0=P[:], in1=xt[tt][:, c * 512:(c + 1) * 512], op=ALU.add), r=[P, xt[tt]], w=[XO])
                    store(x_dst[t * 128:(t + 1) * 128, :], XO[:], XO)


    Q_T = SCR("Q_T", [16, 64, S], BF16); QR_T = SCR("QR_T", [16, 64, S], BF16)
    KCr_T = SCR("KCr_T", [4, 64, S], BF16); VCr_T = SCR("VCr_T", [4, 64, S], BF16)
    KS_T = SCR("KS_T", [4, 64, S], BF16); VS = SCR("VS", [S, 256], BF16)
    KW_T = SCR("KW_T", [4, 64, S], BF16); VW = SCR("VW", [S, 256], BF16)
    GT = SCR("GT", [S, 48], F32)
    KC_T = C.sb("KC_T", [128, 4, 256], BF16, stack=C.es)
    VCa = C.sb("VCa", [128, 2, 4, 129], BF16, stack=C.es)

    def phase_B0():
        with C.phase():
            g = C.sb("g", [128, 8], F32); load_gain(g, inp["norm_mix"][1])
            W = C.sb("W1", [128, 8, 2608], BF16)
            load_w(W, inp["nsa_w_in"], 8, 2608, g)
            gq = bgain(inp["nsa_q_norm"]); gks = bgain(inp["nsa_ksel_norm"]); gkw = bgain(inp["nsa_kwin_norm"])
            hnm = HeadNorm(8, 64, "pool")
            pr = [C.sb("pr", [128, 512], F32) for _ in range(2)]
            qn = [C.sb("qn", [128, 512], BF16) for _ in range(3)]
            qn2 = [C.sb("qn2", [128, 512], BF16) for _ in range(3)]
            ptq = [C.ps("ptq", [64, 8, 128], BF16) for _ in range(2)]
            qT = [C.sb("qT", [64, 8, 128], BF16) for _ in range(3)]
            chunks = [(0, 512), (512, 1024), (1024, 1536), (1536, 2048), (2048, 2560), (2560, 2608)]
            cnt = {"ti": 0}

            def tr_heads(SRC, H, src_off, dst_ap):
                i = cnt["ti"]; cnt["ti"] += 1
                PT = ptq[i % 2]; QT = qT[i % 3]
                for h in range(H):
                    C.op("pe", lambda e: e.transpose(out=PT[:, h, :], in_=SRC[:, src_off + h * 64: src_off + (h + 1) * 64], identity=ident[:]), r=[SRC, ident], w=[PT])
                C.op("dve", lambda e: e.tensor_copy(out=QT[:, 0:H, :], in_=PT[:, 0:H, :]), r=[PT], w=[QT])
                store(dst_ap, QT[:, 0:H, :], QT)

            def make_job(k, t, c, P):
                R = pr[k % 2]; Q = qn[k % 3]; Q2 = qn2[k % 3]
                ts = slice(t * 128, (t + 1) * 128)
                cs8 = cs64[:, t, :]; sn8 = sn64[:, t, :]
                v8 = lambda A_: A_[:].rearrange("p (h d) -> p h d", h=8)
                v4 = lambda A_: A_[:, 0:256].rearrange("p (h d) -> p h d", h=4)

                def post():
                    if c == 5:
                        C.op("act", lambda e: e.activation(out=R[:, 0:48], in_=P[:, 0:48], func=AF.Exp, scale=-1.0), r=[P], w=[R])
                        C.op("dve", lambda e: e.tensor_scalar(out=R[:, 0:48], in0=R[:, 0:48], scalar1=1.0, scalar2=None, op0=ALU.add), r=[R], w=[R])
                        C.op("dve", lambda e: e.reciprocal(out=R[:, 0:48], in_=R[:, 0:48]), r=[R], w=[R])
                        store(GT[ts, :], R[:, 0:48], R)
                        return
                    C.op("act", lambda e: e.activation(out=R[:], in_=P[:], func=AF.Copy), r=[P], w=[R])
                    if c in (0, 1):
                        hnm.run(v8(R), R, 8, gq, v8(Q), Q, rope=(cs8, sn8, 8), out2_ap=v8(Q2), out2T=Q2)
                    elif c == 2:
                        C.op("act", lambda e: e.activation(out=Q[:], in_=R[:], func=AF.Copy), r=[R], w=[Q])
                    else:
                        gg = gks if c == 3 else gkw
                        hnm.run(v4(R), R, 4, gg, v4(Q), Q, rope=(cs8, sn8, 8))
                        C.op("act", lambda e: e.activation(out=Q[:, 256:512], in_=R[:, 256:512], func=AF.Copy), r=[R], w=[Q])

                def trans():
                    if c == 5:
                        return
                    if c in (0, 1):
                        tr_heads(Q, 8, 0, QR_T[c * 8:(c + 1) * 8, :, ts].rearrange("h d s -> d h s"))
                        tr_heads(Q2, 8, 0, Q_T[c * 8:(c + 1) * 8, :, ts].rearrange("h d s -> d h s"))
                    elif c == 2:
                        tr_heads(Q, 4, 0, KCr_T[:, :, ts].rearrange("h d s -> d h s"))
                        tr_heads(Q, 4, 256, VCr_T[:, :, ts].rearrange("h d s -> d h s"))
                    else:
                        store((VS if c == 3 else VW)[ts, :], Q[:, 256:512], Q)
                        tr_heads(Q, 4, 0, (KS_T if c == 3 else KW_T)[:, :, ts].rearrange("h d s -> d h s"))
                return post, trans

            run_proj(XB, W, chunks, make_job)

    def phase_B1():
        with C.phase():
            C.op("pool", lambda e: e.memset(KC_T[:], 0.0), w=[KC_T])
            C.op("pool", lambda e: e.memset(VCa[:], 0.0), w=[VCa])
            C.op("pool", lambda e: e.memset(VCa[:, :, :, 64:65], 1.0), w=[VCa])
            cov = C.sb("cov", [128, 2, 64], F32)
            C.op("pool", lambda e: e.memset(cov[:], 1.0), w=[cov])
            for half in range(2):
                C.op("pool", lambda e: e.affine_select(out=cov[:, half, :], in_=cov[:, half, :], pattern=[[-64, 64]], compare_op=ALU.is_gt, fill=0.0, base=2048 * half + 32, channel_multiplier=16), r=[cov], w=[cov])
                C.op("pool", lambda e: e.affine_select(out=cov[:, half, :], in_=cov[:, half, :], pattern=[[64, 64]], compare_op=ALU.is_gt, fill=0.0, base=64 - 2048 * half, channel_multiplier=-16), r=[cov], w=[cov])
                C.op("dve", lambda e: e.tensor_copy(out=VCa[:, half, :, 65:129], in_=ins_bc(cov[:, half, :], 1, 4)), r=[cov], w=[VCa])
            gkc = bgain(inp["nsa_kcmp_norm"])
            hnm = HeadNorm(1, 64)
            XT = C.sb("XT", [64, 4, S], BF16)
            w1f = C.sb("w1f", [64, 32, 64], F32); w1b = C.sb("w1b", [64, 32, 64], BF16)
            w2f = C.sb("w2f", [64, 64], F32); w2b = C.sb("w2b", [64, 64], BF16)
            posf = C.sb("posf", [64, 32], F32); posrep = C.sb("posrep", [64, 32, 128], BF16)
            cpos = C.sb("cpos", [128, 64], F32)
            ph = [C.ps("ph", [128, 512], F32) for _ in range(2)]
            pt_ = C.ps("pt_", [64, 8, 128], BF16)
            hs = C.sb("hs", [128, 64], F32); hsb = C.sb("hsb", [128, 64], BF16); hsT = C.sb("hsT", [64, 128], BF16)
            o2 = C.sb("o2", [128, 64], F32); o2b = C.sb("o2b", [128, 64], BF16)
            pi = 0
            for kv in range(2):
                src = (KCr_T, VCr_T)[kv]
                C.dma("sp", XT[:], src.rearrange("g d s -> d g s"), w=[XT])
                C.dma("sp", w1f[:], inp[("nsa_cmp_w1_k", "nsa_cmp_w1_v")[kv]].rearrange("(l d) o -> d l o", d=64), w=[w1f])
                C.dma("sp", w2f[:], inp[("nsa_cmp_w2_k", "nsa_cmp_w2_v")[kv]], w=[w2f])
                C.dma("sp", posf[:], inp[("nsa_cmp_pos_k", "nsa_cmp_pos_v")[kv]].rearrange("l d -> d l"), w=[posf], allow_slow_non_contiguous=True)
                C.op("dve", lambda e: e.tensor_copy(out=w1b[:], in_=w1f[:]), r=[w1f], w=[w1b])
                C.op("dve", lambda e: e.tensor_copy(out=w2b[:], in_=w2f[:]), r=[w2f], w=[w2b])
                C.op("dve", lambda e: e.tensor_copy(out=posrep[:], in_=ins_bc(posf[:], 2, 128)), r=[posf], w=[posrep])
                P = ph[pi % 2]; pi += 1
                for l in range(32):
                    C.op("pe", lambda e: e.matmul(P[:, 0:64], lhsT=posrep[:, l, :], rhs=w1b[:, l, :], start=(l == 0), stop=(l == 31)), r=[posrep, w1b], w=[P])
                C.op("dve", lambda e: e.tensor_copy(out=cpos[:], in_=P[:, 0:64]), r=[P], w=[cpos])
                for g_ in range(4):
                    for half in range(2):
                        M = 128 if half == 0 else 127
                        P = ph[pi % 2]; pi += 1
                        for l in range(32):
                            a0 = 2048 * half + l
                            C.op("pe", lambda e: e.matmul(P[0:M, 0:64], lhsT=XT[:, g_, a0:a0 + 16 * (M - 1) + 1:16], rhs=w1b[:, l, :], start=(l == 0), stop=(l == 31)), r=[XT, w1b], w=[P])
                        C.op("dve", lambda e: e.tensor_tensor(out=hs[0:M, :], in0=P[0:M, 0:64], in1=cpos[0:M, :], op=ALU.add), r=[P, cpos], w=[hs])
                        C.op("act", lambda e: e.activation(out=hsb[0:M, :], in_=hs[0:M, :], func=AF.Silu), r=[hs], w=[hsb])
                        C.op("pe", lambda e: e.transpose(out=pt_[:, 0, 0:M], in_=hsb[0:M, :], identity=ident[0:M, 0:M]), r=[hsb, ident], w=[pt_])
                        C.op("dve", lambda e: e.tensor_copy(out=hsT[:, 0:M], in_=pt_[:, 0, 0:M]), r=[pt_], w=[hsT])
                        P2 = ph[pi % 2]; pi += 1
                        C.op("pe", lambda e: e.matmul(P2[0:M, 0:64], lhsT=hsT[:, 0:M], rhs=w2b[:], start=True, stop=True), r=[hsT, w2b], w=[P2])
                        if kv == 0:
                            C.op("act", lambda e: e.activation(out=o2[0:M, :], in_=P2[0:M, 0:64], func=AF.Copy), r=[P2], w=[o2])
                            hnm.run(o2[:].rearrange("p (h d) -> p h d", h=1), o2, 1, gkc, o2b[:].rearrange("p (h d) -> p h d", h=1), o2b)
                            C.op("pe", lambda e: e.transpose(out=pt_[:, 1, 0:M], in_=o2b[0:M, :], identity=ident[0:M, 0:M]), r=[o2b, ident], w=[pt_])
                            C.op("dve", lambda e: e.tensor_copy(out=KC_T[0:64, g_, half * 128:half * 128 + M], in_=pt_[:, 1, 0:M]), r=[pt_], w=[KC_T])
                        else:
                            C.op("act", lambda e: e.activation(out=VCa[0:M, half, g_, 0:64], in_=P2[0:M, 0:64], func=AF.Copy), r=[P2], w=[VCa])

    def phase_B2():
        with C.phase():
            KS = C.sb("KS", [128, 4, S], BF16); C.dma("sp", KS[0:64, :, :], KS_T.rearrange("g d s -> d g s"), w=[KS])
            KW = C.sb("KW", [128, 4, S], BF16)
            C.op("pool", lambda e: e.memset(KW[64:128, :, :], 0.0), w=[KW])
            C.dma("sp", KW[0:64, :, :], KW_T.rearrange("g d s -> d g s"), w=[KW])
            VSa = C.sb("VSa", [128, NT, 4, 65], BF16); VWa = C.sb("VWa", [128, NT, 4, 65], BF16)
            C.op("pool", lambda e: e.memset(VSa[:], 1.0), w=[VSa])
            C.op("pool", lambda e: e.memset(VWa[:], 1.0), w=[VWa])
            for t in range(NT):
                C.dma("sp", VSa[:, t, :, 0:64], VS[t * 128:(t + 1) * 128, :].rearrange("p (g d) -> p g d", g=4), w=[VSa])
                C.dma("pool", VWa[:, t, :, 0:64], VW[t * 128:(t + 1) * 128, :].rearrange("p (g d) -> p g d", g=4), w=[VWa])
            with C.phase():
                Ef = C.sb("E2f", [128, S], F32)
                C.op("pool", lambda e: e.memset(Ef[64:128, :], 1.0), w=[Ef])
                C.op("pool", lambda e: e.affine_select(out=Ef[64:128, :], in_=Ef[64:128, :], pattern=[[1, S]], compare_op=ALU.is_ge, fill=0.0, base=0, channel_multiplier=-64), r=[Ef], w=[Ef])
                C.op("pool", lambda e: e.affine_select(out=Ef[64:128, :], in_=Ef[64:128, :], pattern=[[-1, S]], compare_op=ALU.is_ge, fill=0.0, base=63, channel_multiplier=64), r=[Ef], w=[Ef])
                for g_ in range(4):
                    C.op("dve" if g_ % 2 == 0 else "act", (lambda e: e.tensor_copy(out=KS[64:128, g_, :], in_=Ef[64:128, :])) if g_ % 2 == 0 else (lambda e: e.activation(out=KS[64:128, g_, :], in_=Ef[64:128, :], func=AF.Copy)), r=[Ef], w=[KS])
            V0 = C.sb("V0", [128, 128], F32)
            C.op("pool", lambda e: e.iota(out=V0[:], pattern=[[-1, 128]], base=0, channel_multiplier=16, allow_small_or_imprecise_dtypes=True), w=[V0])
            J = C.sb("J", [128, 64], F32)
            C.op("pool", lambda e: e.iota(out=J[:], pattern=[[1, 64]], base=0, channel_multiplier=0, allow_small_or_imprecise_dtypes=True), w=[J])
            f0 = C.sb("f0", [128, 64], F32)
            C.op("dve", lambda e: e.tensor_scalar(out=f0[:], in0=J[:], scalar1=0.0, scalar2=None, op0=ALU.is_equal), r=[J], w=[f0])
            CURb = C.sb("CURb", [128, 1], F32)
            C.op("pool", lambda e: e.iota(out=CURb[:], pattern=[[0, 1]], base=0, channel_multiplier=1, allow_small_or_imprecise_dtypes=True), w=[CURb])
            C.op("dve", lambda e: e.tensor_scalar(out=CURb[:], in0=CURb[:], scalar1=64.0, scalar2=None, op0=ALU.is_ge), r=[CURb], w=[CURb])
            sm = {n: C.sb(n, [128, 1], F32) for n in ("curq", "cm1")}
            rz4s = [C.sb("rz4", [128, 4], F32) for _ in range(2)]; cf4s = [C.sb("cf4", [128, 4], F32) for _ in range(2)]
            f1 = C.sb("f1", [128, 64], F32); f2 = C.sb("f2", [128, 64], F32)
            FORCE = C.sb("FORCE", [128, 64], F32); FUT = C.sb("FUT", [128, 64], F32)
            imp = C.sb("imp", [128, 64], F32); imp3 = C.sb("imp3", [128, 64], F32); impr = C.sb("impr", [128, 64], F32)
            imt = C.sb("imt", [128, 4, 64], F32)
            m8a = C.sb("m8a", [128, 8], F32); m8b = C.sb("m8b", [128, 8], F32)
            nm = C.sb("nm", [128, 128], BF16)
            C.op("pool", lambda e: e.memset(nm[:], 0.0), w=[nm])
            pnm = C.ps("pnm", [128, 8, 128], BF16)
            cb = [C.sb("cb", [128, 4, 128], BF16) for _ in range(2)]
            qnt = [C.sb("qnt", [128, 16, 128], BF16) for _ in range(2)]
            qrt = [C.sb("qrt", [128, 16, 128], BF16) for _ in range(2)]
            for t_ in qnt + qrt:
                C.op("pool", lambda e: e.memset(t_[:], 0.0), w=[t_])
            gts = [C.sb("gts", [128, 48], F32) for _ in range(2)]
            acc = C.sb("acc", [128, D], F32)
            obf = [C.sb("obf", [128, D], BF16) for _ in range(2)]
            pss = [C.ps("pss", [128, 512], F32) for _ in range(3)]
            pacc = C.ps("pacc", [128, 4, 512], F32)
            PTs = [C.sb("PTs", [128, 512], BF16) for _ in range(3)]
            si = 0; pi = 0; zi = 0
            for qt in range(NT):
                ts = slice(qt * 128, (qt + 1) * 128)
                QN = qnt[qt % 2]; QR = qrt[qt % 2]; Gt = gts[qt % 2]; OB = obf[qt % 2]
                C.dma("sp", QN[0:64, :, :], Q_T[:, :, ts].rearrange("h d s -> d h s"), w=[QN])
                C.dma("sp", QR[0:64, :, :], QR_T[:, :, ts].rearrange("h d s -> d h s"), w=[QR])
                C.dma("sp", Gt[:], GT[ts, :], w=[Gt])
                nhalf = 1 if qt < 16 else 2
                for half in range(nhalf):
                    C.op("dve", lambda e: e.tensor_scalar(out=cb[half][:], in0=ins_bc(V0[:], 1, 4), scalar1=float(128 * qt - 31 - 2048 * half), scalar2=NEG, op0=ALU.is_gt, op1=ALU.mult), r=[V0], w=[cb[half]])
                C.op("dve", lambda e: e.tensor_scalar(out=sm["curq"][:], in0=CURb[:], scalar1=float(2 * qt), scalar2=None, op0=ALU.add), r=[CURb], w=[sm["curq"]])
                C.op("dve", lambda e: e.tensor_scalar(out=sm["cm1"][:], in0=CURb[:], scalar1=float(2 * qt - 1), scalar2=None, op0=ALU.add), r=[CURb], w=[sm["cm1"]])
                C.op("dve", lambda e: e.tensor_scalar(out=f1[:], in0=J[:], scalar1=sm["curq"][:], scalar2=None, op0=ALU.is_equal), r=[J, sm["curq"]], w=[f1])
                C.op("dve", lambda e: e.tensor_scalar(out=f2[:], in0=J[:], scalar1=sm["cm1"][:], scalar2=None, op0=ALU.is_equal), r=[J, sm["cm1"]], w=[f2])
                C.op("dve", lambda e: e.tensor_tensor(out=f1[:], in0=f1[:], in1=f2[:], op=ALU.add), r=[f1, f2], w=[f1])
                C.op("dve", lambda e: e.tensor_tensor(out=f1[:], in0=f1[:], in1=f0[:], op=ALU.add), r=[f1, f0], w=[f1])
                C.op("dve", lambda e: e.tensor_scalar(out=FORCE[:], in0=f1[:], scalar1=1e4, scalar2=None, op0=ALU.mult), r=[f1], w=[FORCE])
                C.op("dve", lambda e: e.tensor_scalar(out=FUT[:], in0=J[:], scalar1=sm["curq"][:], scalar2=-1e30, op0=ALU.is_gt, op1=ALU.mult), r=[J, sm["curq"]], w=[FUT])

                def branch(g_, qk_fn, sts, Vrhs_fn, ncol, gidx, first):
                    nonlocal si, pi, zi
                    n = len(sts)

                    def qk(st):
                        nonlocal si
                        P = pss[si % 3]; si += 1
                        mm = qk_fn(st)
                        for ei, (l_ap, lT, r_ap, rT) in enumerate(mm):
                            C.op("pe", lambda e: e.matmul(P[:], lhsT=l_ap, rhs=r_ap, start=(ei == 0), stop=(ei == len(mm) - 1)), r=[lT, rT], w=[P])
                        return P

                    Pq = [qk(sts[0])]
                    if n > 1:
                        Pq.append(qk(sts[1]))
                    for ix, st in enumerate(sts):
                        if ix + 2 < n:
                            Pq.append(qk(sts[ix + 2]))
                        Pc = Pq.pop(0)
                        PT = PTs[pi % 3]; pi += 1
                        C.op("act", lambda e: e.activation(out=PT[:], in_=Pc[:], func=AF.Exp, scale=0.125), r=[Pc], w=[PT])
                        v_ap, vT = Vrhs_fn(st)
                        for j in range(4):
                            C.op("pe", lambda e: e.matmul(pacc[:, j, 0:ncol], lhsT=PT[:, j * 128:(j + 1) * 128], rhs=v_ap, start=(ix == 0), stop=(ix == n - 1)), r=[PT, vT], w=[pacc])
                    rz4 = rz4s[zi % 2]; cf4 = cf4s[zi % 2]; zi += 1
                    C.op("dve", lambda e: e.tensor_scalar(out=rz4[:], in0=pacc[:, :, 64], scalar1=1e-20, scalar2=None, op0=ALU.max), r=[pacc], w=[rz4])
                    C.op("dve", lambda e: e.reciprocal(out=rz4[:], in_=rz4[:]), r=[rz4], w=[rz4])
                    C.op("dve", lambda e: e.tensor_tensor(out=cf4[:], in0=rz4[:], in1=Gt[:, 12 * g_ + gidx:12 * g_ + gidx + 10:3], op=ALU.mult), r=[rz4, Gt], w=[cf4])
                    for j in range(4):
                        hd = 4 * g_ + j
                        ah = acc[:, hd * 64:(hd + 1) * 64]
                        if first:
                            C.op("act", lambda e: e.activation(out=ah, in_=pacc[:, j, 0:64], func=AF.Copy, scale=cf4[:, j:j + 1]), r=[pacc, cf4], w=[acc])
                        else:
                            C.op("dve", lambda e: e.scalar_tensor_tensor(out=ah, in0=pacc[:, j, 0:64], scalar=cf4[:, j:j + 1], in1=ah, op0=ALU.mult, op1=ALU.add), r=[pacc, cf4, acc], w=[acc])
                    if first:
                        C.op("dve", lambda e: e.tensor_tensor(out=imt[:], in0=pacc[:, :, 65:129], in1=ins_bc(rz4[:], 2, 64), op=ALU.mult), r=[pacc, rz4], w=[imt])
                        C.op("dve", lambda e: e.tensor_reduce(out=imp[:], in_=imt[:].rearrange("p j d -> p d j"), axis=AX.X, op=ALU.add), r=[imt], w=[imp])

                for g_ in range(4):
                    branch(g_, lambda st: [(KC_T[:, g_, st * 128:(st + 1) * 128], KC_T, QN[:, 4 * g_:4 * g_ + 4, :], QN), (ident[:], ident, cb[st][:], cb[st])],
                           list(range(nhalf)), lambda st: (VCa[:, st, g_, :], VCa), 129, 0, True)
                    C.op("dve", lambda e: e.tensor_tensor(out=imp3[:], in0=imp[:], in1=FORCE[:], op=ALU.max), r=[imp, FORCE], w=[imp3])
                    C.op("dve", lambda e: e.tensor_tensor(out=imp3[:], in0=imp3[:], in1=FUT[:], op=ALU.add), r=[imp3, FUT], w=[imp3])
                    C.op("dve", lambda e: e.max(out=m8a[:], in_=imp3[:]), r=[imp3], w=[m8a])
                    C.op("dve", lambda e: e.match_replace(out=impr[:], in_to_replace=m8a[:], in_values=imp3[:], imm_value=-1e30), r=[imp3, m8a], w=[impr])
                    C.op("dve", lambda e: e.max(out=m8b[:], in_=impr[:]), r=[impr], w=[m8b])
                    C.op("dve", lambda e: e.tensor_scalar(out=nm[:, 64:128], in0=imp3[:], scalar1=m8b[:, 7:8], scalar2=NEG, op0=ALU.is_lt, op1=ALU.mult), r=[imp3, m8b], w=[nm])
                    def selqk(st):
                        m = [(KS[:, g_, st * 128:(st + 1) * 128], KS, QR[:, 4 * g_:4 * g_ + 4, :], QR)]
                        if st == qt:
                            m.append((ident[:], ident, TRI4[:], TRI4))
                        return m

                    def winqk(st):
                        m = [(KW[:, g_, st * 128:(st + 1) * 128], KW, QR[:, 4 * g_:4 * g_ + 4, :], QR)]
                        if st == qt:
                            m.append((ident[:], ident, TRI4[:], TRI4))
                        elif st == qt - 4:
                            m.append((ident[:], ident, BLO4[:], BLO4))
                        return m
                    branch(g_, winqk, list(range(max(0, qt - 4), qt + 1)), lambda st: (VWa[:, st, g_, :], VWa), 65, 2, False)
                    C.op("pe", lambda e: e.transpose(out=pnm[:, 0, :], in_=nm[:], identity=ident[:]), r=[nm, ident], w=[pnm])
                    C.op("dve", lambda e: e.tensor_copy(out=QR[64:128, 4 * g_:4 * g_ + 4, :], in_=ins_bc(pnm[64:128, 0, :], 1, 4)), r=[pnm], w=[QR])

                    branch(g_, selqk, list(range(qt + 1)), lambda st: (VSa[:, st, g_, :], VSa), 65, 1, False)
                C.op("act", lambda e: e.activation(out=OB[:], in_=acc[:], func=AF.Copy), r=[acc], w=[OB])
                store(OM[ts, :], OB[:], OB)

    if upto >= 1:
        phase_A0()
    if upto >= 2:
        phase_A1()
    if upto >= 3:
        phase_A2()
    if upto >= 4:
        phase_A3(0, inp["ab_w_out"], x_in, XA)
    if upto >= 5:
        phase_A4(0, XA, XB if (dbg or upto > 5) else y_out)
    if upto >= 6:
        phase_B0()
    if upto >= 7:
        phase_B1()
    if upto >= 8:
        phase_B2()
    if upto >= 9:
        phase_A3(1, inp["nsa_w_out"], XB, XC)
    if upto >= 10:
        phase_A4(1, XC, y_out)
    C.close()
    k.nc = nc; k.dbg_names = dbg_names; k.ninstr = C.ninstr; k.inp_names = list(inp)
    return k


_W_NAMES = ["norm_mix", "norm_mem", "norm_mem_src", "norm_ffn", "mem_w_q", "mem_w_kv", "mem_w_o", "mem_q_norm", "mem_k_norm", "ffn_w_in", "ffn_w_out"]
_W0_NAMES = ["ab_w_in", "ab_w_out", "dsa_q_norm", "dsa_k_norm", "moba_q_norm", "moba_k_norm", "nsa_w_in", "nsa_w_out", "nsa_q_norm", "nsa_kcmp_norm",
             "nsa_ksel_norm", "nsa_kwin_norm", "nsa_cmp_pos_k", "nsa_cmp_pos_v", "nsa_cmp_w1_k", "nsa_cmp_w2_k", "nsa_cmp_w1_v", "nsa_cmp_w2_v"]


def make_in_map(inputs, b):
    m = {"x": np.ascontiguousarray(inputs["x"][b], dtype=np.float32),
         "mem": np.ascontiguousarray(inputs["mem"][b], dtype=np.float32),
         "post": np.ascontiguousarray(np.asarray(inputs["positions"][b]).astype(np.int32).reshape(NT, 128).T)}
    for n in _W_NAMES:
        m[n] = np.ascontiguousarray(inputs[n], dtype=np.float32)
    for n in _W0_NAMES:
        m[n] = np.ascontiguousarray(np.asarray(inputs[n])[0], dtype=np.float32)
    return m


def kernel(**inputs):
    from concourse.bass_utils import run_bass_kernel_spmd
    k = build(dbg=False)
    in_maps = [make_in_map(inputs, b) for b in range(8)]
    res = run_bass_kernel_spmd(k.nc, in_maps, core_ids=list(range(8)))
    return np.stack([np.asarray(r["y"], dtype=np.float32) for r in res.results], axis=0)
```

```python
from contextlib import ExitStack
import numpy as np
import concourse.bass as bass
import concourse.mybir as mybir

F32 = mybir.dt.float32
BF16 = mybir.dt.bfloat16
I32 = mybir.dt.int32
AF = mybir.ActivationFunctionType
ALU = mybir.AluOpType
AX = mybir.AxisListType

N_DMA_SLOTS = 24
N_HW_SLOTS = 14


class Buf:
    __slots__ = ("name", "w", "r")

    def __init__(self, name):
        self.name = name
        self.w = None
        self.r = {}


class T:
    def __init__(self, t, name):
        self.t = t
        self.b = Buf(name)

    def __getitem__(self, k):
        return self.t[k]


class Ctx:
    def __init__(self, nc):
        self.nc = nc
        self.es = ExitStack()
        self.eng = {"pe": nc.tensor, "act": nc.scalar, "dve": nc.vector, "pool": nc.gpsimd, "sp": nc.sync}
        self.sem = {}
        for k in ("pe", "act", "dve", "pool"):
            self.sem[k] = self.es.enter_context(nc.semaphore("sem_" + k))
        for i in range(N_DMA_SLOTS):
            self.sem[("d", i)] = self.es.enter_context(nc.semaphore("semd%d" % i))
        self.cnt = {k: 0 for k in ("pe", "act", "dve", "pool")}
        self.dval = [0] * N_DMA_SLOTS
        self.dnext = 0
        self.dnext_sw = 0
        self.seen = {k: {} for k in self.eng}
        self.uid = 0
        self.phase_stack = None
        self.ninstr = 0

    def _nm(self, name):
        self.uid += 1
        return "%s_%d" % (name, self.uid)

    def sb(self, name, shape, dtype, stack=None):
        st = stack if stack is not None else (self.phase_stack or self.es)
        nm = self._nm(name)
        return T(st.enter_context(self.nc.sbuf_tensor(nm, list(shape), dtype)), nm)

    def ps(self, name, shape, dtype, stack=None):
        st = stack if stack is not None else (self.phase_stack or self.es)
        nm = self._nm(name)
        return T(st.enter_context(self.nc.psum_tensor(nm, list(shape), dtype)), nm)

    def dram(self, name, shape, dtype, kind="Internal"):
        return self.nc.dram_tensor(name, list(shape), dtype, kind=kind).ap()

    def _waits(self, E, reads, writes):
        deps = {}

        def need(k, v):
            if v > deps.get(k, 0):
                deps[k] = v

        for t in reads:
            b = t.b
            if b.w is not None:
                need(*b.w)
        for t in writes:
            b = t.b
            if b.w is not None:
                need(*b.w)
            for k, v in b.r.items():
                need(k, v)
        eng = self.eng[E]
        seen = self.seen[E]
        for k, v in deps.items():
            if k == E and E == "pe":
                continue
            if seen.get(k, 0) >= v:
                continue
            eng.wait_ge(self.sem[k], v)
            self.ninstr += 1
            seen[k] = v

    def op(self, E, fn, r=(), w=()):
        self._waits(E, r, w)
        inst = fn(self.eng[E])
        self.cnt[E] += 1
        self.ninstr += 1
        inst.then_inc(self.sem[E], 1)
        ev = self.cnt[E]
        for t in r:
            if t.b.r.get(E, 0) < ev:
                t.b.r[E] = ev
        for t in w:
            t.b.w = (E, ev)
            t.b.r = {}
        return inst

    def dma(self, Q, out, in_, r=(), w=(), **kw):
        self._waits(Q, r, w)
        slot = self.dnext
        self.dnext = (self.dnext + 1) % N_DMA_SLOTS
        k = ("d", slot)
        eng = self.eng[Q]
        if self.seen[Q].get(k, 0) < self.dval[slot]:
            eng.wait_ge(self.sem[k], self.dval[slot])
            self.seen[Q][k] = self.dval[slot]
            self.ninstr += 1
        inst = eng.dma_start(out=out, in_=in_, **kw)
        self.dval[slot] += 16
        inst.then_inc(self.sem[k], 16)
        self.ninstr += 1
        v = self.dval[slot]
        for t in r:
            if t.b.r.get(k, 0) < v:
                t.b.r[k] = v
        for t in w:
            t.b.w = (k, v)
            t.b.r = {}
        return inst

    def barrier(self):
        for E, eng in self.eng.items():
            seen = self.seen[E]
            for k in ("pe", "act", "dve", "pool"):
                v = self.cnt[k]
                if v == 0 or seen.get(k, 0) >= v or (k == E and E == "pe"):
                    continue
                eng.wait_ge(self.sem[k], v)
                seen[k] = v
                self.ninstr += 1
            for i in range(N_DMA_SLOTS):
                k = ("d", i)
                v = self.dval[i]
                if v == 0 or seen.get(k, 0) >= v:
                    continue
                eng.wait_ge(self.sem[k], v)
                seen[k] = v
                self.ninstr += 1

    def phase(self):
        ctx = self

        class _P:
            def __enter__(s):
                ctx.barrier()
                s.prev = ctx.phase_stack
                s.st = ExitStack()
                ctx.phase_stack = s.st
                return s

            def __exit__(s, *a):
                ctx.barrier()
                ctx.phase_stack = s.prev
                s.st.close()
                return False

        return _P()

    def close(self):
        self.barrier()
        self.es.close()


S = 4096
D = 1024
NT = 32
NEG = -30000.0
EPS = 1e-6
THETA = 500000.0
DFF = 2816


def ins_bc(ap, axis, n):
    l = [list(p) for p in ap.ap]
    l.insert(axis, [0, n])
    return bass.AP(ap.tensor, ap.offset, l)


class K:
    pass


def build(dbg=False, upto=99):
    nc = bass.Bass("TRN2", target_bir_lowering=False)
    k = K()
    inp = {}

    def IN(name, shape, dt=F32):
        inp[name] = nc.dram_tensor(name, list(shape), dt, kind="ExternalInput").ap()
        return inp[name]

    x_in = IN("x", [S, D]); mem_in = IN("mem", [256, D]); pos_in = IN("post", [128, NT], I32)
    for n in ("norm_mix", "norm_mem", "norm_mem_src", "norm_ffn"):
        IN(n, [2, D])
    IN("ab_w_in", [D, 2472]); IN("ab_w_out", [D, D])
    for n in ("dsa_q_norm", "dsa_k_norm", "moba_q_norm", "moba_k_norm", "nsa_q_norm", "nsa_kcmp_norm", "nsa_ksel_norm", "nsa_kwin_norm"):
        IN(n, [64])
    IN("nsa_w_in", [D, 2608]); IN("nsa_w_out", [D, D])
    IN("nsa_cmp_pos_k", [32, 64]); IN("nsa_cmp_pos_v", [32, 64])
    IN("nsa_cmp_w1_k", [2048, 64]); IN("nsa_cmp_w2_k", [64, 64]); IN("nsa_cmp_w1_v", [2048, 64]); IN("nsa_cmp_w2_v", [64, 64])
    IN("mem_w_q", [2, D, 512]); IN("mem_w_kv", [2, D, 1024]); IN("mem_w_o", [2, 512, D])
    IN("mem_q_norm", [2, 128]); IN("mem_k_norm", [2, 128])
    IN("ffn_w_in", [2, D, 2 * DFF]); IN("ffn_w_out", [2, DFF, D])
    y_out = nc.dram_tensor("y", [S, D], F32, kind="ExternalOutput").ap()

    C = Ctx(nc)
    skind = "ExternalOutput" if dbg else "Internal"
    dbg_names = []

    def SCR(name, shape, dt):
        if dbg:
            dbg_names.append(name)
        return nc.dram_tensor(name, list(shape), dt, kind=skind).ap()

    ident = C.sb("ident", [128, 128], BF16)
    I4 = C.sb("I4", [128, 4, 128], BF16)
    epsG = C.sb("epsG", [128, 1], F32)
    C.op("pool", lambda e: e.memset(epsG[:], EPS), w=[epsG])
    TRI4 = C.sb("TRI4", [128, 4, 128], BF16)
    BLO4 = C.sb("BLO4", [128, 4, 128], BF16)
    CAUS = C.sb("CAUS", [128, 4, 4, 128], BF16)
    cs64 = C.sb("cs64", [128, NT, 8], F32); sn64 = C.sb("sn64", [128, NT, 8], F32)
    cs32 = C.sb("cs32", [128, NT, 4], F32); sn32 = C.sb("sn32", [128, NT, 4], F32)
    with C.phase():
        cf = C.sb("cf", [128, 128], F32)
        C.op("pool", lambda e: e.memset(cf[:], 0.0), w=[cf])
        C.op("pool", lambda e: e.affine_select(out=cf[:], in_=cf[:], pattern=[[-1, 128]], compare_op=ALU.not_equal, fill=1.0, base=0, channel_multiplier=1), r=[cf], w=[cf])
        C.op("dve", lambda e: e.tensor_copy(out=ident[:], in_=cf[:]), r=[cf], w=[ident])
        C.op("dve", lambda e: e.tensor_copy(out=I4[:], in_=ins_bc(cf[:], 1, 4)), r=[cf], w=[I4])
        tf = C.sb("tf", [128, 128], F32)
        C.op("pool", lambda e: e.memset(tf[:], 0.0), w=[tf])
        C.op("pool", lambda e: e.affine_select(out=tf[:], in_=tf[:], pattern=[[1, 128]], compare_op=ALU.is_ge, fill=NEG, base=0, channel_multiplier=-1), r=[tf], w=[tf])
        C.op("dve", lambda e: e.tensor_copy(out=TRI4[:], in_=ins_bc(tf[:], 1, 4)), r=[tf], w=[TRI4])
        for j in range(4):
            for qi in range(4):
                if qi > j:
                    C.op("dve", lambda e: e.memset(CAUS[:, j, qi, :], 0.0), w=[CAUS])
                elif qi == j:
                    C.op("dve", lambda e: e.tensor_copy(out=CAUS[:, j, qi, :], in_=tf[:]), r=[tf], w=[CAUS])
                else:
                    C.op("dve", lambda e: e.memset(CAUS[:, j, qi, :], NEG), w=[CAUS])
        tf2 = C.sb("tf2", [128, 128], F32)
        C.op("pool", lambda e: e.memset(tf2[:], 0.0), w=[tf2])
        C.op("pool", lambda e: e.affine_select(out=tf2[:], in_=tf2[:], pattern=[[-1, 128]], compare_op=ALU.is_gt, fill=NEG, base=0, channel_multiplier=1), r=[tf2], w=[tf2])
        C.op("dve", lambda e: e.tensor_copy(out=BLO4[:], in_=ins_bc(tf2[:], 1, 4)), r=[tf2], w=[BLO4])
        pi_ = C.sb("posi", [128, NT], I32); pf = C.sb("posf", [128, NT], F32)
        C.dma("sp", pi_[:], pos_in, w=[pi_])
        C.op("dve", lambda e: e.tensor_copy(out=pf[:], in_=pi_[:]), r=[pi_], w=[pf])
        u = C.sb("u", [128, NT], F32); ui = C.sb("ui", [128, NT], I32); uf = C.sb("uf", [128, NT], F32); fr = C.sb("fr", [128, NT], F32)

        def sincol(dst, col, freq, shift):
            C.op("dve", lambda e: e.tensor_scalar(out=u[:], in0=pf[:], scalar1=float(freq / (2 * np.pi)), scalar2=float(shift), op0=ALU.mult, op1=ALU.add), r=[pf], w=[u])
            C.op("dve", lambda e: e.tensor_copy(out=ui[:], in_=u[:]), r=[u], w=[ui])
            C.op("dve", lambda e: e.tensor_copy(out=uf[:], in_=ui[:]), r=[ui], w=[uf])
            C.op("dve", lambda e: e.tensor_tensor(out=fr[:], in0=u[:], in1=uf[:], op=ALU.subtract), r=[u, uf], w=[fr])
            C.op("dve", lambda e: e.tensor_scalar(out=fr[:], in0=fr[:], scalar1=0.4999, scalar2=-0.4999, op0=ALU.min, op1=ALU.max), r=[fr], w=[fr])
            C.op("act", lambda e: e.activation(out=dst[:, :, col], in_=fr[:], func=AF.Sin, scale=float(2 * np.pi)), r=[fr], w=[dst])

        for i in range(8):
            f = THETA ** (-(i * 2.0 / 16))
            sincol(sn64, i, f, 0.0); sincol(cs64, i, f, 0.25)
        for i in range(4):
            f = THETA ** (-(i * 2.0 / 8))
            sincol(sn32, i, f, 0.0); sincol(cs32, i, f, 0.25)

    def load_gain(dst, src_ap):
        C.dma("sp", dst[:], src_ap.rearrange("(kt p) -> p kt", p=128), w=[dst], allow_slow_non_contiguous=True)

    def load_w(dst, src_ap, KT, N, gain=None):
        srcv = src_ap.rearrange("(kt p) n -> p kt n", p=128)
        with C.phase():
            st = [C.sb("wst", [128, 4, 512], F32) for _ in range(3)]
            i = 0
            for k0 in range(0, KT, 4):
                k1 = min(KT, k0 + 4)
                for c0 in range(0, N, 512):
                    c1 = min(N, c0 + 512)
                    s = st[i % 3]
                    C.dma("sp" if i % 2 == 0 else "pool", s[:, 0:k1 - k0, 0:c1 - c0], srcv[:, k0:k1, c0:c1], w=[s])
                    for kt in range(k0, k1):
                        if kt % 2 == 0:
                            if gain is not None:
                                C.op("dve", lambda e: e.tensor_scalar(out=dst[:, kt, c0:c1], in0=s[:, kt - k0, 0:c1 - c0], scalar1=gain[:, kt:kt + 1], scalar2=None, op0=ALU.mult), r=[s, gain], w=[dst])
                            else:
                                C.op("dve", lambda e: e.tensor_copy(out=dst[:, kt, c0:c1], in_=s[:, kt - k0, 0:c1 - c0]), r=[s], w=[dst])
                        else:
                            if gain is not None:
                                C.op("act", lambda e: e.activation(out=dst[:, kt, c0:c1], in_=s[:, kt - k0, 0:c1 - c0], func=AF.Copy, scale=gain[:, kt:kt + 1]), r=[s, gain], w=[dst])
                            else:
                                C.op("act", lambda e: e.activation(out=dst[:, kt, c0:c1], in_=s[:, kt - k0, 0:c1 - c0], func=AF.Copy), r=[s], w=[dst])
                    i += 1

    class NormT:
        def __init__(s, pT=None):
            s.junk = C.sb("junk", [128, D], F32)
            s.ss = C.sb("ss", [128, 1], F32); s.rs = C.sb("rs", [128, 1], F32); s.epsT = epsG
            s.hn = C.sb("hn", [128, D], BF16)
            s.pT = pT if pT is not None else C.ps("pT", [128, 8, 128], BF16)

        def run(s, X, hT_ap, hT):
            C.op("act", lambda e: e.activation(out=s.junk[:], in_=X[:], func=AF.Square, accum_out=s.ss[:]), r=[X], w=[s.junk, s.ss])
            C.op("act", lambda e: e.activation(out=s.rs[:], in_=s.ss[:], func=AF.Ln, scale=1.0 / D, bias=s.epsT[:]), r=[s.ss, s.epsT], w=[s.rs])
            C.op("act", lambda e: e.activation(out=s.rs[:], in_=s.rs[:], func=AF.Exp, scale=-0.5), r=[s.rs], w=[s.rs])
            C.op("act", lambda e: e.activation(out=s.hn[:], in_=X[:], func=AF.Copy, scale=s.rs[:]), r=[X, s.rs], w=[s.hn])
            for kt in range(8):
                C.op("pe", lambda e: e.transpose(out=s.pT[:, kt, :], in_=s.hn[:, kt * 128:(kt + 1) * 128], identity=ident[:]), r=[s.hn, ident], w=[s.pT])
            C.op("dve", lambda e: e.tensor_copy(out=hT_ap, in_=s.pT[:]), r=[s.pT], w=[hT])

    class HeadNorm:
        def __init__(s, Hmax, Dh, rope_eng="dve"):
            s.sq = C.sb("hsq", [128, Hmax, Dh], F32)
            s.nrms = [C.sb("hnrm", [128, Hmax, Dh], F32) for _ in range(2)]
            s.ssh = C.sb("hss", [128, Hmax], F32); s.rsh = C.sb("hrs", [128, Hmax], F32)
            hf = Dh // 8
            s.t1s = [C.sb("ht1", [128, Hmax, hf], F32) for _ in range(2)]; s.t2s = [C.sb("ht2", [128, Hmax, hf], F32) for _ in range(2)]
            s.Dh = Dh; s.k = 0; s.re = rope_eng

        def run(s, src_ap, srcT, H, gain, out_ap, outT, rope=None, norm=True, out2_ap=None, out2T=None):
            Dh = s.Dh
            s.k += 1
            nrmT = s.nrms[s.k % 2]; t1T = s.t1s[s.k % 2]; t2T = s.t2s[s.k % 2]
            if norm:
                sq = s.sq[:, 0:H, :]; nrm = nrmT[:, 0:H, :]
                C.op("dve", lambda e: e.tensor_tensor(out=sq, in0=src_ap, in1=src_ap, op=ALU.mult), r=[srcT], w=[s.sq])
                C.op("dve", lambda e: e.tensor_reduce(out=s.ssh[:, 0:H], in_=sq, axis=AX.X, op=ALU.add), r=[s.sq], w=[s.ssh])
                C.op("act", lambda e: e.activation(out=s.rsh[:, 0:H], in_=s.ssh[:, 0:H], func=AF.Ln, scale=1.0 / Dh, bias=epsG[:]), r=[s.ssh, epsG], w=[s.rsh])
                C.op("act", lambda e: e.activation(out=s.rsh[:, 0:H], in_=s.rsh[:, 0:H], func=AF.Exp, scale=-0.5), r=[s.rsh], w=[s.rsh])
                C.op("dve", lambda e: e.tensor_tensor(out=nrm, in0=src_ap, in1=ins_bc(s.rsh[:, 0:H], 2, Dh), op=ALU.mult), r=[srcT, s.rsh], w=[nrmT])
                C.op("dve", lambda e: e.tensor_tensor(out=nrm, in0=nrm, in1=ins_bc(gain[:], 1, H), op=ALU.mult), r=[nrmT, gain], w=[nrmT])
                nT = nrmT
            else:
                nrm = src_ap; nT = srcT
            if out2_ap is not None:
                C.op("act", lambda e: e.activation(out=out2_ap, in_=nrm, func=AF.Copy), r=[nT], w=[out2T])
            C.op("act", lambda e: e.activation(out=out_ap, in_=nrm, func=AF.Copy), r=[nT], w=[outT])
            if rope is not None:
                RE = s.re
                cs, sn, hf = rope
                x1 = nrm[:, :, 0:hf]; x2 = nrm[:, :, hf:2 * hf]
                csb = ins_bc(cs, 1, H); snb = ins_bc(sn, 1, H)
                t1 = t1T[:, 0:H, :]; t2 = t2T[:, 0:H, :]
                C.op(RE, lambda e: e.tensor_tensor(out=t1, in0=x1, in1=csb, op=ALU.mult), r=[nT], w=[t1T])
                C.op(RE, lambda e: e.tensor_tensor(out=t2, in0=x2, in1=snb, op=ALU.mult), r=[nT], w=[t2T])
                C.op(RE, lambda e: e.tensor_tensor(out=out_ap[:, :, 0:hf], in0=t1, in1=t2, op=ALU.subtract), r=[t1T, t2T], w=[outT])
                C.op(RE, lambda e: e.tensor_tensor(out=t1, in0=x2, in1=csb, op=ALU.mult), r=[nT], w=[t1T])
                C.op(RE, lambda e: e.tensor_tensor(out=t2, in0=x1, in1=snb, op=ALU.mult), r=[nT], w=[t2T])
                C.op(RE, lambda e: e.tensor_tensor(out=out_ap[:, :, hf:2 * hf], in0=t1, in1=t2, op=ALU.add), r=[t1T, t2T], w=[outT])

    def run_proj(x_src, W, chunks, make_job, npo=3):
        nt = NormT()
        xt = [C.sb("xt", [128, D], F32) for _ in range(2)]
        hT = [C.sb("hT", [128, 8, 128], BF16) for _ in range(2)]
        po = [C.ps("po", [128, 512], F32) for _ in range(npo)]

        def norm(t):
            X = xt[t % 2]
            C.dma("sp", X[:], x_src[t * 128:(t + 1) * 128, :], w=[X])
            nt.run(X, hT[t % 2][:], hT[t % 2])

        jobs = [(t, c) for t in range(NT) for c in range(len(chunks))]

        def mm(k):
            t, c = jobs[k]
            c0, c1 = chunks[c]
            P = po[k % npo]
            H_ = hT[t % 2]
            for kt in range(8):
                C.op("pe", lambda e: e.matmul(P[:, 0:c1 - c0], lhsT=H_[:, kt, :], rhs=W[:, kt, c0:c1], start=(kt == 0), stop=(kt == 7)), r=[H_, W], w=[P])
            return P

        norm(0)
        Ps = {0: mm(0)}
        prev_trans = None
        for k, (t, c) in enumerate(jobs):
            if c == 0 and t + 1 < NT:
                norm(t + 1)
            if k + 1 < len(jobs):
                Ps[k + 1] = mm(k + 1)
            if prev_trans is not None:
                prev_trans()
            post, trans = make_job(k, t, c, Ps.pop(k))
            post()
            prev_trans = trans
        if prev_trans is not None:
            prev_trans()

    def bgain(src_ap, n=64):
        t = C.sb("bg", [128, n], F32)
        C.dma("sp", t[:], src_ap.partition_broadcast(128), w=[t])
        return t


    QA_T = SCR("QA_T", [8, 64, S], BF16); KA_T = SCR("KA_T", [64, S], BF16); VA = SCR("VA", [S, 64], BF16)
    IQ_T = SCR("IQ_T", [8, 32, S], BF16); IK_T = SCR("IK_T", [32, S], BF16); IW = SCR("IW", [S, 8], F32)
    QB_T = SCR("QB_T", [8, 64, S], BF16); KB_T = SCR("KB_T", [8, 64, S], BF16); VB = SCR("VB", [S, 512], BF16)
    OM = SCR("OM", [S, D], BF16)
    XA = SCR("XA", [S, D], F32); XB = SCR("XB", [S, D], F32); XC = SCR("XC", [S, D], F32)

    def store(dst_ap, src_ap, srcT, **kw):
        C.dma("pool", dst_ap, src_ap, r=[srcT], **kw)

    def phase_A0():
        with C.phase():
            g = C.sb("g", [128, 8], F32); load_gain(g, inp["norm_mix"][0])
            W = C.sb("W0", [128, 8, 2472], BF16)
            load_w(W, inp["ab_w_in"], 8, 2472, g)
            gq = bgain(inp["dsa_q_norm"]); gk = bgain(inp["dsa_k_norm"]); gbq = bgain(inp["moba_q_norm"]); gbk = bgain(inp["moba_k_norm"])
            hnm = HeadNorm(8, 64, "pool"); hn32 = HeadNorm(9, 32, "pool")
            pr = [C.sb("pr", [128, 512], F32) for _ in range(2)]
            qn = [C.sb("qn", [128, 512], BF16) for _ in range(3)]
            ptq = [C.ps("ptq", [64, 8, 128], BF16) for _ in range(2)]
            qT = [C.sb("qT", [64, 8, 128], BF16) for _ in range(3)]
            chunks = [(0, 512), (512, 936), (936, 1448), (1448, 1960), (1960, 2472)]
            cnt = {"ti": 0}

            def tr_heads(Q, H, Dh, src_off, dst_ap):
                i = cnt["ti"]; cnt["ti"] += 1
                PT = ptq[i % 2]; QT = qT[i % 3]
                for h in range(H):
                    C.op("pe", lambda e: e.transpose(out=PT[0:Dh, h, :], in_=Q[:, src_off + h * Dh: src_off + (h + 1) * Dh], identity=ident[:]), r=[Q, ident], w=[PT])
                C.op("dve", lambda e: e.tensor_copy(out=QT[0:Dh, 0:H, :], in_=PT[0:Dh, 0:H, :]), r=[PT], w=[QT])
                store(dst_ap, QT[0:Dh, 0:H, :], QT)

            def make_job(k, t, c, P):
                R = pr[k % 2]; Q = qn[k % 3]
                ts = slice(t * 128, (t + 1) * 128)
                cs8 = cs64[:, t, :]; sn8 = sn64[:, t, :]; cs4 = cs32[:, t, :]; sn4 = sn32[:, t, :]
                wd = chunks[c][1] - chunks[c][0]

                def post():
                    C.op("act", lambda e: e.activation(out=R[:, 0:wd], in_=P[:, 0:wd], func=AF.Copy), r=[P], w=[R])
                    if c in (0, 2, 3):
                        gg = (gq, None, gbq, gbk)[c]
                        hnm.run(R[:].rearrange("p (h d) -> p h d", h=8), R, 8, gg, Q[:].rearrange("p (h d) -> p h d", h=8), Q, rope=(cs8, sn8, 8))
                    elif c == 1:
                        hnm.run(R[:, 0:64].rearrange("p (h d) -> p h d", h=1), R, 1, gk, Q[:, 0:64].rearrange("p (h d) -> p h d", h=1), Q, rope=(cs8, sn8, 8))
                        C.op("act", lambda e: e.activation(out=Q[:, 64:128], in_=R[:, 64:128], func=AF.Copy), r=[R], w=[Q])
                        hn32.run(R[:, 128:416].rearrange("p (h d) -> p h d", h=9), R, 9, None, Q[:, 128:416].rearrange("p (h d) -> p h d", h=9), Q, rope=(cs4, sn4, 4), norm=False)
                        store(IW[ts, :], R[:, 416:424], R)
                    else:
                        C.op("act", lambda e: e.activation(out=Q[:], in_=R[:], func=AF.Copy), r=[R], w=[Q])

                def trans():
                    if c in (0, 2, 3):
                        dst = (QA_T, None, QB_T, KB_T)[c]
                        tr_heads(Q, 8, 64, 0, dst[:, :, ts].rearrange("h d s -> d h s"))
                    elif c == 1:
                        store(VA[ts, :], Q[:, 64:128], Q)
                        tr_heads(Q, 1, 64, 0, KA_T[:, ts].rearrange("(h d) s -> d h s", h=1))
                        tr_heads(Q, 8, 32, 128, IQ_T[:, :, ts].rearrange("h d s -> d h s"))
                        tr_heads(Q, 1, 32, 384, IK_T[:, ts].rearrange("(h d) s -> d h s", h=1))
                    else:
                        store(VB[ts, :], Q[:], Q)
                return post, trans

            run_proj(x_in, W, chunks, make_job)

    NIT = 16

    def run_merged(gA, nA, gB, nB):
        a = b = 0
        doneA = gA is None
        doneB = gB is None
        while not (doneA and doneB):
            pickA = (not doneA) and (doneB or a * nB <= b * nA)
            if pickA:
                try:
                    next(gA); a += 1
                except StopIteration:
                    doneA = True
            else:
                try:
                    next(gB); b += 1
                except StopIteration:
                    doneB = True

    def phase_A1():
        with C.phase():
            KT_ = C.sb("KAT", [128, S], BF16)
            C.op("pool", lambda e: e.memset(KT_[64:128, :], 0.0), w=[KT_])
            C.dma("sp", KT_[0:64, :], KA_T, w=[KT_])
            IKT = C.sb("IKT", [128, S], BF16)
            C.op("pool", lambda e: e.memset(IKT[:], 0.0), w=[IKT])
            C.dma("sp", IKT[0:32, :], IK_T, w=[IKT])
            Va = C.sb("VAa", [128, NT, 65], BF16)
            C.op("pool", lambda e: e.memset(Va[:], 1.0), w=[Va])
            C.dma("sp", Va[:, :, 0:64], VA.rearrange("(t p) c -> p t c", p=128), w=[Va])
            IWs = C.sb("IWs", [128, NT, 8], F32); C.dma("sp", IWs[:], IW.rearrange("(t p) c -> p t c", p=128), w=[IWs])
            score = [C.sb("score", [128, S], F32) for _ in range(2)]
            junk = C.sb("junkb", [128, S], BF16)
            nmask = [C.sb("nmask", [128, S], BF16) for _ in range(2)]
            rl = [C.sb("rl", [128, 512], F32) for _ in range(2)]
            qa = [C.sb("qa", [128, 8, 128], BF16) for _ in range(2)]
            iqt = [C.sb("iqt", [128, 8, 128], BF16) for _ in range(2)]
            for t_ in qa + iqt:
                C.op("pool", lambda e: e.memset(t_[:], 0.0), w=[t_])
            psi = [C.ps("psi", [128, 512], F32) for _ in range(2)]
            pss = [C.ps("pss", [128, 512], F32) for _ in range(2)]
            pacc = [C.ps("pacc", [128, 512], F32) for _ in range(4)]
            PTs = [C.sb("PTs", [128, 512], BF16) for _ in range(3)]
            sm = {n: C.sb(n, [128, 1], F32) for n in ("lo", "hi", "mid", "cnt", "ge", "rng", "c255", "sgn")}
            cthr = [C.sb("cthr", [128, 1], F32) for _ in range(2)]
            junkA = C.sb("junkA", [128, S], BF16)
            rzs = [C.sb("rz", [128, 1], F32) for _ in range(2)]
            steps = C.sb("steps", [128, NIT], F32); pw = C.sb("pw", [128, NIT], F32); steps2 = C.sb("steps2", [128, NIT], F32)
            for kk in range(NIT):
                C.op("pool", lambda e: e.memset(pw[:, kk:kk + 1], float(2.0 ** -(kk + 1))), w=[pw])
            C.op("pool", lambda e: e.memset(sm["c255"][:], 255.5), w=[sm["c255"]])
            oq = [C.sb("oq", [128, 512], BF16) for _ in range(2)]
            st_ = {"ii": 0, "si": 0, "pi": 0, "ri": 0}

            def gen_idx(qt):
                nk = (qt + 1) * 128
                IQ = iqt[qt % 2]; SC = score[qt % 2]; NM = nmask[qt % 2]
                ts = slice(qt * 128, (qt + 1) * 128)
                C.dma("sp", IQ[0:32, :, :], IQ_T[:, :, ts].rearrange("h d s -> d h s"), w=[IQ])
                jobs_ = [(c0, min(512, nk - c0), h) for c0 in range(0, nk, 512) for h in range(8)]

                def imm(j):
                    c0, wd, h = jobs_[j]
                    P = psi[st_["ii"] % 2]; st_["ii"] += 1
                    C.op("pe", lambda e: e.matmul(P[:, 0:wd], lhsT=IQ[:, h, :], rhs=IKT[:, c0:c0 + wd], start=True, stop=True), r=[IQ, IKT], w=[P])
                    return P

                Pcur = imm(0)
                for j, (c0, wd, h) in enumerate(jobs_):
                    Pnext = imm(j + 1) if j + 1 < len(jobs_) else None
                    P = Pcur
                    if h == 0:
                        C.op("dve", lambda e: e.tensor_scalar(out=SC[:, c0:c0 + wd], in0=P[:, 0:wd], scalar1=0.0, scalar2=IWs[:, qt, 0:1], op0=ALU.max, op1=ALU.mult), r=[P, IWs], w=[SC])
                    else:
                        R_ = rl[j % 2]
                        C.op("act", lambda e: e.activation(out=R_[:, 0:wd], in_=P[:, 0:wd], func=AF.Relu), r=[P], w=[R_])
                        C.op("dve", lambda e: e.scalar_tensor_tensor(out=SC[:, c0:c0 + wd], in0=R_[:, 0:wd], scalar=IWs[:, qt, h:h + 1], in1=SC[:, c0:c0 + wd], op0=ALU.mult, op1=ALU.add), r=[R_, IWs, SC], w=[SC])
                    Pcur = Pnext
                    if h == 7:
                        yield
                sc = SC[:, 0:nk]
                C.op("dve", lambda e: e.tensor_reduce(out=sm["hi"][:], in_=sc, axis=AX.X, op=ALU.max), r=[SC], w=[sm["hi"]])
                C.op("dve", lambda e: e.tensor_reduce(out=sm["lo"][:], in_=sc, axis=AX.X, op=ALU.min), r=[SC], w=[sm["lo"]])
                C.op("pool", lambda e: e.affine_select(out=SC[:, nk - 128:nk], in_=SC[:, nk - 128:nk], pattern=[[-1, 128]], compare_op=ALU.is_ge, fill=-1e30, base=0, channel_multiplier=1), r=[SC], w=[SC])
                C.op("dve", lambda e: e.tensor_tensor(out=sm["rng"][:], in0=sm["hi"][:], in1=sm["lo"][:], op=ALU.subtract), r=[sm["lo"], sm["hi"]], w=[sm["rng"]])
                C.op("dve", lambda e: e.tensor_scalar(out=steps[:], in0=pw[:], scalar1=sm["rng"][:], scalar2=None, op0=ALU.mult), r=[pw, sm["rng"]], w=[steps])
                yield
                cD = max(8, int(0.52 * nk) // 8 * 8)
                nA = nk - cD
                CT = cthr[qt % 2]
                C.op("pool", lambda e: e.memset(CT[:], float(255.5 - 0.5 * nA)), w=[CT])
                C.op("dve", lambda e: e.tensor_scalar(out=steps2[:], in0=steps[:], scalar1=2.0, scalar2=None, op0=ALU.mult), r=[steps], w=[steps2])
                C.op("dve", lambda e: e.tensor_tensor(out=sm["mid"][:], in0=sm["lo"][:], in1=steps[:, 0:1], op=ALU.add), r=[sm["lo"], steps], w=[sm["mid"]])
                for it in range(NIT):
                    last = it == NIT - 1
                    mul_ap = steps[:, it:it + 1] if last else steps2[:, it + 1:it + 2]
                    sub_ap = steps[:, it:it + 1] if last else steps[:, it + 1:it + 2]
                    C.op("act", lambda e: e.activation(out=junkA[:, 0:nA], in_=SC[:, cD:nk], func=AF.Sign, scale=-1.0, bias=sm["mid"][:], accum_out=sm["sgn"][:]), r=[SC, sm["mid"]], w=[junkA, sm["sgn"]])
                    C.op("dve", lambda e: e.tensor_scalar(out=junk[:, 0:cD], in0=SC[:, 0:cD], scalar1=sm["mid"][:], scalar2=None, op0=ALU.is_ge, op1=ALU.add, accum_out=sm["cnt"][:]), r=[SC, sm["mid"]], w=[junk, sm["cnt"]])
                    C.op("dve", lambda e: e.scalar_tensor_tensor(out=sm["cnt"][:], in0=sm["sgn"][:], scalar=-0.5, in1=sm["cnt"][:], op0=ALU.mult, op1=ALU.add), r=[sm["sgn"], sm["cnt"]], w=[sm["cnt"]])
                    C.op("dve", lambda e: e.tensor_scalar(out=sm["ge"][:], in0=sm["cnt"][:], scalar1=CT[:], scalar2=mul_ap, op0=ALU.is_ge, op1=ALU.mult), r=[sm["cnt"], CT, steps, steps2], w=[sm["ge"]])
                    C.op("dve", lambda e: e.scalar_tensor_tensor(out=sm["mid"][:], in0=sm["ge"][:], scalar=sub_ap, in1=sm["mid"][:], op0=ALU.subtract, op1=ALU.add), r=[sm["ge"], steps, sm["mid"]], w=[sm["mid"]])
                    if it % 2 == 1:
                        yield
                C.op("dve", lambda e: e.scalar_tensor_tensor(out=sm["mid"][:], in0=steps[:, NIT - 1:NIT], scalar=-0.5, in1=sm["mid"][:], op0=ALU.mult, op1=ALU.add), r=[steps, sm["mid"]], w=[sm["mid"]])
                C.op("dve", lambda e: e.tensor_scalar(out=NM[:, 0:nk], in0=sc, scalar1=sm["mid"][:], scalar2=NEG, op0=ALU.is_lt, op1=ALU.mult), r=[SC, sm["mid"]], w=[NM])
                yield

            def n_idx(qt):
                return ((qt + 1) * 128 + 511) // 512 + NIT // 2 + 2

            def gen_att(qt):
                QA = qa[qt % 2]; NM = nmask[qt % 2]; OQ = oq[qt % 2]
                ts = slice(qt * 128, (qt + 1) * 128)
                C.dma("sp", QA[0:64, :, :], QA_T[:, :, ts].rearrange("h d s -> d h s"), w=[QA])

                def qk(hg, st):
                    P = pss[st_["si"] % 2]; st_["si"] += 1
                    ss_ = slice(st * 128, (st + 1) * 128)
                    C.op("pe", lambda e: e.matmul(P[:], lhsT=KT_[:, ss_], rhs=QA[:, hg * 4:(hg + 1) * 4, :], start=True, stop=False), r=[KT_, QA], w=[P])
                    C.op("pe", lambda e: e.matmul(P[:], lhsT=NM[:, ss_], rhs=I4[:], start=False, stop=True), r=[NM, I4], w=[P])
                    return P

                for hg in range(2):
                    Pc = qk(hg, 0)
                    for st in range(qt + 1):
                        Pn = qk(hg, st + 1) if st < qt else None
                        PT = PTs[st_["pi"] % 3]; st_["pi"] += 1
                        C.op("act", lambda e: e.activation(out=PT[:], in_=Pc[:], func=AF.Exp, scale=0.125), r=[Pc], w=[PT])
                        for h in range(4):
                            C.op("pe", lambda e: e.matmul(pacc[h][:, 0:65], lhsT=PT[:, h * 128:(h + 1) * 128], rhs=Va[:, st, :], start=(st == 0), stop=(st == qt)), r=[PT, Va], w=[pacc[h]])
                        Pc = Pn
                        if st % 4 == 3:
                            yield
                    for h in range(4):
                        hd = hg * 4 + h
                        rz = rzs[st_["ri"] % 2]; st_["ri"] += 1
                        C.op("dve", lambda e: e.reciprocal(out=rz[:], in_=pacc[h][:, 64:65]), r=[pacc[h]], w=[rz])
                        C.op("act", lambda e: e.activation(out=OQ[:, hd * 64:(hd + 1) * 64], in_=pacc[h][:, 0:64], func=AF.Copy, scale=rz[:]), r=[pacc[h], rz], w=[OQ])
                    yield
                store(OM[ts, 0:512], OQ[:], OQ)
                yield

            def n_att(qt):
                return 2 * ((qt + 1) // 4 + 1) + 1

            run_merged(gen_idx(0), 1, None, 1)
            for qt in range(NT):
                gB = gen_idx(qt + 1) if qt + 1 < NT else None
                run_merged(gen_att(qt), n_att(qt), gB, n_idx(qt + 1) if gB else 1)

    def phase_A2():
        with C.phase():
            KT_ = C.sb("KBT", [128, S], BF16); QT_ = C.sb("QBT", [128, S], BF16)
            C.op("pool", lambda e: e.memset(KT_[64:128, :], 0.0), w=[KT_])
            C.op("pool", lambda e: e.memset(QT_[64:128, :], 0.0), w=[QT_])
            with C.phase():
                Ef = C.sb("Ef", [80, S], F32)
                C.op("pool", lambda e: e.memset(Ef[64:80, :], 1.0), w=[Ef])
                C.op("pool", lambda e: e.affine_select(out=Ef[64:80, :], in_=Ef[64:80, :], pattern=[[1, S]], compare_op=ALU.is_ge, fill=0.0, base=0, channel_multiplier=-256), r=[Ef], w=[Ef])
                C.op("pool", lambda e: e.affine_select(out=Ef[64:80, :], in_=Ef[64:80, :], pattern=[[-1, S]], compare_op=ALU.is_ge, fill=0.0, base=255, channel_multiplier=256), r=[Ef], w=[Ef])
                C.op("dve", lambda e: e.tensor_copy(out=KT_[64:80, :], in_=Ef[64:80, :]), r=[Ef], w=[KT_])
            cm = C.sb("cm", [128, NT, 16], F32); om = C.sb("om", [128, NT, 16], F32)
            C.op("pool", lambda e: e.memset(cm[:], 0.0), w=[cm])
            C.op("pool", lambda e: e.memset(om[:], 0.0), w=[om])
            for qt in range(NT):
                own = qt // 2
                C.op("pool", lambda e: e.memset(cm[:, qt, own:16], -1e30), w=[cm])
                C.op("pool", lambda e: e.memset(om[:, qt, own:own + 1], 1.0), w=[om])
            Va = C.sb("VBa", [128, NT, 65], BF16)
            C.op("pool", lambda e: e.memset(Va[:], 1.0), w=[Va])
            kmf = C.sb("kmf", [64, 16], F32); kmb = C.sb("kmb", [64, 16], BF16)
            pgp = C.ps("pgp", [128, 512], F32)
            pgv = pgp[:].rearrange("p (t n) -> p t n", n=16)
            pntv = pgp[:].bitcast(BF16).rearrange("p (j q) -> p j q", q=128)
            gm = C.sb("gm", [128, NT, 16], F32); m8 = C.sb("m8", [128, NT, 8], F32)
            sel = C.sb("sel", [128, NT, 16], F32)
            nbx = C.sb("nbx", [128, NT, 80], BF16)
            C.op("pool", lambda e: e.memset(nbx[:], 0.0), w=[nbx])
            pss = [C.ps("pss", [128, 512], F32) for _ in range(3)]
            pacc = [C.ps("pacc", [128, 512], F32) for _ in range(4)]
            PTs = [C.sb("PTs", [128, 512], BF16) for _ in range(3)]
            rzs = [C.sb("rz", [128, 1], F32) for _ in range(2)]
            ob = [C.sb("ob", [128, 64], BF16) for _ in range(3)]
            si = 0; pi = 0; oi = 0; ri = 0
            for h in range(8):
                C.dma("sp", KT_[0:64, :], KB_T[h], w=[KT_])
                C.dma("sp", QT_[0:64, :], QB_T[h], w=[QT_])
                C.dma("sp", Va[:, :, 0:64], VB[:, h * 64:(h + 1) * 64].rearrange("(t p) c -> p t c", p=128), w=[Va])
                C.op("dve", lambda e: e.tensor_reduce(out=kmf[:], in_=KT_[0:64, :].rearrange("d (n s) -> d n s", n=16), axis=AX.X, op=ALU.add), r=[KT_], w=[kmf])
                C.op("dve", lambda e: e.tensor_scalar(out=kmb[:], in0=kmf[:], scalar1=1.0 / 256, scalar2=None, op0=ALU.mult), r=[kmf], w=[kmb])
                for qt in range(NT):
                    C.op("pe", lambda e: e.matmul(pgv[:, qt, :], lhsT=QT_[0:64, qt * 128:(qt + 1) * 128], rhs=kmb[:], start=True, stop=True), r=[QT_, kmb], w=[pgp])
                C.op("dve", lambda e: e.tensor_tensor(out=gm[:], in0=pgv, in1=cm[:], op=ALU.add), r=[pgp, cm], w=[gm])
                for qt in range(NT):
                    C.op("dve", lambda e: e.max(out=m8[:, qt, :], in_=gm[:, qt, :]), r=[gm], w=[m8])
                C.op("dve", lambda e: e.tensor_tensor(out=sel[:], in0=gm[:], in1=ins_bc(m8[:, :, 2], 2, 16), op=ALU.is_ge), r=[gm, m8], w=[sel])
                C.op("dve", lambda e: e.tensor_tensor(out=sel[:], in0=sel[:], in1=om[:], op=ALU.max), r=[sel, om], w=[sel])
                C.op("dve", lambda e: e.tensor_scalar(out=nbx[:, :, 64:80], in0=sel[:], scalar1=1.0, scalar2=-NEG, op0=ALU.subtract, op1=ALU.mult), r=[sel], w=[nbx])
                for r4 in range(4):
                    for j in range(8):
                        qt = r4 * 8 + j
                        C.op("pe", lambda e: e.transpose(out=pntv[0:80, j, :], in_=nbx[:, qt, :], identity=ident[:]), r=[nbx, ident], w=[pgp])
                    C.op("act", lambda e: e.activation(out=QT_[64:80, r4 * 1024:(r4 + 1) * 1024], in_=pntv[64:80, :, :].rearrange("n j q -> n (j q)"), func=AF.Copy), r=[pgp], w=[QT_])
                steps_ = [(G, st) for G in range(8) for st in range(4 * G + 4)]

                def qk(G, st):
                    nonlocal si
                    P = pss[si % 3]; si += 1
                    ss_ = slice(st * 128, (st + 1) * 128); qs = slice(G * 512, (G + 1) * 512)
                    diag = st >= 4 * G
                    C.op("pe", lambda e: e.matmul(P[:], lhsT=KT_[:, ss_], rhs=QT_[:, qs], start=True, stop=not diag), r=[KT_, QT_], w=[P])
                    if diag:
                        C.op("pe", lambda e: e.matmul(P[:], lhsT=ident[:], rhs=CAUS[:, st - 4 * G, :, :], start=False, stop=True), r=[ident, CAUS], w=[P])
                    return P

                Pq = [qk(*steps_[0]), qk(*steps_[1])]
                for k_, (G, st) in enumerate(steps_):
                    if k_ + 2 < len(steps_):
                        Pq.append(qk(*steps_[k_ + 2]))
                    Pc = Pq.pop(0)
                    PT = PTs[pi % 3]; pi += 1
                    C.op("act", lambda e: e.activation(out=PT[:], in_=Pc[:], func=AF.Exp, scale=0.125), r=[Pc], w=[PT])
                    for qi in range(4):
                        qt = 4 * G + qi
                        if qt < st:
                            continue
                        C.op("pe", lambda e: e.matmul(pacc[qi][:, 0:65], lhsT=PT[:, qi * 128:(qi + 1) * 128], rhs=Va[:, st, :], start=(st == 0), stop=(st == qt)), r=[PT, Va], w=[pacc[qi]])
                    if st == 4 * G + 3:
                        for qi in range(4):
                            qt = 4 * G + qi
                            O_ = ob[oi % 3]; oi += 1
                            rz = rzs[ri % 2]; ri += 1
                            C.op("dve", lambda e: e.reciprocal(out=rz[:], in_=pacc[qi][:, 64:65]), r=[pacc[qi]], w=[rz])
                            C.op("act", lambda e: e.activation(out=O_[:], in_=pacc[qi][:, 0:64], func=AF.Copy, scale=rz[:]), r=[pacc[qi], rz], w=[O_])
                            store(OM[qt * 128:(qt + 1) * 128, 512 + h * 64:512 + (h + 1) * 64], O_[:], O_)

    def phase_A3(L, w_out_ap, x_src, x_dst):
        with C.phase():
            Wo = C.sb("Wo", [128, 8, D], BF16); load_w(Wo, w_out_ap, 8, D)
            g = C.sb("g", [128, 8], F32); load_gain(g, inp["norm_mem"][L])
            Wq = C.sb("Wq", [128, 8, 512], BF16); load_w(Wq, inp["mem_w_q"][L], 8, 512, g)
            Wom = C.sb("Wom", [128, 4, D], BF16); load_w(Wom, inp["mem_w_o"][L], 4, D)
            gqn = bgain(inp["mem_q_norm"][L], 128); gkn = bgain(inp["mem_k_norm"][L], 128)
            mK = C.sb("mK", [128, 4, 256], BF16); mV = C.sb("mV", [128, 2, 4, 129], BF16)
            C.op("pool", lambda e: e.memset(mV[:], 1.0), w=[mV])
            nt = NormT(); hnm = HeadNorm(4, 128)
            hT = C.sb("hT", [128, 8, 128], BF16)
            po = [C.ps("po", [128, 512], F32) for _ in range(2)]
            pr = C.sb("pr", [128, 512], F32); qn = C.sb("qn", [128, 512], BF16)
            ptq8 = C.ps("ptq", [128, 8, 128], BF16)
            ptq = T(ptq8.t, "x"); ptq.b = ptq8.b
            ptq_ap = ptq8[:, 0:4, :]
            with C.phase():
                gs = C.sb("gs", [128, 8], F32); load_gain(gs, inp["norm_mem_src"][L])
                Wkv = C.sb("Wkv", [128, 8, D], BF16); load_w(Wkv, inp["mem_w_kv"][L], 8, D, gs)
                mt_ = C.sb("mt", [128, D], F32)
                for mt in range(2):
                    C.dma("sp", mt_[:], mem_in[mt * 128:(mt + 1) * 128, :], w=[mt_])
                    nt.run(mt_, hT[:], hT)
                    for c in range(2):
                        P = po[c]
                        for kt in range(8):
                            C.op("pe", lambda e: e.matmul(P[:], lhsT=hT[:, kt, :], rhs=Wkv[:, kt, c * 512:(c + 1) * 512], start=(kt == 0), stop=(kt == 7)), r=[hT, Wkv], w=[P])
                        if c == 0:
                            C.op("act", lambda e: e.activation(out=pr[:], in_=P[:], func=AF.Copy), r=[P], w=[pr])
                            hnm.run(pr[:].rearrange("p (h d) -> p h d", h=4), pr, 4, gkn, qn[:].rearrange("p (h d) -> p h d", h=4), qn)
                            for h in range(4):
                                C.op("pe", lambda e: e.transpose(out=ptq[:, h, :], in_=qn[:, h * 128:(h + 1) * 128], identity=ident[:]), r=[qn, ident], w=[ptq])
                            C.op("dve", lambda e: e.tensor_copy(out=mK[:, :, mt * 128:(mt + 1) * 128], in_=ptq_ap), r=[ptq], w=[mK])
                        else:
                            C.op("act", lambda e: e.activation(out=mV[:, mt, :, 0:128], in_=P[:].rearrange("p (h d) -> p h d", h=4), func=AF.Copy), r=[P], w=[mV])
            pT = nt.pT
            lanes = []
            for ln in range(2):
                Lb = K()
                Lb.nt = nt if ln == 0 else NormT(pT=pT)
                Lb.hnm = hnm if ln == 0 else HeadNorm(4, 128)
                Lb.ot = C.sb("ot", [128, D], BF16); Lb.xt = C.sb("xt", [128, D], F32)
                Lb.oT = C.sb("oT", [128, 8, 128], BF16); Lb.x1 = C.sb("x1", [128, D], F32); Lb.x2 = C.sb("x2", [128, D], F32)
                Lb.hT = hT if ln == 0 else C.sb("hT", [128, 8, 128], BF16)
                Lb.pr = pr if ln == 0 else C.sb("pr", [128, 512], F32)
                Lb.qn = qn if ln == 0 else C.sb("qn", [128, 512], BF16)
                Lb.qT = C.sb("qT", [128, 4, 128], BF16)
                Lb.po = po[ln]
                Lb.pss = C.ps("pss", [128, 512], F32)
                Lb.PTm = [C.sb("PTm", [128, 512], BF16) for _ in range(2)]
                Lb.pov = C.ps("pov", [128, 2, 256], F32)
                Lb.rz = [C.sb("rz", [128, 1], F32) for _ in range(2)]
                Lb.omx = C.sb("omx", [128, 512], BF16); Lb.omT = C.sb("omT", [128, 4, 128], BF16)
                lanes.append(Lb)

            def gen_tile(Lb, t):
                ts = slice(t * 128, (t + 1) * 128)
                O_ = Lb.ot; X = Lb.xt; X2 = Lb.x2; x1 = Lb.x1; oT = Lb.oT; P = Lb.po
                C.dma("sp", O_[:], OM[ts, :], w=[O_])
                C.dma("sp", X[:], x_src[ts, :], w=[X])
                for kt in range(8):
                    C.op("pe", lambda e: e.transpose(out=pT[:, kt, :], in_=O_[:, kt * 128:(kt + 1) * 128], identity=ident[:]), r=[O_, ident], w=[pT])
                C.op("dve", lambda e: e.tensor_copy(out=oT[:], in_=pT[:]), r=[pT], w=[oT])
                yield
                for c in range(2):
                    for kt in range(8):
                        C.op("pe", lambda e: e.matmul(P[:], lhsT=oT[:, kt, :], rhs=Wo[:, kt, c * 512:(c + 1) * 512], start=(kt == 0), stop=(kt == 7)), r=[oT, Wo], w=[P])
                    C.op("dve", lambda e: e.tensor_tensor(out=x1[:, c * 512:(c + 1) * 512], in0=P[:], in1=X[:, c * 512:(c + 1) * 512], op=ALU.add), r=[P, X], w=[x1])
                    yield
                Lb.nt.run(x1, Lb.hT[:], Lb.hT)
                yield
                for kt in range(8):
                    C.op("pe", lambda e: e.matmul(P[:], lhsT=Lb.hT[:, kt, :], rhs=Wq[:, kt, :], start=(kt == 0), stop=(kt == 7)), r=[Lb.hT, Wq], w=[P])
                C.op("act", lambda e: e.activation(out=Lb.pr[:], in_=P[:], func=AF.Copy), r=[P], w=[Lb.pr])
                yield
                Lb.hnm.run(Lb.pr[:].rearrange("p (h d) -> p h d", h=4), Lb.pr, 4, gqn, Lb.qn[:].rearrange("p (h d) -> p h d", h=4), Lb.qn)
                yield
                for h in range(4):
                    C.op("pe", lambda e: e.transpose(out=ptq[:, h, :], in_=Lb.qn[:, h * 128:(h + 1) * 128], identity=ident[:]), r=[Lb.qn, ident], w=[ptq])
                C.op("dve", lambda e: e.tensor_copy(out=Lb.qT[:], in_=ptq_ap), r=[ptq], w=[Lb.qT])
                yield
                for mt in range(2):
                    PS = Lb.pss
                    for h in range(4):
                        C.op("pe", lambda e: e.matmul(PS[:, h * 128:(h + 1) * 128], lhsT=mK[:, h, mt * 128:(mt + 1) * 128], rhs=Lb.qT[:, h, :], start=True, stop=True), r=[mK, Lb.qT], w=[PS])
                    C.op("act", lambda e: e.activation(out=Lb.PTm[mt][:], in_=PS[:], func=AF.Exp, scale=float(128 ** -0.5)), r=[PS], w=[Lb.PTm[mt]])
                    yield
                for h in range(4):
                    A = Lb.pov
                    for mt in range(2):
                        C.op("pe", lambda e: e.matmul(A[:, h % 2, 0:129], lhsT=Lb.PTm[mt][:, h * 128:(h + 1) * 128], rhs=mV[:, mt, h, :], start=(mt == 0), stop=(mt == 1)), r=[Lb.PTm[mt], mV], w=[A])
                    rz = Lb.rz[h % 2]
                    C.op("dve", lambda e: e.reciprocal(out=rz[:], in_=A[:, h % 2, 128:129]), r=[A], w=[rz])
                    C.op("act", lambda e: e.activation(out=Lb.omx[:, h * 128:(h + 1) * 128], in_=A[:, h % 2, 0:128], func=AF.Copy, scale=rz[:]), r=[A, rz], w=[Lb.omx])
                    yield
                for h in range(4):
                    C.op("pe", lambda e: e.transpose(out=ptq[:, h, :], in_=Lb.omx[:, h * 128:(h + 1) * 128], identity=ident[:]), r=[Lb.omx, ident], w=[ptq])
                C.op("dve", lambda e: e.tensor_copy(out=Lb.omT[:], in_=ptq_ap), r=[ptq], w=[Lb.omT])
                yield
                for c in range(2):
                    for kt in range(4):
                        C.op("pe", lambda e: e.matmul(P[:], lhsT=Lb.omT[:, kt, :], rhs=Wom[:, kt, c * 512:(c + 1) * 512], start=(kt == 0), stop=(kt == 3)), r=[Lb.omT, Wom], w=[P])
                    C.op("dve", lambda e: e.tensor_tensor(out=X2[:, c * 512:(c + 1) * 512], in0=P[:], in1=x1[:, c * 512:(c + 1) * 512], op=ALU.add), r=[P, x1], w=[X2])
                    yield
                store(x_dst[ts, :], X2[:], X2)
                yield

            def lane_gen(ln):
                if ln == 1:
                    for _ in range(9):
                        yield
                for t in range(ln, NT, 2):
                    yield from gen_tile(lanes[ln], t)

            run_merged(lane_gen(0), 1, lane_gen(1), 1)

    def phase_A4(L, x_src, x_dst):
        with C.phase():
            g = C.sb("g", [128, 8], F32); load_gain(g, inp["norm_ffn"][L])
            Wi = C.sb("Wi", [128, 8, 2 * DFF], BF16); load_w(Wi, inp["ffn_w_in"][L], 8, 2 * DFF, g)
            Wd = C.sb("Wd", [128, 22, D], BF16); load_w(Wd, inp["ffn_w_out"][L], 22, D)
            nt = NormT()
            xt = [C.sb("xt", [128, D], F32) for _ in range(4)]
            hT = C.sb("hT", [128, 8, 512], BF16)
            hid = C.sb("hid", [128, 22, 512], BF16)
            psg = [C.ps("psg", [128, 512], F32) for _ in range(2)]
            psu = [C.ps("psu", [128, 512], F32) for _ in range(2)]
            po = [C.ps("po", [128, 512], F32) for _ in range(2)]
            sg = [C.sb("sg", [128, 512], BF16) for _ in range(2)]
            oi = 0
            for G in range(8):
                for tt in range(4):
                    t = G * 4 + tt
                    C.dma("sp", xt[tt][:], x_src[t * 128:(t + 1) * 128, :], w=[xt[tt]])
                    nt.run(xt[tt], hT[:, :, tt * 128:(tt + 1) * 128], hT)
                for ft in range(22):
                    Pg = psg[ft % 2]; Pu = psu[ft % 2]; Sg = sg[ft % 2]
                    for kt in range(8):
                        C.op("pe", lambda e: e.matmul(Pg[:], lhsT=Wi[:, kt, ft * 128:(ft + 1) * 128], rhs=hT[:, kt, :], start=(kt == 0), stop=(kt == 7)), r=[Wi, hT], w=[Pg])
                    for kt in range(8):
                        C.op("pe", lambda e: e.matmul(Pu[:], lhsT=Wi[:, kt, DFF + ft * 128:DFF + (ft + 1) * 128], rhs=hT[:, kt, :], start=(kt == 0), stop=(kt == 7)), r=[Wi, hT], w=[Pu])
                    C.op("act", lambda e: e.activation(out=Sg[:], in_=Pg[:], func=AF.Silu), r=[Pg], w=[Sg])
                    C.op("dve", lambda e: e.tensor_tensor(out=hid[:, ft, :], in0=Sg[:], in1=Pu[:], op=ALU.mult), r=[Sg, Pu], w=[hid])
                for tt in range(4):
                    t = G * 4 + tt
                    XO = xt[tt]
                    for c in range(2):
                        P = po[c]
                        for ft in range(22):
                            C.op("pe", lambda e: e.matmul(P[:], lhsT=hid[:, ft, tt * 128:(tt + 1) * 128], rhs=Wd[:, ft, c * 512:(c + 1) * 512], start=(ft == 0), stop=(ft == 21)), r=[hid, Wd], w=[P])
                        C.op("dve", lambda e: e.tensor_tensor(out=XO[:, c * 512:(c + 1) * 512], in0=P[:], in1=xt[tt][:, c * 512:(c + 1) * 512], op=ALU.add), r=[P, xt[tt]], w=[XO])
                    store(x_dst[t * 128:(t + 1) * 128, :], XO[:], XO)


    Q_T = SCR("Q_T", [16, 64, S], BF16); QR_T = SCR("QR_T", [16, 64, S], BF16)
    KCr_T = SCR("KCr_T", [4, 64, S], BF16); VCr_T = SCR("VCr_T", [4, 64, S], BF16)
    KS_T = SCR("KS_T", [4, 64, S], BF16); VS = SCR("VS", [S, 256], BF16)
    KW_T = SCR("KW_T", [4, 64, S], BF16); VW = SCR("VW", [S, 256], BF16)
    GT = SCR("GT", [S, 48], F32)
    KC_T = C.sb("KC_T", [128, 4, 256], BF16, stack=C.es)
    VCa = C.sb("VCa", [128, 2, 4, 129], BF16, stack=C.es)

    def phase_B0():
        with C.phase():
            g = C.sb("g", [128, 8], F32); load_gain(g, inp["norm_mix"][1])
            W = C.sb("W1", [128, 8, 2608], BF16)
            load_w(W, inp["nsa_w_in"], 8, 2608, g)
            gq = bgain(inp["nsa_q_norm"]); gks = bgain(inp["nsa_ksel_norm"]); gkw = bgain(inp["nsa_kwin_norm"])
            hnm = HeadNorm(8, 64, "pool")
            pr = [C.sb("pr", [128, 512], F32) for _ in range(2)]
            qn = [C.sb("qn", [128, 512], BF16) for _ in range(3)]
            qn2 = [C.sb("qn2", [128, 512], BF16) for _ in range(3)]
            ptq = [C.ps("ptq", [64, 8, 128], BF16) for _ in range(2)]
            qT = [C.sb("qT", [64, 8, 128], BF16) for _ in range(3)]
            chunks = [(0, 512), (512, 1024), (1024, 1536), (1536, 2048), (2048, 2560), (2560, 2608)]
            cnt = {"ti": 0}

            def tr_heads(SRC, H, src_off, dst_ap):
                i = cnt["ti"]; cnt["ti"] += 1
                PT = ptq[i % 2]; QT = qT[i % 3]
                for h in range(H):
                    C.op("pe", lambda e: e.transpose(out=PT[:, h, :], in_=SRC[:, src_off + h * 64: src_off + (h + 1) * 64], identity=ident[:]), r=[SRC, ident], w=[PT])
                C.op("dve", lambda e: e.tensor_copy(out=QT[:, 0:H, :], in_=PT[:, 0:H, :]), r=[PT], w=[QT])
                store(dst_ap, QT[:, 0:H, :], QT)

            def make_job(k, t, c, P):
                R = pr[k % 2]; Q = qn[k % 3]; Q2 = qn2[k % 3]
                ts = slice(t * 128, (t + 1) * 128)
                cs8 = cs64[:, t, :]; sn8 = sn64[:, t, :]
                v8 = lambda A_: A_[:].rearrange("p (h d) -> p h d", h=8)
                v4 = lambda A_: A_[:, 0:256].rearrange("p (h d) -> p h d", h=4)

                def post():
                    if c == 5:
                        C.op("act", lambda e: e.activation(out=R[:, 0:48], in_=P[:, 0:48], func=AF.Exp, scale=-1.0), r=[P], w=[R])
                        C.op("dve", lambda e: e.tensor_scalar(out=R[:, 0:48], in0=R[:, 0:48], scalar1=1.0, scalar2=None, op0=ALU.add), r=[R], w=[R])
                        C.op("dve", lambda e: e.reciprocal(out=R[:, 0:48], in_=R[:, 0:48]), r=[R], w=[R])
                        store(GT[ts, :], R[:, 0:48], R)
                        return
                    C.op("act", lambda e: e.activation(out=R[:], in_=P[:], func=AF.Copy), r=[P], w=[R])
                    if c in (0, 1):
                        hnm.run(v8(R), R, 8, gq, v8(Q), Q, rope=(cs8, sn8, 8), out2_ap=v8(Q2), out2T=Q2)
                    elif c == 2:
                        C.op("act", lambda e: e.activation(out=Q[:], in_=R[:], func=AF.Copy), r=[R], w=[Q])
                    else:
                        gg = gks if c == 3 else gkw
                        hnm.run(v4(R), R, 4, gg, v4(Q), Q, rope=(cs8, sn8, 8))
                        C.op("act", lambda e: e.activation(out=Q[:, 256:512], in_=R[:, 256:512], func=AF.Copy), r=[R], w=[Q])

                def trans():
                    if c == 5:
                        return
                    if c in (0, 1):
                        tr_heads(Q, 8, 0, QR_T[c * 8:(c + 1) * 8, :, ts].rearrange("h d s -> d h s"))
                        tr_heads(Q2, 8, 0, Q_T[c * 8:(c + 1) * 8, :, ts].rearrange("h d s -> d h s"))
                    elif c == 2:
                        tr_heads(Q, 4, 0, KCr_T[:, :, ts].rearrange("h d s -> d h s"))
                        tr_heads(Q, 4, 256, VCr_T[:, :, ts].rearrange("h d s -> d h s"))
                    else:
                        store((VS if c == 3 else VW)[ts, :], Q[:, 256:512], Q)
                        tr_heads(Q, 4, 0, (KS_T if c == 3 else KW_T)[:, :, ts].rearrange("h d s -> d h s"))
                return post, trans

            run_proj(XB, W, chunks, make_job)

    def phase_B1():
        with C.phase():
            C.op("pool", lambda e: e.memset(KC_T[:], 0.0), w=[KC_T])
            C.op("pool", lambda e: e.memset(VCa[:], 0.0), w=[VCa])
            C.op("pool", lambda e: e.memset(VCa[:, :, :, 64:65], 1.0), w=[VCa])
            cov = C.sb("cov", [128, 2, 64], F32)
            C.op("pool", lambda e: e.memset(cov[:], 1.0), w=[cov])
            for half in range(2):
                C.op("pool", lambda e: e.affine_select(out=cov[:, half, :], in_=cov[:, half, :], pattern=[[-64, 64]], compare_op=ALU.is_gt, fill=0.0, base=2048 * half + 32, channel_multiplier=16), r=[cov], w=[cov])
                C.op("pool", lambda e: e.affine_select(out=cov[:, half, :], in_=cov[:, half, :], pattern=[[64, 64]], compare_op=ALU.is_gt, fill=0.0, base=64 - 2048 * half, channel_multiplier=-16), r=[cov], w=[cov])
                C.op("dve", lambda e: e.tensor_copy(out=VCa[:, half, :, 65:129], in_=ins_bc(cov[:, half, :], 1, 4)), r=[cov], w=[VCa])
            gkc = bgain(inp["nsa_kcmp_norm"])
            hnm = HeadNorm(1, 64)
            XT = C.sb("XT", [64, 4, S], BF16)
            w1f = C.sb("w1f", [64, 32, 64], F32); w1b = C.sb("w1b", [64, 32, 64], BF16)
            w2f = C.sb("w2f", [64, 64], F32); w2b = C.sb("w2b", [64, 64], BF16)
            posf = C.sb("posf", [64, 32], F32); posrep = C.sb("posrep", [64, 32, 128], BF16)
            cpos = C.sb("cpos", [128, 64], F32)
            ph = [C.ps("ph", [128, 512], F32) for _ in range(2)]
            pt_ = C.ps("pt_", [64, 8, 128], BF16)
            hs = C.sb("hs", [128, 64], F32); hsb = C.sb("hsb", [128, 64], BF16); hsT = C.sb("hsT", [64, 128], BF16)
            o2 = C.sb("o2", [128, 64], F32); o2b = C.sb("o2b", [128, 64], BF16)
            pi = 0
            for kv in range(2):
                src = (KCr_T, VCr_T)[kv]
                C.dma("sp", XT[:], src.rearrange("g d s -> d g s"), w=[XT])
                C.dma("sp", w1f[:], inp[("nsa_cmp_w1_k", "nsa_cmp_w1_v")[kv]].rearrange("(l d) o -> d l o", d=64), w=[w1f])
                C.dma("sp", w2f[:], inp[("nsa_cmp_w2_k", "nsa_cmp_w2_v")[kv]], w=[w2f])
                C.dma("sp", posf[:], inp[("nsa_cmp_pos_k", "nsa_cmp_pos_v")[kv]].rearrange("l d -> d l"), w=[posf], allow_slow_non_contiguous=True)
                C.op("dve", lambda e: e.tensor_copy(out=w1b[:], in_=w1f[:]), r=[w1f], w=[w1b])
                C.op("dve", lambda e: e.tensor_copy(out=w2b[:], in_=w2f[:]), r=[w2f], w=[w2b])
                C.op("dve", lambda e: e.tensor_copy(out=posrep[:], in_=ins_bc(posf[:], 2, 128)), r=[posf], w=[posrep])
                P = ph[pi % 2]; pi += 1
                for l in range(32):
                    C.op("pe", lambda e: e.matmul(P[:, 0:64], lhsT=posrep[:, l, :], rhs=w1b[:, l, :], start=(l == 0), stop=(l == 31)), r=[posrep, w1b], w=[P])
                C.op("dve", lambda e: e.tensor_copy(out=cpos[:], in_=P[:, 0:64]), r=[P], w=[cpos])
                for g_ in range(4):
                    for half in range(2):
                        M = 128 if half == 0 else 127
                        P = ph[pi % 2]; pi += 1
                        for l in range(32):
                            a0 = 2048 * half + l
                            C.op("pe", lambda e: e.matmul(P[0:M, 0:64], lhsT=XT[:, g_, a0:a0 + 16 * (M - 1) + 1:16], rhs=w1b[:, l, :], start=(l == 0), stop=(l == 31)), r=[XT, w1b], w=[P])
                        C.op("dve", lambda e: e.tensor_tensor(out=hs[0:M, :], in0=P[0:M, 0:64], in1=cpos[0:M, :], op=ALU.add), r=[P, cpos], w=[hs])
                        C.op("act", lambda e: e.activation(out=hsb[0:M, :], in_=hs[0:M, :], func=AF.Silu), r=[hs], w=[hsb])
                        C.op("pe", lambda e: e.transpose(out=pt_[:, 0, 0:M], in_=hsb[0:M, :], identity=ident[0:M, 0:M]), r=[hsb, ident], w=[pt_])
                        C.op("dve", lambda e: e.tensor_copy(out=hsT[:, 0:M], in_=pt_[:, 0, 0:M]), r=[pt_], w=[hsT])
                        P2 = ph[pi % 2]; pi += 1
                        C.op("pe", lambda e: e.matmul(P2[0:M, 0:64], lhsT=hsT[:, 0:M], rhs=w2b[:], start=True, stop=True), r=[hsT, w2b], w=[P2])
                        if kv == 0:
                            C.op("act", lambda e: e.activation(out=o2[0:M, :], in_=P2[0:M, 0:64], func=AF.Copy), r=[P2], w=[o2])
                            hnm.run(o2[:].rearrange("p (h d) -> p h d", h=1), o2, 1, gkc, o2b[:].rearrange("p (h d) -> p h d", h=1), o2b)
                            C.op("pe", lambda e: e.transpose(out=pt_[:, 1, 0:M], in_=o2b[0:M, :], identity=ident[0:M, 0:M]), r=[o2b, ident], w=[pt_])
                            C.op("dve", lambda e: e.tensor_copy(out=KC_T[0:64, g_, half * 128:half * 128 + M], in_=pt_[:, 1, 0:M]), r=[pt_], w=[KC_T])
                        else:
                            C.op("act", lambda e: e.activation(out=VCa[0:M, half, g_, 0:64], in_=P2[0:M, 0:64], func=AF.Copy), r=[P2], w=[VCa])

    def phase_B2():
        with C.phase():
            KS = C.sb("KS", [128, 4, S], BF16); C.dma("sp", KS[0:64, :, :], KS_T.rearrange("g d s -> d g s"), w=[KS])
            KW = C.sb("KW", [128, 4, S], BF16)
            C.op("pool", lambda e: e.memset(KW[64:128, :, :], 0.0), w=[KW])
            C.dma("sp", KW[0:64, :, :], KW_T.rearrange("g d s -> d g s"), w=[KW])
            VSa = C.sb("VSa", [128, NT, 4, 65], BF16); VWa = C.sb("VWa", [128, NT, 4, 65], BF16)
            C.op("pool", lambda e: e.memset(VSa[:], 1.0), w=[VSa])
            C.op("pool", lambda e: e.memset(VWa[:], 1.0), w=[VWa])
            for t in range(NT):
                C.dma("sp", VSa[:, t, :, 0:64], VS[t * 128:(t + 1) * 128, :].rearrange("p (g d) -> p g d", g=4), w=[VSa])
                C.dma("pool", VWa[:, t, :, 0:64], VW[t * 128:(t + 1) * 128, :].rearrange("p (g d) -> p g d", g=4), w=[VWa])
            with C.phase():
                Ef = C.sb("E2f", [128, S], F32)
                C.op("pool", lambda e: e.memset(Ef[64:128, :], 1.0), w=[Ef])
                C.op("pool", lambda e: e.affine_select(out=Ef[64:128, :], in_=Ef[64:128, :], pattern=[[1, S]], compare_op=ALU.is_ge, fill=0.0, base=0, channel_multiplier=-64), r=[Ef], w=[Ef])
                C.op("pool", lambda e: e.affine_select(out=Ef[64:128, :], in_=Ef[64:128, :], pattern=[[-1, S]], compare_op=ALU.is_ge, fill=0.0, base=63, channel_multiplier=64), r=[Ef], w=[Ef])
                for g_ in range(4):
                    C.op("dve" if g_ % 2 == 0 else "act", (lambda e: e.tensor_copy(out=KS[64:128, g_, :], in_=Ef[64:128, :])) if g_ % 2 == 0 else (lambda e: e.activation(out=KS[64:128, g_, :], in_=Ef[64:128, :], func=AF.Copy)), r=[Ef], w=[KS])
            V0 = C.sb("V0", [128, 128], F32)
            C.op("pool", lambda e: e.iota(out=V0[:], pattern=[[-1, 128]], base=0, channel_multiplier=16, allow_small_or_imprecise_dtypes=True), w=[V0])
            J = C.sb("J", [128, 64], F32)
            C.op("pool", lambda e: e.iota(out=J[:], pattern=[[1, 64]], base=0, channel_multiplier=0, allow_small_or_imprecise_dtypes=True), w=[J])
            f0 = C.sb("f0", [128, 64], F32)
            C.op("dve", lambda e: e.tensor_scalar(out=f0[:], in0=J[:], scalar1=0.0, scalar2=None, op0=ALU.is_equal), r=[J], w=[f0])
            CURb = C.sb("CURb", [128, 1], F32)
            C.op("pool", lambda e: e.iota(out=CURb[:], pattern=[[0, 1]], base=0, channel_multiplier=1, allow_small_or_imprecise_dtypes=True), w=[CURb])
            C.op("dve", lambda e: e.tensor_scalar(out=CURb[:], in0=CURb[:], scalar1=64.0, scalar2=None, op0=ALU.is_ge), r=[CURb], w=[CURb])
            sm = {n: C.sb(n, [128, 1], F32) for n in ("curq", "cm1")}
            rz4s = [C.sb("rz4", [128, 4], F32) for _ in range(2)]; cf4s = [C.sb("cf4", [128, 4], F32) for _ in range(2)]
            f1 = C.sb("f1", [128, 64], F32); f2 = C.sb("f2", [128, 64], F32)
            FORCE = C.sb("FORCE", [128, 64], F32); FUT = C.sb("FUT", [128, 64], F32)
            imp = C.sb("imp", [128, 64], F32); imp3 = C.sb("imp3", [128, 64], F32); impr = C.sb("impr", [128, 64], F32)
            imt = C.sb("imt", [128, 4, 64], F32)
            m8a = C.sb("m8a", [128, 8], F32); m8b = C.sb("m8b", [128, 8], F32)
            nm = C.sb("nm", [128, 128], BF16)
            C.op("pool", lambda e: e.memset(nm[:], 0.0), w=[nm])
            pnm = C.ps("pnm", [128, 8, 128], BF16)
            cb = [C.sb("cb", [128, 4, 128], BF16) for _ in range(2)]
            qnt = [C.sb("qnt", [128, 16, 128], BF16) for _ in range(2)]
            qrt = [C.sb("qrt", [128, 16, 128], BF16) for _ in range(2)]
            for t_ in qnt + qrt:
                C.op("pool", lambda e: e.memset(t_[:], 0.0), w=[t_])
            gts = [C.sb("gts", [128, 48], F32) for _ in range(2)]
            acc = C.sb("acc", [128, D], F32)
            obf = [C.sb("obf", [128, D], BF16) for _ in range(2)]
            pss = [C.ps("pss", [128, 512], F32) for _ in range(3)]
            pacc = C.ps("pacc", [128, 4, 512], F32)
            PTs = [C.sb("PTs", [128, 512], BF16) for _ in range(3)]
            si = 0; pi = 0; zi = 0
            for qt in range(NT):
                ts = slice(qt * 128, (qt + 1) * 128)
                QN = qnt[qt % 2]; QR = qrt[qt % 2]; Gt = gts[qt % 2]; OB = obf[qt % 2]
                C.dma("sp", QN[0:64, :, :], Q_T[:, :, ts].rearrange("h d s -> d h s"), w=[QN])
                C.dma("sp", QR[0:64, :, :], QR_T[:, :, ts].rearrange("h d s -> d h s"), w=[QR])
                C.dma("sp", Gt[:], GT[ts, :], w=[Gt])
                nhalf = 1 if qt < 16 else 2
                for half in range(nhalf):
                    C.op("dve", lambda e: e.tensor_scalar(out=cb[half][:], in0=ins_bc(V0[:], 1, 4), scalar1=float(128 * qt - 31 - 2048 * half), scalar2=NEG, op0=ALU.is_gt, op1=ALU.mult), r=[V0], w=[cb[half]])
                C.op("dve", lambda e: e.tensor_scalar(out=sm["curq"][:], in0=CURb[:], scalar1=float(2 * qt), scalar2=None, op0=ALU.add), r=[CURb], w=[sm["curq"]])
                C.op("dve", lambda e: e.tensor_scalar(out=sm["cm1"][:], in0=CURb[:], scalar1=float(2 * qt - 1), scalar2=None, op0=ALU.add), r=[CURb], w=[sm["cm1"]])
                C.op("dve", lambda e: e.tensor_scalar(out=f1[:], in0=J[:], scalar1=sm["curq"][:], scalar2=None, op0=ALU.is_equal), r=[J, sm["curq"]], w=[f1])
                C.op("dve", lambda e: e.tensor_scalar(out=f2[:], in0=J[:], scalar1=sm["cm1"][:], scalar2=None, op0=ALU.is_equal), r=[J, sm["cm1"]], w=[f2])
                C.op("dve", lambda e: e.tensor_tensor(out=f1[:], in0=f1[:], in1=f2[:], op=ALU.add), r=[f1, f2], w=[f1])
                C.op("dve", lambda e: e.tensor_tensor(out=f1[:], in0=f1[:], in1=f0[:], op=ALU.add), r=[f1, f0], w=[f1])
                C.op("dve", lambda e: e.tensor_scalar(out=FORCE[:], in0=f1[:], scalar1=1e4, scalar2=None, op0=ALU.mult), r=[f1], w=[FORCE])
                C.op("dve", lambda e: e.tensor_scalar(out=FUT[:], in0=J[:], scalar1=sm["curq"][:], scalar2=-1e30, op0=ALU.is_gt, op1=ALU.mult), r=[J, sm["curq"]], w=[FUT])

                def branch(g_, qk_fn, sts, Vrhs_fn, ncol, gidx, first):
                    nonlocal si, pi, zi
                    n = len(sts)

                    def qk(st):
                        nonlocal si
                        P = pss[si % 3]; si += 1
                        mm = qk_fn(st)
                        for ei, (l_ap, lT, r_ap, rT) in enumerate(mm):
                            C.op("pe", lambda e: e.matmul(P[:], lhsT=l_ap, rhs=r_ap, start=(ei == 0), stop=(ei == len(mm) - 1)), r=[lT, rT], w=[P])
                        return P

                    Pq = [qk(sts[0])]
                    if n > 1:
                        Pq.append(qk(sts[1]))
                    for ix, st in enumerate(sts):
                        if ix + 2 < n:
                            Pq.append(qk(sts[ix + 2]))
                        Pc = Pq.pop(0)
                        PT = PTs[pi % 3]; pi += 1
                        C.op("act", lambda e: e.activation(out=PT[:], in_=Pc[:], func=AF.Exp, scale=0.125), r=[Pc], w=[PT])
                        v_ap, vT = Vrhs_fn(st)
                        for j in range(4):
                            C.op("pe", lambda e: e.matmul(pacc[:, j, 0:ncol], lhsT=PT[:, j * 128:(j + 1) * 128], rhs=v_ap, start=(ix == 0), stop=(ix == n - 1)), r=[PT, vT], w=[pacc])
                    rz4 = rz4s[zi % 2]; cf4 = cf4s[zi % 2]; zi += 1
                    C.op("dve", lambda e: e.tensor_scalar(out=rz4[:], in0=pacc[:, :, 64], scalar1=1e-20, scalar2=None, op0=ALU.max), r=[pacc], w=[rz4])
                    C.op("dve", lambda e: e.reciprocal(out=rz4[:], in_=rz4[:]), r=[rz4], w=[rz4])
                    C.op("dve", lambda e: e.tensor_tensor(out=cf4[:], in0=rz4[:], in1=Gt[:, 12 * g_ + gidx:12 * g_ + gidx + 10:3], op=ALU.mult), r=[rz4, Gt], w=[cf4])
                    for j in range(4):
                        hd = 4 * g_ + j
                        ah = acc[:, hd * 64:(hd + 1) * 64]
                        if first:
                            C.op("act", lambda e: e.activation(out=ah, in_=pacc[:, j, 0:64], func=AF.Copy, scale=cf4[:, j:j + 1]), r=[pacc, cf4], w=[acc])
                        else:
                            C.op("dve", lambda e: e.scalar_tensor_tensor(out=ah, in0=pacc[:, j, 0:64], scalar=cf4[:, j:j + 1], in1=ah, op0=ALU.mult, op1=ALU.add), r=[pacc, cf4, acc], w=[acc])
                    if first:
                        C.op("dve", lambda e: e.tensor_tensor(out=imt[:], in0=pacc[:, :, 65:129], in1=ins_bc(rz4[:], 2, 64), op=ALU.mult), r=[pacc, rz4], w=[imt])
                        C.op("dve", lambda e: e.tensor_reduce(out=imp[:], in_=imt[:].rearrange("p j d -> p d j"), axis=AX.X, op=ALU.add), r=[imt], w=[imp])

                for g_ in range(4):
                    branch(g_, lambda st: [(KC_T[:, g_, st * 128:(st + 1) * 128], KC_T, QN[:, 4 * g_:4 * g_ + 4, :], QN), (ident[:], ident, cb[st][:], cb[st])],
                           list(range(nhalf)), lambda st: (VCa[:, st, g_, :], VCa), 129, 0, True)
                    C.op("dve", lambda e: e.tensor_tensor(out=imp3[:], in0=imp[:], in1=FORCE[:], op=ALU.max), r=[imp, FORCE], w=[imp3])
                    C.op("dve", lambda e: e.tensor_tensor(out=imp3[:], in0=imp3[:], in1=FUT[:], op=ALU.add), r=[imp3, FUT], w=[imp3])
                    C.op("dve", lambda e: e.max(out=m8a[:], in_=imp3[:]), r=[imp3], w=[m8a])
                    C.op("dve", lambda e: e.match_replace(out=impr[:], in_to_replace=m8a[:], in_values=imp3[:], imm_value=-1e30), r=[imp3, m8a], w=[impr])
                    C.op("dve", lambda e: e.max(out=m8b[:], in_=impr[:]), r=[impr], w=[m8b])
                    C.op("dve", lambda e: e.tensor_scalar(out=nm[:, 64:128], in0=imp3[:], scalar1=m8b[:, 7:8], scalar2=NEG, op0=ALU.is_lt, op1=ALU.mult), r=[imp3, m8b], w=[nm])
                    def selqk(st):
                        m = [(KS[:, g_, st * 128:(st + 1) * 128], KS, QR[:, 4 * g_:4 * g_ + 4, :], QR)]
                        if st == qt:
                            m.append((ident[:], ident, TRI4[:], TRI4))
                        return m

                    def winqk(st):
                        m = [(KW[:, g_, st * 128:(st + 1) * 128], KW, QR[:, 4 * g_:4 * g_ + 4, :], QR)]
                        if st == qt:
                            m.append((ident[:], ident, TRI4[:], TRI4))
                        elif st == qt - 4:
                            m.append((ident[:], ident, BLO4[:], BLO4))
                        return m
                    branch(g_, winqk, list(range(max(0, qt - 4), qt + 1)), lambda st: (VWa[:, st, g_, :], VWa), 65, 2, False)
                    C.op("pe", lambda e: e.transpose(out=pnm[:, 0, :], in_=nm[:], identity=ident[:]), r=[nm, ident], w=[pnm])
                    C.op("dve", lambda e: e.tensor_copy(out=QR[64:128, 4 * g_:4 * g_ + 4, :], in_=ins_bc(pnm[64:128, 0, :], 1, 4)), r=[pnm], w=[QR])

                    branch(g_, selqk, list(range(qt + 1)), lambda st: (VSa[:, st, g_, :], VSa), 65, 1, False)
                C.op("act", lambda e: e.activation(out=OB[:], in_=acc[:], func=AF.Copy), r=[acc], w=[OB])
                store(OM[ts, :], OB[:], OB)

    if upto >= 1:
        phase_A0()
    if upto >= 2:
        phase_A1()
    if upto >= 3:
        phase_A2()
    if upto >= 4:
        phase_A3(0, inp["ab_w_out"], x_in, XA)
    if upto >= 5:
        phase_A4(0, XA, XB if (dbg or upto > 5) else y_out)
    if upto >= 6:
        phase_B0()
    if upto >= 7:
        phase_B1()
    if upto >= 8:
        phase_B2()
    if upto >= 9:
        phase_A3(1, inp["nsa_w_out"], XB, XC)
    if upto >= 10:
        phase_A4(1, XC, y_out)
    C.close()
    k.nc = nc; k.dbg_names = dbg_names; k.ninstr = C.ninstr; k.inp_names = list(inp)
    return k


_W_NAMES = ["norm_mix", "norm_mem", "norm_mem_src", "norm_ffn", "mem_w_q", "mem_w_kv", "mem_w_o", "mem_q_norm", "mem_k_norm", "ffn_w_in", "ffn_w_out"]
_W0_NAMES = ["ab_w_in", "ab_w_out", "dsa_q_norm", "dsa_k_norm", "moba_q_norm", "moba_k_norm", "nsa_w_in", "nsa_w_out", "nsa_q_norm", "nsa_kcmp_norm",
             "nsa_ksel_norm", "nsa_kwin_norm", "nsa_cmp_pos_k", "nsa_cmp_pos_v", "nsa_cmp_w1_k", "nsa_cmp_w2_k", "nsa_cmp_w1_v", "nsa_cmp_w2_v"]


def make_in_map(inputs, b):
    m = {"x": np.ascontiguousarray(inputs["x"][b], dtype=np.float32),
         "mem": np.ascontiguousarray(inputs["mem"][b], dtype=np.float32),
         "post": np.ascontiguousarray(np.asarray(inputs["positions"][b]).astype(np.int32).reshape(NT, 128).T)}
    for n in _W_NAMES:
        m[n] = np.ascontiguousarray(inputs[n], dtype=np.float32)
    for n in _W0_NAMES:
        m[n] = np.ascontiguousarray(np.asarray(inputs[n])[0], dtype=np.float32)
    return m


def kernel(**inputs):
    from concourse.bass_utils import run_bass_kernel_spmd
    k = build(dbg=False)
    in_maps = [make_in_map(inputs, b) for b in range(8)]
    res = run_bass_kernel_spmd(k.nc, in_maps, core_ids=list(range(8)))
    return np.stack([np.asarray(r["y"], dtype=np.float32) for r in res.results], axis=0)
```

```python
from contextlib import ExitStack
import numpy as np
import concourse.bass as bass
import concourse.mybir as mybir

F32 = mybir.dt.float32
BF16 = mybir.dt.bfloat16
I32 = mybir.dt.int32
AF = mybir.ActivationFunctionType
ALU = mybir.AluOpType
AX = mybir.AxisListType

N_DMA_SLOTS = 24
N_HW_SLOTS = 14


class Buf:
    __slots__ = ("name", "w", "r")

    def __init__(self, name):
        self.name = name
        self.w = None
        self.r = {}


class T:
    def __init__(self, t, name):
        self.t = t
        self.b = Buf(name)

    def __getitem__(self, k):
        return self.t[k]


class Ctx:
    def __init__(self, nc):
        self.nc = nc
        self.es = ExitStack()
        self.eng = {"pe": nc.tensor, "act": nc.scalar, "dve": nc.vector, "pool": nc.gpsimd, "sp": nc.sync}
        self.sem = {}
        for k in ("pe", "act", "dve", "pool"):
            self.sem[k] = self.es.enter_context(nc.semaphore("sem_" + k))
        for i in range(N_DMA_SLOTS):
            self.sem[("d", i)] = self.es.enter_context(nc.semaphore("semd%d" % i))
        self.cnt = {k: 0 for k in ("pe", "act", "dve", "pool")}
        self.dval = [0] * N_DMA_SLOTS
        self.dnext = 0
        self.dnext_sw = 0
        self.seen = {k: {} for k in self.eng}
        self.uid = 0
        self.phase_stack = None
        self.ninstr = 0

    def _nm(self, name):
        self.uid += 1
        return "%s_%d" % (name, self.uid)

    def sb(self, name, shape, dtype, stack=None):
        st = stack if stack is not None else (self.phase_stack or self.es)
        nm = self._nm(name)
        return T(st.enter_context(self.nc.sbuf_tensor(nm, list(shape), dtype)), nm)

    def ps(self, name, shape, dtype, stack=None):
        st = stack if stack is not None else (self.phase_stack or self.es)
        nm = self._nm(name)
        return T(st.enter_context(self.nc.psum_tensor(nm, list(shape), dtype)), nm)

    def dram(self, name, shape, dtype, kind="Internal"):
        return self.nc.dram_tensor(name, list(shape), dtype, kind=kind).ap()

    def _waits(self, E, reads, writes):
        deps = {}

        def need(k, v):
            if v > deps.get(k, 0):
                deps[k] = v

        for t in reads:
            b = t.b
            if b.w is not None:
                need(*b.w)
        for t in writes:
            b = t.b
            if b.w is not None:
                need(*b.w)
            for k, v in b.r.items():
                need(k, v)
        eng = self.eng[E]
        seen = self.seen[E]
        for k, v in deps.items():
            if k == E and E == "pe":
                continue
            if seen.get(k, 0) >= v:
                continue
            eng.wait_ge(self.sem[k], v)
            self.ninstr += 1
            seen[k] = v

    def op(self, E, fn, r=(), w=()):
        self._waits(E, r, w)
        inst = fn(self.eng[E])
        self.cnt[E] += 1
        self.ninstr += 1
        inst.then_inc(self.sem[E], 1)
        ev = self.cnt[E]
        for t in r:
            if t.b.r.get(E, 0) < ev:
                t.b.r[E] = ev
        for t in w:
            t.b.w = (E, ev)
            t.b.r = {}
        return inst

    def dma(self, Q, out, in_, r=(), w=(), **kw):
        self._waits(Q, r, w)
        slot = self.dnext
        self.dnext = (self.dnext + 1) % N_DMA_SLOTS
        k = ("d", slot)
        eng = self.eng[Q]
        if self.seen[Q].get(k, 0) < self.dval[slot]:
            eng.wait_ge(self.sem[k], self.dval[slot])
            self.seen[Q][k] = self.dval[slot]
            self.ninstr += 1
        inst = eng.dma_start(out=out, in_=in_, **kw)
        self.dval[slot] += 16
        inst.then_inc(self.sem[k], 16)
        self.ninstr += 1
        v = self.dval[slot]
        for t in r:
            if t.b.r.get(k, 0) < v:
                t.b.r[k] = v
        for t in w:
            t.b.w = (k, v)
            t.b.r = {}
        return inst

    def barrier(self):
        for E, eng in self.eng.items():
            seen = self.seen[E]
            for k in ("pe", "act", "dve", "pool"):
                v = self.cnt[k]
                if v == 0 or seen.get(k, 0) >= v or (k == E and E == "pe"):
                    continue
                eng.wait_ge(self.sem[k], v)
                seen[k] = v
                self.ninstr += 1
            for i in range(N_DMA_SLOTS):
                k = ("d", i)
                v = self.dval[i]
                if v == 0 or seen.get(k, 0) >= v:
                    continue
                eng.wait_ge(self.sem[k], v)
                seen[k] = v
                self.ninstr += 1

    def phase(self):
        ctx = self

        class _P:
            def __enter__(s):
                ctx.barrier()
                s.prev = ctx.phase_stack
                s.st = ExitStack()
                ctx.phase_stack = s.st
                return s

            def __exit__(s, *a):
                ctx.barrier()
                ctx.phase_stack = s.prev
                s.st.close()
                return False

        return _P()

    def close(self):
        self.barrier()
        self.es.close()


S = 4096
D = 1024
NT = 32
NEG = -30000.0
EPS = 1e-6
THETA = 500000.0
DFF = 2816


def ins_bc(ap, axis, n):
    l = [list(p) for p in ap.ap]
    l.insert(axis, [0, n])
    return bass.AP(ap.tensor, ap.offset, l)


class K:
    pass


def build(dbg=False, upto=99):
    nc = bass.Bass("TRN2", target_bir_lowering=False)
    k = K()
    inp = {}

    def IN(name, shape, dt=F32):
        inp[name] = nc.dram_tensor(name, list(shape), dt, kind="ExternalInput").ap()
        return inp[name]

    x_in = IN("x", [S, D]); mem_in = IN("mem", [256, D]); pos_in = IN("post", [128, NT], I32)
    for n in ("norm_mix", "norm_mem", "norm_mem_src", "norm_ffn"):
        IN(n, [2, D])
    IN("ab_w_in", [D, 2472]); IN("ab_w_out", [D, D])
    for n in ("dsa_q_norm", "dsa_k_norm", "moba_q_norm", "moba_k_norm", "nsa_q_norm", "nsa_kcmp_norm", "nsa_ksel_norm", "nsa_kwin_norm"):
        IN(n, [64])
    IN("nsa_w_in", [D, 2608]); IN("nsa_w_out", [D, D])
    IN("nsa_cmp_pos_k", [32, 64]); IN("nsa_cmp_pos_v", [32, 64])
    IN("nsa_cmp_w1_k", [2048, 64]); IN("nsa_cmp_w2_k", [64, 64]); IN("nsa_cmp_w1_v", [2048, 64]); IN("nsa_cmp_w2_v", [64, 64])
    IN("mem_w_q", [2, D, 512]); IN("mem_w_kv", [2, D, 1024]); IN("mem_w_o", [2, 512, D])
    IN("mem_q_norm", [2, 128]); IN("mem_k_norm", [2, 128])
    IN("ffn_w_in", [2, D, 2 * DFF]); IN("ffn_w_out", [2, DFF, D])
    y_out = nc.dram_tensor("y", [S, D], F32, kind="ExternalOutput").ap()

    C = Ctx(nc)
    skind = "ExternalOutput" if dbg else "Internal"
    dbg_names = []

    def SCR(name, shape, dt):
        if dbg:
            dbg_names.append(name)
        return nc.dram_tensor(name, list(shape), dt, kind=skind).ap()

    ident = C.sb("ident", [128, 128], BF16)
    I4 = C.sb("I4", [128, 4, 128], BF16)
    epsG = C.sb("epsG", [128, 1], F32)
    C.op("pool", lambda e: e.memset(epsG[:], EPS), w=[epsG])
    TRI4 = C.sb("TRI4", [128, 4, 128], BF16)
    BLO4 = C.sb("BLO4", [128, 4, 128], BF16)
    CAUS = C.sb("CAUS", [128, 4, 4, 128], BF16)
    cs64 = C.sb("cs64", [128, NT, 8], F32); sn64 = C.sb("sn64", [128, NT, 8], F32)
    cs32 = C.sb("cs32", [128, NT, 4], F32); sn32 = C.sb("sn32", [128, NT, 4], F32)
    with C.phase():
        cf = C.sb("cf", [128, 128], F32)
        C.op("pool", lambda e: e.memset(cf[:], 0.0), w=[cf])
        C.op("pool", lambda e: e.affine_select(out=cf[:], in_=cf[:], pattern=[[-1, 128]], compare_op=ALU.not_equal, fill=1.0, base=0, channel_multiplier=1), r=[cf], w=[cf])
        C.op("dve", lambda e: e.tensor_copy(out=ident[:], in_=cf[:]), r=[cf], w=[ident])
        C.op("dve", lambda e: e.tensor_copy(out=I4[:], in_=ins_bc(cf[:], 1, 4)), r=[cf], w=[I4])
        tf = C.sb("tf", [128, 128], F32)
        C.op("pool", lambda e: e.memset(tf[:], 0.0), w=[tf])
        C.op("pool", lambda e: e.affine_select(out=tf[:], in_=tf[:], pattern=[[1, 128]], compare_op=ALU.is_ge, fill=NEG, base=0, channel_multiplier=-1), r=[tf], w=[tf])
        C.op("dve", lambda e: e.tensor_copy(out=TRI4[:], in_=ins_bc(tf[:], 1, 4)), r=[tf], w=[TRI4])
        for j in range(4):
            for qi in range(4):
                if qi > j:
                    C.op("dve", lambda e: e.memset(CAUS[:, j, qi, :], 0.0), w=[CAUS])
                elif qi == j:
                    C.op("dve", lambda e: e.tensor_copy(out=CAUS[:, j, qi, :], in_=tf[:]), r=[tf], w=[CAUS])
                else:
                    C.op("dve", lambda e: e.memset(CAUS[:, j, qi, :], NEG), w=[CAUS])
        tf2 = C.sb("tf2", [128, 128], F32)
        C.op("pool", lambda e: e.memset(tf2[:], 0.0), w=[tf2])
        C.op("pool", lambda e: e.affine_select(out=tf2[:], in_=tf2[:], pattern=[[-1, 128]], compare_op=ALU.is_gt, fill=NEG, base=0, channel_multiplier=1), r=[tf2], w=[tf2])
        C.op("dve", lambda e: e.tensor_copy(out=BLO4[:], in_=ins_bc(tf2[:], 1, 4)), r=[tf2], w=[BLO4])
        pi_ = C.sb("posi", [128, NT], I32); pf = C.sb("posf", [128, NT], F32)
        C.dma("sp", pi_[:], pos_in, w=[pi_])
        C.op("dve", lambda e: e.tensor_copy(out=pf[:], in_=pi_[:]), r=[pi_], w=[pf])
        u = C.sb("u", [128, NT], F32); ui = C.sb("ui", [128, NT], I32); uf = C.sb("uf", [128, NT], F32); fr = C.sb("fr", [128, NT], F32)

        def sincol(dst, col, freq, shift):
            C.op("dve", lambda e: e.tensor_scalar(out=u[:], in0=pf[:], scalar1=float(freq / (2 * np.pi)), scalar2=float(shift), op0=ALU.mult, op1=ALU.add), r=[pf], w=[u])
            C.op("dve", lambda e: e.tensor_copy(out=ui[:], in_=u[:]), r=[u], w=[ui])
            C.op("dve", lambda e: e.tensor_copy(out=uf[:], in_=ui[:]), r=[ui], w=[uf])
            C.op("dve", lambda e: e.tensor_tensor(out=fr[:], in0=u[:], in1=uf[:], op=ALU.subtract), r=[u, uf], w=[fr])
            C.op("dve", lambda e: e.tensor_scalar(out=fr[:], in0=fr[:], scalar1=0.4999, scalar2=-0.4999, op0=ALU.min, op1=ALU.max), r=[fr], w=[fr])
            C.op("act", lambda e: e.activation(out=dst[:, :, col], in_=fr[:], func=AF.Sin, scale=float(2 * np.pi)), r=[fr], w=[dst])

        for i in range(8):
            f = THETA ** (-(i * 2.0 / 16))
            sincol(sn64, i, f, 0.0); sincol(cs64, i, f, 0.25)
        for i in range(4):
            f = THETA ** (-(i * 2.0 / 8))
            sincol(sn32, i, f, 0.0); sincol(cs32, i, f, 0.25)

    def load_gain(dst, src_ap):
        C.dma("sp", dst[:], src_ap.rearrange("(kt p) -> p kt", p=128), w=[dst], allow_slow_non_contiguous=True)

    def load_w(dst, src_ap, KT, N, gain=None):
        srcv = src_ap.rearrange("(kt p) n -> p kt n", p=128)
        with C.phase():
            st = [C.sb("wst", [128, 4, 512], F32) for _ in range(3)]
            i = 0
            for k0 in range(0, KT, 4):
                k1 = min(KT, k0 + 4)
                for c0 in range(0, N, 512):
                    c1 = min(N, c0 + 512)
                    s = st[i % 3]
                    C.dma("sp" if i % 2 == 0 else "pool", s[:, 0:k1 - k0, 0:c1 - c0], srcv[:, k0:k1, c0:c1], w=[s])
                    for kt in range(k0, k1):
                        if kt % 2 == 0:
                            if gain is not None:
                                C.op("dve", lambda e: e.tensor_scalar(out=dst[:, kt, c0:c1], in0=s[:, kt - k0, 0:c1 - c0], scalar1=gain[:, kt:kt + 1], scalar2=None, op0=ALU.mult), r=[s, gain], w=[dst])
                            else:
                                C.op("dve", lambda e: e.tensor_copy(out=dst[:, kt, c0:c1], in_=s[:, kt - k0, 0:c1 - c0]), r=[s], w=[dst])
                        else:
                            if gain is not None:
                                C.op("act", lambda e: e.activation(out=dst[:, kt, c0:c1], in_=s[:, kt - k0, 0:c1 - c0], func=AF.Copy, scale=gain[:, kt:kt + 1]), r=[s, gain], w=[dst])
                            else:
                                C.op("act", lambda e: e.activation(out=dst[:, kt, c0:c1], in_=s[:, kt - k0, 0:c1 - c0], func=AF.Copy), r=[s], w=[dst])
                    i += 1

    class NormT:
        def __init__(s, pT=None):
            s.junk = C.sb("junk", [128, D], F32)
            s.ss = C.sb("ss", [128, 1], F32); s.rs = C.sb("rs", [128, 1], F32); s.epsT = epsG
            s.hn = C.sb("hn", [128, D], BF16)
            s.pT = pT if pT is not None else C.ps("pT", [128, 8, 128], BF16)

        def run(s, X, hT_ap, hT):
            C.op("act", lambda e: e.activation(out=s.junk[:], in_=X[:], func=AF.Square, accum_out=s.ss[:]), r=[X], w=[s.junk, s.ss])
            C.op("act", lambda e: e.activation(out=s.rs[:], in_=s.ss[:], func=AF.Ln, scale=1.0 / D, bias=s.epsT[:]), r=[s.ss, s.epsT], w=[s.rs])
            C.op("act", lambda e: e.activation(out=s.rs[:], in_=s.rs[:], func=AF.Exp, scale=-0.5), r=[s.rs], w=[s.rs])
            C.op("act", lambda e: e.activation(out=s.hn[:], in_=X[:], func=AF.Copy, scale=s.rs[:]), r=[X, s.rs], w=[s.hn])
            for kt in range(8):
                C.op("pe", lambda e: e.transpose(out=s.pT[:, kt, :], in_=s.hn[:, kt * 128:(kt + 1) * 128], identity=ident[:]), r=[s.hn, ident], w=[s.pT])
            C.op("dve", lambda e: e.tensor_copy(out=hT_ap, in_=s.pT[:]), r=[s.pT], w=[hT])

    class HeadNorm:
        def __init__(s, Hmax, Dh, rope_eng="dve"):
            s.sq = C.sb("hsq", [128, Hmax, Dh], F32)
            s.nrms = [C.sb("hnrm", [128, Hmax, Dh], F32) for _ in range(2)]
            s.ssh = C.sb("hss", [128, Hmax], F32); s.rsh = C.sb("hrs", [128, Hmax], F32)
            hf = Dh // 8
            s.t1s = [C.sb("ht1", [128, Hmax, hf], F32) for _ in range(2)]; s.t2s = [C.sb("ht2", [128, Hmax, hf], F32) for _ in range(2)]
            s.Dh = Dh; s.k = 0; s.re = rope_eng

        def run(s, src_ap, srcT, H, gain, out_ap, outT, rope=None, norm=True, out2_ap=None, out2T=None):
            Dh = s.Dh
            s.k += 1
            nrmT = s.nrms[s.k % 2]; t1T = s.t1s[s.k % 2]; t2T = s.t2s[s.k % 2]
            if norm:
                sq = s.sq[:, 0:H, :]; nrm = nrmT[:, 0:H, :]
                C.op("dve", lambda e: e.tensor_tensor(out=sq, in0=src_ap, in1=src_ap, op=ALU.mult), r=[srcT], w=[s.sq])
                C.op("dve", lambda e: e.tensor_reduce(out=s.ssh[:, 0:H], in_=sq, axis=AX.X, op=ALU.add), r=[s.sq], w=[s.ssh])
                C.op("act", lambda e: e.activation(out=s.rsh[:, 0:H], in_=s.ssh[:, 0:H], func=AF.Ln, scale=1.0 / Dh, bias=epsG[:]), r=[s.ssh, epsG], w=[s.rsh])
                C.op("act", lambda e: e.activation(out=s.rsh[:, 0:H], in_=s.rsh[:, 0:H], func=AF.Exp, scale=-0.5), r=[s.rsh], w=[s.rsh])
                C.op("dve", lambda e: e.tensor_tensor(out=nrm, in0=src_ap, in1=ins_bc(s.rsh[:, 0:H], 2, Dh), op=ALU.mult), r=[srcT, s.rsh], w=[nrmT])
                C.op("dve", lambda e: e.tensor_tensor(out=nrm, in0=nrm, in1=ins_bc(gain[:], 1, H), op=ALU.mult), r=[nrmT, gain], w=[nrmT])
                nT = nrmT
            else:
                nrm = src_ap; nT = srcT
            if out2_ap is not None:
                C.op("act", lambda e: e.activation(out=out2_ap, in_=nrm, func=AF.Copy), r=[nT], w=[out2T])
            C.op("act", lambda e: e.activation(out=out_ap, in_=nrm, func=AF.Copy), r=[nT], w=[outT])
            if rope is not None:
                RE = s.re
                cs, sn, hf = rope
                x1 = nrm[:, :, 0:hf]; x2 = nrm[:, :, hf:2 * hf]
                csb = ins_bc(cs, 1, H); snb = ins_bc(sn, 1, H)
                t1 = t1T[:, 0:H, :]; t2 = t2T[:, 0:H, :]
                C.op(RE, lambda e: e.tensor_tensor(out=t1, in0=x1, in1=csb, op=ALU.mult), r=[nT], w=[t1T])
                C.op(RE, lambda e: e.tensor_tensor(out=t2, in0=x2, in1=snb, op=ALU.mult), r=[nT], w=[t2T])
                C.op(RE, lambda e: e.tensor_tensor(out=out_ap[:, :, 0:hf], in0=t1, in1=t2, op=ALU.subtract), r=[t1T, t2T], w=[outT])
                C.op(RE, lambda e: e.tensor_tensor(out=t1, in0=x2, in1=csb, op=ALU.mult), r=[nT], w=[t1T])
                C.op(RE, lambda e: e.tensor_tensor(out=t2, in0=x1, in1=snb, op=ALU.mult), r=[nT], w=[t2T])
                C.op(RE, lambda e: e.tensor_tensor(out=out_ap[:, :, hf:2 * hf], in0=t1, in1=t2, op=ALU.add), r=[t1T, t2T], w=[outT])

    def run_proj(x_src, W, chunks, make_job, npo=4):
        nt = NormT()
        xt = [C.sb("xt", [128, D], F32) for _ in range(2)]
        hT = [C.sb("hT", [128, 8, 128], BF16) for _ in range(2)]
        po = [C.ps("po", [128, 512], F32) for _ in range(npo)]

        def norm(t):
            X = xt[t % 2]
            C.dma("sp", X[:], x_src[t * 128:(t + 1) * 128, :], w=[X])
            nt.run(X, hT[t % 2][:], hT[t % 2])

        jobs = [(t, c) for t in range(NT) for c in range(len(chunks))]

        def mm(k):
            t, c = jobs[k]
            c0, c1 = chunks[c]
            P = po[k % npo]
            H_ = hT[t % 2]
            for kt in range(8):
                C.op("pe", lambda e: e.matmul(P[:, 0:c1 - c0], lhsT=H_[:, kt, :], rhs=W[:, kt, c0:c1], start=(kt == 0), stop=(kt == 7)), r=[H_, W], w=[P])
            return P

        norm(0)
        Ps = {0: mm(0), 1: mm(1)}
        prev_trans = None
        for k, (t, c) in enumerate(jobs):
            if c == 0 and t + 1 < NT:
                norm(t + 1)
            if k + 2 < len(jobs):
                Ps[k + 2] = mm(k + 2)
            if prev_trans is not None:
                prev_trans()
            post, trans = make_job(k, t, c, Ps.pop(k))
            post()
            prev_trans = trans
        if prev_trans is not None:
            prev_trans()

    def bgain(src_ap, n=64):
        t = C.sb("bg", [128, n], F32)
        C.dma("sp", t[:], src_ap.partition_broadcast(128), w=[t])
        return t


    QA_T = SCR("QA_T", [8, 64, S], BF16); KA_T = SCR("KA_T", [64, S], BF16); VA = SCR("VA", [S, 64], BF16)
    IQ_T = SCR("IQ_T", [8, 32, S], BF16); IK_T = SCR("IK_T", [32, S], BF16); IW = SCR("IW", [S, 8], F32)
    QB_T = SCR("QB_T", [8, 64, S], BF16); KB_T = SCR("KB_T", [8, 64, S], BF16); VB = SCR("VB", [S, 512], BF16)
    OM = SCR("OM", [S, D], BF16)
    XA = SCR("XA", [S, D], F32); XB = SCR("XB", [S, D], F32); XC = SCR("XC", [S, D], F32)

    def store(dst_ap, src_ap, srcT, **kw):
        C.dma("pool", dst_ap, src_ap, r=[srcT], **kw)

    def phase_A0():
        with C.phase():
            g = C.sb("g", [128, 8], F32); load_gain(g, inp["norm_mix"][0])
            W = C.sb("W0", [128, 8, 2472], BF16)
            load_w(W, inp["ab_w_in"], 8, 2472, g)
            gq = bgain(inp["dsa_q_norm"]); gk = bgain(inp["dsa_k_norm"]); gbq = bgain(inp["moba_q_norm"]); gbk = bgain(inp["moba_k_norm"])
            hnm = HeadNorm(8, 64, "pool"); hn32 = HeadNorm(9, 32, "pool")
            pr = [C.sb("pr", [128, 512], F32) for _ in range(2)]
            qn = [C.sb("qn", [128, 512], BF16) for _ in range(3)]
            ptq = [C.ps("ptq", [64, 8, 128], BF16) for _ in range(2)]
            qT = [C.sb("qT", [64, 8, 128], BF16) for _ in range(3)]
            chunks = [(0, 512), (512, 936), (936, 1448), (1448, 1960), (1960, 2472)]
            cnt = {"ti": 0}

            def tr_heads(Q, H, Dh, src_off, dst_ap):
                i = cnt["ti"]; cnt["ti"] += 1
                PT = ptq[i % 2]; QT = qT[i % 3]
                for h in range(H):
                    C.op("pe", lambda e: e.transpose(out=PT[0:Dh, h, :], in_=Q[:, src_off + h * Dh: src_off + (h + 1) * Dh], identity=ident[:]), r=[Q, ident], w=[PT])
                C.op("dve", lambda e: e.tensor_copy(out=QT[0:Dh, 0:H, :], in_=PT[0:Dh, 0:H, :]), r=[PT], w=[QT])
                store(dst_ap, QT[0:Dh, 0:H, :], QT)

            def make_job(k, t, c, P):
                R = pr[k % 2]; Q = qn[k % 3]
                ts = slice(t * 128, (t + 1) * 128)
                cs8 = cs64[:, t, :]; sn8 = sn64[:, t, :]; cs4 = cs32[:, t, :]; sn4 = sn32[:, t, :]
                wd = chunks[c][1] - chunks[c][0]

                def post():
                    C.op("act", lambda e: e.activation(out=R[:, 0:wd], in_=P[:, 0:wd], func=AF.Copy), r=[P], w=[R])
                    if c in (0, 2, 3):
                        gg = (gq, None, gbq, gbk)[c]
                        hnm.run(R[:].rearrange("p (h d) -> p h d", h=8), R, 8, gg, Q[:].rearrange("p (h d) -> p h d", h=8), Q, rope=(cs8, sn8, 8))
                    elif c == 1:
                        hnm.run(R[:, 0:64].rearrange("p (h d) -> p h d", h=1), R, 1, gk, Q[:, 0:64].rearrange("p (h d) -> p h d", h=1), Q, rope=(cs8, sn8, 8))
                        C.op("act", lambda e: e.activation(out=Q[:, 64:128], in_=R[:, 64:128], func=AF.Copy), r=[R], w=[Q])
                        hn32.run(R[:, 128:416].rearrange("p (h d) -> p h d", h=9), R, 9, None, Q[:, 128:416].rearrange("p (h d) -> p h d", h=9), Q, rope=(cs4, sn4, 4), norm=False)
                        store(IW[ts, :], R[:, 416:424], R)
                    else:
                        C.op("act", lambda e: e.activation(out=Q[:], in_=R[:], func=AF.Copy), r=[R], w=[Q])

                def trans():
                    if c in (0, 2, 3):
                        dst = (QA_T, None, QB_T, KB_T)[c]
                        tr_heads(Q, 8, 64, 0, dst[:, :, ts].rearrange("h d s -> d h s"))
                    elif c == 1:
                        store(VA[ts, :], Q[:, 64:128], Q)
                        tr_heads(Q, 1, 64, 0, KA_T[:, ts].rearrange("(h d) s -> d h s", h=1))
                        tr_heads(Q, 8, 32, 128, IQ_T[:, :, ts].rearrange("h d s -> d h s"))
                        tr_heads(Q, 1, 32, 384, IK_T[:, ts].rearrange("(h d) s -> d h s", h=1))
                    else:
                        store(VB[ts, :], Q[:], Q)
                return post, trans

            run_proj(x_in, W, chunks, make_job)

    NIT = 16

    def run_merged(gA, nA, gB, nB):
        a = b = 0
        doneA = gA is None
        doneB = gB is None
        while not (doneA and doneB):
            pickA = (not doneA) and (doneB or a * nB <= b * nA)
            if pickA:
                try:
                    next(gA); a += 1
                except StopIteration:
                    doneA = True
            else:
                try:
                    next(gB); b += 1
                except StopIteration:
                    doneB = True

    def phase_A1():
        with C.phase():
            KT_ = C.sb("KAT", [128, S], BF16)
            C.op("pool", lambda e: e.memset(KT_[64:128, :], 0.0), w=[KT_])
            C.dma("sp", KT_[0:64, :], KA_T, w=[KT_])
            IKT = C.sb("IKT", [128, S], BF16)
            C.op("pool", lambda e: e.memset(IKT[:], 0.0), w=[IKT])
            C.dma("sp", IKT[0:32, :], IK_T, w=[IKT])
            Va = C.sb("VAa", [128, NT, 65], BF16)
            C.op("pool", lambda e: e.memset(Va[:], 1.0), w=[Va])
            C.dma("sp", Va[:, :, 0:64], VA.rearrange("(t p) c -> p t c", p=128), w=[Va])
            IWs = C.sb("IWs", [128, NT, 8], F32); C.dma("sp", IWs[:], IW.rearrange("(t p) c -> p t c", p=128), w=[IWs])
            score = [C.sb("score", [128, S], F32) for _ in range(2)]
            junk = C.sb("junkb", [128, S], BF16)
            nmask = [C.sb("nmask", [128, S], BF16) for _ in range(2)]
            rl = [C.sb("rl", [128, 512], F32) for _ in range(2)]
            qa = [C.sb("qa", [128, 8, 128], BF16) for _ in range(2)]
            iqt = [C.sb("iqt", [128, 8, 128], BF16) for _ in range(2)]
            for t_ in qa + iqt:
                C.op("pool", lambda e: e.memset(t_[:], 0.0), w=[t_])
            psi = [C.ps("psi", [128, 512], F32) for _ in range(2)]
            pss = [C.ps("pss", [128, 512], F32) for _ in range(2)]
            pacc = [C.ps("pacc", [128, 512], F32) for _ in range(4)]
            PTs = [C.sb("PTs", [128, 512], BF16) for _ in range(3)]
            sm = {n: C.sb(n, [128, 1], F32) for n in ("lo", "hi", "mid", "cnt", "ge", "rng", "c255", "sgn")}
            cthr = [C.sb("cthr", [128, 1], F32) for _ in range(2)]
            junkA = C.sb("junkA", [128, S], BF16)
            rzs = [C.sb("rz", [128, 1], F32) for _ in range(2)]
            steps = C.sb("steps", [128, NIT], F32); pw = C.sb("pw", [128, NIT], F32); steps2 = C.sb("steps2", [128, NIT], F32)
            for kk in range(NIT):
                C.op("pool", lambda e: e.memset(pw[:, kk:kk + 1], float(2.0 ** -(kk + 1))), w=[pw])
            C.op("pool", lambda e: e.memset(sm["c255"][:], 255.5), w=[sm["c255"]])
            oq = [C.sb("oq", [128, 512], BF16) for _ in range(2)]
            st_ = {"ii": 0, "si": 0, "pi": 0, "ri": 0}

            def gen_idx(qt):
                nk = (qt + 1) * 128
                IQ = iqt[qt % 2]; SC = score[qt % 2]; NM = nmask[qt % 2]
                ts = slice(qt * 128, (qt + 1) * 128)
                C.dma("sp", IQ[0:32, :, :], IQ_T[:, :, ts].rearrange("h d s -> d h s"), w=[IQ])
                jobs_ = [(c0, min(512, nk - c0), h) for c0 in range(0, nk, 512) for h in range(8)]

                def imm(j):
                    c0, wd, h = jobs_[j]
                    P = psi[st_["ii"] % 2]; st_["ii"] += 1
                    C.op("pe", lambda e: e.matmul(P[:, 0:wd], lhsT=IQ[:, h, :], rhs=IKT[:, c0:c0 + wd], start=True, stop=True), r=[IQ, IKT], w=[P])
                    return P

                Pcur = imm(0)
                for j, (c0, wd, h) in enumerate(jobs_):
                    Pnext = imm(j + 1) if j + 1 < len(jobs_) else None
                    P = Pcur
                    if h == 0:
                        C.op("dve", lambda e: e.tensor_scalar(out=SC[:, c0:c0 + wd], in0=P[:, 0:wd], scalar1=0.0, scalar2=IWs[:, qt, 0:1], op0=ALU.max, op1=ALU.mult), r=[P, IWs], w=[SC])
                    else:
                        R_ = rl[j % 2]
                        C.op("act", lambda e: e.activation(out=R_[:, 0:wd], in_=P[:, 0:wd], func=AF.Relu), r=[P], w=[R_])
                        C.op("dve", lambda e: e.scalar_tensor_tensor(out=SC[:, c0:c0 + wd], in0=R_[:, 0:wd], scalar=IWs[:, qt, h:h + 1], in1=SC[:, c0:c0 + wd], op0=ALU.mult, op1=ALU.add), r=[R_, IWs, SC], w=[SC])
                    Pcur = Pnext
                    if h == 7:
                        yield
                sc = SC[:, 0:nk]
                C.op("dve", lambda e: e.tensor_reduce(out=sm["hi"][:], in_=sc, axis=AX.X, op=ALU.max), r=[SC], w=[sm["hi"]])
                C.op("dve", lambda e: e.tensor_reduce(out=sm["lo"][:], in_=sc, axis=AX.X, op=ALU.min), r=[SC], w=[sm["lo"]])
                C.op("pool", lambda e: e.affine_select(out=SC[:, nk - 128:nk], in_=SC[:, nk - 128:nk], pattern=[[-1, 128]], compare_op=ALU.is_ge, fill=-1e30, base=0, channel_multiplier=1), r=[SC], w=[SC])
                C.op("dve", lambda e: e.tensor_tensor(out=sm["rng"][:], in0=sm["hi"][:], in1=sm["lo"][:], op=ALU.subtract), r=[sm["lo"], sm["hi"]], w=[sm["rng"]])
                C.op("dve", lambda e: e.tensor_scalar(out=steps[:], in0=pw[:], scalar1=sm["rng"][:], scalar2=None, op0=ALU.mult), r=[pw, sm["rng"]], w=[steps])
                yield
                cD = max(8, int(0.52 * nk) // 8 * 8)
                nA = nk - cD
                CT = cthr[qt % 2]
                C.op("pool", lambda e: e.memset(CT[:], float(255.5 - 0.5 * nA)), w=[CT])
                C.op("dve", lambda e: e.tensor_scalar(out=steps2[:], in0=steps[:], scalar1=2.0, scalar2=None, op0=ALU.mult), r=[steps], w=[steps2])
                C.op("dve", lambda e: e.tensor_tensor(out=sm["mid"][:], in0=sm["lo"][:], in1=steps[:, 0:1], op=ALU.add), r=[sm["lo"], steps], w=[sm["mid"]])
                for it in range(NIT):
                    last = it == NIT - 1
                    mul_ap = steps[:, it:it + 1] if last else steps2[:, it + 1:it + 2]
                    sub_ap = steps[:, it:it + 1] if last else steps[:, it + 1:it + 2]
                    C.op("act", lambda e: e.activation(out=junkA[:, 0:nA], in_=SC[:, cD:nk], func=AF.Sign, scale=-1.0, bias=sm["mid"][:], accum_out=sm["sgn"][:]), r=[SC, sm["mid"]], w=[junkA, sm["sgn"]])
                    C.op("dve", lambda e: e.tensor_scalar(out=junk[:, 0:cD], in0=SC[:, 0:cD], scalar1=sm["mid"][:], scalar2=None, op0=ALU.is_ge, op1=ALU.add, accum_out=sm["cnt"][:]), r=[SC, sm["mid"]], w=[junk, sm["cnt"]])
                    C.op("dve", lambda e: e.scalar_tensor_tensor(out=sm["cnt"][:], in0=sm["sgn"][:], scalar=-0.5, in1=sm["cnt"][:], op0=ALU.mult, op1=ALU.add), r=[sm["sgn"], sm["cnt"]], w=[sm["cnt"]])
                    C.op("dve", lambda e: e.tensor_scalar(out=sm["ge"][:], in0=sm["cnt"][:], scalar1=CT[:], scalar2=mul_ap, op0=ALU.is_ge, op1=ALU.mult), r=[sm["cnt"], CT, steps, steps2], w=[sm["ge"]])
                    C.op("dve", lambda e: e.scalar_tensor_tensor(out=sm["mid"][:], in0=sm["ge"][:], scalar=sub_ap, in1=sm["mid"][:], op0=ALU.subtract, op1=ALU.add), r=[sm["ge"], steps, sm["mid"]], w=[sm["mid"]])
                    if it % 2 == 1:
                        yield
                C.op("dve", lambda e: e.scalar_tensor_tensor(out=sm["mid"][:], in0=steps[:, NIT - 1:NIT], scalar=-0.5, in1=sm["mid"][:], op0=ALU.mult, op1=ALU.add), r=[steps, sm["mid"]], w=[sm["mid"]])
                C.op("dve", lambda e: e.tensor_scalar(out=NM[:, 0:nk], in0=sc, scalar1=sm["mid"][:], scalar2=NEG, op0=ALU.is_lt, op1=ALU.mult), r=[SC, sm["mid"]], w=[NM])
                yield

            def n_idx(qt):
                return ((qt + 1) * 128 + 511) // 512 + NIT // 2 + 2

            def gen_att(qt):
                QA = qa[qt % 2]; NM = nmask[qt % 2]; OQ = oq[qt % 2]
                ts = slice(qt * 128, (qt + 1) * 128)
                C.dma("sp", QA[0:64, :, :], QA_T[:, :, ts].rearrange("h d s -> d h s"), w=[QA])

                def qk(hg, st):
                    P = pss[st_["si"] % 2]; st_["si"] += 1
                    ss_ = slice(st * 128, (st + 1) * 128)
                    C.op("pe", lambda e: e.matmul(P[:], lhsT=KT_[:, ss_], rhs=QA[:, hg * 4:(hg + 1) * 4, :], start=True, stop=False), r=[KT_, QA], w=[P])
                    C.op("pe", lambda e: e.matmul(P[:], lhsT=NM[:, ss_], rhs=I4[:], start=False, stop=True), r=[NM, I4], w=[P])
                    return P

                for hg in range(2):
                    Pc = qk(hg, 0)
                    for st in range(qt + 1):
                        Pn = qk(hg, st + 1) if st < qt else None
                        PT = PTs[st_["pi"] % 3]; st_["pi"] += 1
                        C.op("act", lambda e: e.activation(out=PT[:], in_=Pc[:], func=AF.Exp, scale=0.125), r=[Pc], w=[PT])
                        for h in range(4):
                            C.op("pe", lambda e: e.matmul(pacc[h][:, 0:65], lhsT=PT[:, h * 128:(h + 1) * 128], rhs=Va[:, st, :], start=(st == 0), stop=(st == qt)), r=[PT, Va], w=[pacc[h]])
                        Pc = Pn
                        if st % 4 == 3:
                            yield
                    for h in range(4):
                        hd = hg * 4 + h
                        rz = rzs[st_["ri"] % 2]; st_["ri"] += 1
                        C.op("dve", lambda e: e.reciprocal(out=rz[:], in_=pacc[h][:, 64:65]), r=[pacc[h]], w=[rz])
                        C.op("act", lambda e: e.activation(out=OQ[:, hd * 64:(hd + 1) * 64], in_=pacc[h][:, 0:64], func=AF.Copy, scale=rz[:]), r=[pacc[h], rz], w=[OQ])
                    yield
                store(OM[ts, 0:512], OQ[:], OQ)
                yield

            def n_att(qt):
                return 2 * ((qt + 1) // 4 + 1) + 1

            run_merged(gen_idx(0), 1, None, 1)
            for qt in range(NT):
                gB = gen_idx(qt + 1) if qt + 1 < NT else None
                run_merged(gen_att(qt), n_att(qt), gB, n_idx(qt + 1) if gB else 1)

    def phase_A2():
        with C.phase():
            KT_ = C.sb("KBT", [128, S], BF16); QT_ = C.sb("QBT", [128, S], BF16)
            C.op("pool", lambda e: e.memset(KT_[64:128, :], 0.0), w=[KT_])
            C.op("pool", lambda e: e.memset(QT_[64:128, :], 0.0), w=[QT_])
            with C.phase():
                Ef = C.sb("Ef", [80, S], F32)
                C.op("pool", lambda e: e.memset(Ef[64:80, :], 1.0), w=[Ef])
                C.op("pool", lambda e: e.affine_select(out=Ef[64:80, :], in_=Ef[64:80, :], pattern=[[1, S]], compare_op=ALU.is_ge, fill=0.0, base=0, channel_multiplier=-256), r=[Ef], w=[Ef])
                C.op("pool", lambda e: e.affine_select(out=Ef[64:80, :], in_=Ef[64:80, :], pattern=[[-1, S]], compare_op=ALU.is_ge, fill=0.0, base=255, channel_multiplier=256), r=[Ef], w=[Ef])
                C.op("dve", lambda e: e.tensor_copy(out=KT_[64:80, :], in_=Ef[64:80, :]), r=[Ef], w=[KT_])
            cm = C.sb("cm", [128, NT, 16], F32); om = C.sb("om", [128, NT, 16], F32)
            C.op("pool", lambda e: e.memset(cm[:], 0.0), w=[cm])
            C.op("pool", lambda e: e.memset(om[:], 0.0), w=[om])
            for qt in range(NT):
                own = qt // 2
                C.op("pool", lambda e: e.memset(cm[:, qt, own:16], -1e30), w=[cm])
                C.op("pool", lambda e: e.memset(om[:, qt, own:own + 1], 1.0), w=[om])
            Va = C.sb("VBa", [128, NT, 65], BF16)
            C.op("pool", lambda e: e.memset(Va[:], 1.0), w=[Va])
            kmf = C.sb("kmf", [64, 16], F32); kmb = C.sb("kmb", [64, 16], BF16)
            pgp = C.ps("pgp", [128, 512], F32)
            pgv = pgp[:].rearrange("p (t n) -> p t n", n=16)
            pntv = pgp[:].bitcast(BF16).rearrange("p (j q) -> p j q", q=128)
            gm = C.sb("gm", [128, NT, 16], F32); m8 = C.sb("m8", [128, NT, 8], F32)
            sel = C.sb("sel", [128, NT, 16], F32)
            nbx = C.sb("nbx", [128, NT, 80], BF16)
            C.op("pool", lambda e: e.memset(nbx[:], 0.0), w=[nbx])
            pss = [C.ps("pss", [128, 512], F32) for _ in range(3)]
            pacc = [C.ps("pacc", [128, 512], F32) for _ in range(4)]
            PTs = [C.sb("PTs", [128, 512], BF16) for _ in range(3)]
            rzs = [C.sb("rz", [128, 1], F32) for _ in range(2)]
            ob = [C.sb("ob", [128, 64], BF16) for _ in range(3)]
            si = 0; pi = 0; oi = 0; ri = 0
            for h in range(8):
                C.dma("sp", KT_[0:64, :], KB_T[h], w=[KT_])
                C.dma("sp", QT_[0:64, :], QB_T[h], w=[QT_])
                C.dma("sp", Va[:, :, 0:64], VB[:, h * 64:(h + 1) * 64].rearrange("(t p) c -> p t c", p=128), w=[Va])
                C.op("dve", lambda e: e.tensor_reduce(out=kmf[:], in_=KT_[0:64, :].rearrange("d (n s) -> d n s", n=16), axis=AX.X, op=ALU.add), r=[KT_], w=[kmf])
                C.op("dve", lambda e: e.tensor_scalar(out=kmb[:], in0=kmf[:], scalar1=1.0 / 256, scalar2=None, op0=ALU.mult), r=[kmf], w=[kmb])
                for qt in range(NT):
                    C.op("pe", lambda e: e.matmul(pgv[:, qt, :], lhsT=QT_[0:64, qt * 128:(qt + 1) * 128], rhs=kmb[:], start=True, stop=True), r=[QT_, kmb], w=[pgp])
                C.op("dve", lambda e: e.tensor_tensor(out=gm[:], in0=pgv, in1=cm[:], op=ALU.add), r=[pgp, cm], w=[gm])
                for qt in range(NT):
                    C.op("dve", lambda e: e.max(out=m8[:, qt, :], in_=gm[:, qt, :]), r=[gm], w=[m8])
                C.op("dve", lambda e: e.tensor_tensor(out=sel[:], in0=gm[:], in1=ins_bc(m8[:, :, 2], 2, 16), op=ALU.is_ge), r=[gm, m8], w=[sel])
                C.op("dve", lambda e: e.tensor_tensor(out=sel[:], in0=sel[:], in1=om[:], op=ALU.max), r=[sel, om], w=[sel])
                C.op("dve", lambda e: e.tensor_scalar(out=nbx[:, :, 64:80], in0=sel[:], scalar1=1.0, scalar2=-NEG, op0=ALU.subtract, op1=ALU.mult), r=[sel], w=[nbx])
                for r4 in range(4):
                    for j in range(8):
                        qt = r4 * 8 + j
                        C.op("pe", lambda e: e.transpose(out=pntv[0:80, j, :], in_=nbx[:, qt, :], identity=ident[:]), r=[nbx, ident], w=[pgp])
                    C.op("act", lambda e: e.activation(out=QT_[64:80, r4 * 1024:(r4 + 1) * 1024], in_=pntv[64:80, :, :].rearrange("n j q -> n (j q)"), func=AF.Copy), r=[pgp], w=[QT_])
                steps_ = [(G, st) for G in range(8) for st in range(4 * G + 4)]

                def qk(G, st):
                    nonlocal si
                    P = pss[si % 3]; si += 1
                    ss_ = slice(st * 128, (st + 1) * 128); qs = slice(G * 512, (G + 1) * 512)
                    diag = st >= 4 * G
                    C.op("pe", lambda e: e.matmul(P[:], lhsT=KT_[:, ss_], rhs=QT_[:, qs], start=True, stop=not diag), r=[KT_, QT_], w=[P])
                    if diag:
                        C.op("pe", lambda e: e.matmul(P[:], lhsT=ident[:], rhs=CAUS[:, st - 4 * G, :, :], start=False, stop=True), r=[ident, CAUS], w=[P])
                    return P

                Pq = [qk(*steps_[0]), qk(*steps_[1])]
                for k_, (G, st) in enumerate(steps_):
                    if k_ + 2 < len(steps_):
                        Pq.append(qk(*steps_[k_ + 2]))
                    Pc = Pq.pop(0)
                    PT = PTs[pi % 3]; pi += 1
                    C.op("act", lambda e: e.activation(out=PT[:], in_=Pc[:], func=AF.Exp, scale=0.125), r=[Pc], w=[PT])
                    for qi in range(4):
                        qt = 4 * G + qi
                        if qt < st:
                            continue
                        C.op("pe", lambda e: e.matmul(pacc[qi][:, 0:65], lhsT=PT[:, qi * 128:(qi + 1) * 128], rhs=Va[:, st, :], start=(st == 0), stop=(st == qt)), r=[PT, Va], w=[pacc[qi]])
                    if st == 4 * G + 3:
                        for qi in range(4):
                            qt = 4 * G + qi
                            O_ = ob[oi % 3]; oi += 1
                            rz = rzs[ri % 2]; ri += 1
                            C.op("dve", lambda e: e.reciprocal(out=rz[:], in_=pacc[qi][:, 64:65]), r=[pacc[qi]], w=[rz])
                            C.op("act", lambda e: e.activation(out=O_[:], in_=pacc[qi][:, 0:64], func=AF.Copy, scale=rz[:]), r=[pacc[qi], rz], w=[O_])
                            store(OM[qt * 128:(qt + 1) * 128, 512 + h * 64:512 + (h + 1) * 64], O_[:], O_)

    def phase_A3(L, w_out_ap, x_src, x_dst):
        with C.phase():
            Wo = C.sb("Wo", [128, 8, D], BF16); load_w(Wo, w_out_ap, 8, D)
            g = C.sb("g", [128, 8], F32); load_gain(g, inp["norm_mem"][L])
            Wq = C.sb("Wq", [128, 8, 512], BF16); load_w(Wq, inp["mem_w_q"][L], 8, 512, g)
            Wom = C.sb("Wom", [128, 4, D], BF16); load_w(Wom, inp["mem_w_o"][L], 4, D)
            gqn = bgain(inp["mem_q_norm"][L], 128); gkn = bgain(inp["mem_k_norm"][L], 128)
            mK = C.sb("mK", [128, 4, 256], BF16); mV = C.sb("mV", [128, 2, 4, 129], BF16)
            C.op("pool", lambda e: e.memset(mV[:], 1.0), w=[mV])
            nt = NormT(); hnm = HeadNorm(4, 128)
            hT = C.sb("hT", [128, 8, 128], BF16)
            po = [C.ps("po", [128, 512], F32) for _ in range(2)]
            pr = C.sb("pr", [128, 512], F32); qn = C.sb("qn", [128, 512], BF16)
            ptq8 = C.ps("ptq", [128, 8, 128], BF16)
            ptq = T(ptq8.t, "x"); ptq.b = ptq8.b
            ptq_ap = ptq8[:, 0:4, :]
            with C.phase():
                gs = C.sb("gs", [128, 8], F32); load_gain(gs, inp["norm_mem_src"][L])
                Wkv = C.sb("Wkv", [128, 8, D], BF16); load_w(Wkv, inp["mem_w_kv"][L], 8, D, gs)
                mt_ = C.sb("mt", [128, D], F32)
                for mt in range(2):
                    C.dma("sp", mt_[:], mem_in[mt * 128:(mt + 1) * 128, :], w=[mt_])
                    nt.run(mt_, hT[:], hT)
                    for c in range(2):
                        P = po[c]
                        for kt in range(8):
                            C.op("pe", lambda e: e.matmul(P[:], lhsT=hT[:, kt, :], rhs=Wkv[:, kt, c * 512:(c + 1) * 512], start=(kt == 0), stop=(kt == 7)), r=[hT, Wkv], w=[P])
                        if c == 0:
                            C.op("act", lambda e: e.activation(out=pr[:], in_=P[:], func=AF.Copy), r=[P], w=[pr])
                            hnm.run(pr[:].rearrange("p (h d) -> p h d", h=4), pr, 4, gkn, qn[:].rearrange("p (h d) -> p h d", h=4), qn)
                            for h in range(4):
                                C.op("pe", lambda e: e.transpose(out=ptq[:, h, :], in_=qn[:, h * 128:(h + 1) * 128], identity=ident[:]), r=[qn, ident], w=[ptq])
                            C.op("dve", lambda e: e.tensor_copy(out=mK[:, :, mt * 128:(mt + 1) * 128], in_=ptq_ap), r=[ptq], w=[mK])
                        else:
                            C.op("act", lambda e: e.activation(out=mV[:, mt, :, 0:128], in_=P[:].rearrange("p (h d) -> p h d", h=4), func=AF.Copy), r=[P], w=[mV])
            pT = nt.pT
            lanes = []
            for ln in range(2):
                Lb = K()
                Lb.nt = nt if ln == 0 else NormT(pT=pT)
                Lb.hnm = hnm if ln == 0 else HeadNorm(4, 128)
                Lb.ot = C.sb("ot", [128, D], BF16); Lb.xt = C.sb("xt", [128, D], F32)
                Lb.oT = C.sb("oT", [128, 8, 128], BF16); Lb.x1 = C.sb("x1", [128, D], F32); Lb.x2 = C.sb("x2", [128, D], F32)
                Lb.hT = hT if ln == 0 else C.sb("hT", [128, 8, 128], BF16)
                Lb.pr = pr if ln == 0 else C.sb("pr", [128, 512], F32)
                Lb.qn = qn if ln == 0 else C.sb("qn", [128, 512], BF16)
                Lb.qT = C.sb("qT", [128, 4, 128], BF16)
                Lb.po = po[ln]
                Lb.pss = C.ps("pss", [128, 512], F32)
                Lb.PTm = [C.sb("PTm", [128, 512], BF16) for _ in range(2)]
                Lb.pov = C.ps("pov", [128, 2, 256], F32)
                Lb.rz = [C.sb("rz", [128, 1], F32) for _ in range(2)]
                Lb.omx = C.sb("omx", [128, 512], BF16); Lb.omT = C.sb("omT", [128, 4, 128], BF16)
                lanes.append(Lb)

            def gen_tile(Lb, t):
                ts = slice(t * 128, (t + 1) * 128)
                O_ = Lb.ot; X = Lb.xt; X2 = Lb.x2; x1 = Lb.x1; oT = Lb.oT; P = Lb.po
                C.dma("sp", O_[:], OM[ts, :], w=[O_])
                C.dma("sp", X[:], x_src[ts, :], w=[X])
                for kt in range(8):
                    C.op("pe", lambda e: e.transpose(out=pT[:, kt, :], in_=O_[:, kt * 128:(kt + 1) * 128], identity=ident[:]), r=[O_, ident], w=[pT])
                C.op("dve", lambda e: e.tensor_copy(out=oT[:], in_=pT[:]), r=[pT], w=[oT])
                yield
                for c in range(2):
                    for kt in range(8):
                        C.op("pe", lambda e: e.matmul(P[:], lhsT=oT[:, kt, :], rhs=Wo[:, kt, c * 512:(c + 1) * 512], start=(kt == 0), stop=(kt == 7)), r=[oT, Wo], w=[P])
                    C.op("dve", lambda e: e.tensor_tensor(out=x1[:, c * 512:(c + 1) * 512], in0=P[:], in1=X[:, c * 512:(c + 1) * 512], op=ALU.add), r=[P, X], w=[x1])
                    yield
                Lb.nt.run(x1, Lb.hT[:], Lb.hT)
                yield
                for kt in range(8):
                    C.op("pe", lambda e: e.matmul(P[:], lhsT=Lb.hT[:, kt, :], rhs=Wq[:, kt, :], start=(kt == 0), stop=(kt == 7)), r=[Lb.hT, Wq], w=[P])
                C.op("act", lambda e: e.activation(out=Lb.pr[:], in_=P[:], func=AF.Copy), r=[P], w=[Lb.pr])
                yield
                Lb.hnm.run(Lb.pr[:].rearrange("p (h d) -> p h d", h=4), Lb.pr, 4, gqn, Lb.qn[:].rearrange("p (h d) -> p h d", h=4), Lb.qn)
                yield
                for h in range(4):
                    C.op("pe", lambda e: e.transpose(out=ptq[:, h, :], in_=Lb.qn[:, h * 128:(h + 1) * 128], identity=ident[:]), r=[Lb.qn, ident], w=[ptq])
                C.op("dve", lambda e: e.tensor_copy(out=Lb.qT[:], in_=ptq_ap), r=[ptq], w=[Lb.qT])
                yield
                for mt in range(2):
                    PS = Lb.pss
                    for h in range(4):
                        C.op("pe", lambda e: e.matmul(PS[:, h * 128:(h + 1) * 128], lhsT=mK[:, h, mt * 128:(mt + 1) * 128], rhs=Lb.qT[:, h, :], start=True, stop=True), r=[mK, Lb.qT], w=[PS])
                    C.op("act", lambda e: e.activation(out=Lb.PTm[mt][:], in_=PS[:], func=AF.Exp, scale=float(128 ** -0.5)), r=[PS], w=[Lb.PTm[mt]])
                    yield
                for h in range(4):
                    A = Lb.pov
                    for mt in range(2):
                        C.op("pe", lambda e: e.matmul(A[:, h % 2, 0:129], lhsT=Lb.PTm[mt][:, h * 128:(h + 1) * 128], rhs=mV[:, mt, h, :], start=(mt == 0), stop=(mt == 1)), r=[Lb.PTm[mt], mV], w=[A])
                    rz = Lb.rz[h % 2]
                    C.op("dve", lambda e: e.reciprocal(out=rz[:], in_=A[:, h % 2, 128:129]), r=[A], w=[rz])
                    C.op("act", lambda e: e.activation(out=Lb.omx[:, h * 128:(h + 1) * 128], in_=A[:, h % 2, 0:128], func=AF.Copy, scale=rz[:]), r=[A, rz], w=[Lb.omx])
                    yield
                for h in range(4):
                    C.op("pe", lambda e: e.transpose(out=ptq[:, h, :], in_=Lb.omx[:, h * 128:(h + 1) * 128], identity=ident[:]), r=[Lb.omx, ident], w=[ptq])
                C.op("dve", lambda e: e.tensor_copy(out=Lb.omT[:], in_=ptq_ap), r=[ptq], w=[Lb.omT])
                yield
                for c in range(2):
                    for kt in range(4):
                        C.op("pe", lambda e: e.matmul(P[:], lhsT=Lb.omT[:, kt, :], rhs=Wom[:, kt, c * 512:(c + 1) * 512], start=(kt == 0), stop=(kt == 3)), r=[Lb.omT, Wom], w=[P])
                    C.op("dve", lambda e: e.tensor_tensor(out=X2[:, c * 512:(c + 1) * 512], in0=P[:], in1=x1[:, c * 512:(c + 1) * 512], op=ALU.add), r=[P, x1], w=[X2])
                    yield
                store(x_dst[ts, :], X2[:], X2)
                yield

            def lane_gen(ln):
                if ln == 1:
                    for _ in range(9):
                        yield
                for t in range(ln, NT, 2):
                    yield from gen_tile(lanes[ln], t)

            run_merged(lane_gen(0), 1, lane_gen(1), 1)

    def phase_A4(L, x_src, x_dst):
        with C.phase():
            g = C.sb("g", [128, 8], F32); load_gain(g, inp["norm_ffn"][L])
            Wi = C.sb("Wi", [128, 8, 2 * DFF], BF16); load_w(Wi, inp["ffn_w_in"][L], 8, 2 * DFF, g)
            Wd = C.sb("Wd", [128, 22, D], BF16); load_w(Wd, inp["ffn_w_out"][L], 22, D)
            nt = NormT()
            xt = [C.sb("xt", [128, D], F32) for _ in range(4)]
            hT = C.sb("hT", [128, 8, 512], BF16)
            hid = C.sb("hid", [128, 22, 512], BF16)
            psg = [C.ps("psg", [128, 512], F32) for _ in range(2)]
            psu = [C.ps("psu", [128, 512], F32) for _ in range(2)]
            po = [C.ps("po", [128, 512], F32) for _ in range(2)]
            sg = [C.sb("sg", [128, 512], BF16) for _ in range(2)]
            oi = 0
            for G in range(8):
                for tt in range(4):
                    t = G * 4 + tt
                    C.dma("sp", xt[tt][:], x_src[t * 128:(t + 1) * 128, :], w=[xt[tt]])
                    nt.run(xt[tt], hT[:, :, tt * 128:(tt + 1) * 128], hT)
                for ft in range(22):
                    Pg = psg[ft % 2]; Pu = psu[ft % 2]; Sg = sg[ft % 2]
                    for kt in range(8):
                        C.op("pe", lambda e: e.matmul(Pg[:], lhsT=Wi[:, kt, ft * 128:(ft + 1) * 128], rhs=hT[:, kt, :], start=(kt == 0), stop=(kt == 7)), r=[Wi, hT], w=[Pg])
                    for kt in range(8):
                        C.op("pe", lambda e: e.matmul(Pu[:], lhsT=Wi[:, kt, DFF + ft * 128:DFF + (ft + 1) * 128], rhs=hT[:, kt, :], start=(kt == 0), stop=(kt == 7)), r=[Wi, hT], w=[Pu])
                    C.op("act", lambda e: e.activation(out=Sg[:], in_=Pg[:], func=AF.Silu), r=[Pg], w=[Sg])
                    C.op("dve", lambda e: e.tensor_tensor(out=hid[:, ft, :], in0=Sg[:], in1=Pu[:], op=ALU.mult), r=[Sg, Pu], w=[hid])
                for tt in range(4):
                    t = G * 4 + tt
                    XO = xt[tt]
                    for c in range(2):
                        P = po[c]
                        for ft in range(22):
                            C.op("pe", lambda e: e.matmul(P[:], lhsT=hid[:, ft, tt * 128:(tt + 1) * 128], rhs=Wd[:, ft, c * 512:(c + 1) * 512], start=(ft == 0), stop=(ft == 21)), r=[hid, Wd], w=[P])
                        C.op("dve", lambda e: e.tensor_tensor(out=XO[:, c * 512:(c + 1) * 512], in0=P[:], in1=xt[tt][:, c * 512:(c + 1) * 512], op=ALU.add), r=[P, xt[tt]], w=[XO])
                    store(x_dst[t * 128:(t + 1) * 128, :], XO[:], XO)


    Q_T = SCR("Q_T", [16, 64, S], BF16); QR_T = SCR("QR_T", [16, 64, S], BF16)
    KCr_T = SCR("KCr_T", [4, 64, S], BF16); VCr_T = SCR("VCr_T", [4, 64, S], BF16)
    KS_T = SCR("KS_T", [4, 64, S], BF16); VS = SCR("VS", [S, 256], BF16)
    KW_T = SCR("KW_T", [4, 64, S], BF16); VW = SCR("VW", [S, 256], BF16)
    GT = SCR("GT", [S, 48], F32)
    KC_T = C.sb("KC_T", [128, 4, 256], BF16, stack=C.es)
    VCa = C.sb("VCa", [128, 2, 4, 129], BF16, stack=C.es)

    def phase_B0():
        with C.phase():
            g = C.sb("g", [128, 8], F32); load_gain(g, inp["norm_mix"][1])
            W = C.sb("W1", [128, 8, 2608], BF16)
            load_w(W, inp["nsa_w_in"], 8, 2608, g)
            gq = bgain(inp["nsa_q_norm"]); gks = bgain(inp["nsa_ksel_norm"]); gkw = bgain(inp["nsa_kwin_norm"])
            hnm = HeadNorm(8, 64, "pool")
            pr = [C.sb("pr", [128, 512], F32) for _ in range(2)]
            qn = [C.sb("qn", [128, 512], BF16) for _ in range(3)]
            qn2 = [C.sb("qn2", [128, 512], BF16) for _ in range(3)]
            ptq = [C.ps("ptq", [64, 8, 128], BF16) for _ in range(2)]
            qT = [C.sb("qT", [64, 8, 128], BF16) for _ in range(3)]
            chunks = [(0, 512), (512, 1024), (1024, 1536), (1536, 2048), (2048, 2560), (2560, 2608)]
            cnt = {"ti": 0}

            def tr_heads(SRC, H, src_off, dst_ap):
                i = cnt["ti"]; cnt["ti"] += 1
                PT = ptq[i % 2]; QT = qT[i % 3]
                for h in range(H):
                    C.op("pe", lambda e: e.transpose(out=PT[:, h, :], in_=SRC[:, src_off + h * 64: src_off + (h + 1) * 64], identity=ident[:]), r=[SRC, ident], w=[PT])
                C.op("dve", lambda e: e.tensor_copy(out=QT[:, 0:H, :], in_=PT[:, 0:H, :]), r=[PT], w=[QT])
                store(dst_ap, QT[:, 0:H, :], QT)

            def make_job(k, t, c, P):
                R = pr[k % 2]; Q = qn[k % 3]; Q2 = qn2[k % 3]
                ts = slice(t * 128, (t + 1) * 128)
                cs8 = cs64[:, t, :]; sn8 = sn64[:, t, :]
                v8 = lambda A_: A_[:].rearrange("p (h d) -> p h d", h=8)
                v4 = lambda A_: A_[:, 0:256].rearrange("p (h d) -> p h d", h=4)

                def post():
                    if c == 5:
                        C.op("act", lambda e: e.activation(out=R[:, 0:48], in_=P[:, 0:48], func=AF.Exp, scale=-1.0), r=[P], w=[R])
                        C.op("dve", lambda e: e.tensor_scalar(out=R[:, 0:48], in0=R[:, 0:48], scalar1=1.0, scalar2=None, op0=ALU.add), r=[R], w=[R])
                        C.op("dve", lambda e: e.reciprocal(out=R[:, 0:48], in_=R[:, 0:48]), r=[R], w=[R])
                        store(GT[ts, :], R[:, 0:48], R)
                        return
                    C.op("act", lambda e: e.activation(out=R[:], in_=P[:], func=AF.Copy), r=[P], w=[R])
                    if c in (0, 1):
                        hnm.run(v8(R), R, 8, gq, v8(Q), Q, rope=(cs8, sn8, 8), out2_ap=v8(Q2), out2T=Q2)
                    elif c == 2:
                        C.op("act", lambda e: e.activation(out=Q[:], in_=R[:], func=AF.Copy), r=[R], w=[Q])
                    else:
                        gg = gks if c == 3 else gkw
                        hnm.run(v4(R), R, 4, gg, v4(Q), Q, rope=(cs8, sn8, 8))
                        C.op("act", lambda e: e.activation(out=Q[:, 256:512], in_=R[:, 256:512], func=AF.Copy), r=[R], w=[Q])

                def trans():
                    if c == 5:
                        return
                    if c in (0, 1):
                        tr_heads(Q, 8, 0, QR_T[c * 8:(c + 1) * 8, :, ts].rearrange("h d s -> d h s"))
                        tr_heads(Q2, 8, 0, Q_T[c * 8:(c + 1) * 8, :, ts].rearrange("h d s -> d h s"))
                    elif c == 2:
                        tr_heads(Q, 4, 0, KCr_T[:, :, ts].rearrange("h d s -> d h s"))
                        tr_heads(Q, 4, 256, VCr_T[:, :, ts].rearrange("h d s -> d h s"))
                    else:
                        store((VS if c == 3 else VW)[ts, :], Q[:, 256:512], Q)
                        tr_heads(Q, 4, 0, (KS_T if c == 3 else KW_T)[:, :, ts].rearrange("h d s -> d h s"))
                return post, trans

            run_proj(XB, W, chunks, make_job)

    def phase_B1():
        with C.phase():
            C.op("pool", lambda e: e.memset(KC_T[:], 0.0), w=[KC_T])
            C.op("pool", lambda e: e.memset(VCa[:], 0.0), w=[VCa])
            C.op("pool", lambda e: e.memset(VCa[:, :, :, 64:65], 1.0), w=[VCa])
            cov = C.sb("cov", [128, 2, 64], F32)
            C.op("pool", lambda e: e.memset(cov[:], 1.0), w=[cov])
            for half in range(2):
                C.op("pool", lambda e: e.affine_select(out=cov[:, half, :], in_=cov[:, half, :], pattern=[[-64, 64]], compare_op=ALU.is_gt, fill=0.0, base=2048 * half + 32, channel_multiplier=16), r=[cov], w=[cov])
                C.op("pool", lambda e: e.affine_select(out=cov[:, half, :], in_=cov[:, half, :], pattern=[[64, 64]], compare_op=ALU.is_gt, fill=0.0, base=64 - 2048 * half, channel_multiplier=-16), r=[cov], w=[cov])
                C.op("dve", lambda e: e.tensor_copy(out=VCa[:, half, :, 65:129], in_=ins_bc(cov[:, half, :], 1, 4)), r=[cov], w=[VCa])
            gkc = bgain(inp["nsa_kcmp_norm"])
            hnm = HeadNorm(1, 64)
            XT = C.sb("XT", [64, 4, S], BF16)
            w1f = C.sb("w1f", [64, 32, 64], F32); w1b = C.sb("w1b", [64, 32, 64], BF16)
            w2f = C.sb("w2f", [64, 64], F32); w2b = C.sb("w2b", [64, 64], BF16)
            posf = C.sb("posf", [64, 32], F32); posrep = C.sb("posrep", [64, 32, 128], BF16)
            cpos = C.sb("cpos", [128, 64], F32)
            ph = [C.ps("ph", [128, 512], F32) for _ in range(2)]
            pt_ = C.ps("pt_", [64, 8, 128], BF16)
            hs = C.sb("hs", [128, 64], F32); hsb = C.sb("hsb", [128, 64], BF16); hsT = C.sb("hsT", [64, 128], BF16)
            o2 = C.sb("o2", [128, 64], F32); o2b = C.sb("o2b", [128, 64], BF16)
            pi = 0
            for kv in range(2):
                src = (KCr_T, VCr_T)[kv]
                C.dma("sp", XT[:], src.rearrange("g d s -> d g s"), w=[XT])
                C.dma("sp", w1f[:], inp[("nsa_cmp_w1_k", "nsa_cmp_w1_v")[kv]].rearrange("(l d) o -> d l o", d=64), w=[w1f])
                C.dma("sp", w2f[:], inp[("nsa_cmp_w2_k", "nsa_cmp_w2_v")[kv]], w=[w2f])
                C.dma("sp", posf[:], inp[("nsa_cmp_pos_k", "nsa_cmp_pos_v")[kv]].rearrange("l d -> d l"), w=[posf], allow_slow_non_contiguous=True)
                C.op("dve", lambda e: e.tensor_copy(out=w1b[:], in_=w1f[:]), r=[w1f], w=[w1b])
                C.op("dve", lambda e: e.tensor_copy(out=w2b[:], in_=w2f[:]), r=[w2f], w=[w2b])
                C.op("dve", lambda e: e.tensor_copy(out=posrep[:], in_=ins_bc(posf[:], 2, 128)), r=[posf], w=[posrep])
                P = ph[pi % 2]; pi += 1
                for l in range(32):
                    C.op("pe", lambda e: e.matmul(P[:, 0:64], lhsT=posrep[:, l, :], rhs=w1b[:, l, :], start=(l == 0), stop=(l == 31)), r=[posrep, w1b], w=[P])
                C.op("dve", lambda e: e.tensor_copy(out=cpos[:], in_=P[:, 0:64]), r=[P], w=[cpos])
                for g_ in range(4):
                    for half in range(2):
                        M = 128 if half == 0 else 127
                        P = ph[pi % 2]; pi += 1
                        for l in range(32):
                            a0 = 2048 * half + l
                            C.op("pe", lambda e: e.matmul(P[0:M, 0:64], lhsT=XT[:, g_, a0:a0 + 16 * (M - 1) + 1:16], rhs=w1b[:, l, :], start=(l == 0), stop=(l == 31)), r=[XT, w1b], w=[P])
                        C.op("dve", lambda e: e.tensor_tensor(out=hs[0:M, :], in0=P[0:M, 0:64], in1=cpos[0:M, :], op=ALU.add), r=[P, cpos], w=[hs])
                        C.op("act", lambda e: e.activation(out=hsb[0:M, :], in_=hs[0:M, :], func=AF.Silu), r=[hs], w=[hsb])
                        C.op("pe", lambda e: e.transpose(out=pt_[:, 0, 0:M], in_=hsb[0:M, :], identity=ident[0:M, 0:M]), r=[hsb, ident], w=[pt_])
                        C.op("dve", lambda e: e.tensor_copy(out=hsT[:, 0:M], in_=pt_[:, 0, 0:M]), r=[pt_], w=[hsT])
                        P2 = ph[pi % 2]; pi += 1
                        C.op("pe", lambda e: e.matmul(P2[0:M, 0:64], lhsT=hsT[:, 0:M], rhs=w2b[:], start=True, stop=True), r=[hsT, w2b], w=[P2])
                        if kv == 0:
                            C.op("act", lambda e: e.activation(out=o2[0:M, :], in_=P2[0:M, 0:64], func=AF.Copy), r=[P2], w=[o2])
                            hnm.run(o2[:].rearrange("p (h d) -> p h d", h=1), o2, 1, gkc, o2b[:].rearrange("p (h d) -> p h d", h=1), o2b)
                            C.op("pe", lambda e: e.transpose(out=pt_[:, 1, 0:M], in_=o2b[0:M, :], identity=ident[0:M, 0:M]), r=[o2b, ident], w=[pt_])
                            C.op("dve", lambda e: e.tensor_copy(out=KC_T[0:64, g_, half * 128:half * 128 + M], in_=pt_[:, 1, 0:M]), r=[pt_], w=[KC_T])
                        else:
                            C.op("act", lambda e: e.activation(out=VCa[0:M, half, g_, 0:64], in_=P2[0:M, 0:64], func=AF.Copy), r=[P2], w=[VCa])

    def phase_B2():
        with C.phase():
            KS = C.sb("KS", [128, 4, S], BF16); C.dma("sp", KS[0:64, :, :], KS_T.rearrange("g d s -> d g s"), w=[KS])
            KW = C.sb("KW", [128, 4, S], BF16)
            C.op("pool", lambda e: e.memset(KW[64:128, :, :], 0.0), w=[KW])
            C.dma("sp", KW[0:64, :, :], KW_T.rearrange("g d s -> d g s"), w=[KW])
            VSa = C.sb("VSa", [128, NT, 4, 65], BF16); VWa = C.sb("VWa", [128, NT, 4, 65], BF16)
            C.op("pool", lambda e: e.memset(VSa[:], 1.0), w=[VSa])
            C.op("pool", lambda e: e.memset(VWa[:], 1.0), w=[VWa])
            for t in range(NT):
                C.dma("sp", VSa[:, t, :, 0:64], VS[t * 128:(t + 1) * 128, :].rearrange("p (g d) -> p g d", g=4), w=[VSa])
                C.dma("pool", VWa[:, t, :, 0:64], VW[t * 128:(t + 1) * 128, :].rearrange("p (g d) -> p g d", g=4), w=[VWa])
            with C.phase():
                Ef = C.sb("E2f", [128, S], F32)
                C.op("pool", lambda e: e.memset(Ef[64:128, :], 1.0), w=[Ef])
                C.op("pool", lambda e: e.affine_select(out=Ef[64:128, :], in_=Ef[64:128, :], pattern=[[1, S]], compare_op=ALU.is_ge, fill=0.0, base=0, channel_multiplier=-64), r=[Ef], w=[Ef])
                C.op("pool", lambda e: e.affine_select(out=Ef[64:128, :], in_=Ef[64:128, :], pattern=[[-1, S]], compare_op=ALU.is_ge, fill=0.0, base=63, channel_multiplier=64), r=[Ef], w=[Ef])
                for g_ in range(4):
                    C.op("dve" if g_ % 2 == 0 else "act", (lambda e: e.tensor_copy(out=KS[64:128, g_, :], in_=Ef[64:128, :])) if g_ % 2 == 0 else (lambda e: e.activation(out=KS[64:128, g_, :], in_=Ef[64:128, :], func=AF.Copy)), r=[Ef], w=[KS])
            V0 = C.sb("V0", [128, 128], F32)
            C.op("pool", lambda e: e.iota(out=V0[:], pattern=[[-1, 128]], base=0, channel_multiplier=16, allow_small_or_imprecise_dtypes=True), w=[V0])
            J = C.sb("J", [128, 64], F32)
            C.op("pool", lambda e: e.iota(out=J[:], pattern=[[1, 64]], base=0, channel_multiplier=0, allow_small_or_imprecise_dtypes=True), w=[J])
            f0 = C.sb("f0", [128, 64], F32)
            C.op("dve", lambda e: e.tensor_scalar(out=f0[:], in0=J[:], scalar1=0.0, scalar2=None, op0=ALU.is_equal), r=[J], w=[f0])
            CURb = C.sb("CURb", [128, 1], F32)
            C.op("pool", lambda e: e.iota(out=CURb[:], pattern=[[0, 1]], base=0, channel_multiplier=1, allow_small_or_imprecise_dtypes=True), w=[CURb])
            C.op("dve", lambda e: e.tensor_scalar(out=CURb[:], in0=CURb[:], scalar1=64.0, scalar2=None, op0=ALU.is_ge), r=[CURb], w=[CURb])
            sm = {n: C.sb(n, [128, 1], F32) for n in ("curq", "cm1")}
            rz4s = [C.sb("rz4", [128, 4], F32) for _ in range(2)]; cf4s = [C.sb("cf4", [128, 4], F32) for _ in range(2)]
            f1 = C.sb("f1", [128, 64], F32); f2 = C.sb("f2", [128, 64], F32)
            FORCE = C.sb("FORCE", [128, 64], F32); FUT = C.sb("FUT", [128, 64], F32)
            imp = C.sb("imp", [128, 64], F32); imp3 = C.sb("imp3", [128, 64], F32); impr = C.sb("impr", [128, 64], F32)
            imt = C.sb("imt", [128, 4, 64], F32)
            m8a = C.sb("m8a", [128, 8], F32); m8b = C.sb("m8b", [128, 8], F32)
            nm = C.sb("nm", [128, 128], BF16)
            C.op("pool", lambda e: e.memset(nm[:], 0.0), w=[nm])
            pnm = C.ps("pnm", [128, 8, 128], BF16)
            cb = [C.sb("cb", [128, 4, 128], BF16) for _ in range(2)]
            qnt = [C.sb("qnt", [128, 16, 128], BF16) for _ in range(2)]
            qrt = [C.sb("qrt", [128, 16, 128], BF16) for _ in range(2)]
            for t_ in qnt + qrt:
                C.op("pool", lambda e: e.memset(t_[:], 0.0), w=[t_])
            gts = [C.sb("gts", [128, 48], F32) for _ in range(2)]
            acc = C.sb("acc", [128, D], F32)
            obf = [C.sb("obf", [128, D], BF16) for _ in range(2)]
            pss = [C.ps("pss", [128, 512], F32) for _ in range(3)]
            pacc = C.ps("pacc", [128, 4, 512], F32)
            PTs = [C.sb("PTs", [128, 512], BF16) for _ in range(3)]
            si = 0; pi = 0; zi = 0
            for qt in range(NT):
                ts = slice(qt * 128, (qt + 1) * 128)
                QN = qnt[qt % 2]; QR = qrt[qt % 2]; Gt = gts[qt % 2]; OB = obf[qt % 2]
                C.dma("sp", QN[0:64, :, :], Q_T[:, :, ts].rearrange("h d s -> d h s"), w=[QN])
                C.dma("sp", QR[0:64, :, :], QR_T[:, :, ts].rearrange("h d s -> d h s"), w=[QR])
                C.dma("sp", Gt[:], GT[ts, :], w=[Gt])
                nhalf = 1 if qt < 16 else 2
                for half in range(nhalf):
                    C.op("dve", lambda e: e.tensor_scalar(out=cb[half][:], in0=ins_bc(V0[:], 1, 4), scalar1=float(128 * qt - 31 - 2048 * half), scalar2=NEG, op0=ALU.is_gt, op1=ALU.mult), r=[V0], w=[cb[half]])
                C.op("dve", lambda e: e.tensor_scalar(out=sm["curq"][:], in0=CURb[:], scalar1=float(2 * qt), scalar2=None, op0=ALU.add), r=[CURb], w=[sm["curq"]])
                C.op("dve", lambda e: e.tensor_scalar(out=sm["cm1"][:], in0=CURb[:], scalar1=float(2 * qt - 1), scalar2=None, op0=ALU.add), r=[CURb], w=[sm["cm1"]])
                C.op("dve", lambda e: e.tensor_scalar(out=f1[:], in0=J[:], scalar1=sm["curq"][:], scalar2=None, op0=ALU.is_equal), r=[J, sm["curq"]], w=[f1])
                C.op("dve", lambda e: e.tensor_scalar(out=f2[:], in0=J[:], scalar1=sm["cm1"][:], scalar2=None, op0=ALU.is_equal), r=[J, sm["cm1"]], w=[f2])
                C.op("dve", lambda e: e.tensor_tensor(out=f1[:], in0=f1[:], in1=f2[:], op=ALU.add), r=[f1, f2], w=[f1])
                C.op("dve", lambda e: e.tensor_tensor(out=f1[:], in0=f1[:], in1=f0[:], op=ALU.add), r=[f1, f0], w=[f1])
                C.op("dve", lambda e: e.tensor_scalar(out=FORCE[:], in0=f1[:], scalar1=1e4, scalar2=None, op0=ALU.mult), r=[f1], w=[FORCE])
                C.op("dve", lambda e: e.tensor_scalar(out=FUT[:], in0=J[:], scalar1=sm["curq"][:], scalar2=-1e30, op0=ALU.is_gt, op1=ALU.mult), r=[J, sm["curq"]], w=[FUT])

                def branch(g_, qk_fn, sts, Vrhs_fn, ncol, gidx, first):
                    nonlocal si, pi, zi
                    n = len(sts)

                    def qk(st):
                        nonlocal si
                        P = pss[si % 3]; si += 1
                        mm = qk_fn(st)
                        for ei, (l_ap, lT, r_ap, rT) in enumerate(mm):
                            C.op("pe", lambda e: e.matmul(P[:], lhsT=l_ap, rhs=r_ap, start=(ei == 0), stop=(ei == len(mm) - 1)), r=[lT, rT], w=[P])
                        return P

                    Pq = [qk(sts[0])]
                    if n > 1:
                        Pq.append(qk(sts[1]))
                    for ix, st in enumerate(sts):
                        if ix + 2 < n:
                            Pq.append(qk(sts[ix + 2]))
                        Pc = Pq.pop(0)
                        PT = PTs[pi % 3]; pi += 1
                        C.op("act", lambda e: e.activation(out=PT[:], in_=Pc[:], func=AF.Exp, scale=0.125), r=[Pc], w=[PT])
                        v_ap, vT = Vrhs_fn(st)
                        for j in range(4):
                            C.op("pe", lambda e: e.matmul(pacc[:, j, 0:ncol], lhsT=PT[:, j * 128:(j + 1) * 128], rhs=v_ap, start=(ix == 0), stop=(ix == n - 1)), r=[PT, vT], w=[pacc])
                    rz4 = rz4s[zi % 2]; cf4 = cf4s[zi % 2]; zi += 1
                    C.op("dve", lambda e: e.tensor_scalar(out=rz4[:], in0=pacc[:, :, 64], scalar1=1e-20, scalar2=None, op0=ALU.max), r=[pacc], w=[rz4])
                    C.op("dve", lambda e: e.reciprocal(out=rz4[:], in_=rz4[:]), r=[rz4], w=[rz4])
                    C.op("dve", lambda e: e.tensor_tensor(out=cf4[:], in0=rz4[:], in1=Gt[:, 12 * g_ + gidx:12 * g_ + gidx + 10:3], op=ALU.mult), r=[rz4, Gt], w=[cf4])
                    for j in range(4):
                        hd = 4 * g_ + j
                        ah = acc[:, hd * 64:(hd + 1) * 64]
                        if first:
                            C.op("act", lambda e: e.activation(out=ah, in_=pacc[:, j, 0:64], func=AF.Copy, scale=cf4[:, j:j + 1]), r=[pacc, cf4], w=[acc])
                        else:
                            C.op("dve", lambda e: e.scalar_tensor_tensor(out=ah, in0=pacc[:, j, 0:64], scalar=cf4[:, j:j + 1], in1=ah, op0=ALU.mult, op1=ALU.add), r=[pacc, cf4, acc], w=[acc])
                    if first:
                        C.op("dve", lambda e: e.tensor_tensor(out=imt[:], in0=pacc[:, :, 65:129], in1=ins_bc(rz4[:], 2, 64), op=ALU.mult), r=[pacc, rz4], w=[imt])
                        C.op("dve", lambda e: e.tensor_reduce(out=imp[:], in_=imt[:].rearrange("p j d -> p d j"), axis=AX.X, op=ALU.add), r=[imt], w=[imp])

                for g_ in range(4):
                    branch(g_, lambda st: [(KC_T[:, g_, st * 128:(st + 1) * 128], KC_T, QN[:, 4 * g_:4 * g_ + 4, :], QN), (ident[:], ident, cb[st][:], cb[st])],
                           list(range(nhalf)), lambda st: (VCa[:, st, g_, :], VCa), 129, 0, True)
                    C.op("dve", lambda e: e.tensor_tensor(out=imp3[:], in0=imp[:], in1=FORCE[:], op=ALU.max), r=[imp, FORCE], w=[imp3])
                    C.op("dve", lambda e: e.tensor_tensor(out=imp3[:], in0=imp3[:], in1=FUT[:], op=ALU.add), r=[imp3, FUT], w=[imp3])
                    C.op("dve", lambda e: e.max(out=m8a[:], in_=imp3[:]), r=[imp3], w=[m8a])
                    C.op("dve", lambda e: e.match_replace(out=impr[:], in_to_replace=m8a[:], in_values=imp3[:], imm_value=-1e30), r=[imp3, m8a], w=[impr])
                    C.op("dve", lambda e: e.max(out=m8b[:], in_=impr[:]), r=[impr], w=[m8b])
                    C.op("dve", lambda e: e.tensor_scalar(out=nm[:, 64:128], in0=imp3[:], scalar1=m8b[:, 7:8], scalar2=NEG, op0=ALU.is_lt, op1=ALU.mult), r=[imp3, m8b], w=[nm])
                    def selqk(st):
                        m = [(KS[:, g_, st * 128:(st + 1) * 128], KS, QR[:, 4 * g_:4 * g_ + 4, :], QR)]
                        if st == qt:
                            m.append((ident[:], ident, TRI4[:], TRI4))
                        return m

                    def winqk(st):
                        m = [(KW[:, g_, st * 128:(st + 1) * 128], KW, QR[:, 4 * g_:4 * g_ + 4, :], QR)]
                        if st == qt:
                            m.append((ident[:], ident, TRI4[:], TRI4))
                        elif st == qt - 4:
                            m.append((ident[:], ident, BLO4[:], BLO4))
                        return m
                    branch(g_, winqk, list(range(max(0, qt - 4), qt + 1)), lambda st: (VWa[:, st, g_, :], VWa), 65, 2, False)
                    C.op("pe", lambda e: e.transpose(out=pnm[:, 0, :], in_=nm[:], identity=ident[:]), r=[nm, ident], w=[pnm])
                    C.op("dve", lambda e: e.tensor_copy(out=QR[64:128, 4 * g_:4 * g_ + 4, :], in_=ins_bc(pnm[64:128, 0, :], 1, 4)), r=[pnm], w=[QR])

                    branch(g_, selqk, list(range(qt + 1)), lambda st: (VSa[:, st, g_, :], VSa), 65, 1, False)
                C.op("act", lambda e: e.activation(out=OB[:], in_=acc[:], func=AF.Copy), r=[acc], w=[OB])
                store(OM[ts, :], OB[:], OB)

    if upto >= 1:
        phase_A0()
    if upto >= 2:
        phase_A1()
    if upto >= 3:
        phase_A2()
    if upto >= 4:
        phase_A3(0, inp["ab_w_out"], x_in, XA)
    if upto >= 5:
        phase_A4(0, XA, XB if (dbg or upto > 5) else y_out)
    if upto >= 6:
        phase_B0()
    if upto >= 7:
        phase_B1()
    if upto >= 8:
        phase_B2()
    if upto >= 9:
        phase_A3(1, inp["nsa_w_out"], XB, XC)
    if upto >= 10:
        phase_A4(1, XC, y_out)
    C.close()
    k.nc = nc; k.dbg_names = dbg_names; k.ninstr = C.ninstr; k.inp_names = list(inp)
    return k


_W_NAMES = ["norm_mix", "norm_mem", "norm_mem_src", "norm_ffn", "mem_w_q", "mem_w_kv", "mem_w_o", "mem_q_norm", "mem_k_norm", "ffn_w_in", "ffn_w_out"]
_W0_NAMES = ["ab_w_in", "ab_w_out", "dsa_q_norm", "dsa_k_norm", "moba_q_norm", "moba_k_norm", "nsa_w_in", "nsa_w_out", "nsa_q_norm", "nsa_kcmp_norm",
             "nsa_ksel_norm", "nsa_kwin_norm", "nsa_cmp_pos_k", "nsa_cmp_pos_v", "nsa_cmp_w1_k", "nsa_cmp_w2_k", "nsa_cmp_w1_v", "nsa_cmp_w2_v"]


def make_in_map(inputs, b):
    m = {"x": np.ascontiguousarray(inputs["x"][b], dtype=np.float32),
         "mem": np.ascontiguousarray(inputs["mem"][b], dtype=np.float32),
         "post": np.ascontiguousarray(np.asarray(inputs["positions"][b]).astype(np.int32).reshape(NT, 128).T)}
    for n in _W_NAMES:
        m[n] = np.ascontiguousarray(inputs[n], dtype=np.float32)
    for n in _W0_NAMES:
        m[n] = np.ascontiguousarray(np.asarray(inputs[n])[0], dtype=np.float32)
    return m


def kernel(**inputs):
    from concourse.bass_utils import run_bass_kernel_spmd
    k = build(dbg=False)
    in_maps = [make_in_map(inputs, b) for b in range(8)]
    res = run_bass_kernel_spmd(k.nc, in_maps, core_ids=list(range(8)))
    return np.stack([np.asarray(r["y"], dtype=np.float32) for r in res.results], axis=0)
```
